# Optimizing a Trainium2 kernel written in Bass

```python
import jax, jax.numpy as jnp
from jax import lax
import numpy as np

D_MODEL = 1024
BATCH = 16
SEQ = 2048
DEPTH = 2

GRID_W = 64
CTX_LEN = 256
N_EVEN = (DEPTH + 1) // 2
N_ODD = DEPTH // 2
N_MOD = 6
NORM_EPS = 1e-6
CONV_CH = D_MODEL // 2
CONV_WIDTH = 31
FOURIER_CH = D_MODEL // 2
FOURIER_GROUPS = 4
FOURIER_GROUP_CH = FOURIER_CH // FOURIER_GROUPS
AB_IN = 2 * CONV_CH + FOURIER_CH
AB_MIX = CONV_CH + FOURIER_CH
MLA_HEADS = 16
QK_NOPE = 64
QK_ROPE = 32
V_HEAD = 64
QK_HEAD = QK_NOPE + QK_ROPE
Q_LORA = 256
KV_LORA = 128
MLA_IN = Q_LORA + KV_LORA + QK_ROPE
ROPE_BASE = 10000.0
ROPE_FREQS_PER_AXIS = QK_ROPE // 4
ATTN_SCALE = QK_HEAD ** -0.5
Q_BLOCK = 128
N_EXPERTS = 16
EXPERT_FF = 1024
EC_CAPACITY_FACTOR = 2

kernel_name = 'hybrid_conv_fourier_mla_ecmoe_prefix_dit'


def rmsnorm(x, g):
    xf = x.astype(jnp.float32)
    y = xf * lax.rsqrt(jnp.mean(xf * xf, axis=-1, keepdims=True) + NORM_EPS)
    return (y * g.astype(jnp.float32)).astype(x.dtype)


def layernorm(x, g, b):
    xf = x.astype(jnp.float32)
    mu = jnp.mean(xf, axis=-1, keepdims=True)
    var = jnp.mean(jnp.square(xf - mu), axis=-1, keepdims=True)
    y = (xf - mu) * lax.rsqrt(var + NORM_EPS)
    return (y * g.astype(jnp.float32) + b.astype(jnp.float32)).astype(x.dtype)


def adaln(cvec, w, b):
    m = (jax.nn.silu(cvec) @ w + b)[:, None, :]
    return jnp.split(m, N_MOD, axis=-1)


def modulate(h, shift, scale):
    return h * (1 + scale) + shift


def conv_fourier_mixer(h, w_in, conv_w, conv_b, ln_g, ln_b, w_out):
    n, L, _ = h.shape
    z = h @ w_in
    a_val, a_gate, u = jnp.split(z, [CONV_CH, 2 * CONV_CH], axis=-1)
    a = a_val * jax.nn.sigmoid(a_gate)
    a = lax.conv_general_dilated(a, conv_w[:, None, :], window_strides=(1,),
                                 padding=[(CONV_WIDTH // 2, CONV_WIDTH // 2)],
                                 dimension_numbers=('NWC', 'WIO', 'NWC'),
                                 feature_group_count=CONV_CH) + conv_b
    a = jax.nn.silu(layernorm(a, ln_g, ln_b))
    ug = u.astype(jnp.float32).reshape(n, L, FOURIER_GROUPS, FOURIER_GROUP_CH)
    f = jnp.fft.fft2(ug, axes=(1, 3), norm='ortho').real.reshape(n, L, FOURIER_CH).astype(h.dtype)
    return jnp.concatenate([a, f], axis=-1) @ w_out


def axial_rope(L):
    rows = L // GRID_W
    row = jnp.repeat(jnp.arange(rows, dtype=jnp.float32), GRID_W)
    col = jnp.tile(jnp.arange(GRID_W, dtype=jnp.float32), rows)
    inv_freq = ROPE_BASE ** (-jnp.arange(ROPE_FREQS_PER_AXIS, dtype=jnp.float32) / ROPE_FREQS_PER_AXIS)
    ang = jnp.concatenate([row[:, None] * inv_freq, col[:, None] * inv_freq], axis=-1)
    return jnp.cos(ang), jnp.sin(ang)


def apply_rope(x, cos, sin):
    xf = x.astype(jnp.float32).reshape(x.shape[:-1] + (QK_ROPE // 2, 2))
    xe, xo = xf[..., 0], xf[..., 1]
    out = jnp.stack([xe * cos - xo * sin, xe * sin + xo * cos], axis=-1)
    return out.reshape(x.shape).astype(x.dtype)


def mla_queries(cq, q_norm_g, w_uq, rope):
    n, L, _ = cq.shape
    q = (rmsnorm(cq, q_norm_g) @ w_uq).reshape(n, L, MLA_HEADS, QK_HEAD)
    q_nope, q_rope = q[..., :QK_NOPE], q[..., QK_NOPE:]
    if rope is not None:
        q_rope = apply_rope(q_rope, rope[0][:, None, :], rope[1][:, None, :])
    return jnp.concatenate([q_nope, q_rope], axis=-1)


def mla_keys_values(ckv, k_rope, kv_norm_g, w_ukv, rope):
    n, L, _ = ckv.shape
    kv = (rmsnorm(ckv, kv_norm_g) @ w_ukv).reshape(n, L, MLA_HEADS, QK_NOPE + V_HEAD)
    k_nope, v = kv[..., :QK_NOPE], kv[..., QK_NOPE:]
    if rope is not None:
        k_rope = apply_rope(k_rope, rope[0], rope[1])
    k_rope = jnp.broadcast_to(k_rope[:, :, None, :], (n, L, MLA_HEADS, QK_ROPE))
    return jnp.concatenate([k_nope, k_rope], axis=-1), v


def attend(q, k, v):
    s = jnp.einsum('bqhd,bkhd->bhqk', q, k, preferred_element_type=jnp.float32) * ATTN_SCALE
    p = jax.nn.softmax(s, axis=-1).astype(v.dtype)
    return jnp.einsum('bhqk,bkhd->bqhd', p, v)


def blocked_attend(q, k, v):
    n, L, H, dq = q.shape
    nb = L // Q_BLOCK
    qb = q.reshape(n, nb, Q_BLOCK, H, dq).transpose(1, 0, 2, 3, 4)
    ob = lax.map(lambda qi: attend(qi, k, v), qb)
    return ob.transpose(1, 0, 2, 3, 4).reshape(n, L, H * V_HEAD)


def ec_moe(h, w_router, w1, w3, w2):
    n, L, D = h.shape
    cap = EC_CAPACITY_FACTOR * L // N_EXPERTS
    logits = jnp.einsum('bld,de->ble', h, w_router, preferred_element_type=jnp.float32)
    aff = jax.nn.softmax(logits, axis=-1)
    gate, idx = lax.top_k(jnp.swapaxes(aff, 1, 2), cap)
    xg = jax.vmap(lambda hb, ib: hb[ib])(h, idx)
    hid = jax.nn.silu(jnp.einsum('becd,edf->becf', xg, w1)) * jnp.einsum('becd,edf->becf', xg, w3)
    out = jnp.einsum('becf,efd->becd', hid, w2) * gate[..., None].astype(h.dtype)
    return jax.vmap(lambda ib, ob: jnp.zeros((L, D), h.dtype).at[ib.reshape(-1)].add(ob.reshape(-1, D)))(idx, out)


def setup_inputs(seed: int = 0) -> dict:
    key = jax.random.key(seed)
    ks = jax.random.split(key, 32)
    f32 = jnp.float32
    D = D_MODEL

    def nrm(k, shape, fan_in, gain=1.0):
        return gain * fan_in ** -0.5 * jax.random.normal(k, shape, f32)

    def gains(k, shape):
        return 1.0 + 0.05 * jax.random.normal(k, shape, f32)

    def small(k, shape):
        return 0.02 * jax.random.normal(k, shape, f32)

    return {
        'x': jax.random.normal(ks[0], (BATCH, SEQ, D), f32),
        'c': jax.random.normal(ks[1], (BATCH, D), f32),
        'ctx': jax.random.normal(ks[2], (BATCH, CTX_LEN, D), f32),
        'c_ctx': jax.random.normal(ks[3], (D,), f32),
        'mod_w': nrm(ks[4], (DEPTH, D, N_MOD * D), D, 0.5),
        'mod_b': small(ks[5], (DEPTH, N_MOD * D)),
        'norm1_g': gains(ks[6], (DEPTH, D)),
        'norm2_g': gains(ks[7], (DEPTH, D)),
        'ab_w_in': nrm(ks[8], (N_EVEN, D, AB_IN), D),
        'ab_conv_w': nrm(ks[9], (N_EVEN, CONV_WIDTH, CONV_CH), CONV_WIDTH),
        'ab_conv_b': small(ks[10], (N_EVEN, CONV_CH)),
        'ab_ln_g': gains(ks[11], (N_EVEN, CONV_CH)),
        'ab_ln_b': small(ks[12], (N_EVEN, CONV_CH)),
        'ab_w_out': nrm(ks[13], (N_EVEN, AB_MIX, D), AB_MIX),
        'mla_w_in': nrm(ks[14], (N_ODD, D, MLA_IN), D),
        'mla_q_norm_g': gains(ks[15], (N_ODD, Q_LORA)),
        'mla_kv_norm_g': gains(ks[16], (N_ODD, KV_LORA)),
        'mla_w_uq': nrm(ks[17], (N_ODD, Q_LORA, MLA_HEADS * QK_HEAD), Q_LORA),
        'mla_w_ukv': nrm(ks[18], (N_ODD, KV_LORA, MLA_HEADS * (QK_NOPE + V_HEAD)), KV_LORA),
        'mla_w_o': nrm(ks[19], (N_ODD, MLA_HEADS * V_HEAD, D), MLA_HEADS * V_HEAD),
        'moe_w_router': nrm(ks[20], (DEPTH, D, N_EXPERTS), D),
        'moe_w1': nrm(ks[21], (DEPTH, N_EXPERTS, D, EXPERT_FF), D),
        'moe_w3': nrm(ks[22], (DEPTH, N_EXPERTS, D, EXPERT_FF), D),
        'moe_w2': nrm(ks[23], (DEPTH, N_EXPERTS, EXPERT_FF, D), EXPERT_FF),
        'final_g': gains(ks[24], (D,)),
    }


def reference(x, c, ctx, c_ctx, mod_w, mod_b, norm1_g, norm2_g, ab_w_in, ab_conv_w, ab_conv_b,
              ab_ln_g, ab_ln_b, ab_w_out, mla_w_in, mla_q_norm_g, mla_kv_norm_g, mla_w_uq,
              mla_w_ukv, mla_w_o, moe_w_router, moe_w1, moe_w3, moe_w2, final_g):
    n, L, _ = x.shape
    rope = axial_rope(L)
    x_lat, x_ctx = x, ctx
    for i in range(DEPTH):
        ctx_out = i < DEPTH - 1
        j = i // 2
        sh1, sc1, g1, sh2, sc2, g2 = adaln(c, mod_w[i], mod_b[i])
        csh1, csc1, cg1, csh2, csc2, cg2 = adaln(c_ctx[None, :], mod_w[i], mod_b[i])
        u_lat = modulate(rmsnorm(x_lat, norm1_g[i]), sh1, sc1)
        u_ctx = modulate(rmsnorm(x_ctx, norm1_g[i]), csh1, csc1)
        if i % 2 == 0:
            ab = (ab_w_in[j], ab_conv_w[j], ab_conv_b[j], ab_ln_g[j], ab_ln_b[j], ab_w_out[j])
            x_lat = x_lat + g1 * conv_fourier_mixer(u_lat, *ab)
            if ctx_out:
                x_ctx = x_ctx + cg1 * conv_fourier_mixer(u_ctx, *ab)
        else:
            split = [Q_LORA, Q_LORA + KV_LORA]
            cq_lat, ckv_lat, kr_lat = jnp.split(u_lat @ mla_w_in[j], split, axis=-1)
            cq_ctx, ckv_ctx, kr_ctx = jnp.split(u_ctx @ mla_w_in[j], split, axis=-1)
            k_lat, v_lat = mla_keys_values(ckv_lat, kr_lat, mla_kv_norm_g[j], mla_w_ukv[j], rope)
            k_ctx, v_ctx = mla_keys_values(ckv_ctx, kr_ctx, mla_kv_norm_g[j], mla_w_ukv[j], None)
            q_lat = mla_queries(cq_lat, mla_q_norm_g[j], mla_w_uq[j], rope)
            k_all = jnp.concatenate([k_ctx, k_lat], axis=1)
            v_all = jnp.concatenate([v_ctx, v_lat], axis=1)
            x_lat = x_lat + g1 * (blocked_attend(q_lat, k_all, v_all) @ mla_w_o[j])
            if ctx_out:
                q_ctx = mla_queries(cq_ctx, mla_q_norm_g[j], mla_w_uq[j], None)
                o_ctx = attend(q_ctx, k_ctx, v_ctx).reshape(n, x_ctx.shape[1], MLA_HEADS * V_HEAD)
                x_ctx = x_ctx + cg1 * (o_ctx @ mla_w_o[j])
        moe = (moe_w_router[i], moe_w1[i], moe_w3[i], moe_w2[i])
        u_lat = modulate(rmsnorm(x_lat, norm2_g[i]), sh2, sc2)
        x_lat = x_lat + g2 * ec_moe(u_lat, *moe)
        if ctx_out:
            u_ctx = modulate(rmsnorm(x_ctx, norm2_g[i]), csh2, csc2)
            x_ctx = x_ctx + cg2 * ec_moe(u_ctx, *moe)
    return rmsnorm(x_lat, final_g)
```

```python
import numpy as np
import ml_dtypes
from contextlib import ExitStack
import concourse.bass as bass
import concourse.mybir as mybir
from concourse.bass_utils import run_bass_kernel_spmd

F32 = mybir.dt.float32
BF16 = mybir.dt.bfloat16
I32 = mybir.dt.int32
U32 = mybir.dt.uint32
U8 = mybir.dt.uint8
AF = mybir.ActivationFunctionType
ALU = mybir.AluOpType
AX = mybir.AxisListType

D = 1024
L = 2048
LC = 256
NS = 2
NE = 16
EPS = 1e-6
DSZ = {F32: 4, BF16: 2, I32: 4, U32: 4, U8: 1}


class Trk:
    def __init__(self, name):
        self.name = name
        self.w = None
        self.r = []
        self.kids = {}
        self.parent = None

    def sub(self, key):
        if key not in self.kids:
            k = Trk(f"{self.name}.{key}")
            k.parent = self
            self.kids[key] = k
        return self.kids[key]

    def rdeps(self):
        s = set()
        if self.w:
            s.add(self.w)
        if self.parent is not None and self.parent.w:
            s.add(self.parent.w)
        for k in self.kids.values():
            if k.w:
                s.add(k.w)
        return s

    def wdeps(self):
        s = self.rdeps()
        s.update(self.r)
        if self.parent is not None:
            s.update(self.parent.r)
        for k in self.kids.values():
            s.update(k.r)
        return s

    def did_read(self, ev):
        self.r.append(ev)

    def did_write(self, ev):
        self.w = ev
        self.r = []
        for k in self.kids.values():
            k.w = None
            k.r = []


class T(Trk):
    def __init__(self, kb, name, shape, dtype, off):
        super().__init__(name)
        self.kb = kb
        self.shape = shape
        self.dtype = dtype
        self.off = off
        self.h = kb.nc.alloc_sbuf_tensor_at(name, list(shape), dtype, offset=off)

    def view(self, name, shape, dtype, boff=0):
        return self.kb.nc.alloc_sbuf_tensor_at(
            self.kb.uname(name), list(shape), dtype, offset=self.off + boff)

    def __getitem__(self, k):
        return self.h[k]


class Lane:
    def __init__(self, key, sem):
        self.key = key
        self.sem = sem
        self.count = 0


class KB:
    COMPUTE = ["pe", "act", "dve", "pool"]
    QUEUES = ["sp", "pool", "act"]

    def __init__(self, n_lanes=8):
        self.nc = bass.Bass("TRN2", target_bir_lowering=False)
        nc = self.nc
        self.es = ExitStack()
        self.uid = 0
        self.semobj = {}
        self.cnt = {}
        for e in self.COMPUTE:
            self.semobj[e] = self.es.enter_context(nc.semaphore("s_" + e))
            self.cnt[e] = 0
        self.lanes = {}
        self.lane_rr = {}
        for q in self.QUEUES:
            self.lanes[q] = []
            for i in range(n_lanes):
                key = f"d_{q}{i}"
                self.semobj[key] = self.es.enter_context(nc.semaphore(key))
                self.lanes[q].append(Lane(key, self.semobj[key]))
            self.lane_rr[q] = 0
        self.prog = {e: [] for e in ["pe", "act", "dve", "pool", "sp"]}
        self.waited = {e: {} for e in ["pe", "act", "dve", "pool", "sp"]}
        self.arena_bytes = 204 * 1024
        ah = nc.alloc_sbuf_tensor("arena", [128, self.arena_bytes], U8)
        self.abase = nc.lookup_mloc(ah).addr
        self.atop = 0
        self.bnd = {}
        for n in (L - 1, NS * LC + 128 - 1):
            reg = self.es.enter_context(nc.gpsimd.register(f"bnd{n}"))
            self.bnd[n] = reg
            self.prog["pool"].append(lambda en, reg=reg, n=n: en.reg_mov(reg, n))
        self.banks = []
        for i in range(8):
            h = self.es.enter_context(nc.psum_tensor(f"bank{i}", [128, 512], F32))
            t = Trk(f"bank{i}")
            t.h = h
            t.psum = True
            self.banks.append(t)

    def uname(self, n):
        self.uid += 1
        return f"{n}_{self.uid}"

    def alloc(self, name, shape, dtype):
        nbytes = int(np.prod(shape[1:])) * DSZ[dtype]
        nbytes = (nbytes + 63) // 64 * 64
        off = self.atop
        assert off + nbytes <= self.arena_bytes, f"SBUF arena overflow at {name}: {off}+{nbytes}"
        self.atop += nbytes
        return T(self, self.uname(name), shape, dtype, self.abase + off)

    def mark(self):
        return self.atop

    def release(self, m):
        self.barrier()
        self.atop = m

    def dram(self, name, shape, dtype, kind="Internal"):
        if kind == "Internal":
            h = self.nc.dram_tensor(name, list(shape), dtype)
        else:
            h = self.nc.dram_tensor(name, list(shape), dtype, kind=kind)
        t = Trk(name)
        t.h = h
        t.ap = h.ap()
        return t

    def _waits(self, eng, evs):
        best = {}
        for (k, v) in evs:
            if v > best.get(k, 0):
                best[k] = v
        for k, v in best.items():
            if k == "pe" and eng == "pe":
                continue
            if self.waited[eng].get(k, 0) >= v:
                continue
            self.waited[eng][k] = v
            sem = self.semobj[k]
            self.prog[eng].append(lambda e, sem=sem, v=v: e.wait_ge(sem, v))

    def _deps(self, reads, writes, eng=None):
        evs = set()
        for t in reads:
            evs |= t.rdeps()
            root = t if t.parent is None else t.parent
            if getattr(root, "psum", False):
                for ev in root.r:
                    if ev[0] != eng:
                        evs.add(ev)
                for k in root.kids.values():
                    for ev in k.r:
                        if ev[0] != eng:
                            evs.add(ev)
        for t in writes:
            evs |= t.wdeps()
        return evs

    def op(self, eng, fn, reads=(), writes=()):
        evs = self._deps(reads, writes, eng)
        self._waits(eng, evs)
        self.cnt[eng] += 1
        sem = self.semobj[eng]
        self.prog[eng].append(lambda e, fn=fn, sem=sem: fn(e).then_inc(sem, 1))
        ev = (eng, self.cnt[eng])
        for t in reads:
            t.did_read(ev)
        for t in writes:
            t.did_write(ev)
        return ev

    def dma(self, q, fn, reads=(), writes=()):
        evs = self._deps(reads, writes)
        lanes = self.lanes[q]
        lane = lanes[self.lane_rr[q] % len(lanes)]
        self.lane_rr[q] += 1
        if lane.count > 0:
            evs.add((lane.key, lane.count))
        self._waits(q, evs)
        lane.count += 16
        sem = lane.sem
        def run(e, fn=fn, sem=sem):
            try:
                ins = fn(e)
            except Exception:
                print("DMA BUILD FAIL line", fn.__code__.co_firstlineno, "defaults", [str(d)[:80] for d in (fn.__defaults__ or ())])
                raise
            ins.then_inc(sem, 16)
        self.prog[q].append(run)
        ev = (lane.key, lane.count)
        for t in reads:
            t.did_read(ev)
        for t in writes:
            t.did_write(ev)
        return ev

    def barrier(self):
        evs = set()
        for e in self.COMPUTE:
            if self.cnt[e] > 0:
                evs.add((e, self.cnt[e]))
        for q in self.QUEUES:
            for ln in self.lanes[q]:
                if ln.count > 0:
                    evs.add((ln.key, ln.count))
        for e in ["pe", "act", "dve", "pool", "sp"]:
            self._waits(e, evs)

    def finish(self):
        self.barrier()
        nc = self.nc
        with nc.allow_non_contiguous_dma(reason="small strided constant loads"):
            with nc.Block() as block:
                @block.sync
                def _(e):
                    for f in self.prog["sp"]:
                        f(e)

                @block.tensor
                def _(e):
                    for f in self.prog["pe"]:
                        f(e)

                @block.scalar
                def _(e):
                    for f in self.prog["act"]:
                        f(e)

                @block.vector
                def _(e):
                    for f in self.prog["dve"]:
                        f(e)

                @block.gpsimd
                def _(e):
                    for f in self.prog["pool"]:
                        f(e)
        self.es.close()
        return nc


class Prog:
    def __init__(self, phases=("mix0", "moe0", "mla1", "moe1", "final"), copy_in=False):
        self.kb = KB()
        self.phases = phases
        self.copy_in = copy_in
        kb = self.kb
        di = lambda n, s, d=F32: kb.dram(n, s, d, kind="ExternalInput")
        self.x = di("x", [NS, L, D])
        self.ctx = di("ctx", [NS, LC, D])
        self.cv = di("cv", [128, 8, 3])
        self.mod_w = di("mod_w", [2, D, 6 * D])
        self.mod_b = di("mod_b", [2, 6 * D])
        self.n1g = di("norm1_g", [2, D])
        self.n2g = di("norm2_g", [2, D])
        self.final_g = di("final_g", [D])
        self.ab_w_in = di("ab_w_in", [D, 1536])
        self.ab_conv_w = di("ab_conv_w", [31, 512])
        self.ab_conv_b = di("ab_conv_b", [512])
        self.ab_ln_g = di("ab_ln_g", [512])
        self.ab_ln_b = di("ab_ln_b", [512])
        self.ab_w_out = di("ab_w_out", [D, D])
        self.mla_w_in = di("mla_w_in", [D, 416])
        self.mla_qg = di("mla_q_norm_g", [256])
        self.mla_kvg = di("mla_kv_norm_g", [128])
        self.mla_w_uq = di("mla_w_uq", [256, 1536])
        self.mla_w_ukv = di("mla_w_ukv", [128, 2048])
        self.mla_w_o = di("mla_w_o", [D, D])
        self.w_router = di("moe_w_router", [2, D, NE])
        self.w1 = di("moe_w1", [2, NE, D, D])
        self.w3 = di("moe_w3", [2, NE, D, D])
        self.w2 = di("moe_w2", [2, NE, D, D])
        self.c_identb = di("c_identb", [128, 128], BF16)
        self.c_identf = di("c_identf", [128, 128], F32)
        self.c_csc = di("c_csc", [128, 256], BF16)
        self.c_cl = di("c_cl", [L, L], BF16)
        self.c_sl = di("c_sl", [L, L], BF16)
        self.c_clc = di("c_clc", [LC, LC], BF16)
        self.c_slc = di("c_slc", [LC, LC], BF16)
        self.c_rope = di("c_rope", [L, 32], F32)
        self.c_ctxbase = di("c_ctxbase", [128, 1], F32)
        self.out = kb.dram("y", [NS, L, D], F32, kind="ExternalOutput")
        self.xr = [kb.dram(f"xr{s}", [L, D], F32) for s in range(NS)]
        self.xc_all = kb.dram("xc_all", [NS * LC + 128, D], F32)
        self.xnc_all = kb.dram("xnc_all", [NS * LC + 128, D], BF16)
        self.xc = []
        self.xnl = [kb.dram(f"xnl{s}", [L, D], BF16) for s in range(NS)]
        self.xnc = []
        for s in range(NS):
            t = self.xc_all.sub(s); t.ap = self.xc_all.ap[s * LC:(s + 1) * LC, :]; self.xc.append(t)
            t = self.xnc_all.sub(s); t.ap = self.xnc_all.ap[s * LC:(s + 1) * LC, :]; self.xnc.append(t)
        self.xin = []
        self.cin = []
        for s in range(NS):
            t = self.x.sub(s); t.ap = self.x.ap[s]; self.xin.append(t)
            t = self.ctx.sub(s); t.ap = self.ctx.ap[s]; self.cin.append(t)
        self.modd = kb.dram("modd", [2, 3, 6 * D], F32)

    def build(self):
        kb = self.kb
        self.prologue()
        if self.copy_in:
            for s in range(NS):
                kb.dma("sp", lambda e, s=s: e.dma_start(out=self.xr[s].ap, in_=self.x.ap[s]),
                       reads=[self.x], writes=[self.xr[s]])
                kb.dma("sp", lambda e, s=s: e.dma_start(out=self.xc[s].ap, in_=self.ctx.ap[s]),
                       reads=[self.ctx], writes=[self.xc[s]])
            kb.barrier()
        for ph in self.phases:
            m = kb.mark()
            if ph == "mix0":
                self.mixer0()
            elif ph == "moe0":
                self.moe(0, with_ctx=True)
            elif ph == "mla1":
                self.mla()
            elif ph == "moe1":
                self.moe(1, with_ctx=False)
            elif ph == "final":
                self.final()
            elif ph == "dump":
                self.dump()
            kb.release(m)
        return kb.finish()

    def prologue(self):
        kb = self.kb
        self.identb = kb.alloc("identb", [128, 128], BF16)
        self.identf = kb.alloc("identf", [128, 128], F32)
        kb.dma("sp", lambda e: e.dma_start(out=self.identb[:], in_=self.c_identb.ap[:, :]),
               reads=[self.c_identb], writes=[self.identb])
        kb.dma("sp", lambda e: e.dma_start(out=self.identf[:], in_=self.c_identf.ap[:, :]),
               reads=[self.c_identf], writes=[self.identf])
        self.epsc = kb.alloc("epsc", [128, 1], F32)
        kb.op("dve", lambda e: e.memset(self.epsc[:], EPS), writes=[self.epsc])
        self.zeroc = kb.alloc("zeroc", [128, 1], F32)
        kb.op("dve", lambda e: e.memset(self.zeroc[:], 0.0), writes=[self.zeroc])
        self.modc = [kb.alloc(f"modc{l}", [128, 48, 3], F32) for l in range(2)]
        self.mul1c = [kb.alloc(f"mul1c{l}", [128, 8, 3], F32) for l in range(2)]
        self.mul2c = [kb.alloc(f"mul2c{l}", [128, 8, 3], F32) for l in range(2)]
        self.n1gc = kb.alloc("n1gc", [128, 2, 8], F32)
        self.n2gc = kb.alloc("n2gc", [128, 2, 8], F32)
        kb.dma("sp", lambda e: e.dma_start(out=self.n1gc[:], in_=self.n1g.ap.rearrange("l (k p) -> p l k", p=128)),
               reads=[self.n1g], writes=[self.n1gc])
        kb.dma("sp", lambda e: e.dma_start(out=self.n2gc[:], in_=self.n2g.ap.rearrange("l (k p) -> p l k", p=128)),
               reads=[self.n2g], writes=[self.n2gc])
        m0 = kb.mark()
        zf = kb.alloc("zf", [128, D], F32)
        zb = kb.alloc("zb", [128, D], BF16)
        kb.op("dve", lambda e: e.memset(zf[:], 0.0), writes=[zf])
        kb.op("dve", lambda e: e.memset(zb[:], 0.0), writes=[zb])
        kb.dma("sp", lambda e: e.dma_start(out=self.xc_all.ap[NS * LC:NS * LC + 128, :], in_=zf[:]),
               reads=[zf], writes=[self.xc_all.sub("pad")])
        kb.dma("sp", lambda e: e.dma_start(out=self.xnc_all.ap[NS * LC:NS * LC + 128, :], in_=zb[:]),
               reads=[zb], writes=[self.xnc_all.sub("pad")])
        cvt = kb.alloc("cvt", [128, 24], F32)
        sct = kb.alloc("sct", [128, 24], BF16)
        kb.dma("sp", lambda e: e.dma_start(out=cvt[:], in_=self.cv.ap.rearrange("p k r -> p (k r)")),
               reads=[self.cv], writes=[cvt])
        kb.op("act", lambda e: e.activation(out=sct[:], in_=cvt[:], func=AF.Silu), reads=[cvt], writes=[sct])
        mwt = [kb.alloc(f"mwt{i}", [128, 8, 1536], BF16) for i in range(2)]
        mrow = kb.alloc("mrow", [3, 6 * D], F32)
        mb3 = kb.alloc("mb3", [3, 6 * D], F32)
        it = 0
        for l in range(2):
            kb.dma("sp", lambda e, l=l: e.dma_start(out=mb3[:], in_=self.mod_b.ap[l].partition_broadcast(3)),
                   reads=[self.mod_b], writes=[mb3])
            for pc in range(4):
                wt = mwt[it % 2]
                it += 1
                src = self.mod_w.ap[l].rearrange("(k p) n -> p k n", p=128)[:, :, pc * 1536:(pc + 1) * 1536]
                kb.dma("pool", lambda e, wt=wt, src=src: e.dma_start(out=wt[:], in_=src),
                       reads=[self.mod_w], writes=[wt])
                for nb in range(3):
                    bank = kb.banks[(pc * 3 + nb) % 2]
                    for k in range(8):
                        kb.op("pe", lambda e, bank=bank, wt=wt, k=k, nb=nb: e.matmul(
                            out=bank.h[0:3, 0:512], lhsT=sct[:, k * 3:(k + 1) * 3],
                            rhs=wt[:, k, nb * 512:(nb + 1) * 512], start=(k == 0), stop=(k == 7)),
                            reads=[sct, wt], writes=[bank])
                    c0 = pc * 1536 + nb * 512
                    kb.op("dve", lambda e, bank=bank, c0=c0: e.tensor_tensor(
                        out=mrow[0:3, c0:c0 + 512], in0=bank.h[0:3, 0:512], in1=mb3[0:3, c0:c0 + 512], op=ALU.add),
                        reads=[bank, mb3], writes=[mrow.sub(c0)])
            kb.dma("sp", lambda e, l=l: e.dma_start(out=self.modd.ap[l], in_=mrow[0:3, :]),
                   reads=[mrow], writes=[self.modd.sub(l)])
            for r in range(3):
                kb.dma("sp", lambda e, l=l, r=r: e.dma_start(
                    out=self.modc[l][:, :, r], in_=self.modd.ap[l, r].rearrange("(c p) -> p c", p=128)),
                    reads=[self.modd.sub(l)], writes=[self.modc[l].sub(r)])
            for (mulc, gc, v) in ((self.mul1c[l], self.n1gc, 1), (self.mul2c[l], self.n2gc, 4)):
                kb.op("dve", lambda e, mulc=mulc, v=v, l=l: e.tensor_scalar(
                    out=mulc[:], in0=self.modc[l][:, v * 8:(v + 1) * 8, :], scalar1=1.0, scalar2=None, op0=ALU.add),
                    reads=[self.modc[l]], writes=[mulc])
                for r in range(3):
                    kb.op("dve", lambda e, mulc=mulc, gc=gc, r=r, l=l: e.tensor_tensor(
                        out=mulc[:, :, r], in0=mulc[:, :, r], in1=gc[:, l, :], op=ALU.mult),
                        reads=[mulc, gc], writes=[mulc])
        kb.release(m0)

    def norm_batch(self, src, row0, nt, mulc, addc, r, uT, ucol0, bufs, banks, xn_dst=None):
        kb = self.kb
        xb, xnb, junk, stat = bufs
        for j in range(nt):
            xt = xb[self._nb % len(xb)]
            xn = xnb[self._nb % len(xnb)]
            sc = self._nb % 64
            self._nb += 1
            rr = row0 + j * 128
            kb.dma("sp", lambda e, xt=xt, rr=rr: e.dma_start(out=xt[:], in_=src.ap[rr:rr + 128, :]),
                   reads=[src], writes=[xt])
            kb.op("act", lambda e, xt=xt, sc=sc: e.activation(
                out=junk[:], in_=xt[:], func=AF.Square, accum_out=stat[:, sc:sc + 1]),
                reads=[xt], writes=[junk, stat.sub(sc)])
            kb.op("act", lambda e, sc=sc: e.activation(
                out=stat[:, 64 + sc:65 + sc], in_=stat[:, sc:sc + 1], func=AF.Identity, scale=1.0 / D, bias=self.epsc[:, 0:1]),
                reads=[stat.sub(sc), self.epsc], writes=[stat.sub(64 + sc)])
            kb.op("act", lambda e, sc=sc: e.activation(
                out=stat[:, 128 + sc:129 + sc], in_=stat[:, 64 + sc:65 + sc], func=AF.Sqrt),
                reads=[stat.sub(64 + sc)], writes=[stat.sub(128 + sc)])
            kb.op("dve", lambda e, sc=sc: e.reciprocal(out=stat[:, 192 + sc:193 + sc], in_=stat[:, 128 + sc:129 + sc]),
                  reads=[stat.sub(128 + sc)], writes=[stat.sub(192 + sc)])
            kb.op("act", lambda e, xt=xt, xn=xn, sc=sc: e.activation(
                out=xn[:], in_=xt[:], func=AF.Identity, scale=stat[:, 192 + sc:193 + sc]),
                reads=[xt, stat.sub(192 + sc)], writes=[xn])
            if xn_dst is not None:
                dt, drow = xn_dst
                dr0 = drow + j * 128
                kb.dma("sp", lambda e, xn=xn, dt=dt, dr=dr0: e.dma_start(
                    out=dt.ap[dr:dr + 128, :], in_=xn[:]), reads=[xn], writes=[dt.sub(dr0)])
            if getattr(self, "_skip_t", False):
                continue
            for k in range(8):
                bank = banks[2 * j + k // 4]
                pv = bank.h.bitcast(BF16)
                kk = k % 4
                kb.op("pe", lambda e, pv=pv, xn=xn, k=k, kk=kk: e.transpose(
                    out=pv[:, kk * 128:(kk + 1) * 128], in_=xn[:, k * 128:(k + 1) * 128], identity=self.identb[:]),
                    reads=[xn, self.identb], writes=[bank])
            for k in range(8):
                bank = banks[2 * j + k // 4]
                pv = bank.h.bitcast(BF16)
                kk = k % 4
                dst = uT[:, k, ucol0 + j * 128: ucol0 + (j + 1) * 128]
                if k < 4:
                    kb.op("act", lambda e, dst=dst, pv=pv, k=k, kk=kk: e.activation(
                        out=dst, in_=pv[:, kk * 128:(kk + 1) * 128], func=AF.Identity,
                        scale=mulc[:, k, r:r + 1], bias=addc(k, r)),
                        reads=[bank, mulc], writes=[uT.sub((k, ucol0 + j * 128))])
                else:
                    kb.op("dve", lambda e, dst=dst, pv=pv, k=k, kk=kk: e.tensor_scalar(
                        out=dst, in0=pv[:, kk * 128:(kk + 1) * 128], scalar1=mulc[:, k, r:r + 1],
                        scalar2=addc(k, r), op0=ALU.mult, op1=ALU.add),
                        reads=[bank, mulc], writes=[uT.sub((k, ucol0 + j * 128))])

    def norm_bufs(self, nx=2):
        kb = self.kb
        self._nb = 0
        xb = [kb.alloc(f"xb{i}", [128, D], F32) for i in range(nx)]
        xnb = [kb.alloc(f"xnb{i}", [128, D], BF16) for i in range(nx)]
        junk = kb.alloc("junk", [128, D], BF16)
        stat = kb.alloc("stat", [128, 256], F32)
        kb.op("dve", lambda e: e.memset(stat[:], 0.0), writes=[stat])
        return (xb, xnb, junk, stat)

    def dump(self):
        kb = self.kb
        yc = kb.dram("yc", [NS * LC, D], F32, kind="ExternalOutput")
        kb.dma("sp", lambda e: e.dma_start(out=yc.ap[:, :], in_=self.xc_all.ap[0:NS * LC, :]),
               reads=[self.xc_all], writes=[yc])
        for s in range(NS):
            kb.dma("sp", lambda e, s=s: e.dma_start(out=self.out.ap[s], in_=self.xr[s].ap),
                   reads=[self.xr[s]], writes=[self.out.sub(s)])

    def final(self):
        kb = self.kb
        fgb = kb.alloc("fgb", [128, D], F32)
        kb.dma("sp", lambda e: e.dma_start(out=fgb[:], in_=self.final_g.ap.partition_broadcast(128)),
               reads=[self.final_g], writes=[fgb])
        xb = [kb.alloc(f"fxb{i}", [128, D], F32) for i in range(3)]
        yb = [kb.alloc(f"fyb{i}", [128, D], F32) for i in range(3)]
        junk = kb.alloc("fjunk", [128, D], BF16)
        stat = kb.alloc("fstat", [128, 3 * 32], F32)
        kb.op("dve", lambda e: e.memset(stat[:], 0.0), writes=[stat])
        i = 0
        for s in range(NS):
            for t in range(L // 128):
                xt = xb[i % 3]
                yt = yb[i % 3]
                sc = i % 32
                i += 1
                kb.dma("sp", lambda e, xt=xt, s=s, t=t: e.dma_start(out=xt[:], in_=self.xr[s].ap[t * 128:(t + 1) * 128, :]),
                       reads=[self.xr[s]], writes=[xt])
                if i > 32:
                    kb.op("dve", lambda e, sc=sc: e.memset(stat[:, sc:sc + 1], 0.0), writes=[stat.sub(sc)])
                kb.op("act", lambda e, xt=xt, sc=sc: e.activation(
                    out=junk[:], in_=xt[:], func=AF.Square, accum_out=stat[:, sc:sc + 1]),
                    reads=[xt], writes=[junk, stat.sub(sc)])
                kb.op("dve", lambda e, sc=sc: e.tensor_scalar(
                    out=stat[:, 32 + sc:33 + sc], in0=stat[:, sc:sc + 1], scalar1=1.0 / D, scalar2=EPS, op0=ALU.mult, op1=ALU.add),
                    reads=[stat.sub(sc)], writes=[stat.sub(32 + sc)])
                kb.op("act", lambda e, sc=sc: e.activation(
                    out=stat[:, 32 + sc:33 + sc], in_=stat[:, 32 + sc:33 + sc], func=AF.Sqrt),
                    reads=[stat.sub(32 + sc)], writes=[stat.sub(32 + sc)])
                kb.op("dve", lambda e, sc=sc: e.reciprocal(out=stat[:, 64 + sc:65 + sc], in_=stat[:, 32 + sc:33 + sc]),
                      reads=[stat.sub(32 + sc)], writes=[stat.sub(64 + sc)])
                kb.op("dve", lambda e, xt=xt, yt=yt, sc=sc: e.scalar_tensor_tensor(
                    out=yt[:], in0=xt[:], scalar=stat[:, 64 + sc:65 + sc], in1=fgb[:], op0=ALU.mult, op1=ALU.mult),
                    reads=[xt, stat.sub(64 + sc), fgb], writes=[yt])
                kb.dma("sp", lambda e, yt=yt, s=s, t=t: e.dma_start(out=self.out.ap[s, t * 128:(t + 1) * 128, :], in_=yt[:]),
                       reads=[yt], writes=[self.out.sub((s, t))])

    def moe(self, l, with_ctx):
        kb = self.kb
        IOA = bass.IndirectOffsetOnAxis
        wbufs = [[kb.alloc(f"w{n}_{i}", [128, 8, D], BF16) for n in (1, 3, 2)] for i in range(2)]
        wsrc = (self.w1, self.w3, self.w2)

        def load_w(e):
            for n in range(3):
                src = wsrc[n].ap[l, e].rearrange("(k p) f -> p k f", p=128)
                wt = wbufs[e % 2][n]
                kb.dma("pool", lambda en, wt=wt, src=src: en.dma_start(out=wt[:], in_=src),
                       reads=[wsrc[n]], writes=[wt])

        nr = 3 if with_ctx else 2
        g2b = [kb.alloc(f"g2b{r}", [128, D], F32) for r in range(nr)]
        for r in range(nr):
            kb.dma("sp", lambda e, r=r: e.dma_start(
                out=g2b[r][:], in_=self.modd.ap[l, r, 5 * D:6 * D].partition_broadcast(128)),
                reads=[self.modd.sub(l)], writes=[g2b[r]])
        idxT = kb.alloc("idxT", [128, 2, 48], I32)
        gT = kb.alloc("gT", [128, 2, 48], F32)
        idxC = kb.alloc("idxC", [128, NE], I32)
        gC = kb.alloc("gC", [128, NE], F32)
        load_w(0)
        load_w(1)
        addc2 = lambda k, r: self.modc[l][:, 24 + k, r:r + 1]
        mulc2 = self.mul2c[l]

        dbg = getattr(self, "debug_route", 0)
        if dbg == 10:
            return
        m1 = kb.mark()
        bufs = self.norm_bufs()
        uTb = [kb.alloc(f"uTb{i}", [128, 8, 256], BF16) for i in range(2)]
        wr = kb.alloc("wr", [128, 8, NE], BF16)
        wrf = kb.alloc("wrf", [128, 8, NE], F32)
        kb.dma("sp", lambda e: e.dma_start(out=wrf[:], in_=self.w_router.ap[l].rearrange("(k p) e -> p k e", p=128)),
               reads=[self.w_router], writes=[wrf])
        kb.op("dve", lambda e: e.tensor_copy(out=wr[:], in_=wrf[:]), reads=[wrf], writes=[wr])
        aff2 = kb.alloc("aff2", [128, 16, 64], F32)
        affc = kb.alloc("affc", [128, 2, 64], F32)
        kb.op("dve", lambda e: e.memset(aff2[:].rearrange("p a b -> p (a b)"), 0.0), writes=[aff2])
        kb.op("dve", lambda e: e.memset(affc[:].rearrange("p a b -> p (a b)"), 0.0), writes=[affc])
        lg = kb.alloc("lg", [128, 16, 16], F32)
        mx = kb.alloc("mx", [128, 16], F32)
        sm = kb.alloc("sm", [128, 16], F32)
        rs = kb.alloc("rs", [128, 16], F32)
        work = kb.alloc("work", [48, L], F32)
        workc = kb.alloc("workc", [48, LC], F32)
        topv = kb.alloc("topv", [48, 256], F32)
        topi = kb.alloc("topi", [48, 256], U32)
        topif = kb.alloc("topif", [48, 256], F32)
        topvc = kb.alloc("topvc", [48, 32], F32)
        topic = kb.alloc("topic", [48, 32], U32)
        topicf = kb.alloc("topicf", [48, 32], F32)
        nbatch = 0
        seqs = []
        for s in range(NS):
            seqs.append((self.xr[s], self.xnl[s], L // 128, s, aff2, s * 32, kb.banks[4 + s], 0))
        if with_ctx:
            for s in range(NS):
                seqs.append((self.xc[s], self.xnc[s], LC // 128, 2, affc, s * 32, kb.banks[6], s * 32))
        for (src, xnd, ntl, r, afft, acol, lbank, lcol0) in seqs:
            for b in range((ntl + 1) // 2):
                nt = min(2, ntl - b * 2)
                ub = uTb[nbatch % 2]
                nbatch += 1
                self.norm_batch(src, b * 256, nt, mulc2, addc2, r, ub, 0, bufs,
                                [kb.banks[j] for j in range(2 * nt)], xn_dst=(xnd, b * 256))
                for j in range(nt):
                    if dbg == 11:
                        continue
                    c = b * 2 + j
                    for k in range(8):
                        kb.op("pe", lambda e, lbank=lbank, lc=lcol0 + c * 16, ub=ub, k=k, j=j: e.matmul(
                            out=lbank.h[:, lc:lc + 16], lhsT=ub[:, k, j * 128:(j + 1) * 128], rhs=wr[:, k, :],
                            start=(k == 0), stop=(k == 7)),
                            reads=[ub.sub((k, j * 128)), wr], writes=[lbank])
            if dbg == 11:
                continue
            lv = lbank.h[:, lcol0:lcol0 + ntl * 16].rearrange("p (c e) -> p c e", e=16)
            bc = lambda t, ntl=ntl: t[:, 0:ntl].unsqueeze(2).to_broadcast([128, ntl, 16])
            kb.op("dve", lambda e, lv=lv, ntl=ntl: e.tensor_reduce(out=mx[:, 0:ntl], in_=lv, axis=AX.X, op=ALU.max),
                  reads=[lbank], writes=[mx])
            kb.op("dve", lambda e, lv=lv, bc=bc, ntl=ntl: e.tensor_tensor(out=lg[:, 0:ntl, :], in0=lv, in1=bc(mx), op=ALU.subtract),
                  reads=[lbank, mx], writes=[lg])
            kb.op("act", lambda e, ntl=ntl: e.activation(out=lg[:, 0:ntl, :], in_=lg[:, 0:ntl, :], func=AF.Exp),
                  reads=[lg], writes=[lg])
            kb.op("dve", lambda e, ntl=ntl: e.tensor_reduce(out=sm[:, 0:ntl], in_=lg[:, 0:ntl, :], axis=AX.X, op=ALU.add),
                  reads=[lg], writes=[sm])
            kb.op("dve", lambda e, ntl=ntl: e.reciprocal(out=rs[:, 0:ntl], in_=sm[:, 0:ntl]), reads=[sm], writes=[rs])
            kb.op("dve", lambda e, afft=afft, acol=acol, bc=bc, ntl=ntl: e.tensor_tensor(
                out=afft[:, 0:ntl, acol:acol + 16], in0=lg[:, 0:ntl, :], in1=bc(rs), op=ALU.mult),
                reads=[lg, rs], writes=[afft])
        if dbg == 11:
            kb.release(m1)
            return
        if dbg == 1:
            da = kb.dram("dbg_aff", [128, 1024], F32, kind="ExternalOutput")
            kb.dma("sp", lambda e: e.dma_start(out=da.ap[:, :], in_=aff2[:].rearrange("p h c -> p (h c)")), reads=[aff2], writes=[da])
            kb.release(m1)
            return
        for c in range(16):
            bank = kb.banks[c // 4]
            kb.op("pe", lambda e, bank=bank, c=c: e.transpose(
                out=bank.h[0:48, (c % 4) * 128:(c % 4 + 1) * 128], in_=aff2[:, c, 0:48], identity=self.identf[:]),
                reads=[aff2, self.identf], writes=[bank])
        for q in range(4):
            eng = "act" if q % 2 == 0 else "dve"
            if eng == "act":
                kb.op("act", lambda e, q=q: e.copy(out=work[0:48, q * 512:(q + 1) * 512], in_=kb.banks[q].h[0:48, 0:512]),
                      reads=[kb.banks[q]], writes=[work.sub(q)])
            else:
                kb.op("dve", lambda e, q=q: e.tensor_copy(out=work[0:48, q * 512:(q + 1) * 512], in_=kb.banks[q].h[0:48, 0:512]),
                      reads=[kb.banks[q]], writes=[work.sub(q)])
        if with_ctx:
            for c in range(2):
                kb.op("pe", lambda e, c=c: e.transpose(
                    out=kb.banks[7].h[0:48, c * 128:(c + 1) * 128], in_=affc[:, c, 0:48], identity=self.identf[:]),
                    reads=[affc, self.identf], writes=[kb.banks[7]])
            kb.op("act", lambda e: e.copy(out=workc[0:48, :], in_=kb.banks[7].h[0:48, 0:256]),
                  reads=[kb.banks[7]], writes=[workc])

        def topk(wk, tv, ti, niter):
            for it in range(niter):
                sl = slice(it * 8, (it + 1) * 8)
                kb.op("dve", lambda e, sl=sl: e.max(out=tv[:, sl], in_=wk[:]), reads=[wk], writes=[tv.sub(it)])
                kb.op("dve", lambda e, sl=sl: e.max_index(out=ti[:, sl], in_max=tv[:, sl], in_values=wk[:]),
                      reads=[wk, tv.sub(it)], writes=[ti.sub(it)])
                kb.op("dve", lambda e, sl=sl: e.match_replace(out=wk[:], in_to_replace=tv[:, sl], in_values=wk[:], imm_value=-1.0),
                      reads=[tv.sub(it), wk], writes=[wk])

        if dbg == 2:
            da = kb.dram("dbg_work", [48, L], F32, kind="ExternalOutput")
            kb.dma("sp", lambda e: e.dma_start(out=da.ap[:, :], in_=work[:]), reads=[work], writes=[da])
            kb.release(m1)
            return
        topk(work, topv, topi, 32)
        if dbg == 3:
            da = kb.dram("dbg_topv", [48, 256], F32, kind="ExternalOutput")
            kb.dma("sp", lambda e: e.dma_start(out=da.ap[:, :], in_=topv[:]), reads=[topv], writes=[da])
            db_ = kb.dram("dbg_topi", [48, 256], U32, kind="ExternalOutput")
            kb.dma("sp", lambda e: e.dma_start(out=db_.ap[:, :], in_=topi[:]), reads=[topi], writes=[db_])
            kb.release(m1)
            return
        kb.op("dve", lambda e: e.tensor_copy(out=topif[:], in_=topi[:]), reads=[topi], writes=[topif])
        tb = kb.banks[5]
        for h in range(2):
            kb.op("pe", lambda e, h=h: e.transpose(out=tb.h[:, h * 48:(h + 1) * 48], in_=topif[0:48, h * 128:(h + 1) * 128],
                                                   identity=self.identf[0:48, 0:48]),
                  reads=[topif, self.identf], writes=[tb])
            kb.op("pe", lambda e, h=h: e.transpose(out=tb.h[:, 128 + h * 48:128 + (h + 1) * 48], in_=topv[0:48, h * 128:(h + 1) * 128],
                                                   identity=self.identf[0:48, 0:48]),
                  reads=[topv, self.identf], writes=[tb])
        kb.op("dve", lambda e: e.tensor_copy(out=idxT[:], in_=tb.h[:, 0:96].rearrange("p (h c) -> p h c", h=2)),
              reads=[tb], writes=[idxT])
        kb.op("dve", lambda e: e.tensor_copy(out=gT[:], in_=tb.h[:, 128:224].rearrange("p (h c) -> p h c", h=2)),
              reads=[tb], writes=[gT])
        if with_ctx:
            topk(workc, topvc, topic, 4)
            kb.op("dve", lambda e: e.tensor_copy(out=topicf[:], in_=topic[:]), reads=[topic], writes=[topicf])
            cbase = kb.alloc("cbase", [128, 1], F32)
            kb.dma("sp", lambda e: e.dma_start(out=cbase[:], in_=self.c_ctxbase.ap[:, :]), reads=[self.c_ctxbase], writes=[cbase])
            for (srcT, dstT, isidx) in ((topicf, idxC, True), (topvc, gC, False)):
                M = kb.alloc("Mc", [48, 128], F32)
                kb.op("dve", lambda e, M=M: e.memset(M[:], 0.0), writes=[M])
                kb.op("dve", lambda e, M=M, srcT=srcT: e.tensor_copy(out=M[0:16, 0:32], in_=srcT[0:16, 0:32]), reads=[srcT], writes=[M])
                kb.op("dve", lambda e, M=M, srcT=srcT: e.tensor_copy(out=M[32:48, 32:64], in_=srcT[32:48, 0:32]), reads=[srcT], writes=[M])
                kb.op("pe", lambda e, M=M: e.transpose(out=tb.h[:, 256:304], in_=M[0:48, :], identity=self.identf[0:48, 0:48]),
                      reads=[M, self.identf], writes=[tb])
                tcp = kb.alloc("tcp", [128, 48], F32)
                kb.op("dve", lambda e, tcp=tcp: e.tensor_copy(out=tcp[:], in_=tb.h[:, 256:304]), reads=[tb], writes=[tcp])
                tsum = kb.alloc("tsum", [128, NE], F32)
                kb.op("dve", lambda e, tcp=tcp, tsum=tsum: e.tensor_tensor(out=tsum[:], in0=tcp[:, 0:16], in1=tcp[:, 32:48], op=ALU.add),
                      reads=[tcp], writes=[tsum])
                if isidx:
                    kb.op("dve", lambda e, tsum=tsum: e.tensor_scalar(out=tsum[:], in0=tsum[:], scalar1=cbase[:, 0:1], scalar2=None, op0=ALU.add),
                          reads=[tsum, cbase], writes=[tsum])
                kb.op("dve", lambda e, tsum=tsum, dstT=dstT: e.tensor_copy(out=dstT[:], in_=tsum[:]), reads=[tsum], writes=[dstT])
        if dbg == 4:
            di = kb.dram("dbg_idx", [128, 96], I32, kind="ExternalOutput")
            dg = kb.dram("dbg_g", [128, 96], F32, kind="ExternalOutput")
            da = kb.dram("dbg_aff", [128, 1024], F32, kind="ExternalOutput")
            kb.dma("sp", lambda e: e.dma_start(out=di.ap[:, :], in_=idxT[:].rearrange("p h c -> p (h c)")), reads=[idxT], writes=[di])
            kb.dma("sp", lambda e: e.dma_start(out=dg.ap[:, :], in_=gT[:].rearrange("p h c -> p (h c)")), reads=[gT], writes=[dg])
            kb.dma("sp", lambda e: e.dma_start(out=da.ap[:, :], in_=aff2[:].rearrange("p h c -> p (h c)")), reads=[aff2], writes=[da])
            kb.release(m1)
            return
        kb.release(m1)

        G = []
        for s in range(NS):
            for h in range(2):
                G.append(dict(co=s * 256 + h * 128, idx=lambda e, s=s, h=h: idxT[:, h, s * 32 + e:s * 32 + e + 1],
                              gate=lambda e, s=s, h=h: gT[:, h, s * 32 + e:s * 32 + e + 1], r=s,
                              xn=self.xnl[s], dst=self.xr[s], n=L))
        if with_ctx:
            G.append(dict(co=512, idx=lambda e: idxC[:, e:e + 1], gate=lambda e: gC[:, e:e + 1], r=2,
                          xn=self.xnc_all, dst=self.xc_all, n=NS * LC + 128))
        NSL = 128 * len(G)
        HW = NSL // 2
        halves = [(0, HW), (HW, HW)]
        Xg = [[kb.alloc(f"xg{i}_{gi}", [128, D], BF16) for gi in range(len(G))] for i in range(2)]
        XeT = [kb.alloc(f"xeT{i}", [128, 8, NSL], BF16) for i in range(2)]
        hidT = kb.alloc("hidT", [128, 8, NSL], BF16)
        sgt = [kb.alloc(f"sgt{i}", [128, HW], F32) for i in range(2)]
        yo = [kb.alloc(f"yo{i}", [128, D], F32) for i in range(3)]
        nyo = 0
        nyb = 0

        def gathers(e):
            for gi, g in enumerate(G):
                xg = Xg[e % 2][gi]
                kb.dma("pool", lambda en, xg=xg, g=g, e=e: en.indirect_dma_start(
                    out=xg[:], out_offset=None, in_=g["xn"].ap[:, :], in_offset=IOA(ap=g["idx"](e), axis=0)),
                    reads=[g["xn"], idxT, idxC], writes=[xg])

        gathers(0)
        for e in range(NE):
            if e + 1 < NE:
                gathers(e + 1)
            w1t, w3t, w2t = wbufs[e % 2]
            xe = XeT[e % 2]
            for gi, g in enumerate(G):
                co, r = g["co"], g["r"]
                xg = Xg[e % 2][gi]
                for k in range(8):
                    bank = kb.banks[6 + k // 4]
                    pv = bank.h.bitcast(BF16)
                    kk = k % 4
                    kb.op("pe", lambda en, pv=pv, xg=xg, k=k, kk=kk: en.transpose(
                        out=pv[:, kk * 128:(kk + 1) * 128], in_=xg[:, k * 128:(k + 1) * 128],
                        identity=self.identb[:]), reads=[xg, self.identb], writes=[bank])
                for k in range(8):
                    bank = kb.banks[6 + k // 4]
                    pv = bank.h.bitcast(BF16)
                    kk = k % 4
                    dst = xe[:, k, co:co + 128]
                    if k < 4:
                        kb.op("act", lambda en, dst=dst, pv=pv, kk=kk, r=r, k=k: en.activation(
                            out=dst, in_=pv[:, kk * 128:(kk + 1) * 128], func=AF.Identity,
                            scale=mulc2[:, k, r:r + 1], bias=addc2(k, r)),
                            reads=[bank], writes=[xe.sub((k, co))])
                    else:
                        kb.op("dve", lambda en, dst=dst, pv=pv, kk=kk, r=r, k=k: en.tensor_scalar(
                            out=dst, in0=pv[:, kk * 128:(kk + 1) * 128], scalar1=mulc2[:, k, r:r + 1],
                            scalar2=addc2(k, r), op0=ALU.mult, op1=ALU.add),
                            reads=[bank], writes=[xe.sub((k, co))])
            for fc in range(8):
                for hi, (h0, hw) in enumerate(halves):
                    b1 = kb.banks[hi]
                    b3 = kb.banks[2 + hi]
                    for (wt, bm) in ((w1t, b1), (w3t, b3)):
                        for k in range(8):
                            kb.op("pe", lambda en, wt=wt, bm=bm, k=k, fc=fc, xe=xe, h0=h0, hw=hw: en.matmul(
                                out=bm.h[:, 0:hw], lhsT=wt[:, k, fc * 128:(fc + 1) * 128], rhs=xe[:, k, h0:h0 + hw],
                                start=(k == 0), stop=(k == 7)), reads=[wt, xe], writes=[bm])
                    sg = sgt[hi]
                    kb.op("act", lambda en, sg=sg, b1=b1, hw=hw: en.activation(out=sg[:, 0:hw], in_=b1.h[:, 0:hw], func=AF.Silu),
                          reads=[b1], writes=[sg])
                    kb.op("dve", lambda en, sg=sg, b3=b3, fc=fc, h0=h0, hw=hw: en.tensor_tensor(
                        out=hidT[:, fc, h0:h0 + hw], in0=sg[:, 0:hw], in1=b3.h[:, 0:hw], op=ALU.mult),
                        reads=[sg, b3], writes=[hidT.sub((fc, h0))])
            for gi, g in enumerate(G):
                co, r = g["co"], g["r"]
                yt = yo[nyo % 3]
                nyo += 1
                for db in range(2):
                    by = kb.banks[4 + nyb % 2]
                    nyb += 1
                    for fc in range(8):
                        kb.op("pe", lambda en, by=by, fc=fc, co=co, db=db, w2t=w2t: en.matmul(
                            out=by.h[:, 0:512], lhsT=hidT[:, fc, co:co + 128], rhs=w2t[:, fc, db * 512:(db + 1) * 512],
                            start=(fc == 0), stop=(fc == 7)), reads=[hidT, w2t], writes=[by])
                    kb.op("dve", lambda en, by=by, yt=yt, db=db, g=g, e=e, r=r: en.scalar_tensor_tensor(
                        out=yt[:, db * 512:(db + 1) * 512], in0=by.h[:, 0:512], scalar=g["gate"](e),
                        in1=g2b[r][:, db * 512:(db + 1) * 512], op0=ALU.mult, op1=ALU.mult),
                        reads=[by, gT, gC, g2b[r]], writes=[yt.sub(db)])
                kb.dma("pool", lambda en, yt=yt, g=g, e=e: en.indirect_dma_start(
                    out=g["dst"].ap[:, :], out_offset=IOA(ap=g["idx"](e), axis=0), in_=yt[:, :], in_offset=None,
                    compute_op=ALU.add, bounds_check=kb.bnd[g["n"] - 1], oob_is_err=True),
                    reads=[yt, idxT, idxC], writes=[g["dst"]])
            if e + 2 < NE:
                load_w(e + 2)

    def mixer0(self):
        kb = self.kb
        l = 0
        wbuf = kb.alloc("wbuf", [128, 8, 1536], BF16)
        wout = wbuf.view("woutv", [128, 8, D], BF16)
        csc = kb.alloc("csc", [128, 256], BF16)
        kb.dma("sp", lambda e: e.dma_start(out=csc[:], in_=self.c_csc.ap[:, :]), reads=[self.c_csc], writes=[csc])
        cwc = kb.alloc("cwc", [128, 4, 31], F32)
        for j in range(4):
            kb.dma("sp", lambda e, j=j: e.dma_start(out=cwc[:, j, :], in_=self.ab_conv_w.ap[:, j * 128:(j + 1) * 128].rearrange("k p -> p k")),
                   reads=[self.ab_conv_w], writes=[cwc.sub(j)])
        cols = {}
        for nm, dr in (("cb", self.ab_conv_b), ("lg", self.ab_ln_g), ("lb", self.ab_ln_b)):
            t = kb.alloc(nm + "c", [128, 4], F32)
            kb.dma("sp", lambda e, t=t, dr=dr: e.dma_start(out=t[:], in_=dr.ap.rearrange("(j p) -> p j", p=128)), reads=[dr], writes=[t])
            cols[nm] = t
        onesb = kb.alloc("onesb", [128, 128], BF16)
        kb.op("dve", lambda e: e.memset(onesb[:], 1.0 / 512.0), writes=[onesb])
        diag = kb.alloc("diag", [128, 4 * 31, 128], BF16)
        for j in range(4):
            for k in range(31):
                kb.op("dve", lambda e, j=j, k=k: e.tensor_scalar(
                    out=diag[:, j * 31 + k, :], in0=self.identb[:], scalar1=cwc[:, j, k:k + 1], scalar2=None, op0=ALU.mult),
                    reads=[self.identb, cwc], writes=[diag.sub((j, k))])
        bufs = self.norm_bufs(nx=1)
        xb = bufs[0][0]
        uTb = kb.alloc("uTb", [128, 8, 512], BF16)
        aT = kb.alloc("aT", [128, 4, L + 32], BF16)
        ufTb = kb.alloc("ufTb", [128, 4, 512], BF16)
        Yall = kb.alloc("Yall", [128, 16, 1024], BF16)
        mixT = kb.alloc("mixT", [128, 8, L], BF16)
        clb = kb.alloc("clb", [128, 16, 256], BF16)
        slb = kb.alloc("slb", [128, 16, 256], BF16)
        g1b = kb.alloc("g1b", [128, D], F32)
        NBC = 256
        cT = kb.alloc("cT", [128, 4, NBC], F32)
        cbt = kb.alloc("cbt", [128, 4, NBC], BF16)
        c2t = kb.alloc("c2t", [128, 4, NBC], BF16)
        t_mean = kb.alloc("t_mean", [128, NBC], F32)
        t_rstd = kb.alloc("t_rstd", [128, NBC], F32)
        t_tmp = kb.alloc("t_tmp", [128, 512], F32)
        t_tmp2 = kb.alloc("t_tmp2", [128, NBC], F32)
        addc1 = lambda k, r: self.modc[l][:, k, r:r + 1]
        mulc1 = self.mul1c[l]
        B = kb.banks
        seqs = [(self.xin[0], self.xr[0], L, 0, self.c_cl, self.c_sl), (self.xin[1], self.xr[1], L, 1, self.c_cl, self.c_sl),
                (self.cin[0], self.xc[0], LC, 2, self.c_clc, self.c_slc), (self.cin[1], self.xc[1], LC, 2, self.c_clc, self.c_slc)]
        for (src, dst, Ls, r, ctab, stab) in seqs:
            kb.dma("pool", lambda e: e.dma_start(out=wbuf[:], in_=self.ab_w_in.ap.rearrange("(k p) f -> p k f", p=128)),
                   reads=[self.ab_w_in], writes=[wbuf])
            kb.dma("sp", lambda e, r=r: e.dma_start(out=g1b[:], in_=self.modd.ap[l, r, 2 * D:3 * D].partition_broadcast(128)),
                   reads=[self.modd.sub(l)], writes=[g1b])
            for j in range(4):
                kb.op("dve", lambda e, j=j: e.memset(aT[:, j, 0:15], 0.0), writes=[aT.sub((j, "h0"))])
                kb.op("dve", lambda e, j=j, Ls=Ls: e.memset(aT[:, j, 15 + Ls:32 + Ls], 0.0), writes=[aT.sub((j, "h1"))])
            nb = min(512, Ls)
            for blk in range(Ls // nb):
                t0 = blk * nb
                for sb in range(nb // 256):
                    self.norm_batch(src, t0 + sb * 256, 2, mulc1, addc1, r, uTb, sb * 256, bufs, [B[0], B[1], B[2], B[3]])
                def zmm(j, bank):
                    for k in range(8):
                        kb.op("pe", lambda e, j=j, k=k, bank=bank, nb=nb: e.matmul(
                            out=bank.h[:, 0:nb], lhsT=wbuf[:, k, j * 128:(j + 1) * 128], rhs=uTb[:, k, 0:nb],
                            start=(k == 0), stop=(k == 7)), reads=[wbuf, uTb], writes=[bank])
                for jj in range(4):
                    zmm(jj, B[4])
                    zmm(jj + 4, B[5])
                    kb.op("act", lambda e, nb=nb: e.activation(out=t_tmp[:, 0:nb], in_=B[5].h[:, 0:nb], func=AF.Sigmoid),
                          reads=[B[5]], writes=[t_tmp])
                    kb.op("dve", lambda e, jj=jj, nb=nb, t0=t0: e.tensor_tensor(
                        out=aT[:, jj, 15 + t0:15 + t0 + nb], in0=t_tmp[:, 0:nb], in1=B[4].h[:, 0:nb], op=ALU.mult),
                        reads=[t_tmp, B[4]], writes=[aT.sub((jj, t0))])
                for g in range(4):
                    zmm(8 + g, B[6])
                    kb.op("act", lambda e, g=g, nb=nb: e.copy(out=ufTb[:, g, 0:nb], in_=B[6].h[:, 0:nb]),
                          reads=[B[6]], writes=[ufTb.sub(g)])
                for tc in range(nb // 128):
                    c = (t0 // 128) + tc
                    for gp in range(2):
                        for gg in range(2):
                            g = gp * 2 + gg
                            kb.op("pe", lambda e, g=g, gg=gg, tc=tc: e.matmul(
                                out=B[7].h[:, gg * 256:(gg + 1) * 256], lhsT=ufTb[:, g, tc * 128:(tc + 1) * 128], rhs=csc[:, :],
                                start=True, stop=True), reads=[ufTb, csc], writes=[B[7]])
                        kb.op("dve", lambda e, c=c, gp=gp: e.tensor_copy(out=Yall[:, c, gp * 512:(gp + 1) * 512], in_=B[7].h[:, 0:512]),
                              reads=[B[7]], writes=[Yall.sub((c, gp))])
            nbc = min(NBC, Ls)
            for blk in range(Ls // nbc):
                t0 = blk * nbc
                for j in range(4):
                    bank = B[j % 2]
                    for k in range(31):
                        kb.op("pe", lambda e, j=j, k=k, bank=bank, t0=t0, nbc=nbc: e.matmul(
                            out=bank.h[:, 0:nbc], lhsT=diag[:, j * 31 + k, :], rhs=aT[:, j, t0 + k:t0 + k + nbc],
                            start=(k == 0), stop=(k == 30)), reads=[diag, aT], writes=[bank])
                    kb.op("act", lambda e, j=j, bank=bank, nbc=nbc: e.activation(
                        out=cT[:, j, 0:nbc], in_=bank.h[:, 0:nbc], func=AF.Identity, bias=cols["cb"][:, j:j + 1]),
                        reads=[bank, cols["cb"]], writes=[cT.sub(j)])
                    kb.op("pool", lambda e, j=j, nbc=nbc: e.tensor_copy(out=cbt[:, j, 0:nbc], in_=cT[:, j, 0:nbc]),
                          reads=[cT.sub(j)], writes=[cbt.sub(j)])
                    kb.op("pool", lambda e, j=j, nbc=nbc: e.tensor_tensor(out=c2t[:, j, 0:nbc], in0=cT[:, j, 0:nbc], in1=cT[:, j, 0:nbc], op=ALU.mult),
                          reads=[cT.sub(j)], writes=[c2t.sub(j)])
                for j in range(4):
                    kb.op("pe", lambda e, j=j, nbc=nbc: e.matmul(out=B[2].h[:, 0:nbc], lhsT=onesb[:], rhs=cbt[:, j, 0:nbc],
                                                                 start=(j == 0), stop=(j == 3)), reads=[onesb, cbt], writes=[B[2]])
                for j in range(4):
                    kb.op("pe", lambda e, j=j, nbc=nbc: e.matmul(out=B[3].h[:, 0:nbc], lhsT=onesb[:], rhs=c2t[:, j, 0:nbc],
                                                                 start=(j == 0), stop=(j == 3)), reads=[onesb, c2t], writes=[B[3]])
                kb.op("dve", lambda e, nbc=nbc: e.tensor_copy(out=t_mean[:, 0:nbc], in_=B[2].h[:, 0:nbc]), reads=[B[2]], writes=[t_mean])
                kb.op("dve", lambda e, nbc=nbc: e.tensor_tensor(out=t_tmp2[:, 0:nbc], in0=t_mean[:, 0:nbc], in1=t_mean[:, 0:nbc], op=ALU.mult),
                      reads=[t_mean], writes=[t_tmp2])
                kb.op("dve", lambda e, nbc=nbc: e.tensor_tensor(out=t_rstd[:, 0:nbc], in0=B[3].h[:, 0:nbc], in1=t_tmp2[:, 0:nbc], op=ALU.subtract),
                      reads=[B[3], t_tmp2], writes=[t_rstd])
                kb.op("act", lambda e, nbc=nbc: e.activation(out=t_rstd[:, 0:nbc], in_=t_rstd[:, 0:nbc], func=AF.Identity, bias=self.epsc[:, 0:1]),
                      reads=[t_rstd, self.epsc], writes=[t_rstd])
                kb.op("act", lambda e, nbc=nbc: e.activation(out=t_rstd[:, 0:nbc], in_=t_rstd[:, 0:nbc], func=AF.Sqrt),
                      reads=[t_rstd], writes=[t_rstd])
                kb.op("dve", lambda e, nbc=nbc: e.reciprocal(out=t_rstd[:, 0:nbc], in_=t_rstd[:, 0:nbc]), reads=[t_rstd], writes=[t_rstd])
                for j in range(4):
                    kb.op("pool", lambda e, j=j, nbc=nbc: e.tensor_tensor(out=cT[:, j, 0:nbc], in0=cT[:, j, 0:nbc], in1=t_mean[:, 0:nbc], op=ALU.subtract),
                          reads=[cT.sub(j), t_mean], writes=[cT.sub(j)])
                    kb.op("dve", lambda e, j=j, nbc=nbc: e.tensor_tensor(out=cT[:, j, 0:nbc], in0=cT[:, j, 0:nbc], in1=t_rstd[:, 0:nbc], op=ALU.mult),
                          reads=[cT.sub(j), t_rstd], writes=[cT.sub(j)])
                    kb.op("act", lambda e, j=j, nbc=nbc, t0=t0: e.activation(
                        out=mixT[:, j, t0:t0 + nbc], in_=cT[:, j, 0:nbc], func=AF.Silu,
                        scale=cols["lg"][:, j:j + 1], bias=cols["lb"][:, j:j + 1]),
                        reads=[cT.sub(j), cols["lg"], cols["lb"]], writes=[mixT.sub((j, t0))])
            ntc = Ls // 128
            for kbi in range(Ls // 256):
                k0 = kbi * 256
                kb.dma("sp", lambda e, ctab=ctab, k0=k0, ntc=ntc: e.dma_start(
                    out=clb[:, 0:ntc, :], in_=ctab.ap.rearrange("(c p) k -> p c k", p=128)[:, :, k0:k0 + 256]),
                    reads=[ctab], writes=[clb])
                kb.dma("sp", lambda e, stab=stab, k0=k0, ntc=ntc: e.dma_start(
                    out=slb[:, 0:ntc, :], in_=stab.ap.rearrange("(c p) k -> p c k", p=128)[:, :, k0:k0 + 256]),
                    reads=[stab], writes=[slb])
                for g in range(4):
                    bank = B[4 + g % 2]
                    for c in range(ntc):
                        kb.op("pe", lambda e, g=g, c=c, bank=bank: e.matmul(
                            out=bank.h[:, 0:256], lhsT=Yall[:, c, g * 256:g * 256 + 128], rhs=clb[:, c, :],
                            start=(c == 0), stop=False), reads=[Yall, clb], writes=[bank])
                        kb.op("pe", lambda e, g=g, c=c, bank=bank, ntc=ntc: e.matmul(
                            out=bank.h[:, 0:256], lhsT=Yall[:, c, g * 256 + 128:g * 256 + 256], rhs=slb[:, c, :],
                            start=False, stop=(c == ntc - 1)), reads=[Yall, slb], writes=[bank])
                    if g % 2 == 0:
                        kb.op("act", lambda e, g=g, bank=bank, k0=k0: e.copy(out=mixT[:, 4 + g, k0:k0 + 256], in_=bank.h[:, 0:256]),
                              reads=[bank], writes=[mixT.sub((4 + g, k0))])
                    else:
                        kb.op("dve", lambda e, g=g, bank=bank, k0=k0: e.tensor_copy(out=mixT[:, 4 + g, k0:k0 + 256], in_=bank.h[:, 0:256]),
                              reads=[bank], writes=[mixT.sub((4 + g, k0))])
            kb.dma("pool", lambda e: e.dma_start(out=wout[:], in_=self.ab_w_out.ap.rearrange("(k p) f -> p k f", p=128)),
                   reads=[self.ab_w_out], writes=[wbuf])
            for tc in range(ntc):
                kb.dma("sp", lambda e, src=src, tc=tc: e.dma_start(out=xb[:], in_=src.ap[tc * 128:(tc + 1) * 128, :]),
                       reads=[src], writes=[xb])
                for db in range(2):
                    bank = B[6 + db]
                    for m in range(8):
                        kb.op("pe", lambda e, m=m, db=db, bank=bank, tc=tc: e.matmul(
                            out=bank.h[:, 0:512], lhsT=mixT[:, m, tc * 128:(tc + 1) * 128], rhs=wout[:, m, db * 512:(db + 1) * 512],
                            start=(m == 0), stop=(m == 7)), reads=[mixT, wbuf], writes=[bank])
                    kb.op("dve", lambda e, db=db, bank=bank: e.tensor_tensor(
                        out=t_tmp[:, 0:512], in0=bank.h[:, 0:512], in1=g1b[:, db * 512:(db + 1) * 512], op=ALU.mult),
                        reads=[bank, g1b], writes=[t_tmp])
                    kb.op("pool", lambda e, db=db: e.tensor_tensor(
                        out=xb[:, db * 512:(db + 1) * 512], in0=xb[:, db * 512:(db + 1) * 512], in1=t_tmp[:, 0:512], op=ALU.add),
                        reads=[xb, t_tmp], writes=[xb])
                kb.dma("sp", lambda e, dst=dst, tc=tc: e.dma_start(out=dst.ap[tc * 128:(tc + 1) * 128, :], in_=xb[:]),
                       reads=[xb], writes=[dst.sub(("row", tc))])


    def mla(self):
        kb = self.kb
        l = 1
        B = kb.banks
        NKV = LC + L
        NT = NKV // 128
        SCALE = 96.0 ** -0.5
        cast = lambda dst, src_ap, tr: kb.dma("pool", lambda e: e.dma_start(out=dst[:], in_=src_ap), reads=[tr], writes=[dst])
        wi = kb.alloc("wi", [128, 8, 416], BF16)
        cast(wi, self.mla_w_in.ap.rearrange("(k p) f -> p k f", p=128), self.mla_w_in)
        wuq = kb.alloc("wuq", [128, 2, 1536], BF16)
        cast(wuq, self.mla_w_uq.ap.rearrange("(k p) f -> p k f", p=128), self.mla_w_uq)
        wukv = kb.alloc("wukv", [128, 2048], BF16)
        cast(wukv, self.mla_w_ukv.ap[:, :], self.mla_w_ukv)
        wo = kb.alloc("wo", [128, 8, D], BF16)
        cast(wo, self.mla_w_o.ap.rearrange("(k p) f -> p k f", p=128), self.mla_w_o)
        ropt = kb.alloc("ropt", [128, 16, 32], F32)
        kb.dma("sp", lambda e: e.dma_start(out=ropt[:], in_=self.c_rope.ap.rearrange("(c p) f -> p c f", p=128)),
               reads=[self.c_rope], writes=[ropt])
        qgc = kb.alloc("qgc", [128, 2], F32)
        kb.dma("sp", lambda e: e.dma_start(out=qgc[:], in_=self.mla_qg.ap.rearrange("(j p) -> p j", p=128)), reads=[self.mla_qg], writes=[qgc])
        kvgc = kb.alloc("kvgc", [128, 1], F32)
        kb.dma("sp", lambda e: e.dma_start(out=kvgc[:], in_=self.mla_kvg.ap.rearrange("(j p) -> p j", p=128)), reads=[self.mla_kvg], writes=[kvgc])
        ones1 = kb.alloc("ones1", [128, 128], BF16)
        kb.op("dve", lambda e: e.memset(ones1[:], 1.0), writes=[ones1])
        bufs = self.norm_bufs(nx=1)
        xb = bufs[0][0]
        uT = kb.alloc("uTall", [128, 8, NKV], BF16)
        cqnT = kb.alloc("cqnT", [128, 2, L], BF16)
        ckvnT = kb.alloc("ckvnT", [128, NKV], BF16)
        KT = kb.alloc("KT", [128, NKV], BF16)
        QT = kb.alloc("QT", [128, L], BF16)
        qtok = kb.alloc("qtok", [128, 16, 96], BF16)
        Vaug = [kb.alloc(f"Vaug{i}", [128, NT, 128], BF16) for i in range(2)]
        kb.op("dve", lambda e: e.memset(Vaug[0][:].rearrange("p a b -> p (a b)"), 1.0), writes=[Vaug[0]])
        kb.op("dve", lambda e: e.memset(Vaug[1][:].rearrange("p a b -> p (a b)"), 1.0), writes=[Vaug[1]])
        pT = [kb.alloc(f"pT{i}", [128, 512], BF16) for i in range(4)]
        oTs = kb.alloc("oTs", [128, 512], F32)
        recf = kb.alloc("recf", [128, 512], F32)
        rech = kb.alloc("rech", [128, 512], BF16)
        recl = kb.alloc("recl", [128, 512], BF16)
        attnT = kb.alloc("attnT", [128, 8, L], BF16)
        g1b = kb.alloc("g1bm", [128, D], F32)
        t_tmp = kb.alloc("t_tmpm", [128, 512], F32)
        zt = kb.alloc("zt", [128, 416], F32)
        cqn = kb.alloc("cqn", [128, 256], BF16)
        ckvn = kb.alloc("ckvn", [128, 128], BF16)
        ktok = kb.alloc("ktok", [128, 96], BF16)
        kb.op("dve", lambda e: e.memset(ktok[:], 0.0), writes=[ktok])
        rt = [kb.alloc(f"rt{i}", [128, 4, 16], F32) for i in range(4)]
        st2 = kb.alloc("st2", [128, 8 * NT], F32)
        kb.op("dve", lambda e: e.memset(st2[:], 0.0), writes=[st2])
        junk2 = kb.alloc("junk2", [128, 256], BF16)
        addc1 = lambda k, r: self.modc[l][:, k, r:r + 1]
        mulc1 = self.mul1c[l]
        npt = 0
        for s in range(NS):
            kb.dma("sp", lambda e, s=s: e.dma_start(out=g1b[:], in_=self.modd.ap[l, s, 2 * D:3 * D].partition_broadcast(128)),
                   reads=[self.modd.sub(l)], writes=[g1b])
            kb.op("dve", lambda e: e.memset(st2[:], 0.0), writes=[st2])
            for b2 in range(LC // 256):
                self.norm_batch(self.xc[s], b2 * 256, 2, mulc1, addc1, 2, uT, b2 * 256, bufs, [B[0], B[1], B[2], B[3]])
            for b2 in range(L // 256):
                self.norm_batch(self.xr[s], b2 * 256, 2, mulc1, addc1, s, uT, LC + b2 * 256, bufs, [B[0], B[1], B[2], B[3]])
            for c in range(NT):
                lat = c >= 2
                cl_ = c - 2
                zb = B[4 + c % 2]
                for k in range(8):
                    kb.op("pe", lambda e, c=c, k=k, zb=zb: e.matmul(out=zb.h[:, 0:416], lhsT=uT[:, k, c * 128:(c + 1) * 128], rhs=wi[:, k, :],
                                                                  start=(k == 0), stop=(k == 7)), reads=[uT, wi], writes=[zb])
                kb.op("act", lambda e, zb=zb: e.copy(out=zt[:], in_=zb.h[:, 0:416]), reads=[zb], writes=[zt])
                so = c * 8
                parts = [(256, 128, 128.0, ckvn, 0)] + ([(0, 256, 256.0, cqn, 4)] if lat else [])
                for (c0, n, nf, dstt, o) in parts:
                    kb.op("act", lambda e, c0=c0, n=n, so=so, o=o: e.activation(out=junk2[:, 0:n], in_=zt[:, c0:c0 + n], func=AF.Square,
                                                                             accum_out=st2[:, so + o:so + o + 1]), reads=[zt], writes=[junk2, st2.sub(so + o)])
                    kb.op("act", lambda e, so=so, o=o, nf=nf: e.activation(out=st2[:, so + o + 1:so + o + 2], in_=st2[:, so + o:so + o + 1], func=AF.Identity,
                                                                         scale=1.0 / nf, bias=self.epsc[:, 0:1]), reads=[st2.sub(so + o), self.epsc], writes=[st2.sub(so + o + 1)])
                    kb.op("act", lambda e, so=so, o=o: e.activation(out=st2[:, so + o + 2:so + o + 3], in_=st2[:, so + o + 1:so + o + 2], func=AF.Sqrt),
                          reads=[st2.sub(so + o + 1)], writes=[st2.sub(so + o + 2)])
                    kb.op("dve", lambda e, so=so, o=o: e.reciprocal(out=st2[:, so + o + 3:so + o + 4], in_=st2[:, so + o + 2:so + o + 3]),
                          reads=[st2.sub(so + o + 2)], writes=[st2.sub(so + o + 3)])
                    kb.op("act", lambda e, c0=c0, n=n, so=so, o=o, dstt=dstt: e.activation(out=dstt[:, 0:n], in_=zt[:, c0:c0 + n], func=AF.Identity,
                                                                                          scale=st2[:, so + o + 3:so + o + 4]), reads=[zt, st2.sub(so + o + 3)], writes=[dstt])
                if lat:
                    krv = zt[:, 384:416].rearrange("p (i two) -> p i two", two=2)
                    xe, xo = krv[:, :, 0], krv[:, :, 1]
                    cs, sn = ropt[:, cl_, 0:16], ropt[:, cl_, 16:32]
                    ko = ktok[:, 64:96].rearrange("p (i two) -> p i two", two=2)
                    a0, a1, a2, a3 = rt[0][:, 0, :], rt[1][:, 0, :], rt[2][:, 0, :], rt[3][:, 0, :]
                    kb.op("dve", lambda e, xe=xe, cs=cs, a0=a0: e.tensor_tensor(out=a0, in0=xe, in1=cs, op=ALU.mult), reads=[zt, ropt], writes=[rt[0]])
                    kb.op("dve", lambda e, xo=xo, sn=sn, a1=a1: e.tensor_tensor(out=a1, in0=xo, in1=sn, op=ALU.mult), reads=[zt, ropt], writes=[rt[1]])
                    kb.op("dve", lambda e, xe=xe, sn=sn, a2=a2: e.tensor_tensor(out=a2, in0=xe, in1=sn, op=ALU.mult), reads=[zt, ropt], writes=[rt[2]])
                    kb.op("dve", lambda e, xo=xo, cs=cs, a3=a3: e.tensor_tensor(out=a3, in0=xo, in1=cs, op=ALU.mult), reads=[zt, ropt], writes=[rt[3]])
                    kb.op("dve", lambda e, ko=ko, a0=a0, a1=a1: e.tensor_tensor(out=ko[:, :, 0], in0=a0, in1=a1, op=ALU.subtract), reads=[rt[0], rt[1]], writes=[ktok.sub(0)])
                    kb.op("dve", lambda e, ko=ko, a2=a2, a3=a3: e.tensor_tensor(out=ko[:, :, 1], in0=a2, in1=a3, op=ALU.add), reads=[rt[2], rt[3]], writes=[ktok.sub(1)])
                else:
                    kb.op("dve", lambda e: e.tensor_copy(out=ktok[:, 64:96], in_=zt[:, 384:416]), reads=[zt], writes=[ktok.sub(0)])
                tbk = B[6 + c % 2]
                pv = tbk.h.bitcast(BF16)
                if lat:
                    for qk in range(2):
                        kb.op("pe", lambda e, pv=pv, qk=qk: e.transpose(out=pv[:, qk * 128:(qk + 1) * 128], in_=cqn[:, qk * 128:(qk + 1) * 128], identity=self.identb[:]),
                              reads=[cqn, self.identb], writes=[tbk])
                kb.op("pe", lambda e, pv=pv: e.transpose(out=pv[:, 256:384], in_=ckvn[:, :], identity=self.identb[:]), reads=[ckvn, self.identb], writes=[tbk])
                kb.op("pe", lambda e, pv=pv: e.transpose(out=pv[0:96, 384:512], in_=ktok[:, 0:96], identity=self.identb[:]), reads=[ktok, self.identb], writes=[tbk])
                if lat:
                    for qk in range(2):
                        kb.op("act", lambda e, pv=pv, qk=qk, cl_=cl_: e.activation(out=cqnT[:, qk, cl_ * 128:(cl_ + 1) * 128], in_=pv[:, qk * 128:(qk + 1) * 128],
                                                                                 func=AF.Identity, scale=qgc[:, qk:qk + 1]), reads=[tbk, qgc], writes=[cqnT.sub((qk, cl_))])
                kb.op("act", lambda e, pv=pv, c=c: e.activation(out=ckvnT[:, c * 128:(c + 1) * 128], in_=pv[:, 256:384], func=AF.Identity, scale=kvgc[:, 0:1]),
                      reads=[tbk, kvgc], writes=[ckvnT.sub(c)])
                kb.op("act", lambda e, pv=pv, c=c: e.copy(out=KT[64:96, c * 128:(c + 1) * 128], in_=pv[64:96, 384:512]),
                      reads=[tbk], writes=[KT.sub(("r", c))])
            for h in range(NE):
                even = (h % 2 == 0)
                for blk in range(5):
                    n0 = blk * 512
                    nn = min(512, NKV - n0)
                    bk = B[5]
                    kb.op("pe", lambda e, h=h, n0=n0, nn=nn, bk=bk: e.matmul(out=bk.h[0:64, 0:nn], lhsT=wukv[:, h * 128:h * 128 + 64], rhs=ckvnT[:, n0:n0 + nn],
                                                                        start=True, stop=True), reads=[wukv, ckvnT], writes=[bk])
                    kb.op("dve", lambda e, n0=n0, nn=nn, bk=bk: e.tensor_copy(out=KT[0:64, n0:n0 + nn], in_=bk.h[0:64, 0:nn]), reads=[bk], writes=[KT.sub(("n", blk))])
                va = Vaug[h % 2]
                vo = 0 if even else 64
                for vb in range(3):
                    ntl = min(8, NT - vb * 8)
                    bv = B[6]
                    for ci in range(ntl):
                        c = vb * 8 + ci
                        kb.op("pe", lambda e, h=h, c=c, ci=ci, bv=bv: e.matmul(out=bv.h[:, ci * 64:(ci + 1) * 64], lhsT=ckvnT[:, c * 128:(c + 1) * 128],
                                                                             rhs=wukv[:, h * 128 + 64:h * 128 + 128], start=True, stop=True), reads=[ckvnT, wukv], writes=[bv])
                    kb.op("act", lambda e, vb=vb, ntl=ntl, bv=bv, va=va, vo=vo: e.copy(
                        out=va[:, vb * 8:vb * 8 + ntl, vo:vo + 64], in_=bv.h[:, 0:ntl * 64].rearrange("p (c f) -> p c f", f=64)),
                        reads=[bv], writes=[va.sub(vb)])
                for half in range(2):
                    for bq in range(2):
                        bqk = B[4 + bq]
                        for ci in range(4):
                            c = half * 8 + bq * 4 + ci
                            for qk in range(2):
                                kb.op("pe", lambda e, h=h, c=c, ci=ci, qk=qk, bqk=bqk: e.matmul(
                                    out=bqk.h[:, ci * 96:(ci + 1) * 96], lhsT=cqnT[:, qk, c * 128:(c + 1) * 128], rhs=wuq[:, qk, h * 96:(h + 1) * 96],
                                    start=(qk == 0), stop=(qk == 1)), reads=[cqnT, wuq], writes=[bqk])
                        c0 = half * 8 + bq * 4
                        qv = bqk.h[:, 0:384].rearrange("p (c f) -> p c f", f=96)
                        kb.op("act", lambda e, qv=qv, c0=c0: e.copy(out=qtok[:, c0:c0 + 4, 0:64], in_=qv[:, :, 0:64]), reads=[bqk], writes=[qtok.sub((c0, "n"))])
                        xe, xo = qv[:, :, 64:96:2], qv[:, :, 65:96:2]
                        cs, sn = ropt[:, c0:c0 + 4, 0:16], ropt[:, c0:c0 + 4, 16:32]
                        qo = qtok[:, c0:c0 + 4, 64:96].rearrange("p c (i two) -> p c i two", two=2)
                        kb.op("dve", lambda e, xe=xe, cs=cs: e.tensor_tensor(out=rt[0][:], in0=xe, in1=cs, op=ALU.mult), reads=[bqk, ropt], writes=[rt[0]])
                        kb.op("dve", lambda e, xo=xo, sn=sn: e.tensor_tensor(out=rt[1][:], in0=xo, in1=sn, op=ALU.mult), reads=[bqk, ropt], writes=[rt[1]])
                        kb.op("dve", lambda e, xe=xe, sn=sn: e.tensor_tensor(out=rt[2][:], in0=xe, in1=sn, op=ALU.mult), reads=[bqk, ropt], writes=[rt[2]])
                        kb.op("dve", lambda e, xo=xo, cs=cs: e.tensor_tensor(out=rt[3][:], in0=xo, in1=cs, op=ALU.mult), reads=[bqk, ropt], writes=[rt[3]])
                        kb.op("dve", lambda e, qo=qo: e.tensor_tensor(out=qo[:, :, :, 0], in0=rt[0][:], in1=rt[1][:], op=ALU.subtract),
                              reads=[rt[0], rt[1]], writes=[qtok.sub((c0, "e"))])
                        kb.op("dve", lambda e, qo=qo: e.tensor_tensor(out=qo[:, :, :, 1], in0=rt[2][:], in1=rt[3][:], op=ALU.add),
                              reads=[rt[2], rt[3]], writes=[qtok.sub((c0, "o"))])
                        tq = B[6 + bq]
                        pvq = tq.h.bitcast(BF16)
                        for ci in range(4):
                            c = c0 + ci
                            kb.op("pe", lambda e, pvq=pvq, c=c, ci=ci: e.transpose(out=pvq[0:96, ci * 128:(ci + 1) * 128], in_=qtok[:, c, 0:96], identity=self.identb[:]),
                                  reads=[qtok, self.identb], writes=[tq])
                        kb.op("act", lambda e, pvq=pvq, c0=c0: e.copy(out=QT[0:96, c0 * 128:(c0 + 4) * 128], in_=pvq[0:96, 0:512]), reads=[tq], writes=[QT.sub(c0)])
                dp = 64 if even else 0
                op_ = 0 if even else 64
                for qb in range(4):
                    bo = B[2 + qb % 2]
                    for c in range(NT):
                        bs = B[c % 2]
                        kb.op("pe", lambda e, c=c, qb=qb, bs=bs: e.matmul(out=bs.h[:, 0:512], lhsT=KT[0:96, c * 128:(c + 1) * 128], rhs=QT[0:96, qb * 512:(qb + 1) * 512],
                                                                        start=True, stop=True), reads=[KT, QT], writes=[bs])
                        pt = pT[npt % 4]
                        npt += 1
                        kb.op("act", lambda e, bs=bs, pt=pt: e.activation(out=pt[:], in_=bs.h[:, 0:512], func=AF.Exp, scale=SCALE), reads=[bs], writes=[pt])
                        kb.op("pe", lambda e, c=c, bo=bo, pt=pt, va=va: e.matmul(out=bo.h[:, 0:512], lhsT=va[:, c, :], rhs=pt[:],
                                                                               start=(c == 0), stop=(c == NT - 1)), reads=[va, pt], writes=[bo])
                    kb.op("act", lambda e, bo=bo: e.copy(out=oTs[:], in_=bo.h[:, 0:512]), reads=[bo], writes=[oTs])
                    kb.op("dve", lambda e, dp=dp: e.reciprocal(out=recf[dp:dp + 1, :], in_=oTs[dp:dp + 1, :]), reads=[oTs], writes=[recf])
                    kb.op("dve", lambda e, dp=dp: e.tensor_copy(out=rech[dp:dp + 1, :], in_=recf[dp:dp + 1, :]), reads=[recf], writes=[rech])
                    kb.op("dve", lambda e, dp=dp: e.tensor_tensor(out=recl[dp:dp + 1, :], in0=recf[dp:dp + 1, :], in1=rech[dp:dp + 1, :], op=ALU.subtract),
                          reads=[recf, rech], writes=[recl])
                    bb = B[4]
                    kb.op("pe", lambda e, dp=dp, bb=bb: e.matmul(out=bb.h[:, 0:512], lhsT=ones1[dp:dp + 1, :], rhs=rech[dp:dp + 1, :], start=True, stop=False),
                          reads=[ones1, rech], writes=[bb])
                    kb.op("pe", lambda e, dp=dp, bb=bb: e.matmul(out=bb.h[:, 0:512], lhsT=ones1[dp:dp + 1, :], rhs=recl[dp:dp + 1, :], start=False, stop=True),
                          reads=[ones1, recl], writes=[bb])
                    kb.op("dve", lambda e, op_=op_, h=h, qb=qb, bb=bb: e.tensor_tensor(
                        out=attnT[op_:op_ + 64, h // 2, qb * 512:(qb + 1) * 512], in0=oTs[op_:op_ + 64, :], in1=bb.h[op_:op_ + 64, 0:512], op=ALU.mult),
                        reads=[oTs, bb], writes=[attnT.sub((h, qb))])
            for tc in range(L // 128):
                kb.dma("sp", lambda e, s=s, tc=tc: e.dma_start(out=xb[:], in_=self.xr[s].ap[tc * 128:(tc + 1) * 128, :]),
                       reads=[self.xr[s]], writes=[xb])
                for db in range(2):
                    bank = B[6 + db]
                    for m in range(8):
                        kb.op("pe", lambda e, m=m, db=db, bank=bank, tc=tc: e.matmul(
                            out=bank.h[:, 0:512], lhsT=attnT[:, m, tc * 128:(tc + 1) * 128], rhs=wo[:, m, db * 512:(db + 1) * 512],
                            start=(m == 0), stop=(m == 7)), reads=[attnT, wo], writes=[bank])
                    kb.op("dve", lambda e, db=db, bank=bank: e.tensor_tensor(
                        out=t_tmp[:, 0:512], in0=bank.h[:, 0:512], in1=g1b[:, db * 512:(db + 1) * 512], op=ALU.mult),
                        reads=[bank, g1b], writes=[t_tmp])
                    kb.op("pool", lambda e, db=db: e.tensor_tensor(
                        out=xb[:, db * 512:(db + 1) * 512], in0=xb[:, db * 512:(db + 1) * 512], in1=t_tmp[:, 0:512], op=ALU.add),
                        reads=[xb, t_tmp], writes=[xb])
                kb.dma("sp", lambda e, s=s, tc=tc: e.dma_start(out=self.xr[s].ap[tc * 128:(tc + 1) * 128, :], in_=xb[:]),
                       reads=[xb], writes=[self.xr[s].sub(("row", tc))])


def _consts():
    bf = ml_dtypes.bfloat16
    c = {}
    c["c_identb"] = np.eye(128, dtype=np.float32).astype(bf)
    c["c_identf"] = np.eye(128, dtype=np.float32)
    i = np.arange(128, dtype=np.int64)
    ang = 2.0 * np.pi * ((i[:, None] * i[None, :]) % 128).astype(np.float64) / 128.0
    c["c_csc"] = np.concatenate([np.cos(ang), np.sin(ang)], axis=1).astype(np.float32) / np.float32(np.sqrt(128.0))
    c["c_csc"] = c["c_csc"].astype(bf)
    for nm, n in (("", L), ("c", LC)):
        t = np.arange(n, dtype=np.int64)
        a = 2.0 * np.pi * ((t[:, None] * t[None, :]) % n).astype(np.float64) / n
        c["c_cl" + nm] = (np.cos(a) / np.sqrt(n)).astype(np.float32).astype(bf)
        c["c_sl" + nm] = (-np.sin(a) / np.sqrt(n)).astype(np.float32).astype(bf)
    t = np.arange(L)
    row = (t // 64).astype(np.float32)
    col = (t % 64).astype(np.float32)
    inv = (np.float32(10000.0) ** (-np.arange(8, dtype=np.float32) / np.float32(8))).astype(np.float32)
    angr = np.concatenate([row[:, None] * inv[None, :], col[:, None] * inv[None, :]], axis=1).astype(np.float32)
    c["c_rope"] = np.concatenate([np.cos(angr), np.sin(angr)], axis=1).astype(np.float32)
    c["c_ctxbase"] = np.concatenate([np.zeros(32), np.full(32, LC), NS * LC + np.arange(64)]).astype(np.float32).reshape(128, 1)
    return c


def _in_map(inp, core, consts):
    f = lambda a: np.ascontiguousarray(np.asarray(a, dtype=np.float32))
    s0 = core * NS
    m = {}
    m["x"] = f(inp["x"][s0:s0 + NS])
    m["ctx"] = f(inp["ctx"][s0:s0 + NS])
    cv3 = np.stack([inp["c"][s0], inp["c"][s0 + 1], inp["c_ctx"]], axis=0).astype(np.float32)
    m["cv"] = np.ascontiguousarray(cv3.reshape(3, 8, 128).transpose(2, 1, 0))
    for k in ("mod_w", "mod_b", "norm1_g", "norm2_g", "final_g", "moe_w_router", "moe_w1", "moe_w3", "moe_w2"):
        m[k] = f(inp[k])
    for k in ("ab_w_in", "ab_conv_w", "ab_conv_b", "ab_ln_g", "ab_ln_b", "ab_w_out", "mla_w_in", "mla_q_norm_g",
              "mla_kv_norm_g", "mla_w_uq", "mla_w_ukv", "mla_w_o"):
        m[k] = f(inp[k][0])
    m.update(consts)
    return m


_CACHE = {}


def run_prog(inputs, phases, copy_in=False, ncores=8, debug_route=False, raw=False):
    key = (tuple(phases), copy_in, debug_route)
    if key not in _CACHE:
        p = Prog(phases=phases, copy_in=copy_in)
        p.debug_route = debug_route
        _CACHE[key] = p.build()
    nc = _CACHE[key]
    consts = _consts()
    in_maps = [_in_map(inputs, c, consts) for c in range(ncores)]
    res = run_bass_kernel_spmd(nc, in_maps, core_ids=list(range(ncores)))
    if raw:
        return res.results
    return np.concatenate([np.asarray(r["y"]) for r in res.results], axis=0)


def kernel(**inputs):
    out = run_prog(inputs, ("mix0", "moe0", "mla1", "moe1", "final"))
    return out.astype(np.float32)
```

```python
import numpy as np
import ml_dtypes
from contextlib import ExitStack
import concourse.bass as bass
import concourse.mybir as mybir
from concourse.bass_utils import run_bass_kernel_spmd

F32 = mybir.dt.float32
BF16 = mybir.dt.bfloat16
I32 = mybir.dt.int32
U32 = mybir.dt.uint32
U8 = mybir.dt.uint8
AF = mybir.ActivationFunctionType
ALU = mybir.AluOpType
AX = mybir.AxisListType

D = 1024
L = 2048
LC = 256
NS = 2
NE = 16
EPS = 1e-6
DSZ = {F32: 4, BF16: 2, I32: 4, U32: 4, U8: 1}


class Trk:
    def __init__(self, name):
        self.name = name
        self.w = None
        self.r = []
        self.kids = {}
        self.parent = None

    def sub(self, key):
        if key not in self.kids:
            k = Trk(f"{self.name}.{key}")
            k.parent = self
            self.kids[key] = k
        return self.kids[key]

    def rdeps(self):
        s = set()
        if self.w:
            s.add(self.w)
        if self.parent is not None and self.parent.w:
            s.add(self.parent.w)
        for k in self.kids.values():
            if k.w:
                s.add(k.w)
        return s

    def wdeps(self):
        s = self.rdeps()
        s.update(self.r)
        if self.parent is not None:
            s.update(self.parent.r)
        for k in self.kids.values():
            s.update(k.r)
        return s

    def did_read(self, ev):
        self.r.append(ev)

    def did_write(self, ev):
        self.w = ev
        self.r = []
        for k in self.kids.values():
            k.w = None
            k.r = []


class T(Trk):
    def __init__(self, kb, name, shape, dtype, off):
        super().__init__(name)
        self.kb = kb
        self.shape = shape
        self.dtype = dtype
        self.off = off
        self.h = kb.nc.alloc_sbuf_tensor_at(name, list(shape), dtype, offset=off)

    def view(self, name, shape, dtype, boff=0):
        return self.kb.nc.alloc_sbuf_tensor_at(
            self.kb.uname(name), list(shape), dtype, offset=self.off + boff)

    def __getitem__(self, k):
        return self.h[k]


class Lane:
    def __init__(self, key, sem):
        self.key = key
        self.sem = sem
        self.count = 0


class KB:
    COMPUTE = ["pe", "act", "dve", "pool"]
    QUEUES = ["sp", "pool", "act"]

    def __init__(self, n_lanes=8):
        self.nc = bass.Bass("TRN2", target_bir_lowering=False)
        nc = self.nc
        self.es = ExitStack()
        self.uid = 0
        self.semobj = {}
        self.cnt = {}
        for e in self.COMPUTE:
            self.semobj[e] = self.es.enter_context(nc.semaphore("s_" + e))
            self.cnt[e] = 0
        self.lanes = {}
        self.lane_rr = {}
        for q in self.QUEUES:
            self.lanes[q] = []
            for i in range(n_lanes):
                key = f"d_{q}{i}"
                self.semobj[key] = self.es.enter_context(nc.semaphore(key))
                self.lanes[q].append(Lane(key, self.semobj[key]))
            self.lane_rr[q] = 0
        self.prog = {e: [] for e in ["pe", "act", "dve", "pool", "sp"]}
        self.waited = {e: {} for e in ["pe", "act", "dve", "pool", "sp"]}
        self.arena_bytes = 204 * 1024
        ah = nc.alloc_sbuf_tensor("arena", [128, self.arena_bytes], U8)
        self.abase = nc.lookup_mloc(ah).addr
        self.atop = 0
        self.bnd = {}
        for n in (L - 1, NS * LC + 128 - 1):
            reg = self.es.enter_context(nc.gpsimd.register(f"bnd{n}"))
            self.bnd[n] = reg
            self.prog["pool"].append(lambda en, reg=reg, n=n: en.reg_mov(reg, n))
        self.banks = []
        for i in range(8):
            h = self.es.enter_context(nc.psum_tensor(f"bank{i}", [128, 512], F32))
            t = Trk(f"bank{i}")
            t.h = h
            t.psum = True
            self.banks.append(t)

    def uname(self, n):
        self.uid += 1
        return f"{n}_{self.uid}"

    def alloc(self, name, shape, dtype):
        nbytes = int(np.prod(shape[1:])) * DSZ[dtype]
        nbytes = (nbytes + 63) // 64 * 64
        off = self.atop
        assert off + nbytes <= self.arena_bytes, f"SBUF arena overflow at {name}: {off}+{nbytes}"
        self.atop += nbytes
        return T(self, self.uname(name), shape, dtype, self.abase + off)

    def mark(self):
        return self.atop

    def release(self, m):
        self.barrier()
        self.atop = m

    def dram(self, name, shape, dtype, kind="Internal"):
        if kind == "Internal":
            h = self.nc.dram_tensor(name, list(shape), dtype)
        else:
            h = self.nc.dram_tensor(name, list(shape), dtype, kind=kind)
        t = Trk(name)
        t.h = h
        t.ap = h.ap()
        return t

    def _waits(self, eng, evs):
        best = {}
        for (k, v) in evs:
            if v > best.get(k, 0):
                best[k] = v
        for k, v in best.items():
            if k == "pe" and eng == "pe":
                continue
            if self.waited[eng].get(k, 0) >= v:
                continue
            self.waited[eng][k] = v
            sem = self.semobj[k]
            self.prog[eng].append(lambda e, sem=sem, v=v: e.wait_ge(sem, v))

    def _deps(self, reads, writes, eng=None):
        evs = set()
        for t in reads:
            evs |= t.rdeps()
            root = t if t.parent is None else t.parent
            if getattr(root, "psum", False):
                for ev in root.r:
                    if ev[0] != eng:
                        evs.add(ev)
                for k in root.kids.values():
                    for ev in k.r:
                        if ev[0] != eng:
                            evs.add(ev)
        for t in writes:
            evs |= t.wdeps()
        return evs

    def op(self, eng, fn, reads=(), writes=()):
        evs = self._deps(reads, writes, eng)
        self._waits(eng, evs)
        self.cnt[eng] += 1
        sem = self.semobj[eng]
        self.prog[eng].append(lambda e, fn=fn, sem=sem: fn(e).then_inc(sem, 1))
        ev = (eng, self.cnt[eng])
        for t in reads:
            t.did_read(ev)
        for t in writes:
            t.did_write(ev)
        return ev

    def dma(self, q, fn, reads=(), writes=()):
        evs = self._deps(reads, writes)
        lanes = self.lanes[q]
        lane = lanes[self.lane_rr[q] % len(lanes)]
        self.lane_rr[q] += 1
        if lane.count > 0:
            evs.add((lane.key, lane.count))
        self._waits(q, evs)
        lane.count += 16
        sem = lane.sem
        def run(e, fn=fn, sem=sem):
            try:
                ins = fn(e)
            except Exception:
                print("DMA BUILD FAIL line", fn.__code__.co_firstlineno, "defaults", [str(d)[:80] for d in (fn.__defaults__ or ())])
                raise
            ins.then_inc(sem, 16)
        self.prog[q].append(run)
        ev = (lane.key, lane.count)
        for t in reads:
            t.did_read(ev)
        for t in writes:
            t.did_write(ev)
        return ev

    def barrier(self):
        evs = set()
        for e in self.COMPUTE:
            if self.cnt[e] > 0:
                evs.add((e, self.cnt[e]))
        for q in self.QUEUES:
            for ln in self.lanes[q]:
                if ln.count > 0:
                    evs.add((ln.key, ln.count))
        for e in ["pe", "act", "dve", "pool", "sp"]:
            self._waits(e, evs)

    def finish(self):
        self.barrier()
        nc = self.nc
        with nc.allow_non_contiguous_dma(reason="small strided constant loads"):
            with nc.Block() as block:
                @block.sync
                def _(e):
                    for f in self.prog["sp"]:
                        f(e)

                @block.tensor
                def _(e):
                    for f in self.prog["pe"]:
                        f(e)

                @block.scalar
                def _(e):
                    for f in self.prog["act"]:
                        f(e)

                @block.vector
                def _(e):
                    for f in self.prog["dve"]:
                        f(e)

                @block.gpsimd
                def _(e):
                    for f in self.prog["pool"]:
                        f(e)
        self.es.close()
        return nc


class Prog:
    def __init__(self, phases=("mix0", "moe0", "mla1", "moe1", "final"), copy_in=False):
        self.kb = KB()
        self.phases = phases
        self.copy_in = copy_in
        kb = self.kb
        di = lambda n, s, d=F32: kb.dram(n, s, d, kind="ExternalInput")
        self.x = di("x", [NS, L, D])
        self.ctx = di("ctx", [NS, LC, D])
        self.cv = di("cv", [128, 8, 3])
        self.mod_w = di("mod_w", [2, D, 6 * D])
        self.mod_b = di("mod_b", [2, 6 * D])
        self.n1g = di("norm1_g", [2, D])
        self.n2g = di("norm2_g", [2, D])
        self.final_g = di("final_g", [D])
        self.ab_w_in = di("ab_w_in", [D, 1536])
        self.ab_conv_w = di("ab_conv_w", [31, 512])
        self.ab_conv_b = di("ab_conv_b", [512])
        self.ab_ln_g = di("ab_ln_g", [512])
        self.ab_ln_b = di("ab_ln_b", [512])
        self.ab_w_out = di("ab_w_out", [D, D])
        self.mla_w_in = di("mla_w_in", [D, 416])
        self.mla_qg = di("mla_q_norm_g", [256])
        self.mla_kvg = di("mla_kv_norm_g", [128])
        self.mla_w_uq = di("mla_w_uq", [256, 1536])
        self.mla_w_ukv = di("mla_w_ukv", [128, 2048])
        self.mla_w_o = di("mla_w_o", [D, D])
        self.w_router = di("moe_w_router", [2, D, NE])
        self.w1 = di("moe_w1", [2, NE, D, D])
        self.w3 = di("moe_w3", [2, NE, D, D])
        self.w2 = di("moe_w2", [2, NE, D, D])
        self.c_identb = di("c_identb", [128, 128], BF16)
        self.c_identf = di("c_identf", [128, 128], F32)
        self.c_csc = di("c_csc", [128, 256], BF16)
        self.c_cl = di("c_cl", [L, L], BF16)
        self.c_sl = di("c_sl", [L, L], BF16)
        self.c_clc = di("c_clc", [LC, LC], BF16)
        self.c_slc = di("c_slc", [LC, LC], BF16)
        self.c_rope = di("c_rope", [L, 32], F32)
        self.c_ctxbase = di("c_ctxbase", [128, 1], F32)
        self.out = kb.dram("y", [NS, L, D], F32, kind="ExternalOutput")
        self.xr = [kb.dram(f"xr{s}", [L, D], F32) for s in range(NS)]
        self.xc_all = kb.dram("xc_all", [NS * LC + 128, D], F32)
        self.xnc_all = kb.dram("xnc_all", [NS * LC + 128, D], BF16)
        self.xc = []
        self.xnl = [kb.dram(f"xnl{s}", [L, D], BF16) for s in range(NS)]
        self.xnc = []
        for s in range(NS):
            t = self.xc_all.sub(s); t.ap = self.xc_all.ap[s * LC:(s + 1) * LC, :]; self.xc.append(t)
            t = self.xnc_all.sub(s); t.ap = self.xnc_all.ap[s * LC:(s + 1) * LC, :]; self.xnc.append(t)
        self.xin = []
        self.cin = []
        for s in range(NS):
            t = self.x.sub(s); t.ap = self.x.ap[s]; self.xin.append(t)
            t = self.ctx.sub(s); t.ap = self.ctx.ap[s]; self.cin.append(t)
        self.modd = kb.dram("modd", [2, 3, 6 * D], F32)

    def build(self):
        kb = self.kb
        self.prologue()
        if self.copy_in:
            for s in range(NS):
                kb.dma("sp", lambda e, s=s: e.dma_start(out=self.xr[s].ap, in_=self.x.ap[s]),
                       reads=[self.x], writes=[self.xr[s]])
                kb.dma("sp", lambda e, s=s: e.dma_start(out=self.xc[s].ap, in_=self.ctx.ap[s]),
                       reads=[self.ctx], writes=[self.xc[s]])
            kb.barrier()
        for ph in self.phases:
            m = kb.mark()
            if ph == "mix0":
                self.mixer0()
            elif ph == "moe0":
                self.moe(0, with_ctx=True)
            elif ph == "mla1":
                self.mla()
            elif ph == "moe1":
                self.moe(1, with_ctx=False)
            elif ph == "final":
                self.final()
            elif ph == "dump":
                self.dump()
            kb.release(m)
        return kb.finish()

    def prologue(self):
        kb = self.kb
        self.identb = kb.alloc("identb", [128, 128], BF16)
        self.identf = kb.alloc("identf", [128, 128], F32)
        kb.dma("sp", lambda e: e.dma_start(out=self.identb[:], in_=self.c_identb.ap[:, :]),
               reads=[self.c_identb], writes=[self.identb])
        kb.dma("sp", lambda e: e.dma_start(out=self.identf[:], in_=self.c_identf.ap[:, :]),
               reads=[self.c_identf], writes=[self.identf])
        self.epsc = kb.alloc("epsc", [128, 1], F32)
        kb.op("dve", lambda e: e.memset(self.epsc[:], EPS), writes=[self.epsc])
        self.zeroc = kb.alloc("zeroc", [128, 1], F32)
        kb.op("dve", lambda e: e.memset(self.zeroc[:], 0.0), writes=[self.zeroc])
        self.modc = [kb.alloc(f"modc{l}", [128, 48, 3], F32) for l in range(2)]
        self.mul1c = [kb.alloc(f"mul1c{l}", [128, 8, 3], F32) for l in range(2)]
        self.mul2c = [kb.alloc(f"mul2c{l}", [128, 8, 3], F32) for l in range(2)]
        self.n1gc = kb.alloc("n1gc", [128, 2, 8], F32)
        self.n2gc = kb.alloc("n2gc", [128, 2, 8], F32)
        kb.dma("sp", lambda e: e.dma_start(out=self.n1gc[:], in_=self.n1g.ap.rearrange("l (k p) -> p l k", p=128)),
               reads=[self.n1g], writes=[self.n1gc])
        kb.dma("sp", lambda e: e.dma_start(out=self.n2gc[:], in_=self.n2g.ap.rearrange("l (k p) -> p l k", p=128)),
               reads=[self.n2g], writes=[self.n2gc])
        m0 = kb.mark()
        zf = kb.alloc("zf", [128, D], F32)
        zb = kb.alloc("zb", [128, D], BF16)
        kb.op("dve", lambda e: e.memset(zf[:], 0.0), writes=[zf])
        kb.op("dve", lambda e: e.memset(zb[:], 0.0), writes=[zb])
        kb.dma("sp", lambda e: e.dma_start(out=self.xc_all.ap[NS * LC:NS * LC + 128, :], in_=zf[:]),
               reads=[zf], writes=[self.xc_all.sub("pad")])
        kb.dma("sp", lambda e: e.dma_start(out=self.xnc_all.ap[NS * LC:NS * LC + 128, :], in_=zb[:]),
               reads=[zb], writes=[self.xnc_all.sub("pad")])
        cvt = kb.alloc("cvt", [128, 24], F32)
        sct = kb.alloc("sct", [128, 24], BF16)
        kb.dma("sp", lambda e: e.dma_start(out=cvt[:], in_=self.cv.ap.rearrange("p k r -> p (k r)")),
               reads=[self.cv], writes=[cvt])
        kb.op("act", lambda e: e.activation(out=sct[:], in_=cvt[:], func=AF.Silu), reads=[cvt], writes=[sct])
        mwt = [kb.alloc(f"mwt{i}", [128, 8, 1536], BF16) for i in range(2)]
        mrow = kb.alloc("mrow", [3, 6 * D], F32)
        mb3 = kb.alloc("mb3", [3, 6 * D], F32)
        it = 0
        for l in range(2):
            kb.dma("sp", lambda e, l=l: e.dma_start(out=mb3[:], in_=self.mod_b.ap[l].partition_broadcast(3)),
                   reads=[self.mod_b], writes=[mb3])
            for pc in range(4):
                wt = mwt[it % 2]
                it += 1
                src = self.mod_w.ap[l].rearrange("(k p) n -> p k n", p=128)[:, :, pc * 1536:(pc + 1) * 1536]
                kb.dma("pool", lambda e, wt=wt, src=src: e.dma_start(out=wt[:], in_=src),
                       reads=[self.mod_w], writes=[wt])
                for nb in range(3):
                    bank = kb.banks[(pc * 3 + nb) % 2]
                    for k in range(8):
                        kb.op("pe", lambda e, bank=bank, wt=wt, k=k, nb=nb: e.matmul(
                            out=bank.h[0:3, 0:512], lhsT=sct[:, k * 3:(k + 1) * 3],
                            rhs=wt[:, k, nb * 512:(nb + 1) * 512], start=(k == 0), stop=(k == 7)),
                            reads=[sct, wt], writes=[bank])
                    c0 = pc * 1536 + nb * 512
                    kb.op("dve", lambda e, bank=bank, c0=c0: e.tensor_tensor(
                        out=mrow[0:3, c0:c0 + 512], in0=bank.h[0:3, 0:512], in1=mb3[0:3, c0:c0 + 512], op=ALU.add),
                        reads=[bank, mb3], writes=[mrow.sub(c0)])
            kb.dma("sp", lambda e, l=l: e.dma_start(out=self.modd.ap[l], in_=mrow[0:3, :]),
                   reads=[mrow], writes=[self.modd.sub(l)])
            for r in range(3):
                kb.dma("sp", lambda e, l=l, r=r: e.dma_start(
                    out=self.modc[l][:, :, r], in_=self.modd.ap[l, r].rearrange("(c p) -> p c", p=128)),
                    reads=[self.modd.sub(l)], writes=[self.modc[l].sub(r)])
            for (mulc, gc, v) in ((self.mul1c[l], self.n1gc, 1), (self.mul2c[l], self.n2gc, 4)):
                kb.op("dve", lambda e, mulc=mulc, v=v, l=l: e.tensor_scalar(
                    out=mulc[:], in0=self.modc[l][:, v * 8:(v + 1) * 8, :], scalar1=1.0, scalar2=None, op0=ALU.add),
                    reads=[self.modc[l]], writes=[mulc])
                for r in range(3):
                    kb.op("dve", lambda e, mulc=mulc, gc=gc, r=r, l=l: e.tensor_tensor(
                        out=mulc[:, :, r], in0=mulc[:, :, r], in1=gc[:, l, :], op=ALU.mult),
                        reads=[mulc, gc], writes=[mulc])
        kb.release(m0)

    def norm_batch(self, src, row0, nt, mulc, addc, r, uT, ucol0, bufs, banks, xn_dst=None):
        kb = self.kb
        xb, xnb, junk, stat = bufs
        for j in range(nt):
            xt = xb[self._nb % len(xb)]
            xn = xnb[self._nb % len(xnb)]
            sc = self._nb % 64
            self._nb += 1
            rr = row0 + j * 128
            kb.dma("sp", lambda e, xt=xt, rr=rr: e.dma_start(out=xt[:], in_=src.ap[rr:rr + 128, :]),
                   reads=[src], writes=[xt])
            kb.op("act", lambda e, xt=xt, sc=sc: e.activation(
                out=junk[:], in_=xt[:], func=AF.Square, accum_out=stat[:, sc:sc + 1]),
                reads=[xt], writes=[junk, stat.sub(sc)])
            kb.op("act", lambda e, sc=sc: e.activation(
                out=stat[:, 64 + sc:65 + sc], in_=stat[:, sc:sc + 1], func=AF.Identity, scale=1.0 / D, bias=self.epsc[:, 0:1]),
                reads=[stat.sub(sc), self.epsc], writes=[stat.sub(64 + sc)])
            kb.op("act", lambda e, sc=sc: e.activation(
                out=stat[:, 128 + sc:129 + sc], in_=stat[:, 64 + sc:65 + sc], func=AF.Sqrt),
                reads=[stat.sub(64 + sc)], writes=[stat.sub(128 + sc)])
            kb.op("dve", lambda e, sc=sc: e.reciprocal(out=stat[:, 192 + sc:193 + sc], in_=stat[:, 128 + sc:129 + sc]),
                  reads=[stat.sub(128 + sc)], writes=[stat.sub(192 + sc)])
            kb.op("act", lambda e, xt=xt, xn=xn, sc=sc: e.activation(
                out=xn[:], in_=xt[:], func=AF.Identity, scale=stat[:, 192 + sc:193 + sc]),
                reads=[xt, stat.sub(192 + sc)], writes=[xn])
            if xn_dst is not None:
                dt, drow = xn_dst
                dr0 = drow + j * 128
                kb.dma("sp", lambda e, xn=xn, dt=dt, dr=dr0: e.dma_start(
                    out=dt.ap[dr:dr + 128, :], in_=xn[:]), reads=[xn], writes=[dt.sub(dr0)])
            if getattr(self, "_skip_t", False):
                continue
            for k in range(8):
                bank = banks[2 * j + k // 4]
                pv = bank.h.bitcast(BF16)
                kk = k % 4
                kb.op("pe", lambda e, pv=pv, xn=xn, k=k, kk=kk: e.transpose(
                    out=pv[:, kk * 128:(kk + 1) * 128], in_=xn[:, k * 128:(k + 1) * 128], identity=self.identb[:]),
                    reads=[xn, self.identb], writes=[bank])
            for k in range(8):
                bank = banks[2 * j + k // 4]
                pv = bank.h.bitcast(BF16)
                kk = k % 4
                dst = uT[:, k, ucol0 + j * 128: ucol0 + (j + 1) * 128]
                if k < 4:
                    kb.op("act", lambda e, dst=dst, pv=pv, k=k, kk=kk: e.activation(
                        out=dst, in_=pv[:, kk * 128:(kk + 1) * 128], func=AF.Identity,
                        scale=mulc[:, k, r:r + 1], bias=addc(k, r)),
                        reads=[bank, mulc], writes=[uT.sub((k, ucol0 + j * 128))])
                else:
                    kb.op("dve", lambda e, dst=dst, pv=pv, k=k, kk=kk: e.tensor_scalar(
                        out=dst, in0=pv[:, kk * 128:(kk + 1) * 128], scalar1=mulc[:, k, r:r + 1],
                        scalar2=addc(k, r), op0=ALU.mult, op1=ALU.add),
                        reads=[bank, mulc], writes=[uT.sub((k, ucol0 + j * 128))])

    def norm_bufs(self, nx=2):
        kb = self.kb
        self._nb = 0
        xb = [kb.alloc(f"xb{i}", [128, D], F32) for i in range(nx)]
        xnb = [kb.alloc(f"xnb{i}", [128, D], BF16) for i in range(nx)]
        junk = kb.alloc("junk", [128, D], BF16)
        stat = kb.alloc("stat", [128, 256], F32)
        kb.op("dve", lambda e: e.memset(stat[:], 0.0), writes=[stat])
        return (xb, xnb, junk, stat)

    def dump(self):
        kb = self.kb
        yc = kb.dram("yc", [NS * LC, D], F32, kind="ExternalOutput")
        kb.dma("sp", lambda e: e.dma_start(out=yc.ap[:, :], in_=self.xc_all.ap[0:NS * LC, :]),
               reads=[self.xc_all], writes=[yc])
        for s in range(NS):
            kb.dma("sp", lambda e, s=s: e.dma_start(out=self.out.ap[s], in_=self.xr[s].ap),
                   reads=[self.xr[s]], writes=[self.out.sub(s)])

    def final(self):
        kb = self.kb
        fgb = kb.alloc("fgb", [128, D], F32)
        kb.dma("sp", lambda e: e.dma_start(out=fgb[:], in_=self.final_g.ap.partition_broadcast(128)),
               reads=[self.final_g], writes=[fgb])
        xb = [kb.alloc(f"fxb{i}", [128, D], F32) for i in range(3)]
        yb = [kb.alloc(f"fyb{i}", [128, D], F32) for i in range(3)]
        junk = kb.alloc("fjunk", [128, D], BF16)
        stat = kb.alloc("fstat", [128, 3 * 32], F32)
        kb.op("dve", lambda e: e.memset(stat[:], 0.0), writes=[stat])
        i = 0
        for s in range(NS):
            for t in range(L // 128):
                xt = xb[i % 3]
                yt = yb[i % 3]
                sc = i % 32
                i += 1
                kb.dma("sp", lambda e, xt=xt, s=s, t=t: e.dma_start(out=xt[:], in_=self.xr[s].ap[t * 128:(t + 1) * 128, :]),
                       reads=[self.xr[s]], writes=[xt])
                if i > 32:
                    kb.op("dve", lambda e, sc=sc: e.memset(stat[:, sc:sc + 1], 0.0), writes=[stat.sub(sc)])
                kb.op("act", lambda e, xt=xt, sc=sc: e.activation(
                    out=junk[:], in_=xt[:], func=AF.Square, accum_out=stat[:, sc:sc + 1]),
                    reads=[xt], writes=[junk, stat.sub(sc)])
                kb.op("dve", lambda e, sc=sc: e.tensor_scalar(
                    out=stat[:, 32 + sc:33 + sc], in0=stat[:, sc:sc + 1], scalar1=1.0 / D, scalar2=EPS, op0=ALU.mult, op1=ALU.add),
                    reads=[stat.sub(sc)], writes=[stat.sub(32 + sc)])
                kb.op("act", lambda e, sc=sc: e.activation(
                    out=stat[:, 32 + sc:33 + sc], in_=stat[:, 32 + sc:33 + sc], func=AF.Sqrt),
                    reads=[stat.sub(32 + sc)], writes=[stat.sub(32 + sc)])
                kb.op("dve", lambda e, sc=sc: e.reciprocal(out=stat[:, 64 + sc:65 + sc], in_=stat[:, 32 + sc:33 + sc]),
                      reads=[stat.sub(32 + sc)], writes=[stat.sub(64 + sc)])
                kb.op("dve", lambda e, xt=xt, yt=yt, sc=sc: e.scalar_tensor_tensor(
                    out=yt[:], in0=xt[:], scalar=stat[:, 64 + sc:65 + sc], in1=fgb[:], op0=ALU.mult, op1=ALU.mult),
                    reads=[xt, stat.sub(64 + sc), fgb], writes=[yt])
                kb.dma("sp", lambda e, yt=yt, s=s, t=t: e.dma_start(out=self.out.ap[s, t * 128:(t + 1) * 128, :], in_=yt[:]),
                       reads=[yt], writes=[self.out.sub((s, t))])

    def moe(self, l, with_ctx):
        kb = self.kb
        IOA = bass.IndirectOffsetOnAxis
        wbufs = [[kb.alloc(f"w{n}_{i}", [128, 8, D], BF16) for n in (1, 3, 2)] for i in range(2)]
        wsrc = (self.w1, self.w3, self.w2)

        def load_w(e):
            for n in range(3):
                src = wsrc[n].ap[l, e].rearrange("(k p) f -> p k f", p=128)
                wt = wbufs[e % 2][n]
                kb.dma("pool", lambda en, wt=wt, src=src: en.dma_start(out=wt[:], in_=src),
                       reads=[wsrc[n]], writes=[wt])

        nr = 3 if with_ctx else 2
        g2b = [kb.alloc(f"g2b{r}", [128, D], F32) for r in range(nr)]
        for r in range(nr):
            kb.dma("sp", lambda e, r=r: e.dma_start(
                out=g2b[r][:], in_=self.modd.ap[l, r, 5 * D:6 * D].partition_broadcast(128)),
                reads=[self.modd.sub(l)], writes=[g2b[r]])
        idxT = kb.alloc("idxT", [128, 2, 48], I32)
        gT = kb.alloc("gT", [128, 2, 48], F32)
        idxC = kb.alloc("idxC", [128, NE], I32)
        gC = kb.alloc("gC", [128, NE], F32)
        load_w(0)
        load_w(1)
        addc2 = lambda k, r: self.modc[l][:, 24 + k, r:r + 1]
        mulc2 = self.mul2c[l]

        dbg = getattr(self, "debug_route", 0)
        if dbg == 10:
            return
        m1 = kb.mark()
        bufs = self.norm_bufs()
        uTb = [kb.alloc(f"uTb{i}", [128, 8, 256], BF16) for i in range(2)]
        wr = kb.alloc("wr", [128, 8, NE], BF16)
        wrf = kb.alloc("wrf", [128, 8, NE], F32)
        kb.dma("sp", lambda e: e.dma_start(out=wrf[:], in_=self.w_router.ap[l].rearrange("(k p) e -> p k e", p=128)),
               reads=[self.w_router], writes=[wrf])
        kb.op("dve", lambda e: e.tensor_copy(out=wr[:], in_=wrf[:]), reads=[wrf], writes=[wr])
        aff2 = kb.alloc("aff2", [128, 16, 64], F32)
        affc = kb.alloc("affc", [128, 2, 64], F32)
        kb.op("dve", lambda e: e.memset(aff2[:].rearrange("p a b -> p (a b)"), 0.0), writes=[aff2])
        kb.op("dve", lambda e: e.memset(affc[:].rearrange("p a b -> p (a b)"), 0.0), writes=[affc])
        lg = kb.alloc("lg", [128, 16, 16], F32)
        mx = kb.alloc("mx", [128, 16], F32)
        sm = kb.alloc("sm", [128, 16], F32)
        rs = kb.alloc("rs", [128, 16], F32)
        work = kb.alloc("work", [48, L], F32)
        workc = kb.alloc("workc", [48, LC], F32)
        topv = kb.alloc("topv", [48, 256], F32)
        topi = kb.alloc("topi", [48, 256], U32)
        topif = kb.alloc("topif", [48, 256], F32)
        topvc = kb.alloc("topvc", [48, 32], F32)
        topic = kb.alloc("topic", [48, 32], U32)
        topicf = kb.alloc("topicf", [48, 32], F32)
        nbatch = 0
        seqs = []
        for s in range(NS):
            seqs.append((self.xr[s], self.xnl[s], L // 128, s, aff2, s * 32, kb.banks[4 + s], 0))
        if with_ctx:
            for s in range(NS):
                seqs.append((self.xc[s], self.xnc[s], LC // 128, 2, affc, s * 32, kb.banks[6], s * 32))
        for (src, xnd, ntl, r, afft, acol, lbank, lcol0) in seqs:
            for b in range((ntl + 1) // 2):
                nt = min(2, ntl - b * 2)
                ub = uTb[nbatch % 2]
                nbatch += 1
                self.norm_batch(src, b * 256, nt, mulc2, addc2, r, ub, 0, bufs,
                                [kb.banks[j] for j in range(2 * nt)], xn_dst=(xnd, b * 256))
                for j in range(nt):
                    if dbg == 11:
                        continue
                    c = b * 2 + j
                    for k in range(8):
                        kb.op("pe", lambda e, lbank=lbank, lc=lcol0 + c * 16, ub=ub, k=k, j=j: e.matmul(
                            out=lbank.h[:, lc:lc + 16], lhsT=ub[:, k, j * 128:(j + 1) * 128], rhs=wr[:, k, :],
                            start=(k == 0), stop=(k == 7)),
                            reads=[ub.sub((k, j * 128)), wr], writes=[lbank])
            if dbg == 11:
                continue
            lv = lbank.h[:, lcol0:lcol0 + ntl * 16].rearrange("p (c e) -> p c e", e=16)
            bc = lambda t, ntl=ntl: t[:, 0:ntl].unsqueeze(2).to_broadcast([128, ntl, 16])
            kb.op("dve", lambda e, lv=lv, ntl=ntl: e.tensor_reduce(out=mx[:, 0:ntl], in_=lv, axis=AX.X, op=ALU.max),
                  reads=[lbank], writes=[mx])
            kb.op("dve", lambda e, lv=lv, bc=bc, ntl=ntl: e.tensor_tensor(out=lg[:, 0:ntl, :], in0=lv, in1=bc(mx), op=ALU.subtract),
                  reads=[lbank, mx], writes=[lg])
            kb.op("act", lambda e, ntl=ntl: e.activation(out=lg[:, 0:ntl, :], in_=lg[:, 0:ntl, :], func=AF.Exp),
                  reads=[lg], writes=[lg])
            kb.op("dve", lambda e, ntl=ntl: e.tensor_reduce(out=sm[:, 0:ntl], in_=lg[:, 0:ntl, :], axis=AX.X, op=ALU.add),
                  reads=[lg], writes=[sm])
            kb.op("dve", lambda e, ntl=ntl: e.reciprocal(out=rs[:, 0:ntl], in_=sm[:, 0:ntl]), reads=[sm], writes=[rs])
            kb.op("dve", lambda e, afft=afft, acol=acol, bc=bc, ntl=ntl: e.tensor_tensor(
                out=afft[:, 0:ntl, acol:acol + 16], in0=lg[:, 0:ntl, :], in1=bc(rs), op=ALU.mult),
                reads=[lg, rs], writes=[afft])
        if dbg == 11:
            kb.release(m1)
            return
        if dbg == 1:
            da = kb.dram("dbg_aff", [128, 1024], F32, kind="ExternalOutput")
            kb.dma("sp", lambda e: e.dma_start(out=da.ap[:, :], in_=aff2[:].rearrange("p h c -> p (h c)")), reads=[aff2], writes=[da])
            kb.release(m1)
            return
        for c in range(16):
            bank = kb.banks[c // 4]
            kb.op("pe", lambda e, bank=bank, c=c: e.transpose(
                out=bank.h[0:48, (c % 4) * 128:(c % 4 + 1) * 128], in_=aff2[:, c, 0:48], identity=self.identf[:]),
                reads=[aff2, self.identf], writes=[bank])
        for q in range(4):
            eng = "act" if q % 2 == 0 else "dve"
            if eng == "act":
                kb.op("act", lambda e, q=q: e.copy(out=work[0:48, q * 512:(q + 1) * 512], in_=kb.banks[q].h[0:48, 0:512]),
                      reads=[kb.banks[q]], writes=[work.sub(q)])
            else:
                kb.op("dve", lambda e, q=q: e.tensor_copy(out=work[0:48, q * 512:(q + 1) * 512], in_=kb.banks[q].h[0:48, 0:512]),
                      reads=[kb.banks[q]], writes=[work.sub(q)])
        if with_ctx:
            for c in range(2):
                kb.op("pe", lambda e, c=c: e.transpose(
                    out=kb.banks[7].h[0:48, c * 128:(c + 1) * 128], in_=affc[:, c, 0:48], identity=self.identf[:]),
                    reads=[affc, self.identf], writes=[kb.banks[7]])
            kb.op("act", lambda e: e.copy(out=workc[0:48, :], in_=kb.banks[7].h[0:48, 0:256]),
                  reads=[kb.banks[7]], writes=[workc])

        def topk(wk, tv, ti, niter):
            for it in range(niter):
                sl = slice(it * 8, (it + 1) * 8)
                kb.op("dve", lambda e, sl=sl: e.max(out=tv[:, sl], in_=wk[:]), reads=[wk], writes=[tv.sub(it)])
                kb.op("dve", lambda e, sl=sl: e.max_index(out=ti[:, sl], in_max=tv[:, sl], in_values=wk[:]),
                      reads=[wk, tv.sub(it)], writes=[ti.sub(it)])
                kb.op("dve", lambda e, sl=sl: e.match_replace(out=wk[:], in_to_replace=tv[:, sl], in_values=wk[:], imm_value=-1.0),
                      reads=[tv.sub(it), wk], writes=[wk])

        if dbg == 2:
            da = kb.dram("dbg_work", [48, L], F32, kind="ExternalOutput")
            kb.dma("sp", lambda e: e.dma_start(out=da.ap[:, :], in_=work[:]), reads=[work], writes=[da])
            kb.release(m1)
            return
        topk(work, topv, topi, 32)
        if dbg == 3:
            da = kb.dram("dbg_topv", [48, 256], F32, kind="ExternalOutput")
            kb.dma("sp", lambda e: e.dma_start(out=da.ap[:, :], in_=topv[:]), reads=[topv], writes=[da])
            db_ = kb.dram("dbg_topi", [48, 256], U32, kind="ExternalOutput")
            kb.dma("sp", lambda e: e.dma_start(out=db_.ap[:, :], in_=topi[:]), reads=[topi], writes=[db_])
            kb.release(m1)
            return
        kb.op("dve", lambda e: e.tensor_copy(out=topif[:], in_=topi[:]), reads=[topi], writes=[topif])
        tb = kb.banks[5]
        for h in range(2):
            kb.op("pe", lambda e, h=h: e.transpose(out=tb.h[:, h * 48:(h + 1) * 48], in_=topif[0:48, h * 128:(h + 1) * 128],
                                                   identity=self.identf[0:48, 0:48]),
                  reads=[topif, self.identf], writes=[tb])
            kb.op("pe", lambda e, h=h: e.transpose(out=tb.h[:, 128 + h * 48:128 + (h + 1) * 48], in_=topv[0:48, h * 128:(h + 1) * 128],
                                                   identity=self.identf[0:48, 0:48]),
                  reads=[topv, self.identf], writes=[tb])
        kb.op("dve", lambda e: e.tensor_copy(out=idxT[:], in_=tb.h[:, 0:96].rearrange("p (h c) -> p h c", h=2)),
              reads=[tb], writes=[idxT])
        kb.op("dve", lambda e: e.tensor_copy(out=gT[:], in_=tb.h[:, 128:224].rearrange("p (h c) -> p h c", h=2)),
              reads=[tb], writes=[gT])
        if with_ctx:
            topk(workc, topvc, topic, 4)
            kb.op("dve", lambda e: e.tensor_copy(out=topicf[:], in_=topic[:]), reads=[topic], writes=[topicf])
            cbase = kb.alloc("cbase", [128, 1], F32)
            kb.dma("sp", lambda e: e.dma_start(out=cbase[:], in_=self.c_ctxbase.ap[:, :]), reads=[self.c_ctxbase], writes=[cbase])
            for (srcT, dstT, isidx) in ((topicf, idxC, True), (topvc, gC, False)):
                M = kb.alloc("Mc", [48, 128], F32)
                kb.op("dve", lambda e, M=M: e.memset(M[:], 0.0), writes=[M])
                kb.op("dve", lambda e, M=M, srcT=srcT: e.tensor_copy(out=M[0:16, 0:32], in_=srcT[0:16, 0:32]), reads=[srcT], writes=[M])
                kb.op("dve", lambda e, M=M, srcT=srcT: e.tensor_copy(out=M[32:48, 32:64], in_=srcT[32:48, 0:32]), reads=[srcT], writes=[M])
                kb.op("pe", lambda e, M=M: e.transpose(out=tb.h[:, 256:304], in_=M[0:48, :], identity=self.identf[0:48, 0:48]),
                      reads=[M, self.identf], writes=[tb])
                tcp = kb.alloc("tcp", [128, 48], F32)
                kb.op("dve", lambda e, tcp=tcp: e.tensor_copy(out=tcp[:], in_=tb.h[:, 256:304]), reads=[tb], writes=[tcp])
                tsum = kb.alloc("tsum", [128, NE], F32)
                kb.op("dve", lambda e, tcp=tcp, tsum=tsum: e.tensor_tensor(out=tsum[:], in0=tcp[:, 0:16], in1=tcp[:, 32:48], op=ALU.add),
                      reads=[tcp], writes=[tsum])
                if isidx:
                    kb.op("dve", lambda e, tsum=tsum: e.tensor_scalar(out=tsum[:], in0=tsum[:], scalar1=cbase[:, 0:1], scalar2=None, op0=ALU.add),
                          reads=[tsum, cbase], writes=[tsum])
                kb.op("dve", lambda e, tsum=tsum, dstT=dstT: e.tensor_copy(out=dstT[:], in_=tsum[:]), reads=[tsum], writes=[dstT])
        if dbg == 4:
            di = kb.dram("dbg_idx", [128, 96], I32, kind="ExternalOutput")
            dg = kb.dram("dbg_g", [128, 96], F32, kind="ExternalOutput")
            da = kb.dram("dbg_aff", [128, 1024], F32, kind="ExternalOutput")
            kb.dma("sp", lambda e: e.dma_start(out=di.ap[:, :], in_=idxT[:].rearrange("p h c -> p (h c)")), reads=[idxT], writes=[di])
            kb.dma("sp", lambda e: e.dma_start(out=dg.ap[:, :], in_=gT[:].rearrange("p h c -> p (h c)")), reads=[gT], writes=[dg])
            kb.dma("sp", lambda e: e.dma_start(out=da.ap[:, :], in_=aff2[:].rearrange("p h c -> p (h c)")), reads=[aff2], writes=[da])
            kb.release(m1)
            return
        kb.release(m1)

        G = []
        for s in range(NS):
            for h in range(2):
                G.append(dict(co=s * 256 + h * 128, idx=lambda e, s=s, h=h: idxT[:, h, s * 32 + e:s * 32 + e + 1],
                              gate=lambda e, s=s, h=h: gT[:, h, s * 32 + e:s * 32 + e + 1], r=s,
                              xn=self.xnl[s], dst=self.xr[s], n=L))
        if with_ctx:
            G.append(dict(co=512, idx=lambda e: idxC[:, e:e + 1], gate=lambda e: gC[:, e:e + 1], r=2,
                          xn=self.xnc_all, dst=self.xc_all, n=NS * LC + 128))
        NSL = 128 * len(G)
        HW = NSL // 2
        halves = [(0, HW), (HW, HW)]
        Xg = [[kb.alloc(f"xg{i}_{gi}", [128, D], BF16) for gi in range(len(G))] for i in range(2)]
        XeT = [kb.alloc(f"xeT{i}", [128, 8, NSL], BF16) for i in range(2)]
        hidT = kb.alloc("hidT", [128, 8, NSL], BF16)
        sgt = [kb.alloc(f"sgt{i}", [128, HW], F32) for i in range(2)]
        yo = [kb.alloc(f"yo{i}", [128, D], F32) for i in range(3)]
        nyo = 0
        nyb = 0

        def gathers(e):
            for gi, g in enumerate(G):
                xg = Xg[e % 2][gi]
                kb.dma("pool", lambda en, xg=xg, g=g, e=e: en.indirect_dma_start(
                    out=xg[:], out_offset=None, in_=g["xn"].ap[:, :], in_offset=IOA(ap=g["idx"](e), axis=0)),
                    reads=[g["xn"], idxT, idxC], writes=[xg])

        gathers(0)
        for e in range(NE):
            if e + 1 < NE:
                gathers(e + 1)
            w1t, w3t, w2t = wbufs[e % 2]
            xe = XeT[e % 2]
            for gi, g in enumerate(G):
                co, r = g["co"], g["r"]
                xg = Xg[e % 2][gi]
                for k in range(8):
                    bank = kb.banks[6 + k // 4]
                    pv = bank.h.bitcast(BF16)
                    kk = k % 4
                    kb.op("pe", lambda en, pv=pv, xg=xg, k=k, kk=kk: en.transpose(
                        out=pv[:, kk * 128:(kk + 1) * 128], in_=xg[:, k * 128:(k + 1) * 128],
                        identity=self.identb[:]), reads=[xg, self.identb], writes=[bank])
                for k in range(8):
                    bank = kb.banks[6 + k // 4]
                    pv = bank.h.bitcast(BF16)
                    kk = k % 4
                    dst = xe[:, k, co:co + 128]
                    if k < 4:
                        kb.op("act", lambda en, dst=dst, pv=pv, kk=kk, r=r, k=k: en.activation(
                            out=dst, in_=pv[:, kk * 128:(kk + 1) * 128], func=AF.Identity,
                            scale=mulc2[:, k, r:r + 1], bias=addc2(k, r)),
                            reads=[bank], writes=[xe.sub((k, co))])
                    else:
                        kb.op("dve", lambda en, dst=dst, pv=pv, kk=kk, r=r, k=k: en.tensor_scalar(
                            out=dst, in0=pv[:, kk * 128:(kk + 1) * 128], scalar1=mulc2[:, k, r:r + 1],
                            scalar2=addc2(k, r), op0=ALU.mult, op1=ALU.add),
                            reads=[bank], writes=[xe.sub((k, co))])
            for fc in range(8):
                for hi, (h0, hw) in enumerate(halves):
                    b1 = kb.banks[hi]
                    b3 = kb.banks[2 + hi]
                    for (wt, bm) in ((w1t, b1), (w3t, b3)):
                        for k in range(8):
                            kb.op("pe", lambda en, wt=wt, bm=bm, k=k, fc=fc, xe=xe, h0=h0, hw=hw: en.matmul(
                                out=bm.h[:, 0:hw], lhsT=wt[:, k, fc * 128:(fc + 1) * 128], rhs=xe[:, k, h0:h0 + hw],
                                start=(k == 0), stop=(k == 7)), reads=[wt, xe], writes=[bm])
                    sg = sgt[hi]
                    kb.op("act", lambda en, sg=sg, b1=b1, hw=hw: en.activation(out=sg[:, 0:hw], in_=b1.h[:, 0:hw], func=AF.Silu),
                          reads=[b1], writes=[sg])
                    kb.op("dve", lambda en, sg=sg, b3=b3, fc=fc, h0=h0, hw=hw: en.tensor_tensor(
                        out=hidT[:, fc, h0:h0 + hw], in0=sg[:, 0:hw], in1=b3.h[:, 0:hw], op=ALU.mult),
                        reads=[sg, b3], writes=[hidT.sub((fc, h0))])
            for gi, g in enumerate(G):
                co, r = g["co"], g["r"]
                yt = yo[nyo % 3]
                nyo += 1
                for db in range(2):
                    by = kb.banks[4 + nyb % 2]
                    nyb += 1
                    for fc in range(8):
                        kb.op("pe", lambda en, by=by, fc=fc, co=co, db=db, w2t=w2t: en.matmul(
                            out=by.h[:, 0:512], lhsT=hidT[:, fc, co:co + 128], rhs=w2t[:, fc, db * 512:(db + 1) * 512],
                            start=(fc == 0), stop=(fc == 7)), reads=[hidT, w2t], writes=[by])
                    kb.op("dve", lambda en, by=by, yt=yt, db=db, g=g, e=e, r=r: en.scalar_tensor_tensor(
                        out=yt[:, db * 512:(db + 1) * 512], in0=by.h[:, 0:512], scalar=g["gate"](e),
                        in1=g2b[r][:, db * 512:(db + 1) * 512], op0=ALU.mult, op1=ALU.mult),
                        reads=[by, gT, gC, g2b[r]], writes=[yt.sub(db)])
                kb.dma("pool", lambda en, yt=yt, g=g, e=e: en.indirect_dma_start(
                    out=g["dst"].ap[:, :], out_offset=IOA(ap=g["idx"](e), axis=0), in_=yt[:, :], in_offset=None,
                    compute_op=ALU.add, bounds_check=kb.bnd[g["n"] - 1], oob_is_err=True),
                    reads=[yt, idxT, idxC], writes=[g["dst"]])
            if e + 2 < NE:
                load_w(e + 2)

    def mixer0(self):
        kb = self.kb
        l = 0
        wbuf = kb.alloc("wbuf", [128, 8, 1536], BF16)
        wout = wbuf.view("woutv", [128, 8, D], BF16)
        csc = kb.alloc("csc", [128, 256], BF16)
        kb.dma("sp", lambda e: e.dma_start(out=csc[:], in_=self.c_csc.ap[:, :]), reads=[self.c_csc], writes=[csc])
        cwc = kb.alloc("cwc", [128, 4, 31], F32)
        for j in range(4):
            kb.dma("sp", lambda e, j=j: e.dma_start(out=cwc[:, j, :], in_=self.ab_conv_w.ap[:, j * 128:(j + 1) * 128].rearrange("k p -> p k")),
                   reads=[self.ab_conv_w], writes=[cwc.sub(j)])
        cols = {}
        for nm, dr in (("cb", self.ab_conv_b), ("lg", self.ab_ln_g), ("lb", self.ab_ln_b)):
            t = kb.alloc(nm + "c", [128, 4], F32)
            kb.dma("sp", lambda e, t=t, dr=dr: e.dma_start(out=t[:], in_=dr.ap.rearrange("(j p) -> p j", p=128)), reads=[dr], writes=[t])
            cols[nm] = t
        onesb = kb.alloc("onesb", [128, 128], BF16)
        kb.op("dve", lambda e: e.memset(onesb[:], 1.0 / 512.0), writes=[onesb])
        diag = kb.alloc("diag", [128, 4 * 31, 128], BF16)
        for j in range(4):
            for k in range(31):
                kb.op("dve", lambda e, j=j, k=k: e.tensor_scalar(
                    out=diag[:, j * 31 + k, :], in0=self.identb[:], scalar1=cwc[:, j, k:k + 1], scalar2=None, op0=ALU.mult),
                    reads=[self.identb, cwc], writes=[diag.sub((j, k))])
        bufs = self.norm_bufs(nx=1)
        xb = bufs[0][0]
        uTb = kb.alloc("uTb", [128, 8, 512], BF16)
        aT = kb.alloc("aT", [128, 4, L + 32], BF16)
        ufTb = kb.alloc("ufTb", [128, 4, 512], BF16)
        Yall = kb.alloc("Yall", [128, 16, 1024], BF16)
        mixT = kb.alloc("mixT", [128, 8, L], BF16)
        clb = kb.alloc("clb", [128, 16, 256], BF16)
        slb = kb.alloc("slb", [128, 16, 256], BF16)
        g1b = kb.alloc("g1b", [128, D], F32)
        NBC = 256
        cT = kb.alloc("cT", [128, 4, NBC], F32)
        cbt = kb.alloc("cbt", [128, 4, NBC], BF16)
        c2t = kb.alloc("c2t", [128, 4, NBC], BF16)
        t_mean = kb.alloc("t_mean", [128, NBC], F32)
        t_rstd = kb.alloc("t_rstd", [128, NBC], F32)
        t_tmp = kb.alloc("t_tmp", [128, 512], F32)
        t_tmp2 = kb.alloc("t_tmp2", [128, NBC], F32)
        addc1 = lambda k, r: self.modc[l][:, k, r:r + 1]
        mulc1 = self.mul1c[l]
        B = kb.banks
        seqs = [(self.xin[0], self.xr[0], L, 0, self.c_cl, self.c_sl), (self.xin[1], self.xr[1], L, 1, self.c_cl, self.c_sl),
                (self.cin[0], self.xc[0], LC, 2, self.c_clc, self.c_slc), (self.cin[1], self.xc[1], LC, 2, self.c_clc, self.c_slc)]
        for (src, dst, Ls, r, ctab, stab) in seqs:
            kb.dma("pool", lambda e: e.dma_start(out=wbuf[:], in_=self.ab_w_in.ap.rearrange("(k p) f -> p k f", p=128)),
                   reads=[self.ab_w_in], writes=[wbuf])
            kb.dma("sp", lambda e, r=r: e.dma_start(out=g1b[:], in_=self.modd.ap[l, r, 2 * D:3 * D].partition_broadcast(128)),
                   reads=[self.modd.sub(l)], writes=[g1b])
            for j in range(4):
                kb.op("dve", lambda e, j=j: e.memset(aT[:, j, 0:15], 0.0), writes=[aT.sub((j, "h0"))])
                kb.op("dve", lambda e, j=j, Ls=Ls: e.memset(aT[:, j, 15 + Ls:32 + Ls], 0.0), writes=[aT.sub((j, "h1"))])
            nb = min(512, Ls)
            for blk in range(Ls // nb):
                t0 = blk * nb
                for sb in range(nb // 256):
                    self.norm_batch(src, t0 + sb * 256, 2, mulc1, addc1, r, uTb, sb * 256, bufs, [B[0], B[1], B[2], B[3]])
                def zmm(j, bank):
                    for k in range(8):
                        kb.op("pe", lambda e, j=j, k=k, bank=bank, nb=nb: e.matmul(
                            out=bank.h[:, 0:nb], lhsT=wbuf[:, k, j * 128:(j + 1) * 128], rhs=uTb[:, k, 0:nb],
                            start=(k == 0), stop=(k == 7)), reads=[wbuf, uTb], writes=[bank])
                for jj in range(4):
                    zmm(jj, B[4])
                    zmm(jj + 4, B[5])
                    kb.op("act", lambda e, nb=nb: e.activation(out=t_tmp[:, 0:nb], in_=B[5].h[:, 0:nb], func=AF.Sigmoid),
                          reads=[B[5]], writes=[t_tmp])
                    kb.op("dve", lambda e, jj=jj, nb=nb, t0=t0: e.tensor_tensor(
                        out=aT[:, jj, 15 + t0:15 + t0 + nb], in0=t_tmp[:, 0:nb], in1=B[4].h[:, 0:nb], op=ALU.mult),
                        reads=[t_tmp, B[4]], writes=[aT.sub((jj, t0))])
                for g in range(4):
                    zmm(8 + g, B[6])
                    kb.op("act", lambda e, g=g, nb=nb: e.copy(out=ufTb[:, g, 0:nb], in_=B[6].h[:, 0:nb]),
                          reads=[B[6]], writes=[ufTb.sub(g)])
                for tc in range(nb // 128):
                    c = (t0 // 128) + tc
                    for gp in range(2):
                        for gg in range(2):
                            g = gp * 2 + gg
                            kb.op("pe", lambda e, g=g, gg=gg, tc=tc: e.matmul(
                                out=B[7].h[:, gg * 256:(gg + 1) * 256], lhsT=ufTb[:, g, tc * 128:(tc + 1) * 128], rhs=csc[:, :],
                                start=True, stop=True), reads=[ufTb, csc], writes=[B[7]])
                        kb.op("dve", lambda e, c=c, gp=gp: e.tensor_copy(out=Yall[:, c, gp * 512:(gp + 1) * 512], in_=B[7].h[:, 0:512]),
                              reads=[B[7]], writes=[Yall.sub((c, gp))])
            nbc = min(NBC, Ls)
            for blk in range(Ls // nbc):
                t0 = blk * nbc
                for j in range(4):
                    bank = B[j % 2]
                    for k in range(31):
                        kb.op("pe", lambda e, j=j, k=k, bank=bank, t0=t0, nbc=nbc: e.matmul(
                            out=bank.h[:, 0:nbc], lhsT=diag[:, j * 31 + k, :], rhs=aT[:, j, t0 + k:t0 + k + nbc],
                            start=(k == 0), stop=(k == 30)), reads=[diag, aT], writes=[bank])
                    kb.op("act", lambda e, j=j, bank=bank, nbc=nbc: e.activation(
                        out=cT[:, j, 0:nbc], in_=bank.h[:, 0:nbc], func=AF.Identity, bias=cols["cb"][:, j:j + 1]),
                        reads=[bank, cols["cb"]], writes=[cT.sub(j)])
                    kb.op("pool", lambda e, j=j, nbc=nbc: e.tensor_copy(out=cbt[:, j, 0:nbc], in_=cT[:, j, 0:nbc]),
                          reads=[cT.sub(j)], writes=[cbt.sub(j)])
                    kb.op("pool", lambda e, j=j, nbc=nbc: e.tensor_tensor(out=c2t[:, j, 0:nbc], in0=cT[:, j, 0:nbc], in1=cT[:, j, 0:nbc], op=ALU.mult),
                          reads=[cT.sub(j)], writes=[c2t.sub(j)])
                for j in range(4):
                    kb.op("pe", lambda e, j=j, nbc=nbc: e.matmul(out=B[2].h[:, 0:nbc], lhsT=onesb[:], rhs=cbt[:, j, 0:nbc],
                                                                 start=(j == 0), stop=(j == 3)), reads=[onesb, cbt], writes=[B[2]])
                for j in range(4):
                    kb.op("pe", lambda e, j=j, nbc=nbc: e.matmul(out=B[3].h[:, 0:nbc], lhsT=onesb[:], rhs=c2t[:, j, 0:nbc],
                                                                 start=(j == 0), stop=(j == 3)), reads=[onesb, c2t], writes=[B[3]])
                kb.op("dve", lambda e, nbc=nbc: e.tensor_copy(out=t_mean[:, 0:nbc], in_=B[2].h[:, 0:nbc]), reads=[B[2]], writes=[t_mean])
                kb.op("dve", lambda e, nbc=nbc: e.tensor_tensor(out=t_tmp2[:, 0:nbc], in0=t_mean[:, 0:nbc], in1=t_mean[:, 0:nbc], op=ALU.mult),
                      reads=[t_mean], writes=[t_tmp2])
                kb.op("dve", lambda e, nbc=nbc: e.tensor_tensor(out=t_rstd[:, 0:nbc], in0=B[3].h[:, 0:nbc], in1=t_tmp2[:, 0:nbc], op=ALU.subtract),
                      reads=[B[3], t_tmp2], writes=[t_rstd])
                kb.op("act", lambda e, nbc=nbc: e.activation(out=t_rstd[:, 0:nbc], in_=t_rstd[:, 0:nbc], func=AF.Identity, bias=self.epsc[:, 0:1]),
                      reads=[t_rstd, self.epsc], writes=[t_rstd])
                kb.op("act", lambda e, nbc=nbc: e.activation(out=t_rstd[:, 0:nbc], in_=t_rstd[:, 0:nbc], func=AF.Sqrt),
                      reads=[t_rstd], writes=[t_rstd])
                kb.op("dve", lambda e, nbc=nbc: e.reciprocal(out=t_rstd[:, 0:nbc], in_=t_rstd[:, 0:nbc]), reads=[t_rstd], writes=[t_rstd])
                for j in range(4):
                    kb.op("pool", lambda e, j=j, nbc=nbc: e.tensor_tensor(out=cT[:, j, 0:nbc], in0=cT[:, j, 0:nbc], in1=t_mean[:, 0:nbc], op=ALU.subtract),
                          reads=[cT.sub(j), t_mean], writes=[cT.sub(j)])
                    kb.op("dve", lambda e, j=j, nbc=nbc: e.tensor_tensor(out=cT[:, j, 0:nbc], in0=cT[:, j, 0:nbc], in1=t_rstd[:, 0:nbc], op=ALU.mult),
                          reads=[cT.sub(j), t_rstd], writes=[cT.sub(j)])
                    kb.op("act", lambda e, j=j, nbc=nbc, t0=t0: e.activation(
                        out=mixT[:, j, t0:t0 + nbc], in_=cT[:, j, 0:nbc], func=AF.Silu,
                        scale=cols["lg"][:, j:j + 1], bias=cols["lb"][:, j:j + 1]),
                        reads=[cT.sub(j), cols["lg"], cols["lb"]], writes=[mixT.sub((j, t0))])
            ntc = Ls // 128
            for kbi in range(Ls // 256):
                k0 = kbi * 256
                kb.dma("sp", lambda e, ctab=ctab, k0=k0, ntc=ntc: e.dma_start(
                    out=clb[:, 0:ntc, :], in_=ctab.ap.rearrange("(c p) k -> p c k", p=128)[:, :, k0:k0 + 256]),
                    reads=[ctab], writes=[clb])
                kb.dma("sp", lambda e, stab=stab, k0=k0, ntc=ntc: e.dma_start(
                    out=slb[:, 0:ntc, :], in_=stab.ap.rearrange("(c p) k -> p c k", p=128)[:, :, k0:k0 + 256]),
                    reads=[stab], writes=[slb])
                for g in range(4):
                    bank = B[4 + g % 2]
                    for c in range(ntc):
                        kb.op("pe", lambda e, g=g, c=c, bank=bank: e.matmul(
                            out=bank.h[:, 0:256], lhsT=Yall[:, c, g * 256:g * 256 + 128], rhs=clb[:, c, :],
                            start=(c == 0), stop=False), reads=[Yall, clb], writes=[bank])
                        kb.op("pe", lambda e, g=g, c=c, bank=bank, ntc=ntc: e.matmul(
                            out=bank.h[:, 0:256], lhsT=Yall[:, c, g * 256 + 128:g * 256 + 256], rhs=slb[:, c, :],
                            start=False, stop=(c == ntc - 1)), reads=[Yall, slb], writes=[bank])
                    if g % 2 == 0:
                        kb.op("act", lambda e, g=g, bank=bank, k0=k0: e.copy(out=mixT[:, 4 + g, k0:k0 + 256], in_=bank.h[:, 0:256]),
                              reads=[bank], writes=[mixT.sub((4 + g, k0))])
                    else:
                        kb.op("dve", lambda e, g=g, bank=bank, k0=k0: e.tensor_copy(out=mixT[:, 4 + g, k0:k0 + 256], in_=bank.h[:, 0:256]),
                              reads=[bank], writes=[mixT.sub((4 + g, k0))])
            kb.dma("pool", lambda e: e.dma_start(out=wout[:], in_=self.ab_w_out.ap.rearrange("(k p) f -> p k f", p=128)),
                   reads=[self.ab_w_out], writes=[wbuf])
            for tc in range(ntc):
                kb.dma("sp", lambda e, src=src, tc=tc: e.dma_start(out=xb[:], in_=src.ap[tc * 128:(tc + 1) * 128, :]),
                       reads=[src], writes=[xb])
                for db in range(2):
                    bank = B[6 + db]
                    for m in range(8):
                        kb.op("pe", lambda e, m=m, db=db, bank=bank, tc=tc: e.matmul(
                            out=bank.h[:, 0:512], lhsT=mixT[:, m, tc * 128:(tc + 1) * 128], rhs=wout[:, m, db * 512:(db + 1) * 512],
                            start=(m == 0), stop=(m == 7)), reads=[mixT, wbuf], writes=[bank])
                    kb.op("dve", lambda e, db=db, bank=bank: e.tensor_tensor(
                        out=t_tmp[:, 0:512], in0=bank.h[:, 0:512], in1=g1b[:, db * 512:(db + 1) * 512], op=ALU.mult),
                        reads=[bank, g1b], writes=[t_tmp])
                    kb.op("pool", lambda e, db=db: e.tensor_tensor(
                        out=xb[:, db * 512:(db + 1) * 512], in0=xb[:, db * 512:(db + 1) * 512], in1=t_tmp[:, 0:512], op=ALU.add),
                        reads=[xb, t_tmp], writes=[xb])
                kb.dma("sp", lambda e, dst=dst, tc=tc: e.dma_start(out=dst.ap[tc * 128:(tc + 1) * 128, :], in_=xb[:]),
                       reads=[xb], writes=[dst.sub(("row", tc))])


    def mla(self):
        kb = self.kb
        l = 1
        B = kb.banks
        NKV = LC + L
        NT = NKV // 128
        SCALE = 96.0 ** -0.5
        cast = lambda dst, src_ap, tr: kb.dma("pool", lambda e: e.dma_start(out=dst[:], in_=src_ap), reads=[tr], writes=[dst])
        wi = kb.alloc("wi", [128, 8, 416], BF16)
        cast(wi, self.mla_w_in.ap.rearrange("(k p) f -> p k f", p=128), self.mla_w_in)
        wuq = kb.alloc("wuq", [128, 2, 1536], BF16)
        cast(wuq, self.mla_w_uq.ap.rearrange("(k p) f -> p k f", p=128), self.mla_w_uq)
        wukv = kb.alloc("wukv", [128, 2048], BF16)
        cast(wukv, self.mla_w_ukv.ap[:, :], self.mla_w_ukv)
        wo = kb.alloc("wo", [128, 8, D], BF16)
        cast(wo, self.mla_w_o.ap.rearrange("(k p) f -> p k f", p=128), self.mla_w_o)
        ropt = kb.alloc("ropt", [128, 16, 32], F32)
        kb.dma("sp", lambda e: e.dma_start(out=ropt[:], in_=self.c_rope.ap.rearrange("(c p) f -> p c f", p=128)),
               reads=[self.c_rope], writes=[ropt])
        qgc = kb.alloc("qgc", [128, 2], F32)
        kb.dma("sp", lambda e: e.dma_start(out=qgc[:], in_=self.mla_qg.ap.rearrange("(j p) -> p j", p=128)), reads=[self.mla_qg], writes=[qgc])
        kvgc = kb.alloc("kvgc", [128, 1], F32)
        kb.dma("sp", lambda e: e.dma_start(out=kvgc[:], in_=self.mla_kvg.ap.rearrange("(j p) -> p j", p=128)), reads=[self.mla_kvg], writes=[kvgc])
        ones1 = kb.alloc("ones1", [128, 128], BF16)
        kb.op("dve", lambda e: e.memset(ones1[:], 1.0), writes=[ones1])
        bufs = self.norm_bufs(nx=1)
        xb = bufs[0][0]
        uT = kb.alloc("uTall", [128, 8, NKV], BF16)
        cqnT = kb.alloc("cqnT", [128, 2, L], BF16)
        ckvnT = kb.alloc("ckvnT", [128, NKV], BF16)
        KTs = [kb.alloc(f"KT{i}", [128, NKV], BF16) for i in range(2)]
        QTs = [kb.alloc(f"QT{i}", [128, L], BF16) for i in range(2)]
        qtoks = [kb.alloc(f"qtok{i}", [128, 8, 96], BF16) for i in range(2)]
        Vaug = [kb.alloc(f"Vaug{i}", [128, NT, 128], BF16) for i in range(2)]
        kb.op("dve", lambda e: e.memset(Vaug[0][:].rearrange("p a b -> p (a b)"), 1.0), writes=[Vaug[0]])
        kb.op("dve", lambda e: e.memset(Vaug[1][:].rearrange("p a b -> p (a b)"), 1.0), writes=[Vaug[1]])
        pT = [kb.alloc(f"pT{i}", [128, 512], BF16) for i in range(4)]
        oTs = kb.alloc("oTs", [128, 512], F32)
        onesf = kb.alloc("onesf", [128, 512], F32)
        kb.op("dve", lambda e: e.memset(onesf[:], 1.0), writes=[onesf])
        recf = kb.alloc("recf", [128, 512], F32)
        rech = kb.alloc("rech", [128, 512], BF16)
        recl = kb.alloc("recl", [128, 512], BF16)
        attnT = kb.alloc("attnT", [128, 8, L], BF16)
        g1b = kb.alloc("g1bm", [128, D], F32)
        t_tmp = kb.alloc("t_tmpm", [128, 512], F32)
        zt = kb.alloc("zt", [128, 416], F32)
        cqn = kb.alloc("cqn", [128, 256], BF16)
        ckvn = kb.alloc("ckvn", [128, 128], BF16)
        ktok = kb.alloc("ktok", [128, 96], BF16)
        kb.op("dve", lambda e: e.memset(ktok[:], 0.0), writes=[ktok])
        rt = [kb.alloc(f"rt{i}", [128, 4, 16], F32) for i in range(4)]
        st2 = kb.alloc("st2", [128, 8 * NT], F32)
        kb.op("dve", lambda e: e.memset(st2[:], 0.0), writes=[st2])
        junk2 = kb.alloc("junk2", [128, 256], BF16)
        addc1 = lambda k, r: self.modc[l][:, k, r:r + 1]
        mulc1 = self.mul1c[l]
        npt = 0
        for s in range(NS):
            kb.dma("sp", lambda e, s=s: e.dma_start(out=g1b[:], in_=self.modd.ap[l, s, 2 * D:3 * D].partition_broadcast(128)),
                   reads=[self.modd.sub(l)], writes=[g1b])
            kb.op("dve", lambda e: e.memset(st2[:], 0.0), writes=[st2])
            for b2 in range(LC // 256):
                self.norm_batch(self.xc[s], b2 * 256, 2, mulc1, addc1, 2, uT, b2 * 256, bufs, [B[0], B[1], B[2], B[3]])
            for b2 in range(L // 256):
                self.norm_batch(self.xr[s], b2 * 256, 2, mulc1, addc1, s, uT, LC + b2 * 256, bufs, [B[0], B[1], B[2], B[3]])
            for c in range(NT):
                lat = c >= 2
                cl_ = c - 2
                zb = B[4 + c % 2]
                for k in range(8):
                    kb.op("pe", lambda e, c=c, k=k, zb=zb: e.matmul(out=zb.h[:, 0:416], lhsT=uT[:, k, c * 128:(c + 1) * 128], rhs=wi[:, k, :],
                                                                  start=(k == 0), stop=(k == 7)), reads=[uT, wi], writes=[zb])
                kb.op("act", lambda e, zb=zb: e.copy(out=zt[:], in_=zb.h[:, 0:416]), reads=[zb], writes=[zt])
                so = c * 8
                parts = [(256, 128, 128.0, ckvn, 0)] + ([(0, 256, 256.0, cqn, 4)] if lat else [])
                for (c0, n, nf, dstt, o) in parts:
                    kb.op("act", lambda e, c0=c0, n=n, so=so, o=o: e.activation(out=junk2[:, 0:n], in_=zt[:, c0:c0 + n], func=AF.Square,
                                                                             accum_out=st2[:, so + o:so + o + 1]), reads=[zt], writes=[junk2, st2.sub(so + o)])
                    kb.op("act", lambda e, so=so, o=o, nf=nf: e.activation(out=st2[:, so + o + 1:so + o + 2], in_=st2[:, so + o:so + o + 1], func=AF.Identity,
                                                                         scale=1.0 / nf, bias=self.epsc[:, 0:1]), reads=[st2.sub(so + o), self.epsc], writes=[st2.sub(so + o + 1)])
                    kb.op("act", lambda e, so=so, o=o: e.activation(out=st2[:, so + o + 2:so + o + 3], in_=st2[:, so + o + 1:so + o + 2], func=AF.Sqrt),
                          reads=[st2.sub(so + o + 1)], writes=[st2.sub(so + o + 2)])
                    kb.op("dve", lambda e, so=so, o=o: e.reciprocal(out=st2[:, so + o + 3:so + o + 4], in_=st2[:, so + o + 2:so + o + 3]),
                          reads=[st2.sub(so + o + 2)], writes=[st2.sub(so + o + 3)])
                    kb.op("act", lambda e, c0=c0, n=n, so=so, o=o, dstt=dstt: e.activation(out=dstt[:, 0:n], in_=zt[:, c0:c0 + n], func=AF.Identity,
                                                                                          scale=st2[:, so + o + 3:so + o + 4]), reads=[zt, st2.sub(so + o + 3)], writes=[dstt])
                if lat:
                    krv = zt[:, 384:416].rearrange("p (i two) -> p i two", two=2)
                    xe, xo = krv[:, :, 0], krv[:, :, 1]
                    cs, sn = ropt[:, cl_, 0:16], ropt[:, cl_, 16:32]
                    ko = ktok[:, 64:96].rearrange("p (i two) -> p i two", two=2)
                    a0, a1, a2, a3 = rt[0][:, 0, :], rt[1][:, 0, :], rt[2][:, 0, :], rt[3][:, 0, :]
                    kb.op("dve", lambda e, xe=xe, cs=cs, a0=a0: e.tensor_tensor(out=a0, in0=xe, in1=cs, op=ALU.mult), reads=[zt, ropt], writes=[rt[0]])
                    kb.op("dve", lambda e, xo=xo, sn=sn, a1=a1: e.tensor_tensor(out=a1, in0=xo, in1=sn, op=ALU.mult), reads=[zt, ropt], writes=[rt[1]])
                    kb.op("dve", lambda e, xe=xe, sn=sn, a2=a2: e.tensor_tensor(out=a2, in0=xe, in1=sn, op=ALU.mult), reads=[zt, ropt], writes=[rt[2]])
                    kb.op("dve", lambda e, xo=xo, cs=cs, a3=a3: e.tensor_tensor(out=a3, in0=xo, in1=cs, op=ALU.mult), reads=[zt, ropt], writes=[rt[3]])
                    kb.op("dve", lambda e, ko=ko, a0=a0, a1=a1: e.tensor_tensor(out=ko[:, :, 0], in0=a0, in1=a1, op=ALU.subtract), reads=[rt[0], rt[1]], writes=[ktok.sub(0)])
                    kb.op("dve", lambda e, ko=ko, a2=a2, a3=a3: e.tensor_tensor(out=ko[:, :, 1], in0=a2, in1=a3, op=ALU.add), reads=[rt[2], rt[3]], writes=[ktok.sub(1)])
                else:
                    kb.op("dve", lambda e: e.tensor_copy(out=ktok[:, 64:96], in_=zt[:, 384:416]), reads=[zt], writes=[ktok.sub(0)])
                tbk = B[6 + c % 2]
                pv = tbk.h.bitcast(BF16)
                if lat:
                    for qk in range(2):
                        kb.op("pe", lambda e, pv=pv, qk=qk: e.transpose(out=pv[:, qk * 128:(qk + 1) * 128], in_=cqn[:, qk * 128:(qk + 1) * 128], identity=self.identb[:]),
                              reads=[cqn, self.identb], writes=[tbk])
                kb.op("pe", lambda e, pv=pv: e.transpose(out=pv[:, 256:384], in_=ckvn[:, :], identity=self.identb[:]), reads=[ckvn, self.identb], writes=[tbk])
                kb.op("pe", lambda e, pv=pv: e.transpose(out=pv[0:96, 384:512], in_=ktok[:, 0:96], identity=self.identb[:]), reads=[ktok, self.identb], writes=[tbk])
                if lat:
                    for qk in range(2):
                        kb.op("act", lambda e, pv=pv, qk=qk, cl_=cl_: e.activation(out=cqnT[:, qk, cl_ * 128:(cl_ + 1) * 128], in_=pv[:, qk * 128:(qk + 1) * 128],
                                                                                 func=AF.Identity, scale=qgc[:, qk:qk + 1]), reads=[tbk, qgc], writes=[cqnT.sub((qk, cl_))])
                kb.op("act", lambda e, pv=pv, c=c: e.activation(out=ckvnT[:, c * 128:(c + 1) * 128], in_=pv[:, 256:384], func=AF.Identity, scale=kvgc[:, 0:1]),
                      reads=[tbk, kvgc], writes=[ckvnT.sub(c)])
                for KTx in KTs:
                    kb.op("act", lambda e, pv=pv, c=c, KTx=KTx: e.copy(out=KTx[64:96, c * 128:(c + 1) * 128], in_=pv[64:96, 384:512]),
                          reads=[tbk], writes=[KTx.sub(("r", c))])
            def projA(h):
                KTh = KTs[h % 2]
                for blk in range(5):
                    n0 = blk * 512
                    nn = min(512, NKV - n0)
                    bk = B[5]
                    kb.op("pe", lambda e, h=h, n0=n0, nn=nn, bk=bk: e.matmul(out=bk.h[0:64, 0:nn], lhsT=wukv[:, h * 128:h * 128 + 64], rhs=ckvnT[:, n0:n0 + nn],
                                                                        start=True, stop=True), reads=[wukv, ckvnT], writes=[bk])
                    kb.op("dve", lambda e, n0=n0, nn=nn, bk=bk, KTh=KTh: e.tensor_copy(out=KTh[0:64, n0:n0 + nn], in_=bk.h[0:64, 0:nn]),
                          reads=[bk], writes=[KTh.sub(("n", blk))])
                va = Vaug[h % 2]
                vo = 0 if h % 2 == 0 else 64
                for vb in range(3):
                    ntl = min(8, NT - vb * 8)
                    bv = B[6]
                    for ci in range(ntl):
                        c = vb * 8 + ci
                        kb.op("pe", lambda e, h=h, c=c, ci=ci, bv=bv: e.matmul(out=bv.h[:, ci * 64:(ci + 1) * 64], lhsT=ckvnT[:, c * 128:(c + 1) * 128],
                                                                             rhs=wukv[:, h * 128 + 64:h * 128 + 128], start=True, stop=True), reads=[ckvnT, wukv], writes=[bv])
                    kb.op("dve", lambda e, vb=vb, ntl=ntl, bv=bv, va=va, vo=vo: e.tensor_copy(
                        out=va[:, vb * 8:vb * 8 + ntl, vo:vo + 64], in_=bv.h[:, 0:ntl * 64].rearrange("p (c f) -> p c f", f=64)),
                        reads=[bv], writes=[va.sub(vb)])

            def projQ(h, half, stage):
                qtk = qtoks[half]
                QTh = QTs[h % 2]
                for bq in range(2):
                    c0 = half * 8 + bq * 4
                    if stage == 0:
                        bqk = B[4 + bq]
                        for ci in range(4):
                            c = c0 + ci
                            for qk in range(2):
                                kb.op("pe", lambda e, h=h, c=c, ci=ci, qk=qk, bqk=bqk: e.matmul(
                                    out=bqk.h[:, ci * 96:(ci + 1) * 96], lhsT=cqnT[:, qk, c * 128:(c + 1) * 128], rhs=wuq[:, qk, h * 96:(h + 1) * 96],
                                    start=(qk == 0), stop=(qk == 1)), reads=[cqnT, wuq], writes=[bqk])
                        qv = bqk.h[:, 0:384].rearrange("p (c f) -> p c f", f=96)
                        lc0 = bq * 4
                        kb.op("dve", lambda e, qv=qv, lc0=lc0, qtk=qtk: e.tensor_copy(out=qtk[:, lc0:lc0 + 4, 0:64], in_=qv[:, :, 0:64]), reads=[bqk], writes=[qtk.sub((lc0, "n"))])
                        xe, xo = qv[:, :, 64:96:2], qv[:, :, 65:96:2]
                        cs, sn = ropt[:, c0:c0 + 4, 0:16], ropt[:, c0:c0 + 4, 16:32]
                        qo = qtk[:, lc0:lc0 + 4, 64:96].rearrange("p c (i two) -> p c i two", two=2)
                        kb.op("dve", lambda e, xe=xe, cs=cs: e.tensor_tensor(out=rt[0][:], in0=xe, in1=cs, op=ALU.mult), reads=[bqk, ropt], writes=[rt[0]])
                        kb.op("dve", lambda e, xo=xo, sn=sn: e.tensor_tensor(out=rt[1][:], in0=xo, in1=sn, op=ALU.mult), reads=[bqk, ropt], writes=[rt[1]])
                        kb.op("dve", lambda e, xe=xe, sn=sn: e.tensor_tensor(out=rt[2][:], in0=xe, in1=sn, op=ALU.mult), reads=[bqk, ropt], writes=[rt[2]])
                        kb.op("dve", lambda e, xo=xo, cs=cs: e.tensor_tensor(out=rt[3][:], in0=xo, in1=cs, op=ALU.mult), reads=[bqk, ropt], writes=[rt[3]])
                        kb.op("pool", lambda e, qo=qo: e.tensor_tensor(out=qo[:, :, :, 0], in0=rt[0][:], in1=rt[1][:], op=ALU.subtract),
                              reads=[rt[0], rt[1]], writes=[qtk.sub((lc0, "e"))])
                        kb.op("pool", lambda e, qo=qo: e.tensor_tensor(out=qo[:, :, :, 1], in0=rt[2][:], in1=rt[3][:], op=ALU.add),
                              reads=[rt[2], rt[3]], writes=[qtk.sub((lc0, "o"))])
                    else:
                        lc0 = bq * 4
                        tq = B[6 + bq]
                        pvq = tq.h.bitcast(BF16)
                        for ci in range(4):
                            kb.op("pe", lambda e, pvq=pvq, lc=lc0 + ci, ci=ci, qtk=qtk: e.transpose(out=pvq[0:96, ci * 128:(ci + 1) * 128], in_=qtk[:, lc, 0:96], identity=self.identb[:]),
                                  reads=[qtk, self.identb], writes=[tq])
                        kb.op("dve", lambda e, pvq=pvq, c0=c0, QTh=QTh: e.tensor_copy(out=QTh[0:96, c0 * 128:(c0 + 4) * 128], in_=pvq[0:96, 0:512]), reads=[tq], writes=[QTh.sub(c0)])

            def normalize1(h, qb, bo):
                kb.op("dve", lambda e, bo=bo: e.tensor_copy(out=oTs[:], in_=bo.h[:, 0:512]), reads=[bo], writes=[oTs])

            def normalize1b(h, qb):
                dp = 64 if (h % 2 == 0) else 0
                kb.op("dve", lambda e, dp=dp: e.reciprocal(out=recf[dp:dp + 1, :], in_=oTs[dp:dp + 1, :]), reads=[oTs], writes=[recf])
                kb.op("pool", lambda e, dp=dp: e.tensor_copy(out=rech[dp:dp + 1, :], in_=recf[dp:dp + 1, :]), reads=[recf], writes=[rech])
                kb.op("pool", lambda e, dp=dp: e.tensor_tensor(out=recl[dp:dp + 1, :], in0=recf[dp:dp + 1, :], in1=rech[dp:dp + 1, :], op=ALU.subtract),
                      reads=[recf, rech], writes=[recl])

            def normalize2(h, qb):
                even = (h % 2 == 0)
                dp = 64 if even else 0
                op_ = 0 if even else 64
                bb = B[4]
                kb.op("pe", lambda e, dp=dp, bb=bb: e.matmul(out=bb.h[:, 0:512], lhsT=ones1[dp:dp + 1, :], rhs=rech[dp:dp + 1, :], start=True, stop=False),
                      reads=[ones1, rech], writes=[bb])
                kb.op("pe", lambda e, dp=dp, bb=bb: e.matmul(out=bb.h[:, 0:512], lhsT=ones1[dp:dp + 1, :], rhs=recl[dp:dp + 1, :], start=False, stop=True),
                      reads=[ones1, recl], writes=[bb])
                kb.op("dve", lambda e, op_=op_, h=h, qb=qb, bb=bb: e.tensor_tensor(
                    out=attnT[op_:op_ + 64, h // 2, qb * 512:(qb + 1) * 512], in0=oTs[op_:op_ + 64, :], in1=bb.h[op_:op_ + 64, 0:512], op=ALU.mult),
                    reads=[oTs, bb], writes=[attnT.sub((h, qb))])

            projA(0)
            for half in range(2):
                projQ(0, half, 0)
                projQ(0, half, 1)
            pend = []
            pendnorm = []
            nstep = 0

            def issue_pv(item):
                (h, qb, c, bo, pt, va) = item
                kb.op("pe", lambda e, c=c, bo=bo, pt=pt, va=va: e.matmul(out=bo.h[:, 0:512], lhsT=va[:, c, :], rhs=pt[:],
                                                                       start=(c == 0), stop=(c == NT - 1)), reads=[va, pt], writes=[bo])
                if c == NT - 1:
                    normalize1(h, qb, bo)
                    pendnorm.append((nstep + 2, 1, h, qb))
                    pendnorm.append((nstep + 12, 2, h, qb))
                    pendnorm.sort()

            for h in range(NE):
                KTh = KTs[h % 2]
                QTh = QTs[h % 2]
                va = Vaug[h % 2]
                for qb in range(4):
                    bo = B[2 + qb % 2]
                    for c in range(NT):
                        if h + 1 < NE:
                            if qb == 0 and c == 9:
                                projA(h + 1)
                            if qb == 1 and c == 9:
                                projQ(h + 1, 0, 0)
                            if qb == 2 and c == 9:
                                projQ(h + 1, 0, 1)
                            if qb == 3 and c == 8:
                                projQ(h + 1, 1, 0)
                            if qb == 3 and c == 16:
                                projQ(h + 1, 1, 1)
                        while pendnorm and nstep >= pendnorm[0][0]:
                            (_, stg, hh, qq) = pendnorm.pop(0)
                            (normalize1b if stg == 1 else normalize2)(hh, qq)
                        bs = B[nstep % 2]
                        pt = pT[nstep % 4]
                        nstep += 1
                        kb.op("pe", lambda e, c=c, qb=qb, bs=bs, KTh=KTh, QTh=QTh: e.matmul(
                            out=bs.h[:, 0:512], lhsT=KTh[0:96, c * 128:(c + 1) * 128], rhs=QTh[0:96, qb * 512:(qb + 1) * 512],
                            start=True, stop=True), reads=[KTh, QTh], writes=[bs])
                        kb.op("act", lambda e, bs=bs, pt=pt: e.activation(out=pt[:], in_=bs.h[:, 0:512], func=AF.Exp, scale=SCALE), reads=[bs], writes=[pt])
                        pend.append((h, qb, c, bo, pt, va))
                        if len(pend) > 1:
                            issue_pv(pend.pop(0))
            while pend:
                issue_pv(pend.pop(0))
            while pendnorm:
                (_, stg, hh, qq) = pendnorm.pop(0)
                (normalize1b if stg == 1 else normalize2)(hh, qq)
            for tc in range(L // 128):
                kb.dma("sp", lambda e, s=s, tc=tc: e.dma_start(out=xb[:], in_=self.xr[s].ap[tc * 128:(tc + 1) * 128, :]),
                       reads=[self.xr[s]], writes=[xb])
                for db in range(2):
                    bank = B[6 + db]
                    for m in range(8):
                        kb.op("pe", lambda e, m=m, db=db, bank=bank, tc=tc: e.matmul(
                            out=bank.h[:, 0:512], lhsT=attnT[:, m, tc * 128:(tc + 1) * 128], rhs=wo[:, m, db * 512:(db + 1) * 512],
                            start=(m == 0), stop=(m == 7)), reads=[attnT, wo], writes=[bank])
                    kb.op("dve", lambda e, db=db, bank=bank: e.tensor_tensor(
                        out=t_tmp[:, 0:512], in0=bank.h[:, 0:512], in1=g1b[:, db * 512:(db + 1) * 512], op=ALU.mult),
                        reads=[bank, g1b], writes=[t_tmp])
                    kb.op("pool", lambda e, db=db: e.tensor_tensor(
                        out=xb[:, db * 512:(db + 1) * 512], in0=xb[:, db * 512:(db + 1) * 512], in1=t_tmp[:, 0:512], op=ALU.add),
                        reads=[xb, t_tmp], writes=[xb])
                kb.dma("sp", lambda e, s=s, tc=tc: e.dma_start(out=self.xr[s].ap[tc * 128:(tc + 1) * 128, :], in_=xb[:]),
                       reads=[xb], writes=[self.xr[s].sub(("row", tc))])


def _consts():
    bf = ml_dtypes.bfloat16
    c = {}
    c["c_identb"] = np.eye(128, dtype=np.float32).astype(bf)
    c["c_identf"] = np.eye(128, dtype=np.float32)
    i = np.arange(128, dtype=np.int64)
    ang = 2.0 * np.pi * ((i[:, None] * i[None, :]) % 128).astype(np.float64) / 128.0
    c["c_csc"] = np.concatenate([np.cos(ang), np.sin(ang)], axis=1).astype(np.float32) / np.float32(np.sqrt(128.0))
    c["c_csc"] = c["c_csc"].astype(bf)
    for nm, n in (("", L), ("c", LC)):
        t = np.arange(n, dtype=np.int64)
        a = 2.0 * np.pi * ((t[:, None] * t[None, :]) % n).astype(np.float64) / n
        c["c_cl" + nm] = (np.cos(a) / np.sqrt(n)).astype(np.float32).astype(bf)
        c["c_sl" + nm] = (-np.sin(a) / np.sqrt(n)).astype(np.float32).astype(bf)
    t = np.arange(L)
    row = (t // 64).astype(np.float32)
    col = (t % 64).astype(np.float32)
    inv = (np.float32(10000.0) ** (-np.arange(8, dtype=np.float32) / np.float32(8))).astype(np.float32)
    angr = np.concatenate([row[:, None] * inv[None, :], col[:, None] * inv[None, :]], axis=1).astype(np.float32)
    c["c_rope"] = np.concatenate([np.cos(angr), np.sin(angr)], axis=1).astype(np.float32)
    c["c_ctxbase"] = np.concatenate([np.zeros(32), np.full(32, LC), NS * LC + np.arange(64)]).astype(np.float32).reshape(128, 1)
    return c


def _in_map(inp, core, consts):
    f = lambda a: np.ascontiguousarray(np.asarray(a, dtype=np.float32))
    s0 = core * NS
    m = {}
    m["x"] = f(inp["x"][s0:s0 + NS])
    m["ctx"] = f(inp["ctx"][s0:s0 + NS])
    cv3 = np.stack([inp["c"][s0], inp["c"][s0 + 1], inp["c_ctx"]], axis=0).astype(np.float32)
    m["cv"] = np.ascontiguousarray(cv3.reshape(3, 8, 128).transpose(2, 1, 0))
    for k in ("mod_w", "mod_b", "norm1_g", "norm2_g", "final_g", "moe_w_router", "moe_w1", "moe_w3", "moe_w2"):
        m[k] = f(inp[k])
    for k in ("ab_w_in", "ab_conv_w", "ab_conv_b", "ab_ln_g", "ab_ln_b", "ab_w_out", "mla_w_in", "mla_q_norm_g",
              "mla_kv_norm_g", "mla_w_uq", "mla_w_ukv", "mla_w_o"):
        m[k] = f(inp[k][0])
    m.update(consts)
    return m


_CACHE = {}


def run_prog(inputs, phases, copy_in=False, ncores=8, debug_route=False, raw=False):
    key = (tuple(phases), copy_in, debug_route)
    if key not in _CACHE:
        p = Prog(phases=phases, copy_in=copy_in)
        p.debug_route = debug_route
        _CACHE[key] = p.build()
    nc = _CACHE[key]
    consts = _consts()
    in_maps = [_in_map(inputs, c, consts) for c in range(ncores)]
    res = run_bass_kernel_spmd(nc, in_maps, core_ids=list(range(ncores)))
    if raw:
        return res.results
    return np.concatenate([np.asarray(r["y"]) for r in res.results], axis=0)


def kernel(**inputs):
    out = run_prog(inputs, ("mix0", "moe0", "mla1", "moe1", "final"))
    return out.astype(np.float32)
```

```python
import numpy as np
import ml_dtypes
from contextlib import ExitStack
import concourse.bass as bass
import concourse.mybir as mybir
from concourse.bass_utils import run_bass_kernel_spmd

F32 = mybir.dt.float32
BF16 = mybir.dt.bfloat16
I32 = mybir.dt.int32
U32 = mybir.dt.uint32
U8 = mybir.dt.uint8
AF = mybir.ActivationFunctionType
ALU = mybir.AluOpType
AX = mybir.AxisListType

D = 1024
L = 2048
LC = 256
NS = 2
NE = 16
EPS = 1e-6
DSZ = {F32: 4, BF16: 2, I32: 4, U32: 4, U8: 1}


class Trk:
    def __init__(self, name):
        self.name = name
        self.w = None
        self.r = []
        self.kids = {}
        self.parent = None

    def sub(self, key):
        if key not in self.kids:
            k = Trk(f"{self.name}.{key}")
            k.parent = self
            self.kids[key] = k
        return self.kids[key]

    def rdeps(self):
        s = set()
        if self.w:
            s.add(self.w)
        if self.parent is not None and self.parent.w:
            s.add(self.parent.w)
        for k in self.kids.values():
            if k.w:
                s.add(k.w)
        return s

    def wdeps(self):
        s = self.rdeps()
        s.update(self.r)
        if self.parent is not None:
            s.update(self.parent.r)
        for k in self.kids.values():
            s.update(k.r)
        return s

    def did_read(self, ev):
        self.r.append(ev)

    def did_write(self, ev):
        self.w = ev
        self.r = []
        for k in self.kids.values():
            k.w = None
            k.r = []


class T(Trk):
    def __init__(self, kb, name, shape, dtype, off):
        super().__init__(name)
        self.kb = kb
        self.shape = shape
        self.dtype = dtype
        self.off = off
        self.h = kb.nc.alloc_sbuf_tensor_at(name, list(shape), dtype, offset=off)

    def view(self, name, shape, dtype, boff=0):
        return self.kb.nc.alloc_sbuf_tensor_at(
            self.kb.uname(name), list(shape), dtype, offset=self.off + boff)

    def __getitem__(self, k):
        return self.h[k]


class Lane:
    def __init__(self, key, sem):
        self.key = key
        self.sem = sem
        self.count = 0


class KB:
    COMPUTE = ["pe", "act", "dve", "pool"]
    QUEUES = ["sp", "pool", "act"]

    def __init__(self, n_lanes=8):
        self.nc = bass.Bass("TRN2", target_bir_lowering=False)
        nc = self.nc
        self.es = ExitStack()
        self.uid = 0
        self.semobj = {}
        self.cnt = {}
        for e in self.COMPUTE:
            self.semobj[e] = self.es.enter_context(nc.semaphore("s_" + e))
            self.cnt[e] = 0
        self.lanes = {}
        self.lane_rr = {}
        for q in self.QUEUES:
            self.lanes[q] = []
            for i in range(n_lanes):
                key = f"d_{q}{i}"
                self.semobj[key] = self.es.enter_context(nc.semaphore(key))
                self.lanes[q].append(Lane(key, self.semobj[key]))
            self.lane_rr[q] = 0
        self.prog = {e: [] for e in ["pe", "act", "dve", "pool", "sp"]}
        self.waited = {e: {} for e in ["pe", "act", "dve", "pool", "sp"]}
        self.arena_bytes = 206 * 1024
        ah = nc.alloc_sbuf_tensor("arena", [128, self.arena_bytes], U8)
        self.abase = nc.lookup_mloc(ah).addr
        self.atop = 0
        self.bnd = {}
        for n in (L - 1, NS * LC + 128 - 1):
            reg = self.es.enter_context(nc.gpsimd.register(f"bnd{n}"))
            self.bnd[n] = reg
            self.prog["pool"].append(lambda en, reg=reg, n=n: en.reg_mov(reg, n))
        self.banks = []
        self.pairs = []
        for i in range(4):
            ph = self.es.enter_context(nc.psum_tensor(f"pbank{i}", [128, 1024], F32))
            self.pairs.append(ph)
            for j in range(2):
                t = Trk(f"bank{2 * i + j}")
                t.h = ph[:, j * 512:(j + 1) * 512]
                t.psum = True
                self.banks.append(t)

    def uname(self, n):
        self.uid += 1
        return f"{n}_{self.uid}"

    def alloc(self, name, shape, dtype):
        nbytes = int(np.prod(shape[1:])) * DSZ[dtype]
        nbytes = (nbytes + 63) // 64 * 64
        off = self.atop
        assert off + nbytes <= self.arena_bytes, f"SBUF arena overflow at {name}: {off}+{nbytes}"
        self.atop += nbytes
        return T(self, self.uname(name), shape, dtype, self.abase + off)

    def mark(self):
        return self.atop

    def release(self, m):
        self.barrier()
        self.atop = m

    def dram(self, name, shape, dtype, kind="Internal"):
        if kind == "Internal":
            h = self.nc.dram_tensor(name, list(shape), dtype)
        else:
            h = self.nc.dram_tensor(name, list(shape), dtype, kind=kind)
        t = Trk(name)
        t.h = h
        t.ap = h.ap()
        return t

    def _waits(self, eng, evs):
        best = {}
        for (k, v) in evs:
            if v > best.get(k, 0):
                best[k] = v
        for k, v in best.items():
            if k == "pe" and eng == "pe":
                continue
            if self.waited[eng].get(k, 0) >= v:
                continue
            self.waited[eng][k] = v
            sem = self.semobj[k]
            self.prog[eng].append(lambda e, sem=sem, v=v: e.wait_ge(sem, v))

    def _deps(self, reads, writes, eng=None):
        evs = set()
        for t in reads:
            evs |= t.rdeps()
            root = t if t.parent is None else t.parent
            if getattr(root, "psum", False):
                for ev in root.r:
                    if ev[0] != eng:
                        evs.add(ev)
                for k in root.kids.values():
                    for ev in k.r:
                        if ev[0] != eng:
                            evs.add(ev)
        for t in writes:
            evs |= t.wdeps()
        return evs

    def op(self, eng, fn, reads=(), writes=()):
        evs = self._deps(reads, writes, eng)
        self._waits(eng, evs)
        self.cnt[eng] += 1
        sem = self.semobj[eng]
        self.prog[eng].append(lambda e, fn=fn, sem=sem: fn(e).then_inc(sem, 1))
        ev = (eng, self.cnt[eng])
        for t in reads:
            t.did_read(ev)
        for t in writes:
            t.did_write(ev)
        return ev

    def dma(self, q, fn, reads=(), writes=()):
        evs = self._deps(reads, writes)
        lanes = self.lanes[q]
        lane = lanes[self.lane_rr[q] % len(lanes)]
        self.lane_rr[q] += 1
        if lane.count > 0:
            evs.add((lane.key, lane.count))
        self._waits(q, evs)
        lane.count += 16
        sem = lane.sem
        def run(e, fn=fn, sem=sem):
            try:
                ins = fn(e)
            except Exception:
                print("DMA BUILD FAIL line", fn.__code__.co_firstlineno, "defaults", [str(d)[:80] for d in (fn.__defaults__ or ())])
                raise
            ins.then_inc(sem, 16)
        self.prog[q].append(run)
        ev = (lane.key, lane.count)
        for t in reads:
            t.did_read(ev)
        for t in writes:
            t.did_write(ev)
        return ev

    def barrier(self):
        evs = set()
        for e in self.COMPUTE:
            if self.cnt[e] > 0:
                evs.add((e, self.cnt[e]))
        for q in self.QUEUES:
            for ln in self.lanes[q]:
                if ln.count > 0:
                    evs.add((ln.key, ln.count))
        for e in ["pe", "act", "dve", "pool", "sp"]:
            self._waits(e, evs)

    def finish(self):
        self.barrier()
        nc = self.nc
        with nc.allow_non_contiguous_dma(reason="small strided constant loads"):
            with nc.Block() as block:
                @block.sync
                def _(e):
                    for f in self.prog["sp"]:
                        f(e)

                @block.tensor
                def _(e):
                    for f in self.prog["pe"]:
                        f(e)

                @block.scalar
                def _(e):
                    for f in self.prog["act"]:
                        f(e)

                @block.vector
                def _(e):
                    for f in self.prog["dve"]:
                        f(e)

                @block.gpsimd
                def _(e):
                    for f in self.prog["pool"]:
                        f(e)
        self.es.close()
        return nc


class Prog:
    def __init__(self, phases=("mix0", "moe0", "mla1", "moe1", "final"), copy_in=False):
        self.kb = KB()
        self.phases = phases
        self.copy_in = copy_in
        kb = self.kb
        di = lambda n, s, d=F32: kb.dram(n, s, d, kind="ExternalInput")
        self.x = di("x", [NS, L, D])
        self.ctx = di("ctx", [NS, LC, D])
        self.cv = di("cv", [128, 8, 3])
        self.mod_w = di("mod_w", [2, D, 6 * D])
        self.mod_b = di("mod_b", [2, 6 * D])
        self.n1g = di("norm1_g", [2, D])
        self.n2g = di("norm2_g", [2, D])
        self.final_g = di("final_g", [D])
        self.ab_w_in = di("ab_w_in", [D, 1536])
        self.ab_conv_w = di("ab_conv_w", [31, 512])
        self.ab_conv_b = di("ab_conv_b", [512])
        self.ab_ln_g = di("ab_ln_g", [512])
        self.ab_ln_b = di("ab_ln_b", [512])
        self.ab_w_out = di("ab_w_out", [D, D])
        self.mla_w_in = di("mla_w_in", [D, 416])
        self.mla_qg = di("mla_q_norm_g", [256])
        self.mla_kvg = di("mla_kv_norm_g", [128])
        self.mla_w_uq = di("mla_w_uq", [256, 1536])
        self.mla_w_ukv = di("mla_w_ukv", [128, 2048])
        self.mla_w_o = di("mla_w_o", [D, D])
        self.w_router = di("moe_w_router", [2, D, NE])
        self.w1 = di("moe_w1", [2, NE, D, D])
        self.w3 = di("moe_w3", [2, NE, D, D])
        self.w2 = di("moe_w2", [2, NE, D, D])
        self.c_identb = di("c_identb", [128, 128], BF16)
        self.c_identf = di("c_identf", [128, 128], F32)
        self.c_csc = di("c_csc", [128, 256], BF16)
        self.c_cl = di("c_cl", [L, L], BF16)
        self.c_sl = di("c_sl", [L, L], BF16)
        self.c_clc = di("c_clc", [LC, LC], BF16)
        self.c_slc = di("c_slc", [LC, LC], BF16)
        self.c_rope = di("c_rope", [L, 32], F32)
        self.c_ctxbase = di("c_ctxbase", [128, 1], F32)
        self.out = kb.dram("y", [NS, L, D], F32, kind="ExternalOutput")
        self.xr = [kb.dram(f"xr{s}", [L, D], F32) for s in range(NS)]
        self.xc_all = kb.dram("xc_all", [NS * LC + 128, D], F32)
        self.xnc_all = kb.dram("xnc_all", [NS * LC + 128, D], BF16)
        self.xc = []
        self.xnl = [kb.dram(f"xnl{s}", [L, D], BF16) for s in range(NS)]
        self.xnc = []
        for s in range(NS):
            t = self.xc_all.sub(s); t.ap = self.xc_all.ap[s * LC:(s + 1) * LC, :]; self.xc.append(t)
            t = self.xnc_all.sub(s); t.ap = self.xnc_all.ap[s * LC:(s + 1) * LC, :]; self.xnc.append(t)
        self.xin = []
        self.cin = []
        for s in range(NS):
            t = self.x.sub(s); t.ap = self.x.ap[s]; self.xin.append(t)
            t = self.ctx.sub(s); t.ap = self.ctx.ap[s]; self.cin.append(t)
        self.modd = kb.dram("modd", [2, 3, 6 * D], F32)

    def build(self):
        kb = self.kb
        self.prologue()
        if self.copy_in:
            for s in range(NS):
                kb.dma("sp", lambda e, s=s: e.dma_start(out=self.xr[s].ap, in_=self.x.ap[s]),
                       reads=[self.x], writes=[self.xr[s]])
                kb.dma("sp", lambda e, s=s: e.dma_start(out=self.xc[s].ap, in_=self.ctx.ap[s]),
                       reads=[self.ctx], writes=[self.xc[s]])
            kb.barrier()
        for ph in self.phases:
            m = kb.mark()
            if ph == "mix0":
                self.mixer0()
            elif ph == "moe0":
                self.moe(0, with_ctx=True)
            elif ph == "mla1":
                self.mla()
            elif ph == "moe1":
                self.moe(1, with_ctx=False)
            elif ph == "final":
                self.final()
            elif ph == "dump":
                self.dump()
            kb.release(m)
        return kb.finish()

    def prologue(self):
        kb = self.kb
        self.identb = kb.alloc("identb", [128, 128], BF16)
        self.identf = kb.alloc("identf", [128, 128], F32)
        kb.dma("sp", lambda e: e.dma_start(out=self.identb[:], in_=self.c_identb.ap[:, :]),
               reads=[self.c_identb], writes=[self.identb])
        kb.dma("sp", lambda e: e.dma_start(out=self.identf[:], in_=self.c_identf.ap[:, :]),
               reads=[self.c_identf], writes=[self.identf])
        self.epsc = kb.alloc("epsc", [128, 1], F32)
        kb.op("dve", lambda e: e.memset(self.epsc[:], EPS), writes=[self.epsc])
        self.zeroc = kb.alloc("zeroc", [128, 1], F32)
        kb.op("dve", lambda e: e.memset(self.zeroc[:], 0.0), writes=[self.zeroc])
        self.modc = [kb.alloc(f"modc{l}", [128, 48, 3], F32) for l in range(2)]
        self.mul1c = [kb.alloc(f"mul1c{l}", [128, 8, 3], F32) for l in range(2)]
        self.mul2c = [kb.alloc(f"mul2c{l}", [128, 8, 3], F32) for l in range(2)]
        self.n1gc = kb.alloc("n1gc", [128, 2, 8], F32)
        self.n2gc = kb.alloc("n2gc", [128, 2, 8], F32)
        kb.dma("sp", lambda e: e.dma_start(out=self.n1gc[:], in_=self.n1g.ap.rearrange("l (k p) -> p l k", p=128)),
               reads=[self.n1g], writes=[self.n1gc])
        kb.dma("sp", lambda e: e.dma_start(out=self.n2gc[:], in_=self.n2g.ap.rearrange("l (k p) -> p l k", p=128)),
               reads=[self.n2g], writes=[self.n2gc])
        m0 = kb.mark()
        zf = kb.alloc("zf", [128, D], F32)
        zb = kb.alloc("zb", [128, D], BF16)
        kb.op("dve", lambda e: e.memset(zf[:], 0.0), writes=[zf])
        kb.op("dve", lambda e: e.memset(zb[:], 0.0), writes=[zb])
        kb.dma("sp", lambda e: e.dma_start(out=self.xc_all.ap[NS * LC:NS * LC + 128, :], in_=zf[:]),
               reads=[zf], writes=[self.xc_all.sub("pad")])
        kb.dma("sp", lambda e: e.dma_start(out=self.xnc_all.ap[NS * LC:NS * LC + 128, :], in_=zb[:]),
               reads=[zb], writes=[self.xnc_all.sub("pad")])
        cvt = kb.alloc("cvt", [128, 24], F32)
        sct = kb.alloc("sct", [128, 24], BF16)
        kb.dma("sp", lambda e: e.dma_start(out=cvt[:], in_=self.cv.ap.rearrange("p k r -> p (k r)")),
               reads=[self.cv], writes=[cvt])
        kb.op("act", lambda e: e.activation(out=sct[:], in_=cvt[:], func=AF.Silu), reads=[cvt], writes=[sct])
        mwt = [kb.alloc(f"mwt{i}", [128, 8, 1536], BF16) for i in range(2)]
        mrow = kb.alloc("mrow", [3, 6 * D], F32)
        mb3 = kb.alloc("mb3", [3, 6 * D], F32)
        it = 0
        for l in range(2):
            kb.dma("sp", lambda e, l=l: e.dma_start(out=mb3[:], in_=self.mod_b.ap[l].partition_broadcast(3)),
                   reads=[self.mod_b], writes=[mb3])
            for pc in range(4):
                wt = mwt[it % 2]
                it += 1
                src = self.mod_w.ap[l].rearrange("(k p) n -> p k n", p=128)[:, :, pc * 1536:(pc + 1) * 1536]
                kb.dma("pool", lambda e, wt=wt, src=src: e.dma_start(out=wt[:], in_=src),
                       reads=[self.mod_w], writes=[wt])
                for nb in range(3):
                    bank = kb.banks[(pc * 3 + nb) % 2]
                    for k in range(8):
                        kb.op("pe", lambda e, bank=bank, wt=wt, k=k, nb=nb: e.matmul(
                            out=bank.h[0:3, 0:512], lhsT=sct[:, k * 3:(k + 1) * 3],
                            rhs=wt[:, k, nb * 512:(nb + 1) * 512], start=(k == 0), stop=(k == 7)),
                            reads=[sct, wt], writes=[bank])
                    c0 = pc * 1536 + nb * 512
                    kb.op("dve", lambda e, bank=bank, c0=c0: e.tensor_tensor(
                        out=mrow[0:3, c0:c0 + 512], in0=bank.h[0:3, 0:512], in1=mb3[0:3, c0:c0 + 512], op=ALU.add),
                        reads=[bank, mb3], writes=[mrow.sub(c0)])
            kb.dma("sp", lambda e, l=l: e.dma_start(out=self.modd.ap[l], in_=mrow[0:3, :]),
                   reads=[mrow], writes=[self.modd.sub(l)])
            for r in range(3):
                kb.dma("sp", lambda e, l=l, r=r: e.dma_start(
                    out=self.modc[l][:, :, r], in_=self.modd.ap[l, r].rearrange("(c p) -> p c", p=128)),
                    reads=[self.modd.sub(l)], writes=[self.modc[l].sub(r)])
            for (mulc, gc, v) in ((self.mul1c[l], self.n1gc, 1), (self.mul2c[l], self.n2gc, 4)):
                kb.op("dve", lambda e, mulc=mulc, v=v, l=l: e.tensor_scalar(
                    out=mulc[:], in0=self.modc[l][:, v * 8:(v + 1) * 8, :], scalar1=1.0, scalar2=None, op0=ALU.add),
                    reads=[self.modc[l]], writes=[mulc])
                for r in range(3):
                    kb.op("dve", lambda e, mulc=mulc, gc=gc, r=r, l=l: e.tensor_tensor(
                        out=mulc[:, :, r], in0=mulc[:, :, r], in1=gc[:, l, :], op=ALU.mult),
                        reads=[mulc, gc], writes=[mulc])
        kb.release(m0)

    def norm_batch(self, src, row0, nt, mulc, addc, r, uT, ucol0, bufs, banks, xn_dst=None):
        kb = self.kb
        xb, xnb, junk, stat = bufs
        tiles = []
        for j in range(nt):
            xt = xb[self._nb % len(xb)]
            xn = xnb[self._nb % len(xnb)]
            sc = self._nb % 64
            self._nb += 1
            tiles.append((j, xt, xn, sc))
            rr = row0 + j * 128
            kb.dma("sp", lambda e, xt=xt, rr=rr: e.dma_start(out=xt[:], in_=src.ap[rr:rr + 128, :]),
                   reads=[src], writes=[xt])
            kb.op("act", lambda e, xt=xt, sc=sc: e.activation(
                out=junk[:], in_=xt[:], func=AF.Square, accum_out=stat[:, sc:sc + 1]),
                reads=[xt], writes=[junk, stat.sub(sc)])
            kb.op("act", lambda e, sc=sc: e.activation(
                out=stat[:, 64 + sc:65 + sc], in_=stat[:, sc:sc + 1], func=AF.Identity, scale=1.0 / D, bias=self.epsc[:, 0:1]),
                reads=[stat.sub(sc), self.epsc], writes=[stat.sub(64 + sc)])
            kb.op("act", lambda e, sc=sc: e.activation(
                out=stat[:, 128 + sc:129 + sc], in_=stat[:, 64 + sc:65 + sc], func=AF.Sqrt),
                reads=[stat.sub(64 + sc)], writes=[stat.sub(128 + sc)])
            kb.op("dve", lambda e, sc=sc: e.reciprocal(out=stat[:, 192 + sc:193 + sc], in_=stat[:, 128 + sc:129 + sc]),
                  reads=[stat.sub(128 + sc)], writes=[stat.sub(192 + sc)])
            if len(xb) == 1:
                self._norm_tail(tiles.pop(), src, row0, mulc, addc, r, uT, ucol0, banks, xn_dst)
        for t in tiles:
            self._norm_tail(t, src, row0, mulc, addc, r, uT, ucol0, banks, xn_dst)

    def _norm_tail(self, t, src, row0, mulc, addc, r, uT, ucol0, banks, xn_dst):
        kb = self.kb
        (j, xt, xn, sc) = t
        stat = self._stat
        kb.op("act", lambda e, xt=xt, xn=xn, sc=sc: e.activation(
            out=xn[:], in_=xt[:], func=AF.Identity, scale=stat[:, 192 + sc:193 + sc]),
            reads=[xt, stat.sub(192 + sc)], writes=[xn])
        if xn_dst is not None:
            dt, drow = xn_dst
            dr0 = drow + j * 128
            kb.dma("sp", lambda e, xn=xn, dt=dt, dr=dr0: e.dma_start(
                out=dt.ap[dr:dr + 128, :], in_=xn[:]), reads=[xn], writes=[dt.sub(dr0)])
        for k in range(8):
            bank = banks[2 * j + k // 4]
            pv = bank.h.bitcast(BF16)
            kk = k % 4
            kb.op("pe", lambda e, pv=pv, xn=xn, k=k, kk=kk: e.transpose(
                out=pv[:, kk * 128:(kk + 1) * 128], in_=xn[:, k * 128:(k + 1) * 128], identity=self.identb[:]),
                reads=[xn, self.identb], writes=[bank])
        for k in range(8):
            bank = banks[2 * j + k // 4]
            pv = bank.h.bitcast(BF16)
            kk = k % 4
            dst = uT[:, k, ucol0 + j * 128: ucol0 + (j + 1) * 128]
            if k < 4:
                kb.op("act", lambda e, dst=dst, pv=pv, k=k, kk=kk: e.activation(
                    out=dst, in_=pv[:, kk * 128:(kk + 1) * 128], func=AF.Identity,
                    scale=mulc[:, k, r:r + 1], bias=addc(k, r)),
                    reads=[bank, mulc], writes=[uT.sub((k, ucol0 + j * 128))])
            else:
                kb.op("dve", lambda e, dst=dst, pv=pv, k=k, kk=kk: e.tensor_scalar(
                    out=dst, in0=pv[:, kk * 128:(kk + 1) * 128], scalar1=mulc[:, k, r:r + 1],
                    scalar2=addc(k, r), op0=ALU.mult, op1=ALU.add),
                    reads=[bank, mulc], writes=[uT.sub((k, ucol0 + j * 128))])

    def norm_bufs(self, nx=2):
        kb = self.kb
        self._nb = 0
        xb = [kb.alloc(f"xb{i}", [128, D], F32) for i in range(nx)]
        xnb = [kb.alloc(f"xnb{i}", [128, D], BF16) for i in range(nx)]
        junk = kb.alloc("junk", [128, D], BF16)
        stat = kb.alloc("stat", [128, 256], F32)
        kb.op("dve", lambda e: e.memset(stat[:], 0.0), writes=[stat])
        self._stat = stat
        return (xb, xnb, junk, stat)

    def dump(self):
        kb = self.kb
        yc = kb.dram("yc", [NS * LC, D], F32, kind="ExternalOutput")
        kb.dma("sp", lambda e: e.dma_start(out=yc.ap[:, :], in_=self.xc_all.ap[0:NS * LC, :]),
               reads=[self.xc_all], writes=[yc])
        for s in range(NS):
            kb.dma("sp", lambda e, s=s: e.dma_start(out=self.out.ap[s], in_=self.xr[s].ap),
                   reads=[self.xr[s]], writes=[self.out.sub(s)])

    def final(self):
        kb = self.kb
        fgb = kb.alloc("fgb", [128, D], F32)
        kb.dma("sp", lambda e: e.dma_start(out=fgb[:], in_=self.final_g.ap.partition_broadcast(128)),
               reads=[self.final_g], writes=[fgb])
        xb = [kb.alloc(f"fxb{i}", [128, D], F32) for i in range(3)]
        yb = [kb.alloc(f"fyb{i}", [128, D], F32) for i in range(3)]
        junk = kb.alloc("fjunk", [128, D], BF16)
        stat = kb.alloc("fstat", [128, 3 * 32], F32)
        kb.op("dve", lambda e: e.memset(stat[:], 0.0), writes=[stat])
        i = 0
        for s in range(NS):
            for t in range(L // 128):
                xt = xb[i % 3]
                yt = yb[i % 3]
                sc = i % 32
                i += 1
                kb.dma("sp", lambda e, xt=xt, s=s, t=t: e.dma_start(out=xt[:], in_=self.xr[s].ap[t * 128:(t + 1) * 128, :]),
                       reads=[self.xr[s]], writes=[xt])
                if i > 32:
                    kb.op("dve", lambda e, sc=sc: e.memset(stat[:, sc:sc + 1], 0.0), writes=[stat.sub(sc)])
                kb.op("act", lambda e, xt=xt, sc=sc: e.activation(
                    out=junk[:], in_=xt[:], func=AF.Square, accum_out=stat[:, sc:sc + 1]),
                    reads=[xt], writes=[junk, stat.sub(sc)])
                kb.op("dve", lambda e, sc=sc: e.tensor_scalar(
                    out=stat[:, 32 + sc:33 + sc], in0=stat[:, sc:sc + 1], scalar1=1.0 / D, scalar2=EPS, op0=ALU.mult, op1=ALU.add),
                    reads=[stat.sub(sc)], writes=[stat.sub(32 + sc)])
                kb.op("act", lambda e, sc=sc: e.activation(
                    out=stat[:, 32 + sc:33 + sc], in_=stat[:, 32 + sc:33 + sc], func=AF.Sqrt),
                    reads=[stat.sub(32 + sc)], writes=[stat.sub(32 + sc)])
                kb.op("dve", lambda e, sc=sc: e.reciprocal(out=stat[:, 64 + sc:65 + sc], in_=stat[:, 32 + sc:33 + sc]),
                      reads=[stat.sub(32 + sc)], writes=[stat.sub(64 + sc)])
                kb.op("dve", lambda e, xt=xt, yt=yt, sc=sc: e.scalar_tensor_tensor(
                    out=yt[:], in0=xt[:], scalar=stat[:, 64 + sc:65 + sc], in1=fgb[:], op0=ALU.mult, op1=ALU.mult),
                    reads=[xt, stat.sub(64 + sc), fgb], writes=[yt])
                kb.dma("sp", lambda e, yt=yt, s=s, t=t: e.dma_start(out=self.out.ap[s, t * 128:(t + 1) * 128, :], in_=yt[:]),
                       reads=[yt], writes=[self.out.sub((s, t))])

    def moe(self, l, with_ctx):
        kb = self.kb
        IOA = bass.IndirectOffsetOnAxis
        wbufs = [[kb.alloc(f"w{n}_{i}", [128, 8, D], BF16) for n in (1, 3, 2)] for i in range(2)]
        wsrc = (self.w1, self.w3, self.w2)

        def load_w(e):
            for n in range(3):
                src = wsrc[n].ap[l, e].rearrange("(k p) f -> p k f", p=128)
                wt = wbufs[e % 2][n]
                kb.dma("pool", lambda en, wt=wt, src=src: en.dma_start(out=wt[:], in_=src),
                       reads=[wsrc[n]], writes=[wt])

        nr = 3 if with_ctx else 2
        g2b = [kb.alloc(f"g2b{r}", [128, D], F32) for r in range(nr)]
        for r in range(nr):
            kb.dma("sp", lambda e, r=r: e.dma_start(
                out=g2b[r][:], in_=self.modd.ap[l, r, 5 * D:6 * D].partition_broadcast(128)),
                reads=[self.modd.sub(l)], writes=[g2b[r]])
        idxT = kb.alloc("idxT", [128, 2, 48], I32)
        gT = kb.alloc("gT", [128, 2, 48], F32)
        idxC = kb.alloc("idxC", [128, NE], I32)
        gC = kb.alloc("gC", [128, NE], F32)
        load_w(0)
        load_w(1)
        addc2 = lambda k, r: self.modc[l][:, 24 + k, r:r + 1]
        mulc2 = self.mul2c[l]

        dbg = getattr(self, "debug_route", 0)
        if dbg == 10:
            return
        m1 = kb.mark()
        bufs = self.norm_bufs()
        uTb = [kb.alloc(f"uTb{i}", [128, 8, 256], BF16) for i in range(2)]
        wr = kb.alloc("wr", [128, 8, NE], BF16)
        wrf = kb.alloc("wrf", [128, 8, NE], F32)
        kb.dma("sp", lambda e: e.dma_start(out=wrf[:], in_=self.w_router.ap[l].rearrange("(k p) e -> p k e", p=128)),
               reads=[self.w_router], writes=[wrf])
        kb.op("dve", lambda e: e.tensor_copy(out=wr[:], in_=wrf[:]), reads=[wrf], writes=[wr])
        aff2 = kb.alloc("aff2", [128, 16, 64], F32)
        affc = kb.alloc("affc", [128, 2, 64], F32)
        kb.op("dve", lambda e: e.memset(aff2[:].rearrange("p a b -> p (a b)"), 0.0), writes=[aff2])
        kb.op("dve", lambda e: e.memset(affc[:].rearrange("p a b -> p (a b)"), 0.0), writes=[affc])
        lg = kb.alloc("lg", [128, 16, 16], F32)
        mx = kb.alloc("mx", [128, 16], F32)
        sm = kb.alloc("sm", [128, 16], F32)
        rs = kb.alloc("rs", [128, 16], F32)
        work = kb.alloc("work", [48, L], F32)
        workc = kb.alloc("workc", [48, LC], F32)
        topv = kb.alloc("topv", [48, 256], F32)
        topi = kb.alloc("topi", [48, 256], U32)
        topif = kb.alloc("topif", [48, 256], F32)
        topvc = kb.alloc("topvc", [48, 32], F32)
        topic = kb.alloc("topic", [48, 32], U32)
        topicf = kb.alloc("topicf", [48, 32], F32)
        nbatch = 0
        seqs = []
        for s in range(NS):
            seqs.append((self.xr[s], self.xnl[s], L // 128, s, aff2, s * 32, kb.banks[4 + s], 0))
        if with_ctx:
            for s in range(NS):
                seqs.append((self.xc[s], self.xnc[s], LC // 128, 2, affc, s * 32, kb.banks[6], s * 32))
        for (src, xnd, ntl, r, afft, acol, lbank, lcol0) in seqs:
            for b in range((ntl + 1) // 2):
                nt = min(2, ntl - b * 2)
                ub = uTb[nbatch % 2]
                nbatch += 1
                self.norm_batch(src, b * 256, nt, mulc2, addc2, r, ub, 0, bufs,
                                [kb.banks[j] for j in range(2 * nt)], xn_dst=(xnd, b * 256))
                for j in range(nt):
                    if dbg == 11:
                        continue
                    c = b * 2 + j
                    for k in range(8):
                        kb.op("pe", lambda e, lbank=lbank, lc=lcol0 + c * 16, ub=ub, k=k, j=j: e.matmul(
                            out=lbank.h[:, lc:lc + 16], lhsT=ub[:, k, j * 128:(j + 1) * 128], rhs=wr[:, k, :],
                            start=(k == 0), stop=(k == 7)),
                            reads=[ub.sub((k, j * 128)), wr], writes=[lbank])
            if dbg == 11:
                continue
            lv = lbank.h[:, lcol0:lcol0 + ntl * 16].rearrange("p (c e) -> p c e", e=16)
            bc = lambda t, ntl=ntl: t[:, 0:ntl].unsqueeze(2).to_broadcast([128, ntl, 16])
            kb.op("dve", lambda e, lv=lv, ntl=ntl: e.tensor_reduce(out=mx[:, 0:ntl], in_=lv, axis=AX.X, op=ALU.max),
                  reads=[lbank], writes=[mx])
            kb.op("dve", lambda e, lv=lv, bc=bc, ntl=ntl: e.tensor_tensor(out=lg[:, 0:ntl, :], in0=lv, in1=bc(mx), op=ALU.subtract),
                  reads=[lbank, mx], writes=[lg])
            kb.op("act", lambda e, ntl=ntl: e.activation(out=lg[:, 0:ntl, :], in_=lg[:, 0:ntl, :], func=AF.Exp),
                  reads=[lg], writes=[lg])
            kb.op("dve", lambda e, ntl=ntl: e.tensor_reduce(out=sm[:, 0:ntl], in_=lg[:, 0:ntl, :], axis=AX.X, op=ALU.add),
                  reads=[lg], writes=[sm])
            kb.op("dve", lambda e, ntl=ntl: e.reciprocal(out=rs[:, 0:ntl], in_=sm[:, 0:ntl]), reads=[sm], writes=[rs])
            kb.op("dve", lambda e, afft=afft, acol=acol, bc=bc, ntl=ntl: e.tensor_tensor(
                out=afft[:, 0:ntl, acol:acol + 16], in0=lg[:, 0:ntl, :], in1=bc(rs), op=ALU.mult),
                reads=[lg, rs], writes=[afft])
        if dbg == 11:
            kb.release(m1)
            return
        if dbg == 1:
            da = kb.dram("dbg_aff", [128, 1024], F32, kind="ExternalOutput")
            kb.dma("sp", lambda e: e.dma_start(out=da.ap[:, :], in_=aff2[:].rearrange("p h c -> p (h c)")), reads=[aff2], writes=[da])
            kb.release(m1)
            return
        for c in range(16):
            bank = kb.banks[c // 4]
            kb.op("pe", lambda e, bank=bank, c=c: e.transpose(
                out=bank.h[0:48, (c % 4) * 128:(c % 4 + 1) * 128], in_=aff2[:, c, 0:48], identity=self.identf[:]),
                reads=[aff2, self.identf], writes=[bank])
        for q in range(4):
            eng = "act" if q % 2 == 0 else "dve"
            if eng == "act":
                kb.op("act", lambda e, q=q: e.copy(out=work[0:48, q * 512:(q + 1) * 512], in_=kb.banks[q].h[0:48, 0:512]),
                      reads=[kb.banks[q]], writes=[work.sub(q)])
            else:
                kb.op("dve", lambda e, q=q: e.tensor_copy(out=work[0:48, q * 512:(q + 1) * 512], in_=kb.banks[q].h[0:48, 0:512]),
                      reads=[kb.banks[q]], writes=[work.sub(q)])
        if with_ctx:
            for c in range(2):
                kb.op("pe", lambda e, c=c: e.transpose(
                    out=kb.banks[7].h[0:48, c * 128:(c + 1) * 128], in_=affc[:, c, 0:48], identity=self.identf[:]),
                    reads=[affc, self.identf], writes=[kb.banks[7]])
            kb.op("act", lambda e: e.copy(out=workc[0:48, :], in_=kb.banks[7].h[0:48, 0:256]),
                  reads=[kb.banks[7]], writes=[workc])

        def topk(wk, tv, ti, niter):
            for it in range(niter):
                sl = slice(it * 8, (it + 1) * 8)
                kb.op("dve", lambda e, sl=sl: e.max(out=tv[:, sl], in_=wk[:]), reads=[wk], writes=[tv.sub(it)])
                kb.op("dve", lambda e, sl=sl: e.max_index(out=ti[:, sl], in_max=tv[:, sl], in_values=wk[:]),
                      reads=[wk, tv.sub(it)], writes=[ti.sub(it)])
                kb.op("dve", lambda e, sl=sl: e.match_replace(out=wk[:], in_to_replace=tv[:, sl], in_values=wk[:], imm_value=-1.0),
                      reads=[tv.sub(it), wk], writes=[wk])

        if dbg == 2:
            da = kb.dram("dbg_work", [48, L], F32, kind="ExternalOutput")
            kb.dma("sp", lambda e: e.dma_start(out=da.ap[:, :], in_=work[:]), reads=[work], writes=[da])
            kb.release(m1)
            return
        topk(work, topv, topi, 32)
        if dbg == 3:
            da = kb.dram("dbg_topv", [48, 256], F32, kind="ExternalOutput")
            kb.dma("sp", lambda e: e.dma_start(out=da.ap[:, :], in_=topv[:]), reads=[topv], writes=[da])
            db_ = kb.dram("dbg_topi", [48, 256], U32, kind="ExternalOutput")
            kb.dma("sp", lambda e: e.dma_start(out=db_.ap[:, :], in_=topi[:]), reads=[topi], writes=[db_])
            kb.release(m1)
            return
        kb.op("dve", lambda e: e.tensor_copy(out=topif[:], in_=topi[:]), reads=[topi], writes=[topif])
        tb = kb.banks[5]
        for h in range(2):
            kb.op("pe", lambda e, h=h: e.transpose(out=tb.h[:, h * 48:(h + 1) * 48], in_=topif[0:48, h * 128:(h + 1) * 128],
                                                   identity=self.identf[0:48, 0:48]),
                  reads=[topif, self.identf], writes=[tb])
            kb.op("pe", lambda e, h=h: e.transpose(out=tb.h[:, 128 + h * 48:128 + (h + 1) * 48], in_=topv[0:48, h * 128:(h + 1) * 128],
                                                   identity=self.identf[0:48, 0:48]),
                  reads=[topv, self.identf], writes=[tb])
        kb.op("dve", lambda e: e.tensor_copy(out=idxT[:], in_=tb.h[:, 0:96].rearrange("p (h c) -> p h c", h=2)),
              reads=[tb], writes=[idxT])
        kb.op("dve", lambda e: e.tensor_copy(out=gT[:], in_=tb.h[:, 128:224].rearrange("p (h c) -> p h c", h=2)),
              reads=[tb], writes=[gT])
        if with_ctx:
            topk(workc, topvc, topic, 4)
            kb.op("dve", lambda e: e.tensor_copy(out=topicf[:], in_=topic[:]), reads=[topic], writes=[topicf])
            cbase = kb.alloc("cbase", [128, 1], F32)
            kb.dma("sp", lambda e: e.dma_start(out=cbase[:], in_=self.c_ctxbase.ap[:, :]), reads=[self.c_ctxbase], writes=[cbase])
            for (srcT, dstT, isidx) in ((topicf, idxC, True), (topvc, gC, False)):
                M = kb.alloc("Mc", [48, 128], F32)
                kb.op("dve", lambda e, M=M: e.memset(M[:], 0.0), writes=[M])
                kb.op("dve", lambda e, M=M, srcT=srcT: e.tensor_copy(out=M[0:16, 0:32], in_=srcT[0:16, 0:32]), reads=[srcT], writes=[M])
                kb.op("dve", lambda e, M=M, srcT=srcT: e.tensor_copy(out=M[32:48, 32:64], in_=srcT[32:48, 0:32]), reads=[srcT], writes=[M])
                kb.op("pe", lambda e, M=M: e.transpose(out=tb.h[:, 256:304], in_=M[0:48, :], identity=self.identf[0:48, 0:48]),
                      reads=[M, self.identf], writes=[tb])
                tcp = kb.alloc("tcp", [128, 48], F32)
                kb.op("dve", lambda e, tcp=tcp: e.tensor_copy(out=tcp[:], in_=tb.h[:, 256:304]), reads=[tb], writes=[tcp])
                tsum = kb.alloc("tsum", [128, NE], F32)
                kb.op("dve", lambda e, tcp=tcp, tsum=tsum: e.tensor_tensor(out=tsum[:], in0=tcp[:, 0:16], in1=tcp[:, 32:48], op=ALU.add),
                      reads=[tcp], writes=[tsum])
                if isidx:
                    kb.op("dve", lambda e, tsum=tsum: e.tensor_scalar(out=tsum[:], in0=tsum[:], scalar1=cbase[:, 0:1], scalar2=None, op0=ALU.add),
                          reads=[tsum, cbase], writes=[tsum])
                kb.op("dve", lambda e, tsum=tsum, dstT=dstT: e.tensor_copy(out=dstT[:], in_=tsum[:]), reads=[tsum], writes=[dstT])
        if dbg == 4:
            di = kb.dram("dbg_idx", [128, 96], I32, kind="ExternalOutput")
            dg = kb.dram("dbg_g", [128, 96], F32, kind="ExternalOutput")
            da = kb.dram("dbg_aff", [128, 1024], F32, kind="ExternalOutput")
            kb.dma("sp", lambda e: e.dma_start(out=di.ap[:, :], in_=idxT[:].rearrange("p h c -> p (h c)")), reads=[idxT], writes=[di])
            kb.dma("sp", lambda e: e.dma_start(out=dg.ap[:, :], in_=gT[:].rearrange("p h c -> p (h c)")), reads=[gT], writes=[dg])
            kb.dma("sp", lambda e: e.dma_start(out=da.ap[:, :], in_=aff2[:].rearrange("p h c -> p (h c)")), reads=[aff2], writes=[da])
            kb.release(m1)
            return
        kb.release(m1)

        G = []
        for s in range(NS):
            for h in range(2):
                G.append(dict(co=s * 256 + h * 128, idx=lambda e, s=s, h=h: idxT[:, h, s * 32 + e:s * 32 + e + 1],
                              gate=lambda e, s=s, h=h: gT[:, h, s * 32 + e:s * 32 + e + 1], r=s,
                              xn=self.xnl[s], dst=self.xr[s], n=L))
        if with_ctx:
            G.append(dict(co=512, idx=lambda e: idxC[:, e:e + 1], gate=lambda e: gC[:, e:e + 1], r=2,
                          xn=self.xnc_all, dst=self.xc_all, n=NS * LC + 128))
        NSL = 128 * len(G)
        HW = NSL // 2
        halves = [(0, HW), (HW, HW)]
        Xg = [[kb.alloc(f"xg{i}_{gi}", [128, D], BF16) for gi in range(len(G))] for i in range(2)]
        XeT = [kb.alloc(f"xeT{i}", [128, 8, NSL], BF16) for i in range(2)]
        hidT = kb.alloc("hidT", [128, 8, NSL], BF16)
        sgt = [kb.alloc(f"sgt{i}", [128, HW], F32) for i in range(2)]
        yo = [kb.alloc(f"yo{i}", [128, D], F32) for i in range(3)]
        nyo = 0
        nyb = 0

        def gathers(e):
            for gi, g in enumerate(G):
                xg = Xg[e % 2][gi]
                kb.dma("pool", lambda en, xg=xg, g=g, e=e: en.indirect_dma_start(
                    out=xg[:], out_offset=None, in_=g["xn"].ap[:, :], in_offset=IOA(ap=g["idx"](e), axis=0)),
                    reads=[g["xn"], idxT, idxC], writes=[xg])

        gathers(0)
        for e in range(NE):
            if e + 1 < NE:
                gathers(e + 1)
            w1t, w3t, w2t = wbufs[e % 2]
            xe = XeT[e % 2]
            for gi, g in enumerate(G):
                co, r = g["co"], g["r"]
                xg = Xg[e % 2][gi]
                for k in range(8):
                    bank = kb.banks[6 + k // 4]
                    pv = bank.h.bitcast(BF16)
                    kk = k % 4
                    kb.op("pe", lambda en, pv=pv, xg=xg, k=k, kk=kk: en.transpose(
                        out=pv[:, kk * 128:(kk + 1) * 128], in_=xg[:, k * 128:(k + 1) * 128],
                        identity=self.identb[:]), reads=[xg, self.identb], writes=[bank])
                for k in range(8):
                    bank = kb.banks[6 + k // 4]
                    pv = bank.h.bitcast(BF16)
                    kk = k % 4
                    dst = xe[:, k, co:co + 128]
                    if k < 4:
                        kb.op("act", lambda en, dst=dst, pv=pv, kk=kk, r=r, k=k: en.activation(
                            out=dst, in_=pv[:, kk * 128:(kk + 1) * 128], func=AF.Identity,
                            scale=mulc2[:, k, r:r + 1], bias=addc2(k, r)),
                            reads=[bank], writes=[xe.sub((k, co))])
                    else:
                        kb.op("dve", lambda en, dst=dst, pv=pv, kk=kk, r=r, k=k: en.tensor_scalar(
                            out=dst, in0=pv[:, kk * 128:(kk + 1) * 128], scalar1=mulc2[:, k, r:r + 1],
                            scalar2=addc2(k, r), op0=ALU.mult, op1=ALU.add),
                            reads=[bank], writes=[xe.sub((k, co))])
            for fc in range(8):
                for hi, (h0, hw) in enumerate(halves):
                    b1 = kb.banks[hi]
                    b3 = kb.banks[2 + hi]
                    for (wt, bm) in ((w1t, b1), (w3t, b3)):
                        for k in range(8):
                            kb.op("pe", lambda en, wt=wt, bm=bm, k=k, fc=fc, xe=xe, h0=h0, hw=hw: en.matmul(
                                out=bm.h[:, 0:hw], lhsT=wt[:, k, fc * 128:(fc + 1) * 128], rhs=xe[:, k, h0:h0 + hw],
                                start=(k == 0), stop=(k == 7)), reads=[wt, xe], writes=[bm])
                    sg = sgt[hi]
                    kb.op("act", lambda en, sg=sg, b1=b1, hw=hw: en.activation(out=sg[:, 0:hw], in_=b1.h[:, 0:hw], func=AF.Silu),
                          reads=[b1], writes=[sg])
                    kb.op("dve", lambda en, sg=sg, b3=b3, fc=fc, h0=h0, hw=hw: en.tensor_tensor(
                        out=hidT[:, fc, h0:h0 + hw], in0=sg[:, 0:hw], in1=b3.h[:, 0:hw], op=ALU.mult),
                        reads=[sg, b3], writes=[hidT.sub((fc, h0))])
            for gi, g in enumerate(G):
                co, r = g["co"], g["r"]
                yt = yo[nyo % 3]
                nyo += 1
                for db in range(2):
                    by = kb.banks[4 + nyb % 2]
                    nyb += 1
                    for fc in range(8):
                        kb.op("pe", lambda en, by=by, fc=fc, co=co, db=db, w2t=w2t: en.matmul(
                            out=by.h[:, 0:512], lhsT=hidT[:, fc, co:co + 128], rhs=w2t[:, fc, db * 512:(db + 1) * 512],
                            start=(fc == 0), stop=(fc == 7)), reads=[hidT, w2t], writes=[by])
                    kb.op("dve", lambda en, by=by, yt=yt, db=db, g=g, e=e, r=r: en.scalar_tensor_tensor(
                        out=yt[:, db * 512:(db + 1) * 512], in0=by.h[:, 0:512], scalar=g["gate"](e),
                        in1=g2b[r][:, db * 512:(db + 1) * 512], op0=ALU.mult, op1=ALU.mult),
                        reads=[by, gT, gC, g2b[r]], writes=[yt.sub(db)])
                kb.dma("pool", lambda en, yt=yt, g=g, e=e: en.indirect_dma_start(
                    out=g["dst"].ap[:, :], out_offset=IOA(ap=g["idx"](e), axis=0), in_=yt[:, :], in_offset=None,
                    compute_op=ALU.add, bounds_check=kb.bnd[g["n"] - 1], oob_is_err=True),
                    reads=[yt, idxT, idxC], writes=[g["dst"]])
            if e + 2 < NE:
                load_w(e + 2)

    def mixer0(self):
        kb = self.kb
        l = 0
        wbuf = kb.alloc("wbuf", [128, 8, 1536], BF16)
        wout = wbuf.view("woutv", [128, 8, D], BF16)
        csc = kb.alloc("csc", [128, 256], BF16)
        kb.dma("sp", lambda e: e.dma_start(out=csc[:], in_=self.c_csc.ap[:, :]), reads=[self.c_csc], writes=[csc])
        cwc = kb.alloc("cwc", [128, 4, 31], F32)
        for j in range(4):
            kb.dma("sp", lambda e, j=j: e.dma_start(out=cwc[:, j, :], in_=self.ab_conv_w.ap[:, j * 128:(j + 1) * 128].rearrange("k p -> p k")),
                   reads=[self.ab_conv_w], writes=[cwc.sub(j)])
        cols = {}
        for nm, dr in (("cb", self.ab_conv_b), ("lg", self.ab_ln_g), ("lb", self.ab_ln_b)):
            t = kb.alloc(nm + "c", [128, 4], F32)
            kb.dma("sp", lambda e, t=t, dr=dr: e.dma_start(out=t[:], in_=dr.ap.rearrange("(j p) -> p j", p=128)), reads=[dr], writes=[t])
            cols[nm] = t
        onesb = kb.alloc("onesb", [128, 128], BF16)
        kb.op("dve", lambda e: e.memset(onesb[:], 1.0 / 512.0), writes=[onesb])
        diag = kb.alloc("diag", [128, 4 * 31, 128], BF16)
        for j in range(4):
            for k in range(31):
                kb.op("dve", lambda e, j=j, k=k: e.tensor_scalar(
                    out=diag[:, j * 31 + k, :], in0=self.identb[:], scalar1=cwc[:, j, k:k + 1], scalar2=None, op0=ALU.mult),
                    reads=[self.identb, cwc], writes=[diag.sub((j, k))])
        bufs = self.norm_bufs(nx=2)
        xb = bufs[0][0]
        uTb = kb.alloc("uTb", [128, 8, 512], BF16)
        aT = kb.alloc("aT", [128, 4, L + 32], BF16)
        ufTb = kb.alloc("ufTb", [128, 4, 512], BF16)
        Yall = kb.alloc("Yall", [128, 16, 1024], BF16)
        mixT = kb.alloc("mixT", [128, 8, L], BF16)
        clb = kb.alloc("clb", [128, 16, 256], BF16)
        slb = kb.alloc("slb", [128, 16, 256], BF16)
        g1b = kb.alloc("g1b", [128, D], F32)
        NBC = 256
        cT = kb.alloc("cT", [128, 4, NBC], F32)
        cbt = kb.alloc("cbt", [128, 4, NBC], BF16)
        c2t = kb.alloc("c2t", [128, 4, NBC], BF16)
        t_mean = kb.alloc("t_mean", [128, NBC], F32)
        t_rstd = kb.alloc("t_rstd", [128, NBC], F32)
        t_tmp = kb.alloc("t_tmp", [128, 512], F32)
        t_tmp2 = kb.alloc("t_tmp2", [128, NBC], F32)
        addc1 = lambda k, r: self.modc[l][:, k, r:r + 1]
        mulc1 = self.mul1c[l]
        B = kb.banks
        seqs = [(self.xin[0], self.xr[0], L, 0, self.c_cl, self.c_sl), (self.xin[1], self.xr[1], L, 1, self.c_cl, self.c_sl),
                (self.cin[0], self.xc[0], LC, 2, self.c_clc, self.c_slc), (self.cin[1], self.xc[1], LC, 2, self.c_clc, self.c_slc)]
        for (src, dst, Ls, r, ctab, stab) in seqs:
            kb.dma("pool", lambda e: e.dma_start(out=wbuf[:], in_=self.ab_w_in.ap.rearrange("(k p) f -> p k f", p=128)),
                   reads=[self.ab_w_in], writes=[wbuf])
            kb.dma("sp", lambda e, r=r: e.dma_start(out=g1b[:], in_=self.modd.ap[l, r, 2 * D:3 * D].partition_broadcast(128)),
                   reads=[self.modd.sub(l)], writes=[g1b])
            for j in range(4):
                kb.op("dve", lambda e, j=j: e.memset(aT[:, j, 0:15], 0.0), writes=[aT.sub((j, "h0"))])
                kb.op("dve", lambda e, j=j, Ls=Ls: e.memset(aT[:, j, 15 + Ls:32 + Ls], 0.0), writes=[aT.sub((j, "h1"))])
            nb = min(512, Ls)
            for blk in range(Ls // nb):
                t0 = blk * nb
                for sb in range(nb // 256):
                    self.norm_batch(src, t0 + sb * 256, 2, mulc1, addc1, r, uTb, sb * 256, bufs, [B[0], B[1], B[2], B[3]])
                def zmm(j, bank):
                    for k in range(8):
                        kb.op("pe", lambda e, j=j, k=k, bank=bank, nb=nb: e.matmul(
                            out=bank.h[:, 0:nb], lhsT=wbuf[:, k, j * 128:(j + 1) * 128], rhs=uTb[:, k, 0:nb],
                            start=(k == 0), stop=(k == 7)), reads=[wbuf, uTb], writes=[bank])
                for jj in range(4):
                    zmm(jj, B[4])
                    zmm(jj + 4, B[5])
                    kb.op("act", lambda e, nb=nb: e.activation(out=t_tmp[:, 0:nb], in_=B[5].h[:, 0:nb], func=AF.Sigmoid),
                          reads=[B[5]], writes=[t_tmp])
                    kb.op("dve", lambda e, jj=jj, nb=nb, t0=t0: e.tensor_tensor(
                        out=aT[:, jj, 15 + t0:15 + t0 + nb], in0=t_tmp[:, 0:nb], in1=B[4].h[:, 0:nb], op=ALU.mult),
                        reads=[t_tmp, B[4]], writes=[aT.sub((jj, t0))])
                for g in range(4):
                    zmm(8 + g, B[6])
                    kb.op("act", lambda e, g=g, nb=nb: e.copy(out=ufTb[:, g, 0:nb], in_=B[6].h[:, 0:nb]),
                          reads=[B[6]], writes=[ufTb.sub(g)])
                for tc in range(nb // 128):
                    c = (t0 // 128) + tc
                    for gp in range(2):
                        for gg in range(2):
                            g = gp * 2 + gg
                            kb.op("pe", lambda e, g=g, gg=gg, tc=tc: e.matmul(
                                out=B[7].h[:, gg * 256:(gg + 1) * 256], lhsT=ufTb[:, g, tc * 128:(tc + 1) * 128], rhs=csc[:, :],
                                start=True, stop=True), reads=[ufTb, csc], writes=[B[7]])
                        kb.op("dve", lambda e, c=c, gp=gp: e.tensor_copy(out=Yall[:, c, gp * 512:(gp + 1) * 512], in_=B[7].h[:, 0:512]),
                              reads=[B[7]], writes=[Yall.sub((c, gp))])
            nbc = min(NBC, Ls)
            for blk in range(Ls // nbc):
                t0 = blk * nbc
                for j in range(4):
                    bank = B[j % 2]
                    for k in range(31):
                        kb.op("pe", lambda e, j=j, k=k, bank=bank, t0=t0, nbc=nbc: e.matmul(
                            out=bank.h[:, 0:nbc], lhsT=diag[:, j * 31 + k, :], rhs=aT[:, j, t0 + k:t0 + k + nbc],
                            start=(k == 0), stop=(k == 30)), reads=[diag, aT], writes=[bank])
                    kb.op("act", lambda e, j=j, bank=bank, nbc=nbc: e.activation(
                        out=cT[:, j, 0:nbc], in_=bank.h[:, 0:nbc], func=AF.Identity, bias=cols["cb"][:, j:j + 1]),
                        reads=[bank, cols["cb"]], writes=[cT.sub(j)])
                    kb.op("pool", lambda e, j=j, nbc=nbc: e.tensor_copy(out=cbt[:, j, 0:nbc], in_=cT[:, j, 0:nbc]),
                          reads=[cT.sub(j)], writes=[cbt.sub(j)])
                    kb.op("pool", lambda e, j=j, nbc=nbc: e.tensor_tensor(out=c2t[:, j, 0:nbc], in0=cT[:, j, 0:nbc], in1=cT[:, j, 0:nbc], op=ALU.mult),
                          reads=[cT.sub(j)], writes=[c2t.sub(j)])
                for j in range(4):
                    kb.op("pe", lambda e, j=j, nbc=nbc: e.matmul(out=B[2].h[:, 0:nbc], lhsT=onesb[:], rhs=cbt[:, j, 0:nbc],
                                                                 start=(j == 0), stop=(j == 3)), reads=[onesb, cbt], writes=[B[2]])
                for j in range(4):
                    kb.op("pe", lambda e, j=j, nbc=nbc: e.matmul(out=B[3].h[:, 0:nbc], lhsT=onesb[:], rhs=c2t[:, j, 0:nbc],
                                                                 start=(j == 0), stop=(j == 3)), reads=[onesb, c2t], writes=[B[3]])
                kb.op("dve", lambda e, nbc=nbc: e.tensor_copy(out=t_mean[:, 0:nbc], in_=B[2].h[:, 0:nbc]), reads=[B[2]], writes=[t_mean])
                kb.op("dve", lambda e, nbc=nbc: e.tensor_tensor(out=t_tmp2[:, 0:nbc], in0=t_mean[:, 0:nbc], in1=t_mean[:, 0:nbc], op=ALU.mult),
                      reads=[t_mean], writes=[t_tmp2])
                kb.op("dve", lambda e, nbc=nbc: e.tensor_tensor(out=t_rstd[:, 0:nbc], in0=B[3].h[:, 0:nbc], in1=t_tmp2[:, 0:nbc], op=ALU.subtract),
                      reads=[B[3], t_tmp2], writes=[t_rstd])
                kb.op("act", lambda e, nbc=nbc: e.activation(out=t_rstd[:, 0:nbc], in_=t_rstd[:, 0:nbc], func=AF.Identity, bias=self.epsc[:, 0:1]),
                      reads=[t_rstd, self.epsc], writes=[t_rstd])
                kb.op("act", lambda e, nbc=nbc: e.activation(out=t_rstd[:, 0:nbc], in_=t_rstd[:, 0:nbc], func=AF.Sqrt),
                      reads=[t_rstd], writes=[t_rstd])
                kb.op("dve", lambda e, nbc=nbc: e.reciprocal(out=t_rstd[:, 0:nbc], in_=t_rstd[:, 0:nbc]), reads=[t_rstd], writes=[t_rstd])
                for j in range(4):
                    kb.op("pool", lambda e, j=j, nbc=nbc: e.tensor_tensor(out=cT[:, j, 0:nbc], in0=cT[:, j, 0:nbc], in1=t_mean[:, 0:nbc], op=ALU.subtract),
                          reads=[cT.sub(j), t_mean], writes=[cT.sub(j)])
                    kb.op("dve", lambda e, j=j, nbc=nbc: e.tensor_tensor(out=cT[:, j, 0:nbc], in0=cT[:, j, 0:nbc], in1=t_rstd[:, 0:nbc], op=ALU.mult),
                          reads=[cT.sub(j), t_rstd], writes=[cT.sub(j)])
                    kb.op("act", lambda e, j=j, nbc=nbc, t0=t0: e.activation(
                        out=mixT[:, j, t0:t0 + nbc], in_=cT[:, j, 0:nbc], func=AF.Silu,
                        scale=cols["lg"][:, j:j + 1], bias=cols["lb"][:, j:j + 1]),
                        reads=[cT.sub(j), cols["lg"], cols["lb"]], writes=[mixT.sub((j, t0))])
            ntc = Ls // 128
            for kbi in range(Ls // 256):
                k0 = kbi * 256
                kb.dma("sp", lambda e, ctab=ctab, k0=k0, ntc=ntc: e.dma_start(
                    out=clb[:, 0:ntc, :], in_=ctab.ap.rearrange("(c p) k -> p c k", p=128)[:, :, k0:k0 + 256]),
                    reads=[ctab], writes=[clb])
                kb.dma("sp", lambda e, stab=stab, k0=k0, ntc=ntc: e.dma_start(
                    out=slb[:, 0:ntc, :], in_=stab.ap.rearrange("(c p) k -> p c k", p=128)[:, :, k0:k0 + 256]),
                    reads=[stab], writes=[slb])
                for g in range(4):
                    bank = B[4 + g % 2]
                    for c in range(ntc):
                        kb.op("pe", lambda e, g=g, c=c, bank=bank: e.matmul(
                            out=bank.h[:, 0:256], lhsT=Yall[:, c, g * 256:g * 256 + 128], rhs=clb[:, c, :],
                            start=(c == 0), stop=False), reads=[Yall, clb], writes=[bank])
                        kb.op("pe", lambda e, g=g, c=c, bank=bank, ntc=ntc: e.matmul(
                            out=bank.h[:, 0:256], lhsT=Yall[:, c, g * 256 + 128:g * 256 + 256], rhs=slb[:, c, :],
                            start=False, stop=(c == ntc - 1)), reads=[Yall, slb], writes=[bank])
                    if g % 2 == 0:
                        kb.op("act", lambda e, g=g, bank=bank, k0=k0: e.copy(out=mixT[:, 4 + g, k0:k0 + 256], in_=bank.h[:, 0:256]),
                              reads=[bank], writes=[mixT.sub((4 + g, k0))])
                    else:
                        kb.op("dve", lambda e, g=g, bank=bank, k0=k0: e.tensor_copy(out=mixT[:, 4 + g, k0:k0 + 256], in_=bank.h[:, 0:256]),
                              reads=[bank], writes=[mixT.sub((4 + g, k0))])
            kb.dma("pool", lambda e: e.dma_start(out=wout[:], in_=self.ab_w_out.ap.rearrange("(k p) f -> p k f", p=128)),
                   reads=[self.ab_w_out], writes=[wbuf])
            for tc in range(ntc):
                kb.dma("sp", lambda e, src=src, tc=tc: e.dma_start(out=xb[:], in_=src.ap[tc * 128:(tc + 1) * 128, :]),
                       reads=[src], writes=[xb])
                for db in range(2):
                    bank = B[6 + db]
                    for m in range(8):
                        kb.op("pe", lambda e, m=m, db=db, bank=bank, tc=tc: e.matmul(
                            out=bank.h[:, 0:512], lhsT=mixT[:, m, tc * 128:(tc + 1) * 128], rhs=wout[:, m, db * 512:(db + 1) * 512],
                            start=(m == 0), stop=(m == 7)), reads=[mixT, wbuf], writes=[bank])
                    kb.op("dve", lambda e, db=db, bank=bank: e.tensor_tensor(
                        out=t_tmp[:, 0:512], in0=bank.h[:, 0:512], in1=g1b[:, db * 512:(db + 1) * 512], op=ALU.mult),
                        reads=[bank, g1b], writes=[t_tmp])
                    kb.op("pool", lambda e, db=db: e.tensor_tensor(
                        out=xb[:, db * 512:(db + 1) * 512], in0=xb[:, db * 512:(db + 1) * 512], in1=t_tmp[:, 0:512], op=ALU.add),
                        reads=[xb, t_tmp], writes=[xb])
                kb.dma("sp", lambda e, dst=dst, tc=tc: e.dma_start(out=dst.ap[tc * 128:(tc + 1) * 128, :], in_=xb[:]),
                       reads=[xb], writes=[dst.sub(("row", tc))])


    def mla(self):
        kb = self.kb
        l = 1
        B = kb.banks
        NKV = LC + L
        NT = NKV // 128
        SCALE = 96.0 ** -0.5
        cast = lambda dst, src_ap, tr: kb.dma("pool", lambda e: e.dma_start(out=dst[:], in_=src_ap), reads=[tr], writes=[dst])
        wi = kb.alloc("wi", [128, 8, 416], BF16)
        cast(wi, self.mla_w_in.ap.rearrange("(k p) f -> p k f", p=128), self.mla_w_in)
        wuq = kb.alloc("wuq", [128, 2, 1536], BF16)
        cast(wuq, self.mla_w_uq.ap.rearrange("(k p) f -> p k f", p=128), self.mla_w_uq)
        wukv = kb.alloc("wukv", [128, 2048], BF16)
        cast(wukv, self.mla_w_ukv.ap[:, :], self.mla_w_ukv)
        wo = kb.alloc("wo", [128, 8, D], BF16)
        cast(wo, self.mla_w_o.ap.rearrange("(k p) f -> p k f", p=128), self.mla_w_o)
        ropt = kb.alloc("ropt", [128, 16, 32], F32)
        kb.dma("sp", lambda e: e.dma_start(out=ropt[:], in_=self.c_rope.ap.rearrange("(c p) f -> p c f", p=128)),
               reads=[self.c_rope], writes=[ropt])
        qgc = kb.alloc("qgc", [128, 2], F32)
        kb.dma("sp", lambda e: e.dma_start(out=qgc[:], in_=self.mla_qg.ap.rearrange("(j p) -> p j", p=128)), reads=[self.mla_qg], writes=[qgc])
        kvgc = kb.alloc("kvgc", [128, 1], F32)
        kb.dma("sp", lambda e: e.dma_start(out=kvgc[:], in_=self.mla_kvg.ap.rearrange("(j p) -> p j", p=128)), reads=[self.mla_kvg], writes=[kvgc])
        ones1 = kb.alloc("ones1", [128, 128], BF16)
        kb.op("dve", lambda e: e.memset(ones1[:], 1.0), writes=[ones1])
        bufs = self.norm_bufs(nx=2)
        xb = bufs[0][0]
        uT = kb.alloc("uTall", [128, 8, NKV], BF16)
        cqnT = kb.alloc("cqnT", [128, 2, L], BF16)
        ckvnT = kb.alloc("ckvnT", [128, NKV], BF16)
        KTs = [kb.alloc(f"KT{i}", [128, NKV], BF16) for i in range(2)]
        QTs = [kb.alloc(f"QT{i}", [128, L], BF16) for i in range(2)]
        qtoks = [kb.alloc(f"qtok{i}", [128, 8, 96], BF16) for i in range(2)]
        Vaug = [kb.alloc(f"Vaug{i}", [128, NT, 128], BF16) for i in range(2)]
        kb.op("dve", lambda e: e.memset(Vaug[0][:].rearrange("p a b -> p (a b)"), 1.0), writes=[Vaug[0]])
        kb.op("dve", lambda e: e.memset(Vaug[1][:].rearrange("p a b -> p (a b)"), 1.0), writes=[Vaug[1]])
        pT = [kb.alloc(f"pT{i}", [128, 1024], BF16) for i in range(3)]
        oTs = kb.alloc("oTs", [128, 512], F32)
        onesf = kb.alloc("onesf", [128, 512], F32)
        kb.op("dve", lambda e: e.memset(onesf[:], 1.0), writes=[onesf])
        recf = kb.alloc("recf", [128, 512], F32)
        rech = kb.alloc("rech", [128, 512], BF16)
        recl = kb.alloc("recl", [128, 512], BF16)
        attnT = kb.alloc("attnT", [128, 8, L], BF16)
        g1b = kb.alloc("g1bm", [128, D], F32)
        t_tmp = kb.alloc("t_tmpm", [128, 512], F32)
        zt = kb.alloc("zt", [128, 416], F32)
        cqn = kb.alloc("cqn", [128, 256], BF16)
        ckvn = kb.alloc("ckvn", [128, 128], BF16)
        ktok = kb.alloc("ktok", [128, 96], BF16)
        kb.op("dve", lambda e: e.memset(ktok[:], 0.0), writes=[ktok])
        rt = [kb.alloc(f"rt{i}", [128, 4, 16], F32) for i in range(4)]
        st2 = kb.alloc("st2", [128, 8 * NT], F32)
        kb.op("dve", lambda e: e.memset(st2[:], 0.0), writes=[st2])
        junk2 = kb.alloc("junk2", [128, 256], BF16)
        addc1 = lambda k, r: self.modc[l][:, k, r:r + 1]
        mulc1 = self.mul1c[l]
        npt = 0
        for s in range(NS):
            kb.dma("sp", lambda e, s=s: e.dma_start(out=g1b[:], in_=self.modd.ap[l, s, 2 * D:3 * D].partition_broadcast(128)),
                   reads=[self.modd.sub(l)], writes=[g1b])
            kb.op("dve", lambda e: e.memset(st2[:], 0.0), writes=[st2])
            for b2 in range(LC // 256):
                self.norm_batch(self.xc[s], b2 * 256, 2, mulc1, addc1, 2, uT, b2 * 256, bufs, [B[0], B[1], B[2], B[3]])
            for b2 in range(L // 256):
                self.norm_batch(self.xr[s], b2 * 256, 2, mulc1, addc1, s, uT, LC + b2 * 256, bufs, [B[0], B[1], B[2], B[3]])
            for c in range(NT):
                lat = c >= 2
                cl_ = c - 2
                zb = B[4 + c % 2]
                for k in range(8):
                    kb.op("pe", lambda e, c=c, k=k, zb=zb: e.matmul(out=zb.h[:, 0:416], lhsT=uT[:, k, c * 128:(c + 1) * 128], rhs=wi[:, k, :],
                                                                  start=(k == 0), stop=(k == 7)), reads=[uT, wi], writes=[zb])
                kb.op("act", lambda e, zb=zb: e.copy(out=zt[:], in_=zb.h[:, 0:416]), reads=[zb], writes=[zt])
                so = c * 8
                parts = [(256, 128, 128.0, ckvn, 0)] + ([(0, 256, 256.0, cqn, 4)] if lat else [])
                for (c0, n, nf, dstt, o) in parts:
                    kb.op("act", lambda e, c0=c0, n=n, so=so, o=o: e.activation(out=junk2[:, 0:n], in_=zt[:, c0:c0 + n], func=AF.Square,
                                                                             accum_out=st2[:, so + o:so + o + 1]), reads=[zt], writes=[junk2, st2.sub(so + o)])
                    kb.op("act", lambda e, so=so, o=o, nf=nf: e.activation(out=st2[:, so + o + 1:so + o + 2], in_=st2[:, so + o:so + o + 1], func=AF.Identity,
                                                                         scale=1.0 / nf, bias=self.epsc[:, 0:1]), reads=[st2.sub(so + o), self.epsc], writes=[st2.sub(so + o + 1)])
                    kb.op("act", lambda e, so=so, o=o: e.activation(out=st2[:, so + o + 2:so + o + 3], in_=st2[:, so + o + 1:so + o + 2], func=AF.Sqrt),
                          reads=[st2.sub(so + o + 1)], writes=[st2.sub(so + o + 2)])
                    kb.op("dve", lambda e, so=so, o=o: e.reciprocal(out=st2[:, so + o + 3:so + o + 4], in_=st2[:, so + o + 2:so + o + 3]),
                          reads=[st2.sub(so + o + 2)], writes=[st2.sub(so + o + 3)])
                    kb.op("act", lambda e, c0=c0, n=n, so=so, o=o, dstt=dstt: e.activation(out=dstt[:, 0:n], in_=zt[:, c0:c0 + n], func=AF.Identity,
                                                                                          scale=st2[:, so + o + 3:so + o + 4]), reads=[zt, st2.sub(so + o + 3)], writes=[dstt])
                if lat:
                    krv = zt[:, 384:416].rearrange("p (i two) -> p i two", two=2)
                    xe, xo = krv[:, :, 0], krv[:, :, 1]
                    cs, sn = ropt[:, cl_, 0:16], ropt[:, cl_, 16:32]
                    ko = ktok[:, 64:96].rearrange("p (i two) -> p i two", two=2)
                    a0, a1, a2, a3 = rt[0][:, 0, :], rt[1][:, 0, :], rt[2][:, 0, :], rt[3][:, 0, :]
                    kb.op("dve", lambda e, xe=xe, cs=cs, a0=a0: e.tensor_tensor(out=a0, in0=xe, in1=cs, op=ALU.mult), reads=[zt, ropt], writes=[rt[0]])
                    kb.op("dve", lambda e, xo=xo, sn=sn, a1=a1: e.tensor_tensor(out=a1, in0=xo, in1=sn, op=ALU.mult), reads=[zt, ropt], writes=[rt[1]])
                    kb.op("dve", lambda e, xe=xe, sn=sn, a2=a2: e.tensor_tensor(out=a2, in0=xe, in1=sn, op=ALU.mult), reads=[zt, ropt], writes=[rt[2]])
                    kb.op("dve", lambda e, xo=xo, cs=cs, a3=a3: e.tensor_tensor(out=a3, in0=xo, in1=cs, op=ALU.mult), reads=[zt, ropt], writes=[rt[3]])
                    kb.op("dve", lambda e, ko=ko, a0=a0, a1=a1: e.tensor_tensor(out=ko[:, :, 0], in0=a0, in1=a1, op=ALU.subtract), reads=[rt[0], rt[1]], writes=[ktok.sub(0)])
                    kb.op("dve", lambda e, ko=ko, a2=a2, a3=a3: e.tensor_tensor(out=ko[:, :, 1], in0=a2, in1=a3, op=ALU.add), reads=[rt[2], rt[3]], writes=[ktok.sub(1)])
                else:
                    kb.op("dve", lambda e: e.tensor_copy(out=ktok[:, 64:96], in_=zt[:, 384:416]), reads=[zt], writes=[ktok.sub(0)])
                tbk = B[6 + c % 2]
                pv = tbk.h.bitcast(BF16)
                if lat:
                    for qk in range(2):
                        kb.op("pe", lambda e, pv=pv, qk=qk: e.transpose(out=pv[:, qk * 128:(qk + 1) * 128], in_=cqn[:, qk * 128:(qk + 1) * 128], identity=self.identb[:]),
                              reads=[cqn, self.identb], writes=[tbk])
                kb.op("pe", lambda e, pv=pv: e.transpose(out=pv[:, 256:384], in_=ckvn[:, :], identity=self.identb[:]), reads=[ckvn, self.identb], writes=[tbk])
                kb.op("pe", lambda e, pv=pv: e.transpose(out=pv[0:96, 384:512], in_=ktok[:, 0:96], identity=self.identb[:]), reads=[ktok, self.identb], writes=[tbk])
                if lat:
                    for qk in range(2):
                        kb.op("act", lambda e, pv=pv, qk=qk, cl_=cl_: e.activation(out=cqnT[:, qk, cl_ * 128:(cl_ + 1) * 128], in_=pv[:, qk * 128:(qk + 1) * 128],
                                                                                 func=AF.Identity, scale=qgc[:, qk:qk + 1]), reads=[tbk, qgc], writes=[cqnT.sub((qk, cl_))])
                kb.op("act", lambda e, pv=pv, c=c: e.activation(out=ckvnT[:, c * 128:(c + 1) * 128], in_=pv[:, 256:384], func=AF.Identity, scale=kvgc[:, 0:1]),
                      reads=[tbk, kvgc], writes=[ckvnT.sub(c)])
                for KTx in KTs:
                    kb.op("act", lambda e, pv=pv, c=c, KTx=KTx: e.copy(out=KTx[64:96, c * 128:(c + 1) * 128], in_=pv[64:96, 384:512]),
                          reads=[tbk], writes=[KTx.sub(("r", c))])
            def projA(h):
                KTh = KTs[h % 2]
                for blk in range(5):
                    n0 = blk * 512
                    nn = min(512, NKV - n0)
                    bk = B[7]
                    kb.op("pe", lambda e, h=h, n0=n0, nn=nn, bk=bk: e.matmul(out=bk.h[0:64, 0:nn], lhsT=wukv[:, h * 128:h * 128 + 64], rhs=ckvnT[:, n0:n0 + nn],
                                                                        start=True, stop=True), reads=[wukv, ckvnT], writes=[bk])
                    kb.op("dve", lambda e, n0=n0, nn=nn, bk=bk, KTh=KTh: e.tensor_copy(out=KTh[0:64, n0:n0 + nn], in_=bk.h[0:64, 0:nn]),
                          reads=[bk], writes=[KTh.sub(("n", blk))])
                va = Vaug[h % 2]
                vo = 0 if h % 2 == 0 else 64
                for vb in range(3):
                    ntl = min(8, NT - vb * 8)
                    bv = B[7]
                    for ci in range(ntl):
                        c = vb * 8 + ci
                        kb.op("pe", lambda e, h=h, c=c, ci=ci, bv=bv: e.matmul(out=bv.h[:, ci * 64:(ci + 1) * 64], lhsT=ckvnT[:, c * 128:(c + 1) * 128],
                                                                             rhs=wukv[:, h * 128 + 64:h * 128 + 128], start=True, stop=True), reads=[ckvnT, wukv], writes=[bv])
                    kb.op("dve", lambda e, vb=vb, ntl=ntl, bv=bv, va=va, vo=vo: e.tensor_copy(
                        out=va[:, vb * 8:vb * 8 + ntl, vo:vo + 64], in_=bv.h[:, 0:ntl * 64].rearrange("p (c f) -> p c f", f=64)),
                        reads=[bv], writes=[va.sub(vb)])

            def projQ(h, half, stage):
                qtk = qtoks[half]
                QTh = QTs[h % 2]
                for bq in range(2):
                    c0 = half * 8 + bq * 4
                    if stage == 0:
                        bqk = B[6 + bq]
                        for ci in range(4):
                            c = c0 + ci
                            for qk in range(2):
                                kb.op("pe", lambda e, h=h, c=c, ci=ci, qk=qk, bqk=bqk: e.matmul(
                                    out=bqk.h[:, ci * 96:(ci + 1) * 96], lhsT=cqnT[:, qk, c * 128:(c + 1) * 128], rhs=wuq[:, qk, h * 96:(h + 1) * 96],
                                    start=(qk == 0), stop=(qk == 1)), reads=[cqnT, wuq], writes=[bqk])
                        qv = bqk.h[:, 0:384].rearrange("p (c f) -> p c f", f=96)
                        lc0 = bq * 4
                        kb.op("dve", lambda e, qv=qv, lc0=lc0, qtk=qtk: e.tensor_copy(out=qtk[:, lc0:lc0 + 4, 0:64], in_=qv[:, :, 0:64]), reads=[bqk], writes=[qtk.sub((lc0, "n"))])
                        xe, xo = qv[:, :, 64:96:2], qv[:, :, 65:96:2]
                        cs, sn = ropt[:, c0:c0 + 4, 0:16], ropt[:, c0:c0 + 4, 16:32]
                        qo = qtk[:, lc0:lc0 + 4, 64:96].rearrange("p c (i two) -> p c i two", two=2)
                        kb.op("dve", lambda e, xe=xe, cs=cs: e.tensor_tensor(out=rt[0][:], in0=xe, in1=cs, op=ALU.mult), reads=[bqk, ropt], writes=[rt[0]])
                        kb.op("dve", lambda e, xo=xo, sn=sn: e.tensor_tensor(out=rt[1][:], in0=xo, in1=sn, op=ALU.mult), reads=[bqk, ropt], writes=[rt[1]])
                        kb.op("dve", lambda e, xe=xe, sn=sn: e.tensor_tensor(out=rt[2][:], in0=xe, in1=sn, op=ALU.mult), reads=[bqk, ropt], writes=[rt[2]])
                        kb.op("dve", lambda e, xo=xo, cs=cs: e.tensor_tensor(out=rt[3][:], in0=xo, in1=cs, op=ALU.mult), reads=[bqk, ropt], writes=[rt[3]])
                        kb.op("pool", lambda e, qo=qo: e.tensor_tensor(out=qo[:, :, :, 0], in0=rt[0][:], in1=rt[1][:], op=ALU.subtract),
                              reads=[rt[0], rt[1]], writes=[qtk.sub((lc0, "e"))])
                        kb.op("pool", lambda e, qo=qo: e.tensor_tensor(out=qo[:, :, :, 1], in0=rt[2][:], in1=rt[3][:], op=ALU.add),
                              reads=[rt[2], rt[3]], writes=[qtk.sub((lc0, "o"))])
                    else:
                        lc0 = bq * 4
                        tq = B[6 + bq]
                        pvq = tq.h.bitcast(BF16)
                        for ci in range(4):
                            kb.op("pe", lambda e, pvq=pvq, lc=lc0 + ci, ci=ci, qtk=qtk: e.transpose(out=pvq[0:96, ci * 128:(ci + 1) * 128], in_=qtk[:, lc, 0:96], identity=self.identb[:]),
                                  reads=[qtk, self.identb], writes=[tq])
                        kb.op("dve", lambda e, pvq=pvq, c0=c0, QTh=QTh: e.tensor_copy(out=QTh[0:96, c0 * 128:(c0 + 4) * 128], in_=pvq[0:96, 0:512]), reads=[tq], writes=[QTh.sub(c0)])

            def normalize1(h, qb, bo):
                kb.op("dve", lambda e, bo=bo: e.tensor_copy(out=oTs[:], in_=bo.h[:, 0:512]), reads=[bo], writes=[oTs])

            def normalize1b(h, qb):
                dp = 64 if (h % 2 == 0) else 0
                kb.op("dve", lambda e, dp=dp: e.reciprocal(out=recf[dp:dp + 1, :], in_=oTs[dp:dp + 1, :]), reads=[oTs], writes=[recf])
                kb.op("pool", lambda e, dp=dp: e.tensor_copy(out=rech[dp:dp + 1, :], in_=recf[dp:dp + 1, :]), reads=[recf], writes=[rech])
                kb.op("pool", lambda e, dp=dp: e.tensor_tensor(out=recl[dp:dp + 1, :], in0=recf[dp:dp + 1, :], in1=rech[dp:dp + 1, :], op=ALU.subtract),
                      reads=[recf, rech], writes=[recl])

            def normalize2(h, qb):
                even = (h % 2 == 0)
                dp = 64 if even else 0
                op_ = 0 if even else 64
                bb = B[6]
                kb.op("pe", lambda e, dp=dp, bb=bb: e.matmul(out=bb.h[:, 0:512], lhsT=ones1[dp:dp + 1, :], rhs=rech[dp:dp + 1, :], start=True, stop=False),
                      reads=[ones1, rech], writes=[bb])
                kb.op("pe", lambda e, dp=dp, bb=bb: e.matmul(out=bb.h[:, 0:512], lhsT=ones1[dp:dp + 1, :], rhs=recl[dp:dp + 1, :], start=False, stop=True),
                      reads=[ones1, recl], writes=[bb])
                kb.op("dve", lambda e, op_=op_, h=h, qb=qb, bb=bb: e.tensor_tensor(
                    out=attnT[op_:op_ + 64, h // 2, qb * 512:(qb + 1) * 512], in0=oTs[op_:op_ + 64, :], in1=bb.h[op_:op_ + 64, 0:512], op=ALU.mult),
                    reads=[oTs, bb], writes=[attnT.sub((h, qb))])

            projA(0)
            for half in range(2):
                projQ(0, half, 0)
                projQ(0, half, 1)
            pend = []
            pendnorm = []
            nstep = 0

            def issue_pv(item):
                (h, qb, c2, bo, pt, va) = item
                for half in range(2):
                    c = c2 * 2 + half
                    kb.op("pe", lambda e, c=c, bo=bo, pt=pt, va=va, half=half: e.matmul(
                        out=bo.h[:, 0:512], lhsT=va[:, c, :], rhs=pt[:, half * 512:(half + 1) * 512],
                        start=(c == 0), stop=(c == NT - 1)), reads=[va, pt], writes=[bo])
                if c2 == NT // 2 - 1:
                    normalize1(h, qb, bo)
                    pendnorm.append((nstep + 1, 1, h, qb))
                    pendnorm.append((nstep + 6, 2, h, qb))
                    pendnorm.sort()

            for h in range(NE):
                KTh = KTs[h % 2]
                QTh = QTs[h % 2]
                va = Vaug[h % 2]
                for qb in range(4):
                    bo = B[4 + qb % 2]
                    for c2 in range(NT // 2):
                        if h + 1 < NE:
                            if qb == 0 and c2 == 4:
                                projA(h + 1)
                            if qb == 1 and c2 == 4:
                                projQ(h + 1, 0, 0)
                            if qb == 2 and c2 == 4:
                                projQ(h + 1, 0, 1)
                            if qb == 3 and c2 == 3:
                                projQ(h + 1, 1, 0)
                            if qb == 3 and c2 == 8:
                                projQ(h + 1, 1, 1)
                        while pendnorm and nstep >= pendnorm[0][0]:
                            (_, stg, hh, qq) = pendnorm.pop(0)
                            (normalize1b if stg == 1 else normalize2)(hh, qq)
                        pi = nstep % 2
                        pt = pT[nstep % 3]
                        nstep += 1
                        for half in range(2):
                            c = c2 * 2 + half
                            bs = B[2 * pi + half]
                            kb.op("pe", lambda e, c=c, qb=qb, bs=bs, KTh=KTh, QTh=QTh: e.matmul(
                                out=bs.h[:, 0:512], lhsT=KTh[0:96, c * 128:(c + 1) * 128], rhs=QTh[0:96, qb * 512:(qb + 1) * 512],
                                start=True, stop=True), reads=[KTh, QTh], writes=[bs])
                        kb.op("act", lambda e, pi=pi, pt=pt: e.activation(out=pt[:, 0:1024], in_=kb.pairs[pi][:, 0:1024], func=AF.Exp, scale=SCALE),
                              reads=[B[2 * pi], B[2 * pi + 1]], writes=[pt])
                        pend.append((h, qb, c2, bo, pt, va))
                        if len(pend) > 1:
                            issue_pv(pend.pop(0))
            while pend:
                issue_pv(pend.pop(0))
            while pendnorm:
                (_, stg, hh, qq) = pendnorm.pop(0)
                (normalize1b if stg == 1 else normalize2)(hh, qq)
            for tc in range(L // 128):
                kb.dma("sp", lambda e, s=s, tc=tc: e.dma_start(out=xb[:], in_=self.xr[s].ap[tc * 128:(tc + 1) * 128, :]),
                       reads=[self.xr[s]], writes=[xb])
                for db in range(2):
                    bank = B[6 + db]
                    for m in range(8):
                        kb.op("pe", lambda e, m=m, db=db, bank=bank, tc=tc: e.matmul(
                            out=bank.h[:, 0:512], lhsT=attnT[:, m, tc * 128:(tc + 1) * 128], rhs=wo[:, m, db * 512:(db + 1) * 512],
                            start=(m == 0), stop=(m == 7)), reads=[attnT, wo], writes=[bank])
                    kb.op("dve", lambda e, db=db, bank=bank: e.tensor_tensor(
                        out=t_tmp[:, 0:512], in0=bank.h[:, 0:512], in1=g1b[:, db * 512:(db + 1) * 512], op=ALU.mult),
                        reads=[bank, g1b], writes=[t_tmp])
                    kb.op("pool", lambda e, db=db: e.tensor_tensor(
                        out=xb[:, db * 512:(db + 1) * 512], in0=xb[:, db * 512:(db + 1) * 512], in1=t_tmp[:, 0:512], op=ALU.add),
                        reads=[xb, t_tmp], writes=[xb])
                kb.dma("sp", lambda e, s=s, tc=tc: e.dma_start(out=self.xr[s].ap[tc * 128:(tc + 1) * 128, :], in_=xb[:]),
                       reads=[xb], writes=[self.xr[s].sub(("row", tc))])


def _consts():
    bf = ml_dtypes.bfloat16
    c = {}
    c["c_identb"] = np.eye(128, dtype=np.float32).astype(bf)
    c["c_identf"] = np.eye(128, dtype=np.float32)
    i = np.arange(128, dtype=np.int64)
    ang = 2.0 * np.pi * ((i[:, None] * i[None, :]) % 128).astype(np.float64) / 128.0
    c["c_csc"] = np.concatenate([np.cos(ang), np.sin(ang)], axis=1).astype(np.float32) / np.float32(np.sqrt(128.0))
    c["c_csc"] = c["c_csc"].astype(bf)
    for nm, n in (("", L), ("c", LC)):
        t = np.arange(n, dtype=np.int64)
        a = 2.0 * np.pi * ((t[:, None] * t[None, :]) % n).astype(np.float64) / n
        c["c_cl" + nm] = (np.cos(a) / np.sqrt(n)).astype(np.float32).astype(bf)
        c["c_sl" + nm] = (-np.sin(a) / np.sqrt(n)).astype(np.float32).astype(bf)
    t = np.arange(L)
    row = (t // 64).astype(np.float32)
    col = (t % 64).astype(np.float32)
    inv = (np.float32(10000.0) ** (-np.arange(8, dtype=np.float32) / np.float32(8))).astype(np.float32)
    angr = np.concatenate([row[:, None] * inv[None, :], col[:, None] * inv[None, :]], axis=1).astype(np.float32)
    c["c_rope"] = np.concatenate([np.cos(angr), np.sin(angr)], axis=1).astype(np.float32)
    c["c_ctxbase"] = np.concatenate([np.zeros(32), np.full(32, LC), NS * LC + np.arange(64)]).astype(np.float32).reshape(128, 1)
    return c


def _in_map(inp, core, consts):
    f = lambda a: np.ascontiguousarray(np.asarray(a, dtype=np.float32))
    s0 = core * NS
    m = {}
    m["x"] = f(inp["x"][s0:s0 + NS])
    m["ctx"] = f(inp["ctx"][s0:s0 + NS])
    cv3 = np.stack([inp["c"][s0], inp["c"][s0 + 1], inp["c_ctx"]], axis=0).astype(np.float32)
    m["cv"] = np.ascontiguousarray(cv3.reshape(3, 8, 128).transpose(2, 1, 0))
    for k in ("mod_w", "mod_b", "norm1_g", "norm2_g", "final_g", "moe_w_router", "moe_w1", "moe_w3", "moe_w2"):
        m[k] = f(inp[k])
    for k in ("ab_w_in", "ab_conv_w", "ab_conv_b", "ab_ln_g", "ab_ln_b", "ab_w_out", "mla_w_in", "mla_q_norm_g",
              "mla_kv_norm_g", "mla_w_uq", "mla_w_ukv", "mla_w_o"):
        m[k] = f(inp[k][0])
    m.update(consts)
    return m


_CACHE = {}


def run_prog(inputs, phases, copy_in=False, ncores=8, debug_route=False, raw=False):
    key = (tuple(phases), copy_in, debug_route)
    if key not in _CACHE:
        p = Prog(phases=phases, copy_in=copy_in)
        p.debug_route = debug_route
        _CACHE[key] = p.build()
    nc = _CACHE[key]
    consts = _consts()
    in_maps = [_in_map(inputs, c, consts) for c in range(ncores)]
    res = run_bass_kernel_spmd(nc, in_maps, core_ids=list(range(ncores)))
    if raw:
        return res.results
    return np.concatenate([np.asarray(r["y"]) for r in res.results], axis=0)


def kernel(**inputs):
    out = run_prog(inputs, ("mix0", "moe0", "mla1", "moe1", "final"))
    return out.astype(np.float32)
```

```python
import numpy as np
import ml_dtypes
from contextlib import ExitStack
import concourse.bass as bass
import concourse.mybir as mybir
from concourse.bass_utils import run_bass_kernel_spmd

F32 = mybir.dt.float32
BF16 = mybir.dt.bfloat16
I32 = mybir.dt.int32
U32 = mybir.dt.uint32
U8 = mybir.dt.uint8
AF = mybir.ActivationFunctionType
ALU = mybir.AluOpType
AX = mybir.AxisListType

D = 1024
L = 2048
LC = 256
NS = 2
NE = 16
EPS = 1e-6
DSZ = {F32: 4, BF16: 2, I32: 4, U32: 4, U8: 1}


class Trk:
    def __init__(self, name):
        self.name = name
        self.w = None
        self.r = []
        self.kids = {}
        self.parent = None

    def sub(self, key):
        if key not in self.kids:
            k = Trk(f"{self.name}.{key}")
            k.parent = self
            self.kids[key] = k
        return self.kids[key]

    def rdeps(self):
        s = set()
        if self.w:
            s.add(self.w)
        if self.parent is not None and self.parent.w:
            s.add(self.parent.w)
        for k in self.kids.values():
            if k.w:
                s.add(k.w)
        return s

    def wdeps(self):
        s = self.rdeps()
        s.update(self.r)
        if self.parent is not None:
            s.update(self.parent.r)
        for k in self.kids.values():
            s.update(k.r)
        return s

    def did_read(self, ev):
        self.r.append(ev)

    def did_write(self, ev):
        self.w = ev
        self.r = []
        for k in self.kids.values():
            k.w = None
            k.r = []


class T(Trk):
    def __init__(self, kb, name, shape, dtype, off):
        super().__init__(name)
        self.kb = kb
        self.shape = shape
        self.dtype = dtype
        self.off = off
        self.h = kb.nc.alloc_sbuf_tensor_at(name, list(shape), dtype, offset=off)

    def view(self, name, shape, dtype, boff=0):
        return self.kb.nc.alloc_sbuf_tensor_at(
            self.kb.uname(name), list(shape), dtype, offset=self.off + boff)

    def __getitem__(self, k):
        return self.h[k]


class Lane:
    def __init__(self, key, sem):
        self.key = key
        self.sem = sem
        self.count = 0


class KB:
    COMPUTE = ["pe", "act", "dve", "pool"]
    QUEUES = ["sp", "pool", "act"]

    def __init__(self, n_lanes=8):
        self.nc = bass.Bass("TRN2", target_bir_lowering=False)
        nc = self.nc
        self.es = ExitStack()
        self.uid = 0
        self.semobj = {}
        self.cnt = {}
        for e in self.COMPUTE:
            self.semobj[e] = self.es.enter_context(nc.semaphore("s_" + e))
            self.cnt[e] = 0
        self.lanes = {}
        self.lane_rr = {}
        for q in self.QUEUES:
            self.lanes[q] = []
            for i in range(n_lanes):
                key = f"d_{q}{i}"
                self.semobj[key] = self.es.enter_context(nc.semaphore(key))
                self.lanes[q].append(Lane(key, self.semobj[key]))
            self.lane_rr[q] = 0
        self.prog = {e: [] for e in ["pe", "act", "dve", "pool", "sp"]}
        self.waited = {e: {} for e in ["pe", "act", "dve", "pool", "sp"]}
        self.arena_bytes = 206 * 1024
        ah = nc.alloc_sbuf_tensor("arena", [128, self.arena_bytes], U8)
        self.abase = nc.lookup_mloc(ah).addr
        self.atop = 0
        self.bnd = {}
        for n in (L - 1, NS * LC + 128 - 1):
            reg = self.es.enter_context(nc.gpsimd.register(f"bnd{n}"))
            self.bnd[n] = reg
            self.prog["pool"].append(lambda en, reg=reg, n=n: en.reg_mov(reg, n))
        self.banks = []
        self.pairs = []
        for i in range(4):
            ph = self.es.enter_context(nc.psum_tensor(f"pbank{i}", [128, 1024], F32))
            self.pairs.append(ph)
            for j in range(2):
                t = Trk(f"bank{2 * i + j}")
                t.h = ph[:, j * 512:(j + 1) * 512]
                t.psum = True
                self.banks.append(t)

    def uname(self, n):
        self.uid += 1
        return f"{n}_{self.uid}"

    def alloc(self, name, shape, dtype):
        nbytes = int(np.prod(shape[1:])) * DSZ[dtype]
        nbytes = (nbytes + 63) // 64 * 64
        off = self.atop
        assert off + nbytes <= self.arena_bytes, f"SBUF arena overflow at {name}: {off}+{nbytes}"
        self.atop += nbytes
        return T(self, self.uname(name), shape, dtype, self.abase + off)

    def mark(self):
        return self.atop

    def release(self, m):
        self.barrier()
        self.atop = m

    def dram(self, name, shape, dtype, kind="Internal"):
        if kind == "Internal":
            h = self.nc.dram_tensor(name, list(shape), dtype)
        else:
            h = self.nc.dram_tensor(name, list(shape), dtype, kind=kind)
        t = Trk(name)
        t.h = h
        t.ap = h.ap()
        return t

    def _waits(self, eng, evs):
        best = {}
        for (k, v) in evs:
            if v > best.get(k, 0):
                best[k] = v
        for k, v in best.items():
            if k == "pe" and eng == "pe":
                continue
            if self.waited[eng].get(k, 0) >= v:
                continue
            self.waited[eng][k] = v
            sem = self.semobj[k]
            self.prog[eng].append(lambda e, sem=sem, v=v: e.wait_ge(sem, v))

    def _deps(self, reads, writes, eng=None):
        evs = set()
        for t in reads:
            evs |= t.rdeps()
            root = t if t.parent is None else t.parent
            if getattr(root, "psum", False):
                for ev in root.r:
                    if ev[0] != eng:
                        evs.add(ev)
                for k in root.kids.values():
                    for ev in k.r:
                        if ev[0] != eng:
                            evs.add(ev)
        for t in writes:
            evs |= t.wdeps()
        return evs

    def op(self, eng, fn, reads=(), writes=()):
        evs = self._deps(reads, writes, eng)
        self._waits(eng, evs)
        self.cnt[eng] += 1
        sem = self.semobj[eng]
        self.prog[eng].append(lambda e, fn=fn, sem=sem: fn(e).then_inc(sem, 1))
        ev = (eng, self.cnt[eng])
        for t in reads:
            t.did_read(ev)
        for t in writes:
            t.did_write(ev)
        return ev

    def dma(self, q, fn, reads=(), writes=()):
        evs = self._deps(reads, writes)
        lanes = self.lanes[q]
        lane = lanes[self.lane_rr[q] % len(lanes)]
        self.lane_rr[q] += 1
        if lane.count > 0:
            evs.add((lane.key, lane.count))
        self._waits(q, evs)
        lane.count += 16
        sem = lane.sem
        def run(e, fn=fn, sem=sem):
            try:
                ins = fn(e)
            except Exception:
                print("DMA BUILD FAIL line", fn.__code__.co_firstlineno, "defaults", [str(d)[:80] for d in (fn.__defaults__ or ())])
                raise
            ins.then_inc(sem, 16)
        self.prog[q].append(run)
        ev = (lane.key, lane.count)
        for t in reads:
            t.did_read(ev)
        for t in writes:
            t.did_write(ev)
        return ev

    def barrier(self):
        evs = set()
        for e in self.COMPUTE:
            if self.cnt[e] > 0:
                evs.add((e, self.cnt[e]))
        for q in self.QUEUES:
            for ln in self.lanes[q]:
                if ln.count > 0:
                    evs.add((ln.key, ln.count))
        for e in ["pe", "act", "dve", "pool", "sp"]:
            self._waits(e, evs)

    def finish(self):
        self.barrier()
        nc = self.nc
        with nc.allow_non_contiguous_dma(reason="small strided constant loads"):
            with nc.Block() as block:
                @block.sync
                def _(e):
                    for f in self.prog["sp"]:
                        f(e)

                @block.tensor
                def _(e):
                    for f in self.prog["pe"]:
                        f(e)

                @block.scalar
                def _(e):
                    for f in self.prog["act"]:
                        f(e)

                @block.vector
                def _(e):
                    for f in self.prog["dve"]:
                        f(e)

                @block.gpsimd
                def _(e):
                    for f in self.prog["pool"]:
                        f(e)
        self.es.close()
        return nc


class Prog:
    def __init__(self, phases=("mix0", "moe0", "mla1", "moe1", "final"), copy_in=False):
        self.kb = KB()
        self.phases = phases
        self.copy_in = copy_in
        kb = self.kb
        di = lambda n, s, d=F32: kb.dram(n, s, d, kind="ExternalInput")
        self.x = di("x", [NS, L, D])
        self.ctx = di("ctx", [NS, LC, D])
        self.cv = di("cv", [128, 8, 3])
        self.mod_w = di("mod_w", [2, D, 6 * D])
        self.mod_b = di("mod_b", [2, 6 * D])
        self.n1g = di("norm1_g", [2, D])
        self.n2g = di("norm2_g", [2, D])
        self.final_g = di("final_g", [D])
        self.ab_w_in = di("ab_w_in", [D, 1536])
        self.ab_conv_w = di("ab_conv_w", [31, 512])
        self.ab_conv_b = di("ab_conv_b", [512])
        self.ab_ln_g = di("ab_ln_g", [512])
        self.ab_ln_b = di("ab_ln_b", [512])
        self.ab_w_out = di("ab_w_out", [D, D])
        self.mla_w_in = di("mla_w_in", [D, 416])
        self.mla_qg = di("mla_q_norm_g", [256])
        self.mla_kvg = di("mla_kv_norm_g", [128])
        self.mla_w_uq = di("mla_w_uq", [256, 1536])
        self.mla_w_ukv = di("mla_w_ukv", [128, 2048])
        self.mla_w_o = di("mla_w_o", [D, D])
        self.w_router = di("moe_w_router", [2, D, NE])
        self.w1 = di("moe_w1", [2, NE, D, D])
        self.w3 = di("moe_w3", [2, NE, D, D])
        self.w2 = di("moe_w2", [2, NE, D, D])
        self.c_identb = di("c_identb", [128, 128], BF16)
        self.c_identf = di("c_identf", [128, 128], F32)
        self.c_csc = di("c_csc", [128, 256], BF16)
        self.c_cl = di("c_cl", [L, L], BF16)
        self.c_sl = di("c_sl", [L, L], BF16)
        self.c_clc = di("c_clc", [LC, LC], BF16)
        self.c_slc = di("c_slc", [LC, LC], BF16)
        self.c_rope = di("c_rope", [L, 32], F32)
        self.c_ctxbase = di("c_ctxbase", [128, 1], F32)
        self.out = kb.dram("y", [NS, L, D], F32, kind="ExternalOutput")
        self.xr = [kb.dram(f"xr{s}", [L, D], F32) for s in range(NS)]
        self.xc_all = kb.dram("xc_all", [NS * LC + 128, D], F32)
        self.xnc_all = kb.dram("xnc_all", [NS * LC + 128, D], BF16)
        self.xc = []
        self.xnl = [kb.dram(f"xnl{s}", [L, D], BF16) for s in range(NS)]
        self.xnc = []
        for s in range(NS):
            t = self.xc_all.sub(s); t.ap = self.xc_all.ap[s * LC:(s + 1) * LC, :]; self.xc.append(t)
            t = self.xnc_all.sub(s); t.ap = self.xnc_all.ap[s * LC:(s + 1) * LC, :]; self.xnc.append(t)
        self.xin = []
        self.cin = []
        for s in range(NS):
            t = self.x.sub(s); t.ap = self.x.ap[s]; self.xin.append(t)
            t = self.ctx.sub(s); t.ap = self.ctx.ap[s]; self.cin.append(t)
        self.modd = kb.dram("modd", [2, 3, 6 * D], F32)

    def build(self):
        kb = self.kb
        self.prologue()
        if self.copy_in:
            for s in range(NS):
                kb.dma("sp", lambda e, s=s: e.dma_start(out=self.xr[s].ap, in_=self.x.ap[s]),
                       reads=[self.x], writes=[self.xr[s]])
                kb.dma("sp", lambda e, s=s: e.dma_start(out=self.xc[s].ap, in_=self.ctx.ap[s]),
                       reads=[self.ctx], writes=[self.xc[s]])
            kb.barrier()
        for ph in self.phases:
            m = kb.mark()
            if ph == "mix0":
                self.mixer0()
            elif ph == "moe0":
                self.moe(0, with_ctx=True)
            elif ph == "mla1":
                self.mla()
            elif ph == "moe1":
                self.moe(1, with_ctx=False)
            elif ph == "final":
                self.final()
            elif ph == "dump":
                self.dump()
            kb.release(m)
        return kb.finish()

    def prologue(self):
        kb = self.kb
        self.identb = kb.alloc("identb", [128, 128], BF16)
        self.identf = kb.alloc("identf", [128, 128], F32)
        kb.dma("sp", lambda e: e.dma_start(out=self.identb[:], in_=self.c_identb.ap[:, :]),
               reads=[self.c_identb], writes=[self.identb])
        kb.dma("sp", lambda e: e.dma_start(out=self.identf[:], in_=self.c_identf.ap[:, :]),
               reads=[self.c_identf], writes=[self.identf])
        self.epsc = kb.alloc("epsc", [128, 1], F32)
        kb.op("dve", lambda e: e.memset(self.epsc[:], EPS), writes=[self.epsc])
        self.zeroc = kb.alloc("zeroc", [128, 1], F32)
        kb.op("dve", lambda e: e.memset(self.zeroc[:], 0.0), writes=[self.zeroc])
        self.modc = [kb.alloc(f"modc{l}", [128, 48, 3], F32) for l in range(2)]
        self.mul1c = [kb.alloc(f"mul1c{l}", [128, 8, 3], F32) for l in range(2)]
        self.mul2c = [kb.alloc(f"mul2c{l}", [128, 8, 3], F32) for l in range(2)]
        self.n1gc = kb.alloc("n1gc", [128, 2, 8], F32)
        self.n2gc = kb.alloc("n2gc", [128, 2, 8], F32)
        kb.dma("sp", lambda e: e.dma_start(out=self.n1gc[:], in_=self.n1g.ap.rearrange("l (k p) -> p l k", p=128)),
               reads=[self.n1g], writes=[self.n1gc])
        kb.dma("sp", lambda e: e.dma_start(out=self.n2gc[:], in_=self.n2g.ap.rearrange("l (k p) -> p l k", p=128)),
               reads=[self.n2g], writes=[self.n2gc])
        m0 = kb.mark()
        zf = kb.alloc("zf", [128, D], F32)
        zb = kb.alloc("zb", [128, D], BF16)
        kb.op("dve", lambda e: e.memset(zf[:], 0.0), writes=[zf])
        kb.op("dve", lambda e: e.memset(zb[:], 0.0), writes=[zb])
        kb.dma("sp", lambda e: e.dma_start(out=self.xc_all.ap[NS * LC:NS * LC + 128, :], in_=zf[:]),
               reads=[zf], writes=[self.xc_all.sub("pad")])
        kb.dma("sp", lambda e: e.dma_start(out=self.xnc_all.ap[NS * LC:NS * LC + 128, :], in_=zb[:]),
               reads=[zb], writes=[self.xnc_all.sub("pad")])
        cvt = kb.alloc("cvt", [128, 24], F32)
        sct = kb.alloc("sct", [128, 24], BF16)
        kb.dma("sp", lambda e: e.dma_start(out=cvt[:], in_=self.cv.ap.rearrange("p k r -> p (k r)")),
               reads=[self.cv], writes=[cvt])
        kb.op("act", lambda e: e.activation(out=sct[:], in_=cvt[:], func=AF.Silu), reads=[cvt], writes=[sct])
        mwt = [kb.alloc(f"mwt{i}", [128, 8, 1536], BF16) for i in range(2)]
        mrow = kb.alloc("mrow", [3, 6 * D], F32)
        mb3 = kb.alloc("mb3", [3, 6 * D], F32)
        it = 0
        for l in range(2):
            kb.dma("sp", lambda e, l=l: e.dma_start(out=mb3[:], in_=self.mod_b.ap[l].partition_broadcast(3)),
                   reads=[self.mod_b], writes=[mb3])
            for pc in range(4):
                wt = mwt[it % 2]
                it += 1
                src = self.mod_w.ap[l].rearrange("(k p) n -> p k n", p=128)[:, :, pc * 1536:(pc + 1) * 1536]
                kb.dma("pool", lambda e, wt=wt, src=src: e.dma_start(out=wt[:], in_=src),
                       reads=[self.mod_w], writes=[wt])
                for nb in range(3):
                    bank = kb.banks[(pc * 3 + nb) % 2]
                    for k in range(8):
                        kb.op("pe", lambda e, bank=bank, wt=wt, k=k, nb=nb: e.matmul(
                            out=bank.h[0:3, 0:512], lhsT=sct[:, k * 3:(k + 1) * 3],
                            rhs=wt[:, k, nb * 512:(nb + 1) * 512], start=(k == 0), stop=(k == 7)),
                            reads=[sct, wt], writes=[bank])
                    c0 = pc * 1536 + nb * 512
                    kb.op("dve", lambda e, bank=bank, c0=c0: e.tensor_tensor(
                        out=mrow[0:3, c0:c0 + 512], in0=bank.h[0:3, 0:512], in1=mb3[0:3, c0:c0 + 512], op=ALU.add),
                        reads=[bank, mb3], writes=[mrow.sub(c0)])
            kb.dma("sp", lambda e, l=l: e.dma_start(out=self.modd.ap[l], in_=mrow[0:3, :]),
                   reads=[mrow], writes=[self.modd.sub(l)])
            for r in range(3):
                kb.dma("sp", lambda e, l=l, r=r: e.dma_start(
                    out=self.modc[l][:, :, r], in_=self.modd.ap[l, r].rearrange("(c p) -> p c", p=128)),
                    reads=[self.modd.sub(l)], writes=[self.modc[l].sub(r)])
            for (mulc, gc, v) in ((self.mul1c[l], self.n1gc, 1), (self.mul2c[l], self.n2gc, 4)):
                kb.op("dve", lambda e, mulc=mulc, v=v, l=l: e.tensor_scalar(
                    out=mulc[:], in0=self.modc[l][:, v * 8:(v + 1) * 8, :], scalar1=1.0, scalar2=None, op0=ALU.add),
                    reads=[self.modc[l]], writes=[mulc])
                for r in range(3):
                    kb.op("dve", lambda e, mulc=mulc, gc=gc, r=r, l=l: e.tensor_tensor(
                        out=mulc[:, :, r], in0=mulc[:, :, r], in1=gc[:, l, :], op=ALU.mult),
                        reads=[mulc, gc], writes=[mulc])
        kb.release(m0)

    def norm_batch(self, src, row0, nt, mulc, addc, r, uT, ucol0, bufs, banks, xn_dst=None):
        kb = self.kb
        xb, xnb, junk, stat = bufs
        tiles = []
        for j in range(nt):
            xt = xb[self._nb % len(xb)]
            xn = xnb[self._nb % len(xnb)]
            sc = self._nb % 64
            self._nb += 1
            tiles.append((j, xt, xn, sc))
            rr = row0 + j * 128
            kb.dma("sp", lambda e, xt=xt, rr=rr: e.dma_start(out=xt[:], in_=src.ap[rr:rr + 128, :]),
                   reads=[src], writes=[xt])
            kb.op("act", lambda e, xt=xt, sc=sc: e.activation(
                out=junk[:], in_=xt[:], func=AF.Square, accum_out=stat[:, sc:sc + 1]),
                reads=[xt], writes=[junk, stat.sub(sc)])
            kb.op("act", lambda e, sc=sc: e.activation(
                out=stat[:, 64 + sc:65 + sc], in_=stat[:, sc:sc + 1], func=AF.Identity, scale=1.0 / D, bias=self.epsc[:, 0:1]),
                reads=[stat.sub(sc), self.epsc], writes=[stat.sub(64 + sc)])
            kb.op("act", lambda e, sc=sc: e.activation(
                out=stat[:, 128 + sc:129 + sc], in_=stat[:, 64 + sc:65 + sc], func=AF.Sqrt),
                reads=[stat.sub(64 + sc)], writes=[stat.sub(128 + sc)])
            kb.op("dve", lambda e, sc=sc: e.reciprocal(out=stat[:, 192 + sc:193 + sc], in_=stat[:, 128 + sc:129 + sc]),
                  reads=[stat.sub(128 + sc)], writes=[stat.sub(192 + sc)])
            if len(xb) == 1:
                self._norm_tail(tiles.pop(), src, row0, mulc, addc, r, uT, ucol0, banks, xn_dst)
        for t in tiles:
            self._norm_tail(t, src, row0, mulc, addc, r, uT, ucol0, banks, xn_dst)

    def _norm_tail(self, t, src, row0, mulc, addc, r, uT, ucol0, banks, xn_dst):
        kb = self.kb
        (j, xt, xn, sc) = t
        stat = self._stat
        kb.op("act", lambda e, xt=xt, xn=xn, sc=sc: e.activation(
            out=xn[:], in_=xt[:], func=AF.Identity, scale=stat[:, 192 + sc:193 + sc]),
            reads=[xt, stat.sub(192 + sc)], writes=[xn])
        if xn_dst is not None:
            dt, drow = xn_dst
            dr0 = drow + j * 128
            kb.dma("sp", lambda e, xn=xn, dt=dt, dr=dr0: e.dma_start(
                out=dt.ap[dr:dr + 128, :], in_=xn[:]), reads=[xn], writes=[dt.sub(dr0)])
        for k in range(8):
            bank = banks[2 * j + k // 4]
            pv = bank.h.bitcast(BF16)
            kk = k % 4
            kb.op("pe", lambda e, pv=pv, xn=xn, k=k, kk=kk: e.transpose(
                out=pv[:, kk * 128:(kk + 1) * 128], in_=xn[:, k * 128:(k + 1) * 128], identity=self.identb[:]),
                reads=[xn, self.identb], writes=[bank])
        for k in range(8):
            bank = banks[2 * j + k // 4]
            pv = bank.h.bitcast(BF16)
            kk = k % 4
            dst = uT[:, k, ucol0 + j * 128: ucol0 + (j + 1) * 128]
            if k < 4:
                kb.op("act", lambda e, dst=dst, pv=pv, k=k, kk=kk: e.activation(
                    out=dst, in_=pv[:, kk * 128:(kk + 1) * 128], func=AF.Identity,
                    scale=mulc[:, k, r:r + 1], bias=addc(k, r)),
                    reads=[bank, mulc], writes=[uT.sub((k, ucol0 + j * 128))])
            else:
                kb.op("dve", lambda e, dst=dst, pv=pv, k=k, kk=kk: e.tensor_scalar(
                    out=dst, in0=pv[:, kk * 128:(kk + 1) * 128], scalar1=mulc[:, k, r:r + 1],
                    scalar2=addc(k, r), op0=ALU.mult, op1=ALU.add),
                    reads=[bank, mulc], writes=[uT.sub((k, ucol0 + j * 128))])

    def norm_bufs(self, nx=2):
        kb = self.kb
        self._nb = 0
        xb = [kb.alloc(f"xb{i}", [128, D], F32) for i in range(nx)]
        xnb = [kb.alloc(f"xnb{i}", [128, D], BF16) for i in range(nx)]
        junk = kb.alloc("junk", [128, D], BF16)
        stat = kb.alloc("stat", [128, 256], F32)
        kb.op("dve", lambda e: e.memset(stat[:], 0.0), writes=[stat])
        self._stat = stat
        return (xb, xnb, junk, stat)

    def dump(self):
        kb = self.kb
        yc = kb.dram("yc", [NS * LC, D], F32, kind="ExternalOutput")
        kb.dma("sp", lambda e: e.dma_start(out=yc.ap[:, :], in_=self.xc_all.ap[0:NS * LC, :]),
               reads=[self.xc_all], writes=[yc])
        for s in range(NS):
            kb.dma("sp", lambda e, s=s: e.dma_start(out=self.out.ap[s], in_=self.xr[s].ap),
                   reads=[self.xr[s]], writes=[self.out.sub(s)])

    def final(self):
        kb = self.kb
        fgb = kb.alloc("fgb", [128, D], F32)
        kb.dma("sp", lambda e: e.dma_start(out=fgb[:], in_=self.final_g.ap.partition_broadcast(128)),
               reads=[self.final_g], writes=[fgb])
        xb = [kb.alloc(f"fxb{i}", [128, D], F32) for i in range(3)]
        yb = [kb.alloc(f"fyb{i}", [128, D], F32) for i in range(3)]
        junk = kb.alloc("fjunk", [128, D], BF16)
        stat = kb.alloc("fstat", [128, 3 * 32], F32)
        kb.op("dve", lambda e: e.memset(stat[:], 0.0), writes=[stat])
        i = 0
        for s in range(NS):
            for t in range(L // 128):
                xt = xb[i % 3]
                yt = yb[i % 3]
                sc = i % 32
                i += 1
                kb.dma("sp", lambda e, xt=xt, s=s, t=t: e.dma_start(out=xt[:], in_=self.xr[s].ap[t * 128:(t + 1) * 128, :]),
                       reads=[self.xr[s]], writes=[xt])
                if i > 32:
                    kb.op("dve", lambda e, sc=sc: e.memset(stat[:, sc:sc + 1], 0.0), writes=[stat.sub(sc)])
                kb.op("act", lambda e, xt=xt, sc=sc: e.activation(
                    out=junk[:], in_=xt[:], func=AF.Square, accum_out=stat[:, sc:sc + 1]),
                    reads=[xt], writes=[junk, stat.sub(sc)])
                kb.op("dve", lambda e, sc=sc: e.tensor_scalar(
                    out=stat[:, 32 + sc:33 + sc], in0=stat[:, sc:sc + 1], scalar1=1.0 / D, scalar2=EPS, op0=ALU.mult, op1=ALU.add),
                    reads=[stat.sub(sc)], writes=[stat.sub(32 + sc)])
                kb.op("act", lambda e, sc=sc: e.activation(
                    out=stat[:, 32 + sc:33 + sc], in_=stat[:, 32 + sc:33 + sc], func=AF.Sqrt),
                    reads=[stat.sub(32 + sc)], writes=[stat.sub(32 + sc)])
                kb.op("dve", lambda e, sc=sc: e.reciprocal(out=stat[:, 64 + sc:65 + sc], in_=stat[:, 32 + sc:33 + sc]),
                      reads=[stat.sub(32 + sc)], writes=[stat.sub(64 + sc)])
                kb.op("dve", lambda e, xt=xt, yt=yt, sc=sc: e.scalar_tensor_tensor(
                    out=yt[:], in0=xt[:], scalar=stat[:, 64 + sc:65 + sc], in1=fgb[:], op0=ALU.mult, op1=ALU.mult),
                    reads=[xt, stat.sub(64 + sc), fgb], writes=[yt])
                kb.dma("sp", lambda e, yt=yt, s=s, t=t: e.dma_start(out=self.out.ap[s, t * 128:(t + 1) * 128, :], in_=yt[:]),
                       reads=[yt], writes=[self.out.sub((s, t))])

    def moe(self, l, with_ctx):
        kb = self.kb
        IOA = bass.IndirectOffsetOnAxis
        wbufs = [[kb.alloc(f"w{n}_{i}", [128, 8, D], BF16) for n in (1, 3, 2)] for i in range(2)]
        wsrc = (self.w1, self.w3, self.w2)

        def load_w(e):
            for n in range(3):
                src = wsrc[n].ap[l, e].rearrange("(k p) f -> p k f", p=128)
                wt = wbufs[e % 2][n]
                kb.dma("pool", lambda en, wt=wt, src=src: en.dma_start(out=wt[:], in_=src),
                       reads=[wsrc[n]], writes=[wt])

        nr = 3 if with_ctx else 2
        g2b = [kb.alloc(f"g2b{r}", [128, D], F32) for r in range(nr)]
        for r in range(nr):
            kb.dma("sp", lambda e, r=r: e.dma_start(
                out=g2b[r][:], in_=self.modd.ap[l, r, 5 * D:6 * D].partition_broadcast(128)),
                reads=[self.modd.sub(l)], writes=[g2b[r]])
        idxT = kb.alloc("idxT", [128, 2, 48], I32)
        gT = kb.alloc("gT", [128, 2, 48], F32)
        idxC = kb.alloc("idxC", [128, NE], I32)
        gC = kb.alloc("gC", [128, NE], F32)
        load_w(0)
        load_w(1)
        addc2 = lambda k, r: self.modc[l][:, 24 + k, r:r + 1]
        mulc2 = self.mul2c[l]

        dbg = getattr(self, "debug_route", 0)
        if dbg == 10:
            return
        m1 = kb.mark()
        bufs = self.norm_bufs(nx=4)
        uTb = [kb.alloc(f"uTb{i}", [128, 8, 256], BF16) for i in range(2)]
        wr = kb.alloc("wr", [128, 8, NE], BF16)
        wrf = kb.alloc("wrf", [128, 8, NE], F32)
        kb.dma("sp", lambda e: e.dma_start(out=wrf[:], in_=self.w_router.ap[l].rearrange("(k p) e -> p k e", p=128)),
               reads=[self.w_router], writes=[wrf])
        kb.op("dve", lambda e: e.tensor_copy(out=wr[:], in_=wrf[:]), reads=[wrf], writes=[wr])
        aff2 = kb.alloc("aff2", [128, 16, 64], F32)
        affc = kb.alloc("affc", [128, 2, 64], F32)
        kb.op("dve", lambda e: e.memset(aff2[:].rearrange("p a b -> p (a b)"), 0.0), writes=[aff2])
        kb.op("dve", lambda e: e.memset(affc[:].rearrange("p a b -> p (a b)"), 0.0), writes=[affc])
        lg = kb.alloc("lg", [128, 16, 16], F32)
        mx = kb.alloc("mx", [128, 16], F32)
        sm = kb.alloc("sm", [128, 16], F32)
        rs = kb.alloc("rs", [128, 16], F32)
        work = kb.alloc("work", [48, L], F32)
        workc = kb.alloc("workc", [48, LC], F32)
        topv = kb.alloc("topv", [48, 256], F32)
        topi = kb.alloc("topi", [48, 256], U32)
        topif = kb.alloc("topif", [48, 256], F32)
        topvc = kb.alloc("topvc", [48, 32], F32)
        topic = kb.alloc("topic", [48, 32], U32)
        topicf = kb.alloc("topicf", [48, 32], F32)
        nbatch = 0
        seqs = []
        for s in range(NS):
            seqs.append((self.xr[s], self.xnl[s], L // 128, s, aff2, s * 32, kb.banks[4 + s], 0))
        if with_ctx:
            for s in range(NS):
                seqs.append((self.xc[s], self.xnc[s], LC // 128, 2, affc, s * 32, kb.banks[6], s * 32))
        for (src, xnd, ntl, r, afft, acol, lbank, lcol0) in seqs:
            for b in range((ntl + 1) // 2):
                nt = min(2, ntl - b * 2)
                ub = uTb[nbatch % 2]
                nbatch += 1
                self.norm_batch(src, b * 256, nt, mulc2, addc2, r, ub, 0, bufs,
                                [kb.banks[j] for j in range(2 * nt)], xn_dst=(xnd, b * 256))
                for j in range(nt):
                    if dbg == 11:
                        continue
                    c = b * 2 + j
                    for k in range(8):
                        kb.op("pe", lambda e, lbank=lbank, lc=lcol0 + c * 16, ub=ub, k=k, j=j: e.matmul(
                            out=lbank.h[:, lc:lc + 16], lhsT=ub[:, k, j * 128:(j + 1) * 128], rhs=wr[:, k, :],
                            start=(k == 0), stop=(k == 7)),
                            reads=[ub.sub((k, j * 128)), wr], writes=[lbank])
            if dbg == 11:
                continue
            lv = lbank.h[:, lcol0:lcol0 + ntl * 16].rearrange("p (c e) -> p c e", e=16)
            bc = lambda t, ntl=ntl: t[:, 0:ntl].unsqueeze(2).to_broadcast([128, ntl, 16])
            kb.op("dve", lambda e, lv=lv, ntl=ntl: e.tensor_reduce(out=mx[:, 0:ntl], in_=lv, axis=AX.X, op=ALU.max),
                  reads=[lbank], writes=[mx])
            kb.op("dve", lambda e, lv=lv, bc=bc, ntl=ntl: e.tensor_tensor(out=lg[:, 0:ntl, :], in0=lv, in1=bc(mx), op=ALU.subtract),
                  reads=[lbank, mx], writes=[lg])
            kb.op("act", lambda e, ntl=ntl: e.activation(out=lg[:, 0:ntl, :], in_=lg[:, 0:ntl, :], func=AF.Exp),
                  reads=[lg], writes=[lg])
            kb.op("dve", lambda e, ntl=ntl: e.tensor_reduce(out=sm[:, 0:ntl], in_=lg[:, 0:ntl, :], axis=AX.X, op=ALU.add),
                  reads=[lg], writes=[sm])
            kb.op("dve", lambda e, ntl=ntl: e.reciprocal(out=rs[:, 0:ntl], in_=sm[:, 0:ntl]), reads=[sm], writes=[rs])
            kb.op("dve", lambda e, afft=afft, acol=acol, bc=bc, ntl=ntl: e.tensor_tensor(
                out=afft[:, 0:ntl, acol:acol + 16], in0=lg[:, 0:ntl, :], in1=bc(rs), op=ALU.mult),
                reads=[lg, rs], writes=[afft])
        if dbg == 11:
            kb.release(m1)
            return
        if dbg == 1:
            da = kb.dram("dbg_aff", [128, 1024], F32, kind="ExternalOutput")
            kb.dma("sp", lambda e: e.dma_start(out=da.ap[:, :], in_=aff2[:].rearrange("p h c -> p (h c)")), reads=[aff2], writes=[da])
            kb.release(m1)
            return
        for c in range(16):
            bank = kb.banks[c // 4]
            kb.op("pe", lambda e, bank=bank, c=c: e.transpose(
                out=bank.h[0:48, (c % 4) * 128:(c % 4 + 1) * 128], in_=aff2[:, c, 0:48], identity=self.identf[:]),
                reads=[aff2, self.identf], writes=[bank])
        for q in range(4):
            eng = "act" if q % 2 == 0 else "dve"
            if eng == "act":
                kb.op("act", lambda e, q=q: e.copy(out=work[0:48, q * 512:(q + 1) * 512], in_=kb.banks[q].h[0:48, 0:512]),
                      reads=[kb.banks[q]], writes=[work.sub(q)])
            else:
                kb.op("dve", lambda e, q=q: e.tensor_copy(out=work[0:48, q * 512:(q + 1) * 512], in_=kb.banks[q].h[0:48, 0:512]),
                      reads=[kb.banks[q]], writes=[work.sub(q)])
        if with_ctx:
            for c in range(2):
                kb.op("pe", lambda e, c=c: e.transpose(
                    out=kb.banks[7].h[0:48, c * 128:(c + 1) * 128], in_=affc[:, c, 0:48], identity=self.identf[:]),
                    reads=[affc, self.identf], writes=[kb.banks[7]])
            kb.op("act", lambda e: e.copy(out=workc[0:48, :], in_=kb.banks[7].h[0:48, 0:256]),
                  reads=[kb.banks[7]], writes=[workc])

        def topk(wk, tv, ti, niter):
            for it in range(niter):
                sl = slice(it * 8, (it + 1) * 8)
                kb.op("dve", lambda e, sl=sl: e.max(out=tv[:, sl], in_=wk[:]), reads=[wk], writes=[tv.sub(it)])
                kb.op("dve", lambda e, sl=sl: e.max_index(out=ti[:, sl], in_max=tv[:, sl], in_values=wk[:]),
                      reads=[wk, tv.sub(it)], writes=[ti.sub(it)])
                kb.op("dve", lambda e, sl=sl: e.match_replace(out=wk[:], in_to_replace=tv[:, sl], in_values=wk[:], imm_value=-1.0),
                      reads=[tv.sub(it), wk], writes=[wk])

        if dbg == 2:
            da = kb.dram("dbg_work", [48, L], F32, kind="ExternalOutput")
            kb.dma("sp", lambda e: e.dma_start(out=da.ap[:, :], in_=work[:]), reads=[work], writes=[da])
            kb.release(m1)
            return
        topk(work, topv, topi, 32)
        if dbg == 3:
            da = kb.dram("dbg_topv", [48, 256], F32, kind="ExternalOutput")
            kb.dma("sp", lambda e: e.dma_start(out=da.ap[:, :], in_=topv[:]), reads=[topv], writes=[da])
            db_ = kb.dram("dbg_topi", [48, 256], U32, kind="ExternalOutput")
            kb.dma("sp", lambda e: e.dma_start(out=db_.ap[:, :], in_=topi[:]), reads=[topi], writes=[db_])
            kb.release(m1)
            return
        kb.op("dve", lambda e: e.tensor_copy(out=topif[:], in_=topi[:]), reads=[topi], writes=[topif])
        tb = kb.banks[5]
        for h in range(2):
            kb.op("pe", lambda e, h=h: e.transpose(out=tb.h[:, h * 48:(h + 1) * 48], in_=topif[0:48, h * 128:(h + 1) * 128],
                                                   identity=self.identf[0:48, 0:48]),
                  reads=[topif, self.identf], writes=[tb])
            kb.op("pe", lambda e, h=h: e.transpose(out=tb.h[:, 128 + h * 48:128 + (h + 1) * 48], in_=topv[0:48, h * 128:(h + 1) * 128],
                                                   identity=self.identf[0:48, 0:48]),
                  reads=[topv, self.identf], writes=[tb])
        kb.op("dve", lambda e: e.tensor_copy(out=idxT[:], in_=tb.h[:, 0:96].rearrange("p (h c) -> p h c", h=2)),
              reads=[tb], writes=[idxT])
        kb.op("dve", lambda e: e.tensor_copy(out=gT[:], in_=tb.h[:, 128:224].rearrange("p (h c) -> p h c", h=2)),
              reads=[tb], writes=[gT])
        if with_ctx:
            topk(workc, topvc, topic, 4)
            kb.op("dve", lambda e: e.tensor_copy(out=topicf[:], in_=topic[:]), reads=[topic], writes=[topicf])
            cbase = kb.alloc("cbase", [128, 1], F32)
            kb.dma("sp", lambda e: e.dma_start(out=cbase[:], in_=self.c_ctxbase.ap[:, :]), reads=[self.c_ctxbase], writes=[cbase])
            for (srcT, dstT, isidx) in ((topicf, idxC, True), (topvc, gC, False)):
                M = kb.alloc("Mc", [48, 128], F32)
                kb.op("dve", lambda e, M=M: e.memset(M[:], 0.0), writes=[M])
                kb.op("dve", lambda e, M=M, srcT=srcT: e.tensor_copy(out=M[0:16, 0:32], in_=srcT[0:16, 0:32]), reads=[srcT], writes=[M])
                kb.op("dve", lambda e, M=M, srcT=srcT: e.tensor_copy(out=M[32:48, 32:64], in_=srcT[32:48, 0:32]), reads=[srcT], writes=[M])
                kb.op("pe", lambda e, M=M: e.transpose(out=tb.h[:, 256:304], in_=M[0:48, :], identity=self.identf[0:48, 0:48]),
                      reads=[M, self.identf], writes=[tb])
                tcp = kb.alloc("tcp", [128, 48], F32)
                kb.op("dve", lambda e, tcp=tcp: e.tensor_copy(out=tcp[:], in_=tb.h[:, 256:304]), reads=[tb], writes=[tcp])
                tsum = kb.alloc("tsum", [128, NE], F32)
                kb.op("dve", lambda e, tcp=tcp, tsum=tsum: e.tensor_tensor(out=tsum[:], in0=tcp[:, 0:16], in1=tcp[:, 32:48], op=ALU.add),
                      reads=[tcp], writes=[tsum])
                if isidx:
                    kb.op("dve", lambda e, tsum=tsum: e.tensor_scalar(out=tsum[:], in0=tsum[:], scalar1=cbase[:, 0:1], scalar2=None, op0=ALU.add),
                          reads=[tsum, cbase], writes=[tsum])
                kb.op("dve", lambda e, tsum=tsum, dstT=dstT: e.tensor_copy(out=dstT[:], in_=tsum[:]), reads=[tsum], writes=[dstT])
        if dbg == 4:
            di = kb.dram("dbg_idx", [128, 96], I32, kind="ExternalOutput")
            dg = kb.dram("dbg_g", [128, 96], F32, kind="ExternalOutput")
            da = kb.dram("dbg_aff", [128, 1024], F32, kind="ExternalOutput")
            kb.dma("sp", lambda e: e.dma_start(out=di.ap[:, :], in_=idxT[:].rearrange("p h c -> p (h c)")), reads=[idxT], writes=[di])
            kb.dma("sp", lambda e: e.dma_start(out=dg.ap[:, :], in_=gT[:].rearrange("p h c -> p (h c)")), reads=[gT], writes=[dg])
            kb.dma("sp", lambda e: e.dma_start(out=da.ap[:, :], in_=aff2[:].rearrange("p h c -> p (h c)")), reads=[aff2], writes=[da])
            kb.release(m1)
            return
        kb.release(m1)

        G = []
        for s in range(NS):
            for h in range(2):
                G.append(dict(co=s * 256 + h * 128, idx=lambda e, s=s, h=h: idxT[:, h, s * 32 + e:s * 32 + e + 1],
                              gate=lambda e, s=s, h=h: gT[:, h, s * 32 + e:s * 32 + e + 1], r=s,
                              xn=self.xnl[s], dst=self.xr[s], n=L))
        if with_ctx:
            G.append(dict(co=512, idx=lambda e: idxC[:, e:e + 1], gate=lambda e: gC[:, e:e + 1], r=2,
                          xn=self.xnc_all, dst=self.xc_all, n=NS * LC + 128))
        NSL = 128 * len(G)
        HW = NSL // 2
        halves = [(0, HW), (HW, HW)]
        Xg = [[kb.alloc(f"xg{i}_{gi}", [128, D], BF16) for gi in range(len(G))] for i in range(2)]
        XeT = [kb.alloc(f"xeT{i}", [128, 8, NSL], BF16) for i in range(2)]
        hidT = kb.alloc("hidT", [128, 8, NSL], BF16)
        sgt = [kb.alloc(f"sgt{i}", [128, HW], F32) for i in range(2)]
        yo = [kb.alloc(f"yo{i}", [128, D], F32) for i in range(3)]
        nyo = 0
        nyb = 0

        def gathers(e):
            for gi, g in enumerate(G):
                xg = Xg[e % 2][gi]
                kb.dma("pool", lambda en, xg=xg, g=g, e=e: en.indirect_dma_start(
                    out=xg[:], out_offset=None, in_=g["xn"].ap[:, :], in_offset=IOA(ap=g["idx"](e), axis=0)),
                    reads=[g["xn"], idxT, idxC], writes=[xg])

        gathers(0)
        for e in range(NE):
            if e + 1 < NE:
                gathers(e + 1)
            w1t, w3t, w2t = wbufs[e % 2]
            xe = XeT[e % 2]
            for gi, g in enumerate(G):
                co, r = g["co"], g["r"]
                xg = Xg[e % 2][gi]
                for k in range(8):
                    bank = kb.banks[6 + k // 4]
                    pv = bank.h.bitcast(BF16)
                    kk = k % 4
                    kb.op("pe", lambda en, pv=pv, xg=xg, k=k, kk=kk: en.transpose(
                        out=pv[:, kk * 128:(kk + 1) * 128], in_=xg[:, k * 128:(k + 1) * 128],
                        identity=self.identb[:]), reads=[xg, self.identb], writes=[bank])
                for k in range(8):
                    bank = kb.banks[6 + k // 4]
                    pv = bank.h.bitcast(BF16)
                    kk = k % 4
                    dst = xe[:, k, co:co + 128]
                    if k < 4:
                        kb.op("act", lambda en, dst=dst, pv=pv, kk=kk, r=r, k=k: en.activation(
                            out=dst, in_=pv[:, kk * 128:(kk + 1) * 128], func=AF.Identity,
                            scale=mulc2[:, k, r:r + 1], bias=addc2(k, r)),
                            reads=[bank], writes=[xe.sub((k, co))])
                    else:
                        kb.op("dve", lambda en, dst=dst, pv=pv, kk=kk, r=r, k=k: en.tensor_scalar(
                            out=dst, in0=pv[:, kk * 128:(kk + 1) * 128], scalar1=mulc2[:, k, r:r + 1],
                            scalar2=addc2(k, r), op0=ALU.mult, op1=ALU.add),
                            reads=[bank], writes=[xe.sub((k, co))])
            for fc in range(8):
                for hi, (h0, hw) in enumerate(halves):
                    b1 = kb.banks[hi]
                    b3 = kb.banks[2 + hi]
                    for (wt, bm) in ((w1t, b1), (w3t, b3)):
                        for k in range(8):
                            kb.op("pe", lambda en, wt=wt, bm=bm, k=k, fc=fc, xe=xe, h0=h0, hw=hw: en.matmul(
                                out=bm.h[:, 0:hw], lhsT=wt[:, k, fc * 128:(fc + 1) * 128], rhs=xe[:, k, h0:h0 + hw],
                                start=(k == 0), stop=(k == 7)), reads=[wt, xe], writes=[bm])
                    sg = sgt[hi]
                    kb.op("act", lambda en, sg=sg, b1=b1, hw=hw: en.activation(out=sg[:, 0:hw], in_=b1.h[:, 0:hw], func=AF.Silu),
                          reads=[b1], writes=[sg])
                    kb.op("dve", lambda en, sg=sg, b3=b3, fc=fc, h0=h0, hw=hw: en.tensor_tensor(
                        out=hidT[:, fc, h0:h0 + hw], in0=sg[:, 0:hw], in1=b3.h[:, 0:hw], op=ALU.mult),
                        reads=[sg, b3], writes=[hidT.sub((fc, h0))])
            for gi, g in enumerate(G):
                co, r = g["co"], g["r"]
                yt = yo[nyo % 3]
                nyo += 1
                for db in range(2):
                    by = kb.banks[4 + nyb % 2]
                    nyb += 1
                    for fc in range(8):
                        kb.op("pe", lambda en, by=by, fc=fc, co=co, db=db, w2t=w2t: en.matmul(
                            out=by.h[:, 0:512], lhsT=hidT[:, fc, co:co + 128], rhs=w2t[:, fc, db * 512:(db + 1) * 512],
                            start=(fc == 0), stop=(fc == 7)), reads=[hidT, w2t], writes=[by])
                    kb.op("dve", lambda en, by=by, yt=yt, db=db, g=g, e=e, r=r: en.scalar_tensor_tensor(
                        out=yt[:, db * 512:(db + 1) * 512], in0=by.h[:, 0:512], scalar=g["gate"](e),
                        in1=g2b[r][:, db * 512:(db + 1) * 512], op0=ALU.mult, op1=ALU.mult),
                        reads=[by, gT, gC, g2b[r]], writes=[yt.sub(db)])
                kb.dma("pool", lambda en, yt=yt, g=g, e=e: en.indirect_dma_start(
                    out=g["dst"].ap[:, :], out_offset=IOA(ap=g["idx"](e), axis=0), in_=yt[:, :], in_offset=None,
                    compute_op=ALU.add, bounds_check=kb.bnd[g["n"] - 1], oob_is_err=True),
                    reads=[yt, idxT, idxC], writes=[g["dst"]])
            if e + 2 < NE:
                load_w(e + 2)

    def mixer0(self):
        kb = self.kb
        l = 0
        wbuf = kb.alloc("wbuf", [128, 8, 1536], BF16)
        wout = wbuf.view("woutv", [128, 8, D], BF16)
        csc = kb.alloc("csc", [128, 256], BF16)
        kb.dma("sp", lambda e: e.dma_start(out=csc[:], in_=self.c_csc.ap[:, :]), reads=[self.c_csc], writes=[csc])
        cwc = kb.alloc("cwc", [128, 4, 31], F32)
        for j in range(4):
            kb.dma("sp", lambda e, j=j: e.dma_start(out=cwc[:, j, :], in_=self.ab_conv_w.ap[:, j * 128:(j + 1) * 128].rearrange("k p -> p k")),
                   reads=[self.ab_conv_w], writes=[cwc.sub(j)])
        cols = {}
        for nm, dr in (("cb", self.ab_conv_b), ("lg", self.ab_ln_g), ("lb", self.ab_ln_b)):
            t = kb.alloc(nm + "c", [128, 4], F32)
            kb.dma("sp", lambda e, t=t, dr=dr: e.dma_start(out=t[:], in_=dr.ap.rearrange("(j p) -> p j", p=128)), reads=[dr], writes=[t])
            cols[nm] = t
        onesb = kb.alloc("onesb", [128, 128], BF16)
        kb.op("dve", lambda e: e.memset(onesb[:], 1.0 / 512.0), writes=[onesb])
        diag = kb.alloc("diag", [128, 4 * 31, 128], BF16)
        for j in range(4):
            for k in range(31):
                kb.op("dve", lambda e, j=j, k=k: e.tensor_scalar(
                    out=diag[:, j * 31 + k, :], in0=self.identb[:], scalar1=cwc[:, j, k:k + 1], scalar2=None, op0=ALU.mult),
                    reads=[self.identb, cwc], writes=[diag.sub((j, k))])
        bufs = self.norm_bufs(nx=2)
        xb = bufs[0][0]
        uTb = kb.alloc("uTb", [128, 8, 512], BF16)
        aT = kb.alloc("aT", [128, 4, L + 32], BF16)
        ufTb = kb.alloc("ufTb", [128, 4, 512], BF16)
        Yall = kb.alloc("Yall", [128, 16, 1024], BF16)
        mixT = kb.alloc("mixT", [128, 8, L], BF16)
        clb = kb.alloc("clb", [128, 16, 256], BF16)
        slb = kb.alloc("slb", [128, 16, 256], BF16)
        g1b = kb.alloc("g1b", [128, D], F32)
        cl2h = aT.view("cl2", [128, 16, 256], BF16, boff=0)
        sl2h = aT.view("sl2", [128, 16, 256], BF16, boff=8192)
        NBC = 256
        cT = kb.alloc("cT", [128, 4, NBC], F32)
        cbt = kb.alloc("cbt", [128, 4, NBC], BF16)
        c2t = kb.alloc("c2t", [128, 4, NBC], BF16)
        t_mean = kb.alloc("t_mean", [128, NBC], F32)
        t_rstd = kb.alloc("t_rstd", [128, NBC], F32)
        t_tmp = kb.alloc("t_tmp", [128, 512], F32)
        t_tmp2 = kb.alloc("t_tmp2", [128, NBC], F32)
        addc1 = lambda k, r: self.modc[l][:, k, r:r + 1]
        mulc1 = self.mul1c[l]
        B = kb.banks
        seqs = [(self.xin[0], self.xr[0], L, 0, self.c_cl, self.c_sl), (self.xin[1], self.xr[1], L, 1, self.c_cl, self.c_sl),
                (self.cin[0], self.xc[0], LC, 2, self.c_clc, self.c_slc), (self.cin[1], self.xc[1], LC, 2, self.c_clc, self.c_slc)]
        for (src, dst, Ls, r, ctab, stab) in seqs:
            kb.dma("pool", lambda e: e.dma_start(out=wbuf[:], in_=self.ab_w_in.ap.rearrange("(k p) f -> p k f", p=128)),
                   reads=[self.ab_w_in], writes=[wbuf])
            kb.dma("sp", lambda e, r=r: e.dma_start(out=g1b[:], in_=self.modd.ap[l, r, 2 * D:3 * D].partition_broadcast(128)),
                   reads=[self.modd.sub(l)], writes=[g1b])
            for j in range(4):
                kb.op("dve", lambda e, j=j: e.memset(aT[:, j, 0:15], 0.0), writes=[aT.sub((j, "h0"))])
                kb.op("dve", lambda e, j=j, Ls=Ls: e.memset(aT[:, j, 15 + Ls:32 + Ls], 0.0), writes=[aT.sub((j, "h1"))])
            nb = min(512, Ls)
            for blk in range(Ls // nb):
                t0 = blk * nb
                for sb in range(nb // 256):
                    self.norm_batch(src, t0 + sb * 256, 2, mulc1, addc1, r, uTb, sb * 256, bufs, [B[0], B[1], B[2], B[3]])
                def zmm(j, bank):
                    for k in range(8):
                        kb.op("pe", lambda e, j=j, k=k, bank=bank, nb=nb: e.matmul(
                            out=bank.h[:, 0:nb], lhsT=wbuf[:, k, j * 128:(j + 1) * 128], rhs=uTb[:, k, 0:nb],
                            start=(k == 0), stop=(k == 7)), reads=[wbuf, uTb], writes=[bank])
                for jj in range(4):
                    zmm(jj, B[4])
                    zmm(jj + 4, B[5])
                    kb.op("act", lambda e, nb=nb: e.activation(out=t_tmp[:, 0:nb], in_=B[5].h[:, 0:nb], func=AF.Sigmoid),
                          reads=[B[5]], writes=[t_tmp])
                    kb.op("dve", lambda e, jj=jj, nb=nb, t0=t0: e.tensor_tensor(
                        out=aT[:, jj, 15 + t0:15 + t0 + nb], in0=t_tmp[:, 0:nb], in1=B[4].h[:, 0:nb], op=ALU.mult),
                        reads=[t_tmp, B[4]], writes=[aT.sub((jj, t0))])
                for g in range(4):
                    zmm(8 + g, B[6])
                    kb.op("act", lambda e, g=g, nb=nb: e.copy(out=ufTb[:, g, 0:nb], in_=B[6].h[:, 0:nb]),
                          reads=[B[6]], writes=[ufTb.sub(g)])
                for tc in range(nb // 128):
                    c = (t0 // 128) + tc
                    for gp in range(2):
                        for gg in range(2):
                            g = gp * 2 + gg
                            kb.op("pe", lambda e, g=g, gg=gg, tc=tc: e.matmul(
                                out=B[7].h[:, gg * 256:(gg + 1) * 256], lhsT=ufTb[:, g, tc * 128:(tc + 1) * 128], rhs=csc[:, :],
                                start=True, stop=True), reads=[ufTb, csc], writes=[B[7]])
                        kb.op("dve", lambda e, c=c, gp=gp: e.tensor_copy(out=Yall[:, c, gp * 512:(gp + 1) * 512], in_=B[7].h[:, 0:512]),
                              reads=[B[7]], writes=[Yall.sub((c, gp))])
            nbc = min(NBC, Ls)
            for blk in range(Ls // nbc):
                t0 = blk * nbc
                for j in range(4):
                    bank = B[j % 2]
                    for k in range(31):
                        kb.op("pe", lambda e, j=j, k=k, bank=bank, t0=t0, nbc=nbc: e.matmul(
                            out=bank.h[:, 0:nbc], lhsT=diag[:, j * 31 + k, :], rhs=aT[:, j, t0 + k:t0 + k + nbc],
                            start=(k == 0), stop=(k == 30)), reads=[diag, aT], writes=[bank])
                    kb.op("act", lambda e, j=j, bank=bank, nbc=nbc: e.activation(
                        out=cT[:, j, 0:nbc], in_=bank.h[:, 0:nbc], func=AF.Identity, bias=cols["cb"][:, j:j + 1]),
                        reads=[bank, cols["cb"]], writes=[cT.sub(j)])
                    kb.op("pool", lambda e, j=j, nbc=nbc: e.tensor_copy(out=cbt[:, j, 0:nbc], in_=cT[:, j, 0:nbc]),
                          reads=[cT.sub(j)], writes=[cbt.sub(j)])
                    kb.op("pool", lambda e, j=j, nbc=nbc: e.tensor_tensor(out=c2t[:, j, 0:nbc], in0=cT[:, j, 0:nbc], in1=cT[:, j, 0:nbc], op=ALU.mult),
                          reads=[cT.sub(j)], writes=[c2t.sub(j)])
                for j in range(4):
                    kb.op("pe", lambda e, j=j, nbc=nbc: e.matmul(out=B[2].h[:, 0:nbc], lhsT=onesb[:], rhs=cbt[:, j, 0:nbc],
                                                                 start=(j == 0), stop=(j == 3)), reads=[onesb, cbt], writes=[B[2]])
                for j in range(4):
                    kb.op("pe", lambda e, j=j, nbc=nbc: e.matmul(out=B[3].h[:, 0:nbc], lhsT=onesb[:], rhs=c2t[:, j, 0:nbc],
                                                                 start=(j == 0), stop=(j == 3)), reads=[onesb, c2t], writes=[B[3]])
                kb.op("dve", lambda e, nbc=nbc: e.tensor_copy(out=t_mean[:, 0:nbc], in_=B[2].h[:, 0:nbc]), reads=[B[2]], writes=[t_mean])
                kb.op("dve", lambda e, nbc=nbc: e.tensor_tensor(out=t_tmp2[:, 0:nbc], in0=t_mean[:, 0:nbc], in1=t_mean[:, 0:nbc], op=ALU.mult),
                      reads=[t_mean], writes=[t_tmp2])
                kb.op("dve", lambda e, nbc=nbc: e.tensor_tensor(out=t_rstd[:, 0:nbc], in0=B[3].h[:, 0:nbc], in1=t_tmp2[:, 0:nbc], op=ALU.subtract),
                      reads=[B[3], t_tmp2], writes=[t_rstd])
                kb.op("act", lambda e, nbc=nbc: e.activation(out=t_rstd[:, 0:nbc], in_=t_rstd[:, 0:nbc], func=AF.Identity, bias=self.epsc[:, 0:1]),
                      reads=[t_rstd, self.epsc], writes=[t_rstd])
                kb.op("act", lambda e, nbc=nbc: e.activation(out=t_rstd[:, 0:nbc], in_=t_rstd[:, 0:nbc], func=AF.Sqrt),
                      reads=[t_rstd], writes=[t_rstd])
                kb.op("dve", lambda e, nbc=nbc: e.reciprocal(out=t_rstd[:, 0:nbc], in_=t_rstd[:, 0:nbc]), reads=[t_rstd], writes=[t_rstd])
                for j in range(4):
                    kb.op("pool", lambda e, j=j, nbc=nbc: e.tensor_tensor(out=cT[:, j, 0:nbc], in0=cT[:, j, 0:nbc], in1=t_mean[:, 0:nbc], op=ALU.subtract),
                          reads=[cT.sub(j), t_mean], writes=[cT.sub(j)])
                    kb.op("dve", lambda e, j=j, nbc=nbc: e.tensor_tensor(out=cT[:, j, 0:nbc], in0=cT[:, j, 0:nbc], in1=t_rstd[:, 0:nbc], op=ALU.mult),
                          reads=[cT.sub(j), t_rstd], writes=[cT.sub(j)])
                    kb.op("act", lambda e, j=j, nbc=nbc, t0=t0: e.activation(
                        out=mixT[:, j, t0:t0 + nbc], in_=cT[:, j, 0:nbc], func=AF.Silu,
                        scale=cols["lg"][:, j:j + 1], bias=cols["lb"][:, j:j + 1]),
                        reads=[cT.sub(j), cols["lg"], cols["lb"]], writes=[mixT.sub((j, t0))])
            ntc = Ls // 128
            for kbi in range(Ls // 256):
                k0 = kbi * 256
                if kbi % 2 == 0:
                    clh, slh, trc, trs = clb.h, slb.h, clb, slb
                else:
                    clh, slh, trc, trs = cl2h, sl2h, aT, aT
                kb.dma("sp", lambda e, ctab=ctab, k0=k0, ntc=ntc, clh=clh: e.dma_start(
                    out=clh[:, 0:ntc, :], in_=ctab.ap.rearrange("(c p) k -> p c k", p=128)[:, :, k0:k0 + 256]),
                    reads=[ctab], writes=[trc])
                kb.dma("sp", lambda e, stab=stab, k0=k0, ntc=ntc, slh=slh: e.dma_start(
                    out=slh[:, 0:ntc, :], in_=stab.ap.rearrange("(c p) k -> p c k", p=128)[:, :, k0:k0 + 256]),
                    reads=[stab], writes=[trs])
                for g in range(4):
                    bank = B[4 + g % 2]
                    for c in range(ntc):
                        kb.op("pe", lambda e, g=g, c=c, bank=bank, clh=clh: e.matmul(
                            out=bank.h[:, 0:256], lhsT=Yall[:, c, g * 256:g * 256 + 128], rhs=clh[:, c, :],
                            start=(c == 0), stop=False), reads=[Yall, trc], writes=[bank])
                        kb.op("pe", lambda e, g=g, c=c, bank=bank, ntc=ntc, slh=slh: e.matmul(
                            out=bank.h[:, 0:256], lhsT=Yall[:, c, g * 256 + 128:g * 256 + 256], rhs=slh[:, c, :],
                            start=False, stop=(c == ntc - 1)), reads=[Yall, trs], writes=[bank])
                    if g % 2 == 0:
                        kb.op("act", lambda e, g=g, bank=bank, k0=k0: e.copy(out=mixT[:, 4 + g, k0:k0 + 256], in_=bank.h[:, 0:256]),
                              reads=[bank], writes=[mixT.sub((4 + g, k0))])
                    else:
                        kb.op("dve", lambda e, g=g, bank=bank, k0=k0: e.tensor_copy(out=mixT[:, 4 + g, k0:k0 + 256], in_=bank.h[:, 0:256]),
                              reads=[bank], writes=[mixT.sub((4 + g, k0))])
            kb.dma("pool", lambda e: e.dma_start(out=wout[:], in_=self.ab_w_out.ap.rearrange("(k p) f -> p k f", p=128)),
                   reads=[self.ab_w_out], writes=[wbuf])
            for tc in range(ntc):
                kb.dma("sp", lambda e, src=src, tc=tc: e.dma_start(out=xb[:], in_=src.ap[tc * 128:(tc + 1) * 128, :]),
                       reads=[src], writes=[xb])
                for db in range(2):
                    bank = B[6 + db]
                    for m in range(8):
                        kb.op("pe", lambda e, m=m, db=db, bank=bank, tc=tc: e.matmul(
                            out=bank.h[:, 0:512], lhsT=mixT[:, m, tc * 128:(tc + 1) * 128], rhs=wout[:, m, db * 512:(db + 1) * 512],
                            start=(m == 0), stop=(m == 7)), reads=[mixT, wbuf], writes=[bank])
                    kb.op("dve", lambda e, db=db, bank=bank: e.tensor_tensor(
                        out=t_tmp[:, 0:512], in0=bank.h[:, 0:512], in1=g1b[:, db * 512:(db + 1) * 512], op=ALU.mult),
                        reads=[bank, g1b], writes=[t_tmp])
                    kb.op("pool", lambda e, db=db: e.tensor_tensor(
                        out=xb[:, db * 512:(db + 1) * 512], in0=xb[:, db * 512:(db + 1) * 512], in1=t_tmp[:, 0:512], op=ALU.add),
                        reads=[xb, t_tmp], writes=[xb])
                kb.dma("sp", lambda e, dst=dst, tc=tc: e.dma_start(out=dst.ap[tc * 128:(tc + 1) * 128, :], in_=xb[:]),
                       reads=[xb], writes=[dst.sub(("row", tc))])


    def mla(self):
        kb = self.kb
        l = 1
        B = kb.banks
        NKV = LC + L
        NT = NKV // 128
        SCALE = 96.0 ** -0.5
        cast = lambda dst, src_ap, tr: kb.dma("pool", lambda e: e.dma_start(out=dst[:], in_=src_ap), reads=[tr], writes=[dst])
        wi = kb.alloc("wi", [128, 8, 416], BF16)
        cast(wi, self.mla_w_in.ap.rearrange("(k p) f -> p k f", p=128), self.mla_w_in)
        wuq = kb.alloc("wuq", [128, 2, 1536], BF16)
        cast(wuq, self.mla_w_uq.ap.rearrange("(k p) f -> p k f", p=128), self.mla_w_uq)
        wukv = kb.alloc("wukv", [128, 2048], BF16)
        cast(wukv, self.mla_w_ukv.ap[:, :], self.mla_w_ukv)
        wo = kb.alloc("wo", [128, 8, D], BF16)
        cast(wo, self.mla_w_o.ap.rearrange("(k p) f -> p k f", p=128), self.mla_w_o)
        ropt = kb.alloc("ropt", [128, 16, 32], F32)
        kb.dma("sp", lambda e: e.dma_start(out=ropt[:], in_=self.c_rope.ap.rearrange("(c p) f -> p c f", p=128)),
               reads=[self.c_rope], writes=[ropt])
        qgc = kb.alloc("qgc", [128, 2], F32)
        kb.dma("sp", lambda e: e.dma_start(out=qgc[:], in_=self.mla_qg.ap.rearrange("(j p) -> p j", p=128)), reads=[self.mla_qg], writes=[qgc])
        kvgc = kb.alloc("kvgc", [128, 1], F32)
        kb.dma("sp", lambda e: e.dma_start(out=kvgc[:], in_=self.mla_kvg.ap.rearrange("(j p) -> p j", p=128)), reads=[self.mla_kvg], writes=[kvgc])
        ones1 = kb.alloc("ones1", [128, 128], BF16)
        kb.op("dve", lambda e: e.memset(ones1[:], 1.0), writes=[ones1])
        bufs = self.norm_bufs(nx=2)
        xb = bufs[0][0]
        uT = kb.alloc("uTall", [128, 8, NKV], BF16)
        cqnT = kb.alloc("cqnT", [128, 2, L], BF16)
        ckvnT = kb.alloc("ckvnT", [128, NKV], BF16)
        KTs = [kb.alloc(f"KT{i}", [128, NKV], BF16) for i in range(2)]
        QTs = [kb.alloc(f"QT{i}", [128, L], BF16) for i in range(2)]
        qtoks = [kb.alloc(f"qtok{i}", [128, 8, 96], BF16) for i in range(2)]
        Vaug = [kb.alloc(f"Vaug{i}", [128, NT, 128], BF16) for i in range(2)]
        kb.op("dve", lambda e: e.memset(Vaug[0][:].rearrange("p a b -> p (a b)"), 1.0), writes=[Vaug[0]])
        kb.op("dve", lambda e: e.memset(Vaug[1][:].rearrange("p a b -> p (a b)"), 1.0), writes=[Vaug[1]])
        pT = [kb.alloc(f"pT{i}", [128, 1024], BF16) for i in range(3)]
        oTs = kb.alloc("oTs", [128, 512], F32)
        onesf = kb.alloc("onesf", [128, 512], F32)
        kb.op("dve", lambda e: e.memset(onesf[:], 1.0), writes=[onesf])
        recf = kb.alloc("recf", [128, 512], F32)
        rech = kb.alloc("rech", [128, 512], BF16)
        recl = kb.alloc("recl", [128, 512], BF16)
        attnT = kb.alloc("attnT", [128, 8, L], BF16)
        g1b = kb.alloc("g1bm", [128, D], F32)
        t_tmp = kb.alloc("t_tmpm", [128, 512], F32)
        zts = [kb.alloc(f"zt{i}", [128, 416], F32) for i in range(2)]
        cqns = [kb.alloc(f"cqn{i}", [128, 256], BF16) for i in range(2)]
        ckvns = [kb.alloc(f"ckvn{i}", [128, 128], BF16) for i in range(2)]
        ktoks = [kb.alloc(f"ktok{i}", [128, 96], BF16) for i in range(2)]
        for kt_ in ktoks:
            kb.op("dve", lambda e, kt_=kt_: e.memset(kt_[:], 0.0), writes=[kt_])
        rtk = [[kb.alloc(f"rtk{i}_{j}", [128, 16], F32) for j in range(4)] for i in range(2)]
        rt = [kb.alloc(f"rt{i}", [128, 4, 16], F32) for i in range(4)]
        st2 = kb.alloc("st2", [128, 8 * NT], F32)
        kb.op("dve", lambda e: e.memset(st2[:], 0.0), writes=[st2])
        junk2 = kb.alloc("junk2", [128, 256], BF16)
        addc1 = lambda k, r: self.modc[l][:, k, r:r + 1]
        mulc1 = self.mul1c[l]
        npt = 0
        for s in range(NS):
            kb.dma("sp", lambda e, s=s: e.dma_start(out=g1b[:], in_=self.modd.ap[l, s, 2 * D:3 * D].partition_broadcast(128)),
                   reads=[self.modd.sub(l)], writes=[g1b])
            kb.op("dve", lambda e: e.memset(st2[:], 0.0), writes=[st2])
            for b2 in range(LC // 256):
                self.norm_batch(self.xc[s], b2 * 256, 2, mulc1, addc1, 2, uT, b2 * 256, bufs, [B[0], B[1], B[2], B[3]])
            for b2 in range(L // 256):
                self.norm_batch(self.xr[s], b2 * 256, 2, mulc1, addc1, s, uT, LC + b2 * 256, bufs, [B[0], B[1], B[2], B[3]])
            for c in range(NT):
                lat = c >= 2
                cl_ = c - 2
                zt, cqn, ckvn, ktok = zts[c % 2], cqns[c % 2], ckvns[c % 2], ktoks[c % 2]
                rk = rtk[c % 2]
                zb = B[4 + c % 2]
                for k in range(8):
                    kb.op("pe", lambda e, c=c, k=k, zb=zb: e.matmul(out=zb.h[:, 0:416], lhsT=uT[:, k, c * 128:(c + 1) * 128], rhs=wi[:, k, :],
                                                                  start=(k == 0), stop=(k == 7)), reads=[uT, wi], writes=[zb])
                kb.op("act", lambda e, zb=zb, zt=zt: e.copy(out=zt[:], in_=zb.h[:, 0:416]), reads=[zb], writes=[zt])
                so = c * 8
                parts = [(256, 128, 128.0, ckvn, 0)] + ([(0, 256, 256.0, cqn, 4)] if lat else [])
                for (c0, n, nf, dstt, o) in parts:
                    kb.op("act", lambda e, c0=c0, n=n, so=so, o=o, zt=zt: e.activation(out=junk2[:, 0:n], in_=zt[:, c0:c0 + n], func=AF.Square,
                                                                             accum_out=st2[:, so + o:so + o + 1]), reads=[zt], writes=[junk2, st2.sub(so + o)])
                    kb.op("act", lambda e, so=so, o=o, nf=nf: e.activation(out=st2[:, so + o + 1:so + o + 2], in_=st2[:, so + o:so + o + 1], func=AF.Identity,
                                                                         scale=1.0 / nf, bias=self.epsc[:, 0:1]), reads=[st2.sub(so + o), self.epsc], writes=[st2.sub(so + o + 1)])
                    kb.op("act", lambda e, so=so, o=o: e.activation(out=st2[:, so + o + 2:so + o + 3], in_=st2[:, so + o + 1:so + o + 2], func=AF.Sqrt),
                          reads=[st2.sub(so + o + 1)], writes=[st2.sub(so + o + 2)])
                    kb.op("dve", lambda e, so=so, o=o: e.reciprocal(out=st2[:, so + o + 3:so + o + 4], in_=st2[:, so + o + 2:so + o + 3]),
                          reads=[st2.sub(so + o + 2)], writes=[st2.sub(so + o + 3)])
                    kb.op("act", lambda e, c0=c0, n=n, so=so, o=o, dstt=dstt, zt=zt: e.activation(out=dstt[:, 0:n], in_=zt[:, c0:c0 + n], func=AF.Identity,
                                                                                          scale=st2[:, so + o + 3:so + o + 4]), reads=[zt, st2.sub(so + o + 3)], writes=[dstt])
                if lat:
                    krv = zt[:, 384:416].rearrange("p (i two) -> p i two", two=2)
                    xe, xo = krv[:, :, 0], krv[:, :, 1]
                    cs, sn = ropt[:, cl_, 0:16], ropt[:, cl_, 16:32]
                    ko = ktok[:, 64:96].rearrange("p (i two) -> p i two", two=2)
                    a0, a1, a2, a3 = rk[0][:, :], rk[1][:, :], rk[2][:, :], rk[3][:, :]
                    kb.op("pool", lambda e, xe=xe, cs=cs, a0=a0: e.tensor_tensor(out=a0, in0=xe, in1=cs, op=ALU.mult), reads=[zt, ropt], writes=[rk[0]])
                    kb.op("pool", lambda e, xo=xo, sn=sn, a1=a1: e.tensor_tensor(out=a1, in0=xo, in1=sn, op=ALU.mult), reads=[zt, ropt], writes=[rk[1]])
                    kb.op("pool", lambda e, xe=xe, sn=sn, a2=a2: e.tensor_tensor(out=a2, in0=xe, in1=sn, op=ALU.mult), reads=[zt, ropt], writes=[rk[2]])
                    kb.op("pool", lambda e, xo=xo, cs=cs, a3=a3: e.tensor_tensor(out=a3, in0=xo, in1=cs, op=ALU.mult), reads=[zt, ropt], writes=[rk[3]])
                    kb.op("pool", lambda e, ko=ko, a0=a0, a1=a1: e.tensor_tensor(out=ko[:, :, 0], in0=a0, in1=a1, op=ALU.subtract), reads=[rk[0], rk[1]], writes=[ktok.sub(0)])
                    kb.op("pool", lambda e, ko=ko, a2=a2, a3=a3: e.tensor_tensor(out=ko[:, :, 1], in0=a2, in1=a3, op=ALU.add), reads=[rk[2], rk[3]], writes=[ktok.sub(1)])
                else:
                    kb.op("pool", lambda e, ktok=ktok, zt=zt: e.tensor_copy(out=ktok[:, 64:96], in_=zt[:, 384:416]), reads=[zt], writes=[ktok.sub(0)])
                tbk = B[6 + c % 2]
                pv = tbk.h.bitcast(BF16)
                if lat:
                    for qk in range(2):
                        kb.op("pe", lambda e, pv=pv, qk=qk, cqn=cqn: e.transpose(out=pv[:, qk * 128:(qk + 1) * 128], in_=cqn[:, qk * 128:(qk + 1) * 128], identity=self.identb[:]),
                              reads=[cqn, self.identb], writes=[tbk])
                kb.op("pe", lambda e, pv=pv, ckvn=ckvn: e.transpose(out=pv[:, 256:384], in_=ckvn[:, :], identity=self.identb[:]), reads=[ckvn, self.identb], writes=[tbk])
                kb.op("pe", lambda e, pv=pv, ktok=ktok: e.transpose(out=pv[0:96, 384:512], in_=ktok[:, 0:96], identity=self.identb[:]), reads=[ktok, self.identb], writes=[tbk])
                if lat:
                    for qk in range(2):
                        kb.op("act", lambda e, pv=pv, qk=qk, cl_=cl_: e.activation(out=cqnT[:, qk, cl_ * 128:(cl_ + 1) * 128], in_=pv[:, qk * 128:(qk + 1) * 128],
                                                                                 func=AF.Identity, scale=qgc[:, qk:qk + 1]), reads=[tbk, qgc], writes=[cqnT.sub((qk, cl_))])
                kb.op("act", lambda e, pv=pv, c=c: e.activation(out=ckvnT[:, c * 128:(c + 1) * 128], in_=pv[:, 256:384], func=AF.Identity, scale=kvgc[:, 0:1]),
                      reads=[tbk, kvgc], writes=[ckvnT.sub(c)])
                for KTx in KTs:
                    kb.op("act", lambda e, pv=pv, c=c, KTx=KTx: e.copy(out=KTx[64:96, c * 128:(c + 1) * 128], in_=pv[64:96, 384:512]),
                          reads=[tbk], writes=[KTx.sub(("r", c))])
            def projA(h):
                KTh = KTs[h % 2]
                for blk in range(5):
                    n0 = blk * 512
                    nn = min(512, NKV - n0)
                    bk = B[7]
                    kb.op("pe", lambda e, h=h, n0=n0, nn=nn, bk=bk: e.matmul(out=bk.h[0:64, 0:nn], lhsT=wukv[:, h * 128:h * 128 + 64], rhs=ckvnT[:, n0:n0 + nn],
                                                                        start=True, stop=True), reads=[wukv, ckvnT], writes=[bk])
                    kb.op("dve", lambda e, n0=n0, nn=nn, bk=bk, KTh=KTh: e.tensor_copy(out=KTh[0:64, n0:n0 + nn], in_=bk.h[0:64, 0:nn]),
                          reads=[bk], writes=[KTh.sub(("n", blk))])
                va = Vaug[h % 2]
                vo = 0 if h % 2 == 0 else 64
                for vb in range(3):
                    ntl = min(8, NT - vb * 8)
                    bv = B[7]
                    for ci in range(ntl):
                        c = vb * 8 + ci
                        kb.op("pe", lambda e, h=h, c=c, ci=ci, bv=bv: e.matmul(out=bv.h[:, ci * 64:(ci + 1) * 64], lhsT=ckvnT[:, c * 128:(c + 1) * 128],
                                                                             rhs=wukv[:, h * 128 + 64:h * 128 + 128], start=True, stop=True), reads=[ckvnT, wukv], writes=[bv])
                    kb.op("dve", lambda e, vb=vb, ntl=ntl, bv=bv, va=va, vo=vo: e.tensor_copy(
                        out=va[:, vb * 8:vb * 8 + ntl, vo:vo + 64], in_=bv.h[:, 0:ntl * 64].rearrange("p (c f) -> p c f", f=64)),
                        reads=[bv], writes=[va.sub(vb)])

            def projQ(h, half, stage):
                qtk = qtoks[half]
                QTh = QTs[h % 2]
                for bq in range(2):
                    c0 = half * 8 + bq * 4
                    if stage == 0:
                        bqk = B[6 + bq]
                        for ci in range(4):
                            c = c0 + ci
                            for qk in range(2):
                                kb.op("pe", lambda e, h=h, c=c, ci=ci, qk=qk, bqk=bqk: e.matmul(
                                    out=bqk.h[:, ci * 96:(ci + 1) * 96], lhsT=cqnT[:, qk, c * 128:(c + 1) * 128], rhs=wuq[:, qk, h * 96:(h + 1) * 96],
                                    start=(qk == 0), stop=(qk == 1)), reads=[cqnT, wuq], writes=[bqk])
                        qv = bqk.h[:, 0:384].rearrange("p (c f) -> p c f", f=96)
                        lc0 = bq * 4
                        kb.op("dve", lambda e, qv=qv, lc0=lc0, qtk=qtk: e.tensor_copy(out=qtk[:, lc0:lc0 + 4, 0:64], in_=qv[:, :, 0:64]), reads=[bqk], writes=[qtk.sub((lc0, "n"))])
                        xe, xo = qv[:, :, 64:96:2], qv[:, :, 65:96:2]
                        cs, sn = ropt[:, c0:c0 + 4, 0:16], ropt[:, c0:c0 + 4, 16:32]
                        qo = qtk[:, lc0:lc0 + 4, 64:96].rearrange("p c (i two) -> p c i two", two=2)
                        kb.op("dve", lambda e, xe=xe, cs=cs: e.tensor_tensor(out=rt[0][:], in0=xe, in1=cs, op=ALU.mult), reads=[bqk, ropt], writes=[rt[0]])
                        kb.op("dve", lambda e, xo=xo, sn=sn: e.tensor_tensor(out=rt[1][:], in0=xo, in1=sn, op=ALU.mult), reads=[bqk, ropt], writes=[rt[1]])
                        kb.op("dve", lambda e, xe=xe, sn=sn: e.tensor_tensor(out=rt[2][:], in0=xe, in1=sn, op=ALU.mult), reads=[bqk, ropt], writes=[rt[2]])
                        kb.op("dve", lambda e, xo=xo, cs=cs: e.tensor_tensor(out=rt[3][:], in0=xo, in1=cs, op=ALU.mult), reads=[bqk, ropt], writes=[rt[3]])
                        kb.op("pool", lambda e, qo=qo: e.tensor_tensor(out=qo[:, :, :, 0], in0=rt[0][:], in1=rt[1][:], op=ALU.subtract),
                              reads=[rt[0], rt[1]], writes=[qtk.sub((lc0, "e"))])
                        kb.op("pool", lambda e, qo=qo: e.tensor_tensor(out=qo[:, :, :, 1], in0=rt[2][:], in1=rt[3][:], op=ALU.add),
                              reads=[rt[2], rt[3]], writes=[qtk.sub((lc0, "o"))])
                    else:
                        lc0 = bq * 4
                        tq = B[6 + bq]
                        pvq = tq.h.bitcast(BF16)
                        for ci in range(4):
                            kb.op("pe", lambda e, pvq=pvq, lc=lc0 + ci, ci=ci, qtk=qtk: e.transpose(out=pvq[0:96, ci * 128:(ci + 1) * 128], in_=qtk[:, lc, 0:96], identity=self.identb[:]),
                                  reads=[qtk, self.identb], writes=[tq])
                        kb.op("dve", lambda e, pvq=pvq, c0=c0, QTh=QTh: e.tensor_copy(out=QTh[0:96, c0 * 128:(c0 + 4) * 128], in_=pvq[0:96, 0:512]), reads=[tq], writes=[QTh.sub(c0)])

            def normalize1(h, qb, bo):
                kb.op("dve", lambda e, bo=bo: e.tensor_copy(out=oTs[:], in_=bo.h[:, 0:512]), reads=[bo], writes=[oTs])

            def normalize1b(h, qb):
                dp = 64 if (h % 2 == 0) else 0
                kb.op("dve", lambda e, dp=dp: e.reciprocal(out=recf[dp:dp + 1, :], in_=oTs[dp:dp + 1, :]), reads=[oTs], writes=[recf])
                kb.op("pool", lambda e, dp=dp: e.tensor_copy(out=rech[dp:dp + 1, :], in_=recf[dp:dp + 1, :]), reads=[recf], writes=[rech])
                kb.op("pool", lambda e, dp=dp: e.tensor_tensor(out=recl[dp:dp + 1, :], in0=recf[dp:dp + 1, :], in1=rech[dp:dp + 1, :], op=ALU.subtract),
                      reads=[recf, rech], writes=[recl])

            def normalize2(h, qb):
                even = (h % 2 == 0)
                dp = 64 if even else 0
                op_ = 0 if even else 64
                bb = B[6]
                kb.op("pe", lambda e, dp=dp, bb=bb: e.matmul(out=bb.h[:, 0:512], lhsT=ones1[dp:dp + 1, :], rhs=rech[dp:dp + 1, :], start=True, stop=False),
                      reads=[ones1, rech], writes=[bb])
                kb.op("pe", lambda e, dp=dp, bb=bb: e.matmul(out=bb.h[:, 0:512], lhsT=ones1[dp:dp + 1, :], rhs=recl[dp:dp + 1, :], start=False, stop=True),
                      reads=[ones1, recl], writes=[bb])
                kb.op("dve", lambda e, op_=op_, h=h, qb=qb, bb=bb: e.tensor_tensor(
                    out=attnT[op_:op_ + 64, h // 2, qb * 512:(qb + 1) * 512], in0=oTs[op_:op_ + 64, :], in1=bb.h[op_:op_ + 64, 0:512], op=ALU.mult),
                    reads=[oTs, bb], writes=[attnT.sub((h, qb))])

            projA(0)
            for half in range(2):
                projQ(0, half, 0)
                projQ(0, half, 1)
            pend = []
            pendnorm = []
            nstep = 0

            def issue_pv(item):
                (h, qb, c2, bo, pt, va) = item
                for half in range(2):
                    c = c2 * 2 + half
                    kb.op("pe", lambda e, c=c, bo=bo, pt=pt, va=va, half=half: e.matmul(
                        out=bo.h[:, 0:512], lhsT=va[:, c, :], rhs=pt[:, half * 512:(half + 1) * 512],
                        start=(c == 0), stop=(c == NT - 1)), reads=[va, pt], writes=[bo])
                if c2 == NT // 2 - 1:
                    normalize1(h, qb, bo)
                    pendnorm.append((nstep + 1, 1, h, qb))
                    pendnorm.append((nstep + 6, 2, h, qb))
                    pendnorm.sort()

            for h in range(NE):
                KTh = KTs[h % 2]
                QTh = QTs[h % 2]
                va = Vaug[h % 2]
                for qb in range(4):
                    bo = B[4 + qb % 2]
                    for c2 in range(NT // 2):
                        if h + 1 < NE:
                            if qb == 0 and c2 == 4:
                                projA(h + 1)
                            if qb == 1 and c2 == 4:
                                projQ(h + 1, 0, 0)
                            if qb == 2 and c2 == 4:
                                projQ(h + 1, 0, 1)
                            if qb == 3 and c2 == 3:
                                projQ(h + 1, 1, 0)
                            if qb == 3 and c2 == 8:
                                projQ(h + 1, 1, 1)
                        while pendnorm and nstep >= pendnorm[0][0]:
                            (_, stg, hh, qq) = pendnorm.pop(0)
                            (normalize1b if stg == 1 else normalize2)(hh, qq)
                        pi = nstep % 2
                        pt = pT[nstep % 3]
                        nstep += 1
                        for half in range(2):
                            c = c2 * 2 + half
                            bs = B[2 * pi + half]
                            kb.op("pe", lambda e, c=c, qb=qb, bs=bs, KTh=KTh, QTh=QTh: e.matmul(
                                out=bs.h[:, 0:512], lhsT=KTh[0:96, c * 128:(c + 1) * 128], rhs=QTh[0:96, qb * 512:(qb + 1) * 512],
                                start=True, stop=True), reads=[KTh, QTh], writes=[bs])
                        kb.op("act", lambda e, pi=pi, pt=pt: e.activation(out=pt[:, 0:1024], in_=kb.pairs[pi][:, 0:1024], func=AF.Exp, scale=SCALE),
                              reads=[B[2 * pi], B[2 * pi + 1]], writes=[pt])
                        pend.append((h, qb, c2, bo, pt, va))
                        if len(pend) > 1:
                            issue_pv(pend.pop(0))
            while pend:
                issue_pv(pend.pop(0))
            while pendnorm:
                (_, stg, hh, qq) = pendnorm.pop(0)
                (normalize1b if stg == 1 else normalize2)(hh, qq)
            for tc in range(L // 128):
                kb.dma("sp", lambda e, s=s, tc=tc: e.dma_start(out=xb[:], in_=self.xr[s].ap[tc * 128:(tc + 1) * 128, :]),
                       reads=[self.xr[s]], writes=[xb])
                for db in range(2):
                    bank = B[6 + db]
                    for m in range(8):
                        kb.op("pe", lambda e, m=m, db=db, bank=bank, tc=tc: e.matmul(
                            out=bank.h[:, 0:512], lhsT=attnT[:, m, tc * 128:(tc + 1) * 128], rhs=wo[:, m, db * 512:(db + 1) * 512],
                            start=(m == 0), stop=(m == 7)), reads=[attnT, wo], writes=[bank])
                    kb.op("dve", lambda e, db=db, bank=bank: e.tensor_tensor(
                        out=t_tmp[:, 0:512], in0=bank.h[:, 0:512], in1=g1b[:, db * 512:(db + 1) * 512], op=ALU.mult),
                        reads=[bank, g1b], writes=[t_tmp])
                    kb.op("pool", lambda e, db=db: e.tensor_tensor(
                        out=xb[:, db * 512:(db + 1) * 512], in0=xb[:, db * 512:(db + 1) * 512], in1=t_tmp[:, 0:512], op=ALU.add),
                        reads=[xb, t_tmp], writes=[xb])
                kb.dma("sp", lambda e, s=s, tc=tc: e.dma_start(out=self.xr[s].ap[tc * 128:(tc + 1) * 128, :], in_=xb[:]),
                       reads=[xb], writes=[self.xr[s].sub(("row", tc))])


def _consts():
    bf = ml_dtypes.bfloat16
    c = {}
    c["c_identb"] = np.eye(128, dtype=np.float32).astype(bf)
    c["c_identf"] = np.eye(128, dtype=np.float32)
    i = np.arange(128, dtype=np.int64)
    ang = 2.0 * np.pi * ((i[:, None] * i[None, :]) % 128).astype(np.float64) / 128.0
    c["c_csc"] = np.concatenate([np.cos(ang), np.sin(ang)], axis=1).astype(np.float32) / np.float32(np.sqrt(128.0))
    c["c_csc"] = c["c_csc"].astype(bf)
    for nm, n in (("", L), ("c", LC)):
        t = np.arange(n, dtype=np.int64)
        a = 2.0 * np.pi * ((t[:, None] * t[None, :]) % n).astype(np.float64) / n
        c["c_cl" + nm] = (np.cos(a) / np.sqrt(n)).astype(np.float32).astype(bf)
        c["c_sl" + nm] = (-np.sin(a) / np.sqrt(n)).astype(np.float32).astype(bf)
    t = np.arange(L)
    row = (t // 64).astype(np.float32)
    col = (t % 64).astype(np.float32)
    inv = (np.float32(10000.0) ** (-np.arange(8, dtype=np.float32) / np.float32(8))).astype(np.float32)
    angr = np.concatenate([row[:, None] * inv[None, :], col[:, None] * inv[None, :]], axis=1).astype(np.float32)
    c["c_rope"] = np.concatenate([np.cos(angr), np.sin(angr)], axis=1).astype(np.float32)
    c["c_ctxbase"] = np.concatenate([np.zeros(32), np.full(32, LC), NS * LC + np.arange(64)]).astype(np.float32).reshape(128, 1)
    return c


def _in_map(inp, core, consts):
    f = lambda a: np.ascontiguousarray(np.asarray(a, dtype=np.float32))
    s0 = core * NS
    m = {}
    m["x"] = f(inp["x"][s0:s0 + NS])
    m["ctx"] = f(inp["ctx"][s0:s0 + NS])
    cv3 = np.stack([inp["c"][s0], inp["c"][s0 + 1], inp["c_ctx"]], axis=0).astype(np.float32)
    m["cv"] = np.ascontiguousarray(cv3.reshape(3, 8, 128).transpose(2, 1, 0))
    for k in ("mod_w", "mod_b", "norm1_g", "norm2_g", "final_g", "moe_w_router", "moe_w1", "moe_w3", "moe_w2"):
        m[k] = f(inp[k])
    for k in ("ab_w_in", "ab_conv_w", "ab_conv_b", "ab_ln_g", "ab_ln_b", "ab_w_out", "mla_w_in", "mla_q_norm_g",
              "mla_kv_norm_g", "mla_w_uq", "mla_w_ukv", "mla_w_o"):
        m[k] = f(inp[k][0])
    m.update(consts)
    return m


_CACHE = {}


def run_prog(inputs, phases, copy_in=False, ncores=8, debug_route=False, raw=False):
    key = (tuple(phases), copy_in, debug_route)
    if key not in _CACHE:
        p = Prog(phases=phases, copy_in=copy_in)
        p.debug_route = debug_route
        _CACHE[key] = p.build()
    nc = _CACHE[key]
    consts = _consts()
    in_maps = [_in_map(inputs, c, consts) for c in range(ncores)]
    res = run_bass_kernel_spmd(nc, in_maps, core_ids=list(range(ncores)))
    if raw:
        return res.results
    return np.concatenate([np.asarray(r["y"]) for r in res.results], axis=0)


def kernel(**inputs):
    out = run_prog(inputs, ("mix0", "moe0", "mla1", "moe1", "final"))
    return out.astype(np.float32)
```

```python
import numpy as np
import ml_dtypes
from contextlib import ExitStack
import concourse.bass as bass
import concourse.mybir as mybir
from concourse.bass_utils import run_bass_kernel_spmd

F32 = mybir.dt.float32
BF16 = mybir.dt.bfloat16
I32 = mybir.dt.int32
U32 = mybir.dt.uint32
U8 = mybir.dt.uint8
AF = mybir.ActivationFunctionType
ALU = mybir.AluOpType
AX = mybir.AxisListType

D = 1024
L = 2048
LC = 256
NS = 2
NE = 16
EPS = 1e-6
DSZ = {F32: 4, BF16: 2, I32: 4, U32: 4, U8: 1}


class Trk:
    def __init__(self, name):
        self.name = name
        self.w = None
        self.r = []
        self.kids = {}
        self.parent = None

    def sub(self, key):
        if key not in self.kids:
            k = Trk(f"{self.name}.{key}")
            k.parent = self
            self.kids[key] = k
        return self.kids[key]

    def rdeps(self):
        s = set()
        if self.w:
            s.add(self.w)
        if self.parent is not None and self.parent.w:
            s.add(self.parent.w)
        for k in self.kids.values():
            if k.w:
                s.add(k.w)
        return s

    def wdeps(self):
        s = self.rdeps()
        s.update(self.r)
        if self.parent is not None:
            s.update(self.parent.r)
        for k in self.kids.values():
            s.update(k.r)
        return s

    def did_read(self, ev):
        self.r.append(ev)

    def did_write(self, ev):
        self.w = ev
        self.r = []
        for k in self.kids.values():
            k.w = None
            k.r = []


class T(Trk):
    def __init__(self, kb, name, shape, dtype, off):
        super().__init__(name)
        self.kb = kb
        self.shape = shape
        self.dtype = dtype
        self.off = off
        self.h = kb.nc.alloc_sbuf_tensor_at(name, list(shape), dtype, offset=off)

    def view(self, name, shape, dtype, boff=0):
        return self.kb.nc.alloc_sbuf_tensor_at(
            self.kb.uname(name), list(shape), dtype, offset=self.off + boff)

    def __getitem__(self, k):
        return self.h[k]


class Lane:
    def __init__(self, key, sem):
        self.key = key
        self.sem = sem
        self.count = 0


class KB:
    COMPUTE = ["pe", "act", "dve", "pool"]
    QUEUES = ["sp", "pool", "act"]

    def __init__(self, n_lanes=8):
        self.nc = bass.Bass("TRN2", target_bir_lowering=False)
        nc = self.nc
        self.es = ExitStack()
        self.uid = 0
        self.semobj = {}
        self.cnt = {}
        for e in self.COMPUTE:
            self.semobj[e] = self.es.enter_context(nc.semaphore("s_" + e))
            self.cnt[e] = 0
        self.lanes = {}
        self.lane_rr = {}
        for q in self.QUEUES:
            self.lanes[q] = []
            for i in range(n_lanes):
                key = f"d_{q}{i}"
                self.semobj[key] = self.es.enter_context(nc.semaphore(key))
                self.lanes[q].append(Lane(key, self.semobj[key]))
            self.lane_rr[q] = 0
        self.prog = {e: [] for e in ["pe", "act", "dve", "pool", "sp"]}
        self.waited = {e: {} for e in ["pe", "act", "dve", "pool", "sp"]}
        self.arena_bytes = 206 * 1024
        ah = nc.alloc_sbuf_tensor("arena", [128, self.arena_bytes], U8)
        self.abase = nc.lookup_mloc(ah).addr
        self.atop = 0
        self.bnd = {}
        for n in (L - 1, NS * LC + 128 - 1):
            reg = self.es.enter_context(nc.gpsimd.register(f"bnd{n}"))
            self.bnd[n] = reg
            self.prog["pool"].append(lambda en, reg=reg, n=n: en.reg_mov(reg, n))
        self.banks = []
        self.pairs = []
        for i in range(4):
            ph = self.es.enter_context(nc.psum_tensor(f"pbank{i}", [128, 1024], F32))
            self.pairs.append(ph)
            for j in range(2):
                t = Trk(f"bank{2 * i + j}")
                t.h = ph[:, j * 512:(j + 1) * 512]
                t.psum = True
                self.banks.append(t)

    def uname(self, n):
        self.uid += 1
        return f"{n}_{self.uid}"

    def alloc(self, name, shape, dtype):
        nbytes = int(np.prod(shape[1:])) * DSZ[dtype]
        nbytes = (nbytes + 63) // 64 * 64
        off = self.atop
        assert off + nbytes <= self.arena_bytes, f"SBUF arena overflow at {name}: {off}+{nbytes}"
        self.atop += nbytes
        return T(self, self.uname(name), shape, dtype, self.abase + off)

    def mark(self):
        return self.atop

    def release(self, m):
        self.barrier()
        self.atop = m

    def dram(self, name, shape, dtype, kind="Internal"):
        if kind == "Internal":
            h = self.nc.dram_tensor(name, list(shape), dtype)
        else:
            h = self.nc.dram_tensor(name, list(shape), dtype, kind=kind)
        t = Trk(name)
        t.h = h
        t.ap = h.ap()
        return t

    def _waits(self, eng, evs):
        best = {}
        for (k, v) in evs:
            if v > best.get(k, 0):
                best[k] = v
        for k, v in best.items():
            if k == "pe" and eng == "pe":
                continue
            if self.waited[eng].get(k, 0) >= v:
                continue
            self.waited[eng][k] = v
            sem = self.semobj[k]
            self.prog[eng].append(lambda e, sem=sem, v=v: e.wait_ge(sem, v))

    def _deps(self, reads, writes, eng=None):
        evs = set()
        for t in reads:
            evs |= t.rdeps()
            root = t if t.parent is None else t.parent
            if getattr(root, "psum", False):
                for ev in root.r:
                    if ev[0] != eng:
                        evs.add(ev)
                for k in root.kids.values():
                    for ev in k.r:
                        if ev[0] != eng:
                            evs.add(ev)
        for t in writes:
            evs |= t.wdeps()
        return evs

    def op(self, eng, fn, reads=(), writes=()):
        evs = self._deps(reads, writes, eng)
        self._waits(eng, evs)
        self.cnt[eng] += 1
        sem = self.semobj[eng]
        self.prog[eng].append(lambda e, fn=fn, sem=sem: fn(e).then_inc(sem, 1))
        ev = (eng, self.cnt[eng])
        for t in reads:
            t.did_read(ev)
        for t in writes:
            t.did_write(ev)
        return ev

    def dma(self, q, fn, reads=(), writes=()):
        evs = self._deps(reads, writes)
        lanes = self.lanes[q]
        lane = lanes[self.lane_rr[q] % len(lanes)]
        self.lane_rr[q] += 1
        if lane.count > 0:
            evs.add((lane.key, lane.count))
        self._waits(q, evs)
        lane.count += 16
        sem = lane.sem
        def run(e, fn=fn, sem=sem):
            try:
                ins = fn(e)
            except Exception:
                print("DMA BUILD FAIL line", fn.__code__.co_firstlineno, "defaults", [str(d)[:80] for d in (fn.__defaults__ or ())])
                raise
            ins.then_inc(sem, 16)
        self.prog[q].append(run)
        ev = (lane.key, lane.count)
        for t in reads:
            t.did_read(ev)
        for t in writes:
            t.did_write(ev)
        return ev

    def barrier(self):
        evs = set()
        for e in self.COMPUTE:
            if self.cnt[e] > 0:
                evs.add((e, self.cnt[e]))
        for q in self.QUEUES:
            for ln in self.lanes[q]:
                if ln.count > 0:
                    evs.add((ln.key, ln.count))
        for e in ["pe", "act", "dve", "pool", "sp"]:
            self._waits(e, evs)

    def finish(self):
        self.barrier()
        nc = self.nc
        with nc.allow_non_contiguous_dma(reason="small strided constant loads"):
            with nc.Block() as block:
                @block.sync
                def _(e):
                    for f in self.prog["sp"]:
                        f(e)

                @block.tensor
                def _(e):
                    for f in self.prog["pe"]:
                        f(e)

                @block.scalar
                def _(e):
                    for f in self.prog["act"]:
                        f(e)

                @block.vector
                def _(e):
                    for f in self.prog["dve"]:
                        f(e)

                @block.gpsimd
                def _(e):
                    for f in self.prog["pool"]:
                        f(e)
        self.es.close()
        return nc


class Prog:
    def __init__(self, phases=("mix0", "moe0", "mla1", "moe1", "final"), copy_in=False):
        self.kb = KB()
        self.phases = phases
        self.copy_in = copy_in
        kb = self.kb
        di = lambda n, s, d=F32: kb.dram(n, s, d, kind="ExternalInput")
        self.x = di("x", [NS, L, D])
        self.ctx = di("ctx", [NS, LC, D])
        self.cv = di("cv", [128, 8, 3])
        self.mod_w = di("mod_w", [2, D, 6 * D])
        self.mod_b = di("mod_b", [2, 6 * D])
        self.n1g = di("norm1_g", [2, D])
        self.n2g = di("norm2_g", [2, D])
        self.final_g = di("final_g", [D])
        self.ab_w_in = di("ab_w_in", [D, 1536])
        self.ab_conv_w = di("ab_conv_w", [31, 512])
        self.ab_conv_b = di("ab_conv_b", [512])
        self.ab_ln_g = di("ab_ln_g", [512])
        self.ab_ln_b = di("ab_ln_b", [512])
        self.ab_w_out = di("ab_w_out", [D, D])
        self.mla_w_in = di("mla_w_in", [D, 416])
        self.mla_qg = di("mla_q_norm_g", [256])
        self.mla_kvg = di("mla_kv_norm_g", [128])
        self.mla_w_uq = di("mla_w_uq", [256, 1536])
        self.mla_w_ukv = di("mla_w_ukv", [128, 2048])
        self.mla_w_o = di("mla_w_o", [D, D])
        self.w_router = di("moe_w_router", [2, D, NE])
        self.w1 = di("moe_w1", [2, NE, D, D])
        self.w3 = di("moe_w3", [2, NE, D, D])
        self.w2 = di("moe_w2", [2, NE, D, D])
        self.c_identb = di("c_identb", [128, 128], BF16)
        self.c_identf = di("c_identf", [128, 128], F32)
        self.c_csc = di("c_csc", [128, 256], BF16)
        self.c_cl = di("c_cl", [L, L], BF16)
        self.c_sl = di("c_sl", [L, L], BF16)
        self.c_clc = di("c_clc", [LC, LC], BF16)
        self.c_slc = di("c_slc", [LC, LC], BF16)
        self.c_rope = di("c_rope", [L, 32], F32)
        self.c_ctxbase = di("c_ctxbase", [128, 1], F32)
        self.out = kb.dram("y", [NS, L, D], F32, kind="ExternalOutput")
        self.xr = [kb.dram(f"xr{s}", [L, D], F32) for s in range(NS)]
        self.xc_all = kb.dram("xc_all", [NS * LC + 128, D], F32)
        self.xnc_all = kb.dram("xnc_all", [NS * LC + 128, D], BF16)
        self.xc = []
        self.xnl = [kb.dram(f"xnl{s}", [L, D], BF16) for s in range(NS)]
        self.xnc = []
        for s in range(NS):
            t = self.xc_all.sub(s); t.ap = self.xc_all.ap[s * LC:(s + 1) * LC, :]; self.xc.append(t)
            t = self.xnc_all.sub(s); t.ap = self.xnc_all.ap[s * LC:(s + 1) * LC, :]; self.xnc.append(t)
        self.xin = []
        self.cin = []
        for s in range(NS):
            t = self.x.sub(s); t.ap = self.x.ap[s]; self.xin.append(t)
            t = self.ctx.sub(s); t.ap = self.ctx.ap[s]; self.cin.append(t)
        self.modd = kb.dram("modd", [2, 3, 6 * D], F32)

    def build(self):
        kb = self.kb
        self.prologue()
        if self.copy_in:
            for s in range(NS):
                kb.dma("sp", lambda e, s=s: e.dma_start(out=self.xr[s].ap, in_=self.x.ap[s]),
                       reads=[self.x], writes=[self.xr[s]])
                kb.dma("sp", lambda e, s=s: e.dma_start(out=self.xc[s].ap, in_=self.ctx.ap[s]),
                       reads=[self.ctx], writes=[self.xc[s]])
            kb.barrier()
        for ph in self.phases:
            m = kb.mark()
            if ph == "mix0":
                self.mixer0()
            elif ph == "moe0":
                self.moe(0, with_ctx=True)
            elif ph == "mla1":
                self.mla()
            elif ph == "moe1":
                self.moe(1, with_ctx=False)
            elif ph == "final":
                self.final()
            elif ph == "dump":
                self.dump()
            kb.release(m)
        return kb.finish()

    def prologue(self):
        kb = self.kb
        self.identb = kb.alloc("identb", [128, 128], BF16)
        self.identf = kb.alloc("identf", [128, 128], F32)
        kb.dma("sp", lambda e: e.dma_start(out=self.identb[:], in_=self.c_identb.ap[:, :]),
               reads=[self.c_identb], writes=[self.identb])
        kb.dma("sp", lambda e: e.dma_start(out=self.identf[:], in_=self.c_identf.ap[:, :]),
               reads=[self.c_identf], writes=[self.identf])
        self.epsc = kb.alloc("epsc", [128, 1], F32)
        kb.op("dve", lambda e: e.memset(self.epsc[:], EPS), writes=[self.epsc])
        self.zeroc = kb.alloc("zeroc", [128, 1], F32)
        kb.op("dve", lambda e: e.memset(self.zeroc[:], 0.0), writes=[self.zeroc])
        self.modc = [kb.alloc(f"modc{l}", [128, 48, 3], F32) for l in range(2)]
        self.mul1c = [kb.alloc(f"mul1c{l}", [128, 8, 3], F32) for l in range(2)]
        self.mul2c = [kb.alloc(f"mul2c{l}", [128, 8, 3], F32) for l in range(2)]
        self.n1gc = kb.alloc("n1gc", [128, 2, 8], F32)
        self.n2gc = kb.alloc("n2gc", [128, 2, 8], F32)
        kb.dma("sp", lambda e: e.dma_start(out=self.n1gc[:], in_=self.n1g.ap.rearrange("l (k p) -> p l k", p=128)),
               reads=[self.n1g], writes=[self.n1gc])
        kb.dma("sp", lambda e: e.dma_start(out=self.n2gc[:], in_=self.n2g.ap.rearrange("l (k p) -> p l k", p=128)),
               reads=[self.n2g], writes=[self.n2gc])
        m0 = kb.mark()
        zf = kb.alloc("zf", [128, D], F32)
        zb = kb.alloc("zb", [128, D], BF16)
        kb.op("dve", lambda e: e.memset(zf[:], 0.0), writes=[zf])
        kb.op("dve", lambda e: e.memset(zb[:], 0.0), writes=[zb])
        kb.dma("sp", lambda e: e.dma_start(out=self.xc_all.ap[NS * LC:NS * LC + 128, :], in_=zf[:]),
               reads=[zf], writes=[self.xc_all.sub("pad")])
        kb.dma("sp", lambda e: e.dma_start(out=self.xnc_all.ap[NS * LC:NS * LC + 128, :], in_=zb[:]),
               reads=[zb], writes=[self.xnc_all.sub("pad")])
        cvt = kb.alloc("cvt", [128, 24], F32)
        sct = kb.alloc("sct", [128, 24], BF16)
        kb.dma("sp", lambda e: e.dma_start(out=cvt[:], in_=self.cv.ap.rearrange("p k r -> p (k r)")),
               reads=[self.cv], writes=[cvt])
        kb.op("act", lambda e: e.activation(out=sct[:], in_=cvt[:], func=AF.Silu), reads=[cvt], writes=[sct])
        mwt = [kb.alloc(f"mwt{i}", [128, 8, 1536], BF16) for i in range(3)]
        mrow = kb.alloc("mrow", [3, 6 * D], F32)
        mb3 = kb.alloc("mb3", [3, 6 * D], F32)
        it = 0
        for l in range(2):
            kb.dma("sp", lambda e, l=l: e.dma_start(out=mb3[:], in_=self.mod_b.ap[l].partition_broadcast(3)),
                   reads=[self.mod_b], writes=[mb3])
            for pc in range(4):
                wt = mwt[it % 3]
                it += 1
                src = self.mod_w.ap[l].rearrange("(k p) n -> p k n", p=128)[:, :, pc * 1536:(pc + 1) * 1536]
                kb.dma("pool", lambda e, wt=wt, src=src: e.dma_start(out=wt[:], in_=src),
                       reads=[self.mod_w], writes=[wt])
                for nb in range(3):
                    bank = kb.banks[(pc * 3 + nb) % 2]
                    for k in range(8):
                        kb.op("pe", lambda e, bank=bank, wt=wt, k=k, nb=nb: e.matmul(
                            out=bank.h[0:3, 0:512], lhsT=sct[:, k * 3:(k + 1) * 3],
                            rhs=wt[:, k, nb * 512:(nb + 1) * 512], start=(k == 0), stop=(k == 7)),
                            reads=[sct, wt], writes=[bank])
                    c0 = pc * 1536 + nb * 512
                    kb.op("dve", lambda e, bank=bank, c0=c0: e.tensor_tensor(
                        out=mrow[0:3, c0:c0 + 512], in0=bank.h[0:3, 0:512], in1=mb3[0:3, c0:c0 + 512], op=ALU.add),
                        reads=[bank, mb3], writes=[mrow.sub(c0)])
            kb.dma("sp", lambda e, l=l: e.dma_start(out=self.modd.ap[l], in_=mrow[0:3, :]),
                   reads=[mrow], writes=[self.modd.sub(l)])
            for r in range(3):
                kb.dma("sp", lambda e, l=l, r=r: e.dma_start(
                    out=self.modc[l][:, :, r], in_=self.modd.ap[l, r].rearrange("(c p) -> p c", p=128)),
                    reads=[self.modd.sub(l)], writes=[self.modc[l].sub(r)])
            for (mulc, gc, v) in ((self.mul1c[l], self.n1gc, 1), (self.mul2c[l], self.n2gc, 4)):
                kb.op("dve", lambda e, mulc=mulc, v=v, l=l: e.tensor_scalar(
                    out=mulc[:], in0=self.modc[l][:, v * 8:(v + 1) * 8, :], scalar1=1.0, scalar2=None, op0=ALU.add),
                    reads=[self.modc[l]], writes=[mulc])
                for r in range(3):
                    kb.op("dve", lambda e, mulc=mulc, gc=gc, r=r, l=l: e.tensor_tensor(
                        out=mulc[:, :, r], in0=mulc[:, :, r], in1=gc[:, l, :], op=ALU.mult),
                        reads=[mulc, gc], writes=[mulc])
        kb.release(m0)

    def norm_batch(self, src, row0, nt, mulc, addc, r, uT, ucol0, bufs, banks, xn_dst=None):
        kb = self.kb
        xb, xnb, junk, stat = bufs
        tiles = []
        for j in range(nt):
            xt = xb[self._nb % len(xb)]
            xn = xnb[self._nb % len(xnb)]
            sc = self._nb % 64
            self._nb += 1
            tiles.append((j, xt, xn, sc))
            rr = row0 + j * 128
            kb.dma("sp", lambda e, xt=xt, rr=rr: e.dma_start(out=xt[:], in_=src.ap[rr:rr + 128, :]),
                   reads=[src], writes=[xt])
            kb.op("act", lambda e, xt=xt, sc=sc: e.activation(
                out=junk[:], in_=xt[:], func=AF.Square, accum_out=stat[:, sc:sc + 1]),
                reads=[xt], writes=[junk, stat.sub(sc)])
            kb.op("act", lambda e, sc=sc: e.activation(
                out=stat[:, 64 + sc:65 + sc], in_=stat[:, sc:sc + 1], func=AF.Identity, scale=1.0 / D, bias=self.epsc[:, 0:1]),
                reads=[stat.sub(sc), self.epsc], writes=[stat.sub(64 + sc)])
            kb.op("act", lambda e, sc=sc: e.activation(
                out=stat[:, 128 + sc:129 + sc], in_=stat[:, 64 + sc:65 + sc], func=AF.Sqrt),
                reads=[stat.sub(64 + sc)], writes=[stat.sub(128 + sc)])
            kb.op("dve", lambda e, sc=sc: e.reciprocal(out=stat[:, 192 + sc:193 + sc], in_=stat[:, 128 + sc:129 + sc]),
                  reads=[stat.sub(128 + sc)], writes=[stat.sub(192 + sc)])
            if len(xb) == 1:
                self._norm_tail(tiles.pop(), src, row0, mulc, addc, r, uT, ucol0, banks, xn_dst)
        for t in tiles:
            self._norm_tail(t, src, row0, mulc, addc, r, uT, ucol0, banks, xn_dst)

    def _norm_tail(self, t, src, row0, mulc, addc, r, uT, ucol0, banks, xn_dst):
        kb = self.kb
        (j, xt, xn, sc) = t
        stat = self._stat
        kb.op("act", lambda e, xt=xt, xn=xn, sc=sc: e.activation(
            out=xn[:], in_=xt[:], func=AF.Identity, scale=stat[:, 192 + sc:193 + sc]),
            reads=[xt, stat.sub(192 + sc)], writes=[xn])
        if xn_dst is not None:
            dt, drow = xn_dst
            dr0 = drow + j * 128
            kb.dma("sp", lambda e, xn=xn, dt=dt, dr=dr0: e.dma_start(
                out=dt.ap[dr:dr + 128, :], in_=xn[:]), reads=[xn], writes=[dt.sub(dr0)])
        for k in range(8):
            bank = banks[2 * j + k // 4]
            pv = bank.h.bitcast(BF16)
            kk = k % 4
            kb.op("pe", lambda e, pv=pv, xn=xn, k=k, kk=kk: e.transpose(
                out=pv[:, kk * 128:(kk + 1) * 128], in_=xn[:, k * 128:(k + 1) * 128], identity=self.identb[:]),
                reads=[xn, self.identb], writes=[bank])
        for k in range(8):
            bank = banks[2 * j + k // 4]
            pv = bank.h.bitcast(BF16)
            kk = k % 4
            dst = uT[:, k, ucol0 + j * 128: ucol0 + (j + 1) * 128]
            if k < 4:
                kb.op("act", lambda e, dst=dst, pv=pv, k=k, kk=kk: e.activation(
                    out=dst, in_=pv[:, kk * 128:(kk + 1) * 128], func=AF.Identity,
                    scale=mulc[:, k, r:r + 1], bias=addc(k, r)),
                    reads=[bank, mulc], writes=[uT.sub((k, ucol0 + j * 128))])
            else:
                kb.op("dve", lambda e, dst=dst, pv=pv, k=k, kk=kk: e.tensor_scalar(
                    out=dst, in0=pv[:, kk * 128:(kk + 1) * 128], scalar1=mulc[:, k, r:r + 1],
                    scalar2=addc(k, r), op0=ALU.mult, op1=ALU.add),
                    reads=[bank, mulc], writes=[uT.sub((k, ucol0 + j * 128))])

    def norm_bufs(self, nx=2):
        kb = self.kb
        self._nb = 0
        xb = [kb.alloc(f"xb{i}", [128, D], F32) for i in range(nx)]
        xnb = [kb.alloc(f"xnb{i}", [128, D], BF16) for i in range(nx)]
        junk = kb.alloc("junk", [128, D], BF16)
        stat = kb.alloc("stat", [128, 256], F32)
        kb.op("dve", lambda e: e.memset(stat[:], 0.0), writes=[stat])
        self._stat = stat
        return (xb, xnb, junk, stat)

    def dump(self):
        kb = self.kb
        yc = kb.dram("yc", [NS * LC, D], F32, kind="ExternalOutput")
        kb.dma("sp", lambda e: e.dma_start(out=yc.ap[:, :], in_=self.xc_all.ap[0:NS * LC, :]),
               reads=[self.xc_all], writes=[yc])
        for s in range(NS):
            kb.dma("sp", lambda e, s=s: e.dma_start(out=self.out.ap[s], in_=self.xr[s].ap),
                   reads=[self.xr[s]], writes=[self.out.sub(s)])

    def final(self):
        kb = self.kb
        fgb = kb.alloc("fgb", [128, D], F32)
        kb.dma("sp", lambda e: e.dma_start(out=fgb[:], in_=self.final_g.ap.partition_broadcast(128)),
               reads=[self.final_g], writes=[fgb])
        NB = 8
        xb = [kb.alloc(f"fxb{i}", [128, D], F32) for i in range(NB)]
        yb = [kb.alloc(f"fyb{i}", [128, D], F32) for i in range(NB)]
        junk = kb.alloc("fjunk", [128, D], BF16)
        stat = kb.alloc("fstat", [128, 4 * 64], F32)
        kb.op("dve", lambda e: e.memset(stat[:], 0.0), writes=[stat])
        tiles = [(s, t) for s in range(NS) for t in range(L // 128)]
        for b0 in range(0, len(tiles), 4):
            batch = tiles[b0:b0 + 4]
            info = []
            for bi, (s, t) in enumerate(batch):
                i = b0 + bi
                xt = xb[i % NB]
                yt = yb[i % NB]
                sc = i % 64
                info.append((s, t, xt, yt, sc))
                kb.dma("sp", lambda e, xt=xt, s=s, t=t: e.dma_start(out=xt[:], in_=self.xr[s].ap[t * 128:(t + 1) * 128, :]),
                       reads=[self.xr[s]], writes=[xt])
                kb.op("act", lambda e, xt=xt, sc=sc: e.activation(
                    out=junk[:], in_=xt[:], func=AF.Square, accum_out=stat[:, sc:sc + 1]),
                    reads=[xt], writes=[junk, stat.sub(sc)])
                kb.op("act", lambda e, sc=sc: e.activation(
                    out=stat[:, 64 + sc:65 + sc], in_=stat[:, sc:sc + 1], func=AF.Identity, scale=1.0 / D, bias=self.epsc[:, 0:1]),
                    reads=[stat.sub(sc), self.epsc], writes=[stat.sub(64 + sc)])
                kb.op("act", lambda e, sc=sc: e.activation(
                    out=stat[:, 128 + sc:129 + sc], in_=stat[:, 64 + sc:65 + sc], func=AF.Sqrt),
                    reads=[stat.sub(64 + sc)], writes=[stat.sub(128 + sc)])
                kb.op("dve", lambda e, sc=sc: e.reciprocal(out=stat[:, 192 + sc:193 + sc], in_=stat[:, 128 + sc:129 + sc]),
                      reads=[stat.sub(128 + sc)], writes=[stat.sub(192 + sc)])
            for bi, (s, t, xt, yt, sc) in enumerate(info):
                eng = "dve"
                kb.op(eng, lambda e, xt=xt, yt=yt, sc=sc: e.scalar_tensor_tensor(
                    out=yt[:], in0=xt[:], scalar=stat[:, 192 + sc:193 + sc], in1=fgb[:], op0=ALU.mult, op1=ALU.mult),
                    reads=[xt, stat.sub(192 + sc), fgb], writes=[yt])
                kb.dma("sp", lambda e, yt=yt, s=s, t=t: e.dma_start(out=self.out.ap[s, t * 128:(t + 1) * 128, :], in_=yt[:]),
                       reads=[yt], writes=[self.out.sub((s, t))])

    def moe(self, l, with_ctx):
        kb = self.kb
        IOA = bass.IndirectOffsetOnAxis
        wbufs = [[kb.alloc(f"w{n}_{i}", [128, 8, D], BF16) for n in (1, 3, 2)] for i in range(2)]
        wsrc = (self.w1, self.w3, self.w2)

        def load_w(e):
            for n in range(3):
                src = wsrc[n].ap[l, e].rearrange("(k p) f -> p k f", p=128)
                wt = wbufs[e % 2][n]
                kb.dma("pool", lambda en, wt=wt, src=src: en.dma_start(out=wt[:], in_=src),
                       reads=[wsrc[n]], writes=[wt])

        nr = 3 if with_ctx else 2
        g2b = [kb.alloc(f"g2b{r}", [128, D], F32) for r in range(nr)]
        for r in range(nr):
            kb.dma("sp", lambda e, r=r: e.dma_start(
                out=g2b[r][:], in_=self.modd.ap[l, r, 5 * D:6 * D].partition_broadcast(128)),
                reads=[self.modd.sub(l)], writes=[g2b[r]])
        idxT = kb.alloc("idxT", [128, 2, 48], I32)
        gT = kb.alloc("gT", [128, 2, 48], F32)
        idxC = kb.alloc("idxC", [128, NE], I32)
        gC = kb.alloc("gC", [128, NE], F32)
        load_w(0)
        load_w(1)
        addc2 = lambda k, r: self.modc[l][:, 24 + k, r:r + 1]
        mulc2 = self.mul2c[l]

        dbg = getattr(self, "debug_route", 0)
        if dbg == 10:
            return
        m1 = kb.mark()
        bufs = self.norm_bufs(nx=4)
        uTb = [kb.alloc(f"uTb{i}", [128, 8, 256], BF16) for i in range(2)]
        wr = kb.alloc("wr", [128, 8, NE], BF16)
        wrf = kb.alloc("wrf", [128, 8, NE], F32)
        kb.dma("sp", lambda e: e.dma_start(out=wrf[:], in_=self.w_router.ap[l].rearrange("(k p) e -> p k e", p=128)),
               reads=[self.w_router], writes=[wrf])
        kb.op("dve", lambda e: e.tensor_copy(out=wr[:], in_=wrf[:]), reads=[wrf], writes=[wr])
        aff2 = kb.alloc("aff2", [128, 16, 64], F32)
        affc = kb.alloc("affc", [128, 2, 64], F32)
        kb.op("dve", lambda e: e.memset(aff2[:].rearrange("p a b -> p (a b)"), 0.0), writes=[aff2])
        kb.op("dve", lambda e: e.memset(affc[:].rearrange("p a b -> p (a b)"), 0.0), writes=[affc])
        lg = kb.alloc("lg", [128, 16, 16], F32)
        mx = kb.alloc("mx", [128, 16], F32)
        sm = kb.alloc("sm", [128, 16], F32)
        rs = kb.alloc("rs", [128, 16], F32)
        work = kb.alloc("work", [48, L], F32)
        workc = kb.alloc("workc", [48, LC], F32)
        topv = kb.alloc("topv", [48, 256], F32)
        topi = kb.alloc("topi", [48, 256], U32)
        topif = kb.alloc("topif", [48, 256], F32)
        topvc = kb.alloc("topvc", [48, 32], F32)
        topic = kb.alloc("topic", [48, 32], U32)
        topicf = kb.alloc("topicf", [48, 32], F32)
        nbatch = 0
        seqs = []
        for s in range(NS):
            seqs.append((self.xr[s], self.xnl[s], L // 128, s, aff2, s * 32, kb.banks[4 + s], 0))
        if with_ctx:
            for s in range(NS):
                seqs.append((self.xc[s], self.xnc[s], LC // 128, 2, affc, s * 32, kb.banks[6], s * 32))
        for (src, xnd, ntl, r, afft, acol, lbank, lcol0) in seqs:
            for b in range((ntl + 1) // 2):
                nt = min(2, ntl - b * 2)
                ub = uTb[nbatch % 2]
                nbatch += 1
                self.norm_batch(src, b * 256, nt, mulc2, addc2, r, ub, 0, bufs,
                                [kb.banks[j] for j in range(2 * nt)], xn_dst=(xnd, b * 256))
                for j in range(nt):
                    if dbg == 11:
                        continue
                    c = b * 2 + j
                    for k in range(8):
                        kb.op("pe", lambda e, lbank=lbank, lc=lcol0 + c * 16, ub=ub, k=k, j=j: e.matmul(
                            out=lbank.h[:, lc:lc + 16], lhsT=ub[:, k, j * 128:(j + 1) * 128], rhs=wr[:, k, :],
                            start=(k == 0), stop=(k == 7)),
                            reads=[ub.sub((k, j * 128)), wr], writes=[lbank])
            if dbg == 11:
                continue
            lv = lbank.h[:, lcol0:lcol0 + ntl * 16].rearrange("p (c e) -> p c e", e=16)
            bc = lambda t, ntl=ntl: t[:, 0:ntl].unsqueeze(2).to_broadcast([128, ntl, 16])
            kb.op("dve", lambda e, lv=lv, ntl=ntl: e.tensor_reduce(out=mx[:, 0:ntl], in_=lv, axis=AX.X, op=ALU.max),
                  reads=[lbank], writes=[mx])
            kb.op("dve", lambda e, lv=lv, bc=bc, ntl=ntl: e.tensor_tensor(out=lg[:, 0:ntl, :], in0=lv, in1=bc(mx), op=ALU.subtract),
                  reads=[lbank, mx], writes=[lg])
            kb.op("act", lambda e, ntl=ntl: e.activation(out=lg[:, 0:ntl, :], in_=lg[:, 0:ntl, :], func=AF.Exp),
                  reads=[lg], writes=[lg])
            kb.op("dve", lambda e, ntl=ntl: e.tensor_reduce(out=sm[:, 0:ntl], in_=lg[:, 0:ntl, :], axis=AX.X, op=ALU.add),
                  reads=[lg], writes=[sm])
            kb.op("dve", lambda e, ntl=ntl: e.reciprocal(out=rs[:, 0:ntl], in_=sm[:, 0:ntl]), reads=[sm], writes=[rs])
            kb.op("dve", lambda e, afft=afft, acol=acol, bc=bc, ntl=ntl: e.tensor_tensor(
                out=afft[:, 0:ntl, acol:acol + 16], in0=lg[:, 0:ntl, :], in1=bc(rs), op=ALU.mult),
                reads=[lg, rs], writes=[afft])
        if dbg == 11:
            kb.release(m1)
            return
        if dbg == 1:
            da = kb.dram("dbg_aff", [128, 1024], F32, kind="ExternalOutput")
            kb.dma("sp", lambda e: e.dma_start(out=da.ap[:, :], in_=aff2[:].rearrange("p h c -> p (h c)")), reads=[aff2], writes=[da])
            kb.release(m1)
            return
        for c in range(16):
            bank = kb.banks[c // 4]
            kb.op("pe", lambda e, bank=bank, c=c: e.transpose(
                out=bank.h[0:48, (c % 4) * 128:(c % 4 + 1) * 128], in_=aff2[:, c, 0:48], identity=self.identf[:]),
                reads=[aff2, self.identf], writes=[bank])
        for q in range(4):
            eng = "act" if q % 2 == 0 else "dve"
            if eng == "act":
                kb.op("act", lambda e, q=q: e.copy(out=work[0:48, q * 512:(q + 1) * 512], in_=kb.banks[q].h[0:48, 0:512]),
                      reads=[kb.banks[q]], writes=[work.sub(q)])
            else:
                kb.op("dve", lambda e, q=q: e.tensor_copy(out=work[0:48, q * 512:(q + 1) * 512], in_=kb.banks[q].h[0:48, 0:512]),
                      reads=[kb.banks[q]], writes=[work.sub(q)])
        if with_ctx:
            for c in range(2):
                kb.op("pe", lambda e, c=c: e.transpose(
                    out=kb.banks[7].h[0:48, c * 128:(c + 1) * 128], in_=affc[:, c, 0:48], identity=self.identf[:]),
                    reads=[affc, self.identf], writes=[kb.banks[7]])
            kb.op("act", lambda e: e.copy(out=workc[0:48, :], in_=kb.banks[7].h[0:48, 0:256]),
                  reads=[kb.banks[7]], writes=[workc])

        def topk(wk, tv, ti, niter):
            for it in range(niter):
                sl = slice(it * 8, (it + 1) * 8)
                kb.op("dve", lambda e, sl=sl: e.max(out=tv[:, sl], in_=wk[:]), reads=[wk], writes=[tv.sub(it)])
                kb.op("dve", lambda e, sl=sl: e.max_index(out=ti[:, sl], in_max=tv[:, sl], in_values=wk[:]),
                      reads=[wk, tv.sub(it)], writes=[ti.sub(it)])
                kb.op("dve", lambda e, sl=sl: e.match_replace(out=wk[:], in_to_replace=tv[:, sl], in_values=wk[:], imm_value=-1.0),
                      reads=[tv.sub(it), wk], writes=[wk])

        if dbg == 2:
            da = kb.dram("dbg_work", [48, L], F32, kind="ExternalOutput")
            kb.dma("sp", lambda e: e.dma_start(out=da.ap[:, :], in_=work[:]), reads=[work], writes=[da])
            kb.release(m1)
            return
        topk(work, topv, topi, 32)
        if dbg == 3:
            da = kb.dram("dbg_topv", [48, 256], F32, kind="ExternalOutput")
            kb.dma("sp", lambda e: e.dma_start(out=da.ap[:, :], in_=topv[:]), reads=[topv], writes=[da])
            db_ = kb.dram("dbg_topi", [48, 256], U32, kind="ExternalOutput")
            kb.dma("sp", lambda e: e.dma_start(out=db_.ap[:, :], in_=topi[:]), reads=[topi], writes=[db_])
            kb.release(m1)
            return
        kb.op("dve", lambda e: e.tensor_copy(out=topif[:], in_=topi[:]), reads=[topi], writes=[topif])
        tb = kb.banks[5]
        for h in range(2):
            kb.op("pe", lambda e, h=h: e.transpose(out=tb.h[:, h * 48:(h + 1) * 48], in_=topif[0:48, h * 128:(h + 1) * 128],
                                                   identity=self.identf[0:48, 0:48]),
                  reads=[topif, self.identf], writes=[tb])
            kb.op("pe", lambda e, h=h: e.transpose(out=tb.h[:, 128 + h * 48:128 + (h + 1) * 48], in_=topv[0:48, h * 128:(h + 1) * 128],
                                                   identity=self.identf[0:48, 0:48]),
                  reads=[topv, self.identf], writes=[tb])
        kb.op("dve", lambda e: e.tensor_copy(out=idxT[:], in_=tb.h[:, 0:96].rearrange("p (h c) -> p h c", h=2)),
              reads=[tb], writes=[idxT])
        kb.op("dve", lambda e: e.tensor_copy(out=gT[:], in_=tb.h[:, 128:224].rearrange("p (h c) -> p h c", h=2)),
              reads=[tb], writes=[gT])
        if with_ctx:
            topk(workc, topvc, topic, 4)
            kb.op("dve", lambda e: e.tensor_copy(out=topicf[:], in_=topic[:]), reads=[topic], writes=[topicf])
            cbase = kb.alloc("cbase", [128, 1], F32)
            kb.dma("sp", lambda e: e.dma_start(out=cbase[:], in_=self.c_ctxbase.ap[:, :]), reads=[self.c_ctxbase], writes=[cbase])
            for (srcT, dstT, isidx) in ((topicf, idxC, True), (topvc, gC, False)):
                M = kb.alloc("Mc", [48, 128], F32)
                kb.op("dve", lambda e, M=M: e.memset(M[:], 0.0), writes=[M])
                kb.op("dve", lambda e, M=M, srcT=srcT: e.tensor_copy(out=M[0:16, 0:32], in_=srcT[0:16, 0:32]), reads=[srcT], writes=[M])
                kb.op("dve", lambda e, M=M, srcT=srcT: e.tensor_copy(out=M[32:48, 32:64], in_=srcT[32:48, 0:32]), reads=[srcT], writes=[M])
                kb.op("pe", lambda e, M=M: e.transpose(out=tb.h[:, 256:304], in_=M[0:48, :], identity=self.identf[0:48, 0:48]),
                      reads=[M, self.identf], writes=[tb])
                tcp = kb.alloc("tcp", [128, 48], F32)
                kb.op("dve", lambda e, tcp=tcp: e.tensor_copy(out=tcp[:], in_=tb.h[:, 256:304]), reads=[tb], writes=[tcp])
                tsum = kb.alloc("tsum", [128, NE], F32)
                kb.op("dve", lambda e, tcp=tcp, tsum=tsum: e.tensor_tensor(out=tsum[:], in0=tcp[:, 0:16], in1=tcp[:, 32:48], op=ALU.add),
                      reads=[tcp], writes=[tsum])
                if isidx:
                    kb.op("dve", lambda e, tsum=tsum: e.tensor_scalar(out=tsum[:], in0=tsum[:], scalar1=cbase[:, 0:1], scalar2=None, op0=ALU.add),
                          reads=[tsum, cbase], writes=[tsum])
                kb.op("dve", lambda e, tsum=tsum, dstT=dstT: e.tensor_copy(out=dstT[:], in_=tsum[:]), reads=[tsum], writes=[dstT])
        if dbg == 4:
            di = kb.dram("dbg_idx", [128, 96], I32, kind="ExternalOutput")
            dg = kb.dram("dbg_g", [128, 96], F32, kind="ExternalOutput")
            da = kb.dram("dbg_aff", [128, 1024], F32, kind="ExternalOutput")
            kb.dma("sp", lambda e: e.dma_start(out=di.ap[:, :], in_=idxT[:].rearrange("p h c -> p (h c)")), reads=[idxT], writes=[di])
            kb.dma("sp", lambda e: e.dma_start(out=dg.ap[:, :], in_=gT[:].rearrange("p h c -> p (h c)")), reads=[gT], writes=[dg])
            kb.dma("sp", lambda e: e.dma_start(out=da.ap[:, :], in_=aff2[:].rearrange("p h c -> p (h c)")), reads=[aff2], writes=[da])
            kb.release(m1)
            return
        kb.release(m1)

        G = []
        for s in range(NS):
            for h in range(2):
                G.append(dict(co=s * 256 + h * 128, idx=lambda e, s=s, h=h: idxT[:, h, s * 32 + e:s * 32 + e + 1],
                              gate=lambda e, s=s, h=h: gT[:, h, s * 32 + e:s * 32 + e + 1], r=s,
                              xn=self.xnl[s], dst=self.xr[s], n=L))
        if with_ctx:
            G.append(dict(co=512, idx=lambda e: idxC[:, e:e + 1], gate=lambda e: gC[:, e:e + 1], r=2,
                          xn=self.xnc_all, dst=self.xc_all, n=NS * LC + 128))
        NSL = 128 * len(G)
        HW = NSL // 2
        halves = [(0, HW), (HW, HW)]
        Xg = [[kb.alloc(f"xg{i}_{gi}", [128, D], BF16) for gi in range(len(G))] for i in range(2)]
        XeT = [kb.alloc(f"xeT{i}", [128, 8, NSL], BF16) for i in range(2)]
        hidT = kb.alloc("hidT", [128, 8, NSL], BF16)
        sgt = [kb.alloc(f"sgt{i}", [128, HW], F32) for i in range(2)]
        yo = [kb.alloc(f"yo{i}", [128, D], F32) for i in range(3)]
        nyo = 0
        nyb = 0

        def gathers(e):
            for gi, g in enumerate(G):
                xg = Xg[e % 2][gi]
                kb.dma("pool", lambda en, xg=xg, g=g, e=e: en.indirect_dma_start(
                    out=xg[:], out_offset=None, in_=g["xn"].ap[:, :], in_offset=IOA(ap=g["idx"](e), axis=0)),
                    reads=[g["xn"], idxT, idxC], writes=[xg])

        def transposes(e, gi):
            g = G[gi]
            co, r = g["co"], g["r"]
            xg = Xg[e % 2][gi]
            xe_ = XeT[e % 2]
            for k in range(8):
                bank = kb.banks[6 + k // 4]
                pv = bank.h.bitcast(BF16)
                kk = k % 4
                kb.op("pe", lambda en, pv=pv, xg=xg, k=k, kk=kk: en.transpose(
                    out=pv[:, kk * 128:(kk + 1) * 128], in_=xg[:, k * 128:(k + 1) * 128],
                    identity=self.identb[:]), reads=[xg, self.identb], writes=[bank])
            for k in range(8):
                bank = kb.banks[6 + k // 4]
                pv = bank.h.bitcast(BF16)
                kk = k % 4
                dst = xe_[:, k, co:co + 128]
                if k < 4:
                    kb.op("act", lambda en, dst=dst, pv=pv, kk=kk, r=r, k=k: en.activation(
                        out=dst, in_=pv[:, kk * 128:(kk + 1) * 128], func=AF.Identity,
                        scale=mulc2[:, k, r:r + 1], bias=addc2(k, r)),
                        reads=[bank], writes=[xe_.sub((k, co))])
                else:
                    kb.op("dve", lambda en, dst=dst, pv=pv, kk=kk, r=r, k=k: en.tensor_scalar(
                        out=dst, in0=pv[:, kk * 128:(kk + 1) * 128], scalar1=mulc2[:, k, r:r + 1],
                        scalar2=addc2(k, r), op0=ALU.mult, op1=ALU.add),
                        reads=[bank], writes=[xe_.sub((k, co))])

        gathers(0)
        gathers(1)
        for gi in range(len(G)):
            transposes(0, gi)
        for e in range(NE):
            if e + 2 < NE:
                pass
            w1t, w3t, w2t = wbufs[e % 2]
            xe = XeT[e % 2]
            for fc in range(8):
                for hi, (h0, hw) in enumerate(halves):
                    b1 = kb.banks[hi]
                    b3 = kb.banks[2 + hi]
                    for (wt, bm) in ((w1t, b1), (w3t, b3)):
                        for k in range(8):
                            kb.op("pe", lambda en, wt=wt, bm=bm, k=k, fc=fc, xe=xe, h0=h0, hw=hw: en.matmul(
                                out=bm.h[:, 0:hw], lhsT=wt[:, k, fc * 128:(fc + 1) * 128], rhs=xe[:, k, h0:h0 + hw],
                                start=(k == 0), stop=(k == 7)), reads=[wt, xe], writes=[bm])
                    sg = sgt[hi]
                    kb.op("act", lambda en, sg=sg, b1=b1, hw=hw: en.activation(out=sg[:, 0:hw], in_=b1.h[:, 0:hw], func=AF.Silu),
                          reads=[b1], writes=[sg])
                    kb.op("dve", lambda en, sg=sg, b3=b3, fc=fc, h0=h0, hw=hw: en.tensor_tensor(
                        out=hidT[:, fc, h0:h0 + hw], in0=sg[:, 0:hw], in1=b3.h[:, 0:hw], op=ALU.mult),
                        reads=[sg, b3], writes=[hidT.sub((fc, h0))])
            for gi, g in enumerate(G):
                if e + 1 < NE:
                    transposes(e + 1, gi)
                co, r = g["co"], g["r"]
                yt = yo[nyo % 3]
                nyo += 1
                for db in range(2):
                    by = kb.banks[4 + nyb % 2]
                    nyb += 1
                    for fc in range(8):
                        kb.op("pe", lambda en, by=by, fc=fc, co=co, db=db, w2t=w2t: en.matmul(
                            out=by.h[:, 0:512], lhsT=hidT[:, fc, co:co + 128], rhs=w2t[:, fc, db * 512:(db + 1) * 512],
                            start=(fc == 0), stop=(fc == 7)), reads=[hidT, w2t], writes=[by])
                    kb.op("dve", lambda en, by=by, yt=yt, db=db, g=g, e=e, r=r: en.scalar_tensor_tensor(
                        out=yt[:, db * 512:(db + 1) * 512], in0=by.h[:, 0:512], scalar=g["gate"](e),
                        in1=g2b[r][:, db * 512:(db + 1) * 512], op0=ALU.mult, op1=ALU.mult),
                        reads=[by, gT, gC, g2b[r]], writes=[yt.sub(db)])
                kb.dma("pool", lambda en, yt=yt, g=g, e=e: en.indirect_dma_start(
                    out=g["dst"].ap[:, :], out_offset=IOA(ap=g["idx"](e), axis=0), in_=yt[:, :], in_offset=None,
                    compute_op=ALU.add, bounds_check=kb.bnd[g["n"] - 1], oob_is_err=True),
                    reads=[yt, idxT, idxC], writes=[g["dst"]])
            if e + 2 < NE:
                gathers(e + 2)
                load_w(e + 2)

    def mixer0(self):
        kb = self.kb
        l = 0
        wbuf = kb.alloc("wbuf", [128, 8, 1536], BF16)
        wout = wbuf.view("woutv", [128, 8, D], BF16)
        csc = kb.alloc("csc", [128, 256], BF16)
        kb.dma("sp", lambda e: e.dma_start(out=csc[:], in_=self.c_csc.ap[:, :]), reads=[self.c_csc], writes=[csc])
        cwc = kb.alloc("cwc", [128, 4, 31], F32)
        for j in range(4):
            kb.dma("sp", lambda e, j=j: e.dma_start(out=cwc[:, j, :], in_=self.ab_conv_w.ap[:, j * 128:(j + 1) * 128].rearrange("k p -> p k")),
                   reads=[self.ab_conv_w], writes=[cwc.sub(j)])
        cols = {}
        for nm, dr in (("cb", self.ab_conv_b), ("lg", self.ab_ln_g), ("lb", self.ab_ln_b)):
            t = kb.alloc(nm + "c", [128, 4], F32)
            kb.dma("sp", lambda e, t=t, dr=dr: e.dma_start(out=t[:], in_=dr.ap.rearrange("(j p) -> p j", p=128)), reads=[dr], writes=[t])
            cols[nm] = t
        onesb = kb.alloc("onesb", [128, 128], BF16)
        kb.op("dve", lambda e: e.memset(onesb[:], 1.0 / 512.0), writes=[onesb])
        diag = kb.alloc("diag", [128, 4 * 31, 128], BF16)
        for j in range(4):
            for k in range(31):
                kb.op("dve", lambda e, j=j, k=k: e.tensor_scalar(
                    out=diag[:, j * 31 + k, :], in0=self.identb[:], scalar1=cwc[:, j, k:k + 1], scalar2=None, op0=ALU.mult),
                    reads=[self.identb, cwc], writes=[diag.sub((j, k))])
        bufs = self.norm_bufs(nx=2)
        xb = bufs[0][0]
        uTb = kb.alloc("uTb", [128, 8, 512], BF16)
        aT = kb.alloc("aT", [128, 4, L + 32], BF16)
        ufTb = kb.alloc("ufTb", [128, 4, 512], BF16)
        Yall = kb.alloc("Yall", [128, 16, 1024], BF16)
        mixT = kb.alloc("mixT", [128, 8, L], BF16)
        clb = kb.alloc("clb", [128, 16, 256], BF16)
        slb = kb.alloc("slb", [128, 16, 256], BF16)
        g1b = kb.alloc("g1b", [128, D], F32)
        cl2h = aT.view("cl2", [128, 16, 256], BF16, boff=0)
        sl2h = aT.view("sl2", [128, 16, 256], BF16, boff=8192)
        NBC = 256
        cT = kb.alloc("cT", [128, 4, NBC], F32)
        cbt = kb.alloc("cbt", [128, 4, NBC], BF16)
        c2t = kb.alloc("c2t", [128, 4, NBC], BF16)
        t_mean = kb.alloc("t_mean", [128, NBC], F32)
        t_rstd = kb.alloc("t_rstd", [128, NBC], F32)
        t_tmp = kb.alloc("t_tmp", [128, 512], F32)
        t_tmp2 = kb.alloc("t_tmp2", [128, NBC], F32)
        addc1 = lambda k, r: self.modc[l][:, k, r:r + 1]
        mulc1 = self.mul1c[l]
        B = kb.banks
        seqs = [(self.xin[0], self.xr[0], L, 0, self.c_cl, self.c_sl), (self.xin[1], self.xr[1], L, 1, self.c_cl, self.c_sl),
                (self.cin[0], self.xc[0], LC, 2, self.c_clc, self.c_slc), (self.cin[1], self.xc[1], LC, 2, self.c_clc, self.c_slc)]
        for (src, dst, Ls, r, ctab, stab) in seqs:
            kb.dma("pool", lambda e: e.dma_start(out=wbuf[:], in_=self.ab_w_in.ap.rearrange("(k p) f -> p k f", p=128)),
                   reads=[self.ab_w_in], writes=[wbuf])
            kb.dma("sp", lambda e, r=r: e.dma_start(out=g1b[:], in_=self.modd.ap[l, r, 2 * D:3 * D].partition_broadcast(128)),
                   reads=[self.modd.sub(l)], writes=[g1b])
            for j in range(4):
                kb.op("dve", lambda e, j=j: e.memset(aT[:, j, 0:15], 0.0), writes=[aT.sub((j, "h0"))])
                kb.op("dve", lambda e, j=j, Ls=Ls: e.memset(aT[:, j, 15 + Ls:32 + Ls], 0.0), writes=[aT.sub((j, "h1"))])
            nb = min(512, Ls)
            for blk in range(Ls // nb):
                t0 = blk * nb
                for sb in range(nb // 256):
                    self.norm_batch(src, t0 + sb * 256, 2, mulc1, addc1, r, uTb, sb * 256, bufs, [B[0], B[1], B[2], B[3]])
                def zmm(j, bank):
                    for k in range(8):
                        kb.op("pe", lambda e, j=j, k=k, bank=bank, nb=nb: e.matmul(
                            out=bank.h[:, 0:nb], lhsT=wbuf[:, k, j * 128:(j + 1) * 128], rhs=uTb[:, k, 0:nb],
                            start=(k == 0), stop=(k == 7)), reads=[wbuf, uTb], writes=[bank])
                for jj in range(4):
                    zmm(jj, B[4])
                    zmm(jj + 4, B[5])
                    kb.op("act", lambda e, nb=nb: e.activation(out=t_tmp[:, 0:nb], in_=B[5].h[:, 0:nb], func=AF.Sigmoid),
                          reads=[B[5]], writes=[t_tmp])
                    kb.op("dve", lambda e, jj=jj, nb=nb, t0=t0: e.tensor_tensor(
                        out=aT[:, jj, 15 + t0:15 + t0 + nb], in0=t_tmp[:, 0:nb], in1=B[4].h[:, 0:nb], op=ALU.mult),
                        reads=[t_tmp, B[4]], writes=[aT.sub((jj, t0))])
                for g in range(4):
                    zmm(8 + g, B[6])
                    kb.op("act", lambda e, g=g, nb=nb: e.copy(out=ufTb[:, g, 0:nb], in_=B[6].h[:, 0:nb]),
                          reads=[B[6]], writes=[ufTb.sub(g)])
                for tc in range(nb // 128):
                    c = (t0 // 128) + tc
                    for gp in range(2):
                        for gg in range(2):
                            g = gp * 2 + gg
                            kb.op("pe", lambda e, g=g, gg=gg, tc=tc: e.matmul(
                                out=B[7].h[:, gg * 256:(gg + 1) * 256], lhsT=ufTb[:, g, tc * 128:(tc + 1) * 128], rhs=csc[:, :],
                                start=True, stop=True), reads=[ufTb, csc], writes=[B[7]])
                        kb.op("dve", lambda e, c=c, gp=gp: e.tensor_copy(out=Yall[:, c, gp * 512:(gp + 1) * 512], in_=B[7].h[:, 0:512]),
                              reads=[B[7]], writes=[Yall.sub((c, gp))])
            nbc = min(NBC, Ls)
            for blk in range(Ls // nbc):
                t0 = blk * nbc
                for j in range(4):
                    bank = B[j % 2]
                    for k in range(31):
                        kb.op("pe", lambda e, j=j, k=k, bank=bank, t0=t0, nbc=nbc: e.matmul(
                            out=bank.h[:, 0:nbc], lhsT=diag[:, j * 31 + k, :], rhs=aT[:, j, t0 + k:t0 + k + nbc],
                            start=(k == 0), stop=(k == 30)), reads=[diag, aT], writes=[bank])
                    kb.op("act", lambda e, j=j, bank=bank, nbc=nbc: e.activation(
                        out=cT[:, j, 0:nbc], in_=bank.h[:, 0:nbc], func=AF.Identity, bias=cols["cb"][:, j:j + 1]),
                        reads=[bank, cols["cb"]], writes=[cT.sub(j)])
                    kb.op("pool", lambda e, j=j, nbc=nbc: e.tensor_copy(out=cbt[:, j, 0:nbc], in_=cT[:, j, 0:nbc]),
                          reads=[cT.sub(j)], writes=[cbt.sub(j)])
                    kb.op("pool", lambda e, j=j, nbc=nbc: e.tensor_tensor(out=c2t[:, j, 0:nbc], in0=cT[:, j, 0:nbc], in1=cT[:, j, 0:nbc], op=ALU.mult),
                          reads=[cT.sub(j)], writes=[c2t.sub(j)])
                for j in range(4):
                    kb.op("pe", lambda e, j=j, nbc=nbc: e.matmul(out=B[2].h[:, 0:nbc], lhsT=onesb[:], rhs=cbt[:, j, 0:nbc],
                                                                 start=(j == 0), stop=(j == 3)), reads=[onesb, cbt], writes=[B[2]])
                for j in range(4):
                    kb.op("pe", lambda e, j=j, nbc=nbc: e.matmul(out=B[3].h[:, 0:nbc], lhsT=onesb[:], rhs=c2t[:, j, 0:nbc],
                                                                 start=(j == 0), stop=(j == 3)), reads=[onesb, c2t], writes=[B[3]])
                kb.op("dve", lambda e, nbc=nbc: e.tensor_copy(out=t_mean[:, 0:nbc], in_=B[2].h[:, 0:nbc]), reads=[B[2]], writes=[t_mean])
                kb.op("dve", lambda e, nbc=nbc: e.tensor_tensor(out=t_tmp2[:, 0:nbc], in0=t_mean[:, 0:nbc], in1=t_mean[:, 0:nbc], op=ALU.mult),
                      reads=[t_mean], writes=[t_tmp2])
                kb.op("dve", lambda e, nbc=nbc: e.tensor_tensor(out=t_rstd[:, 0:nbc], in0=B[3].h[:, 0:nbc], in1=t_tmp2[:, 0:nbc], op=ALU.subtract),
                      reads=[B[3], t_tmp2], writes=[t_rstd])
                kb.op("act", lambda e, nbc=nbc: e.activation(out=t_rstd[:, 0:nbc], in_=t_rstd[:, 0:nbc], func=AF.Identity, bias=self.epsc[:, 0:1]),
                      reads=[t_rstd, self.epsc], writes=[t_rstd])
                kb.op("act", lambda e, nbc=nbc: e.activation(out=t_rstd[:, 0:nbc], in_=t_rstd[:, 0:nbc], func=AF.Sqrt),
                      reads=[t_rstd], writes=[t_rstd])
                kb.op("dve", lambda e, nbc=nbc: e.reciprocal(out=t_rstd[:, 0:nbc], in_=t_rstd[:, 0:nbc]), reads=[t_rstd], writes=[t_rstd])
                for j in range(4):
                    kb.op("pool", lambda e, j=j, nbc=nbc: e.tensor_tensor(out=cT[:, j, 0:nbc], in0=cT[:, j, 0:nbc], in1=t_mean[:, 0:nbc], op=ALU.subtract),
                          reads=[cT.sub(j), t_mean], writes=[cT.sub(j)])
                    kb.op("dve", lambda e, j=j, nbc=nbc: e.tensor_tensor(out=cT[:, j, 0:nbc], in0=cT[:, j, 0:nbc], in1=t_rstd[:, 0:nbc], op=ALU.mult),
                          reads=[cT.sub(j), t_rstd], writes=[cT.sub(j)])
                    kb.op("act", lambda e, j=j, nbc=nbc, t0=t0: e.activation(
                        out=mixT[:, j, t0:t0 + nbc], in_=cT[:, j, 0:nbc], func=AF.Silu,
                        scale=cols["lg"][:, j:j + 1], bias=cols["lb"][:, j:j + 1]),
                        reads=[cT.sub(j), cols["lg"], cols["lb"]], writes=[mixT.sub((j, t0))])
            ntc = Ls // 128
            for kbi in range(Ls // 256):
                k0 = kbi * 256
                if kbi % 2 == 0:
                    clh, slh, trc, trs = clb.h, slb.h, clb, slb
                else:
                    clh, slh, trc, trs = cl2h, sl2h, aT, aT
                kb.dma("sp", lambda e, ctab=ctab, k0=k0, ntc=ntc, clh=clh: e.dma_start(
                    out=clh[:, 0:ntc, :], in_=ctab.ap.rearrange("(c p) k -> p c k", p=128)[:, :, k0:k0 + 256]),
                    reads=[ctab], writes=[trc])
                kb.dma("sp", lambda e, stab=stab, k0=k0, ntc=ntc, slh=slh: e.dma_start(
                    out=slh[:, 0:ntc, :], in_=stab.ap.rearrange("(c p) k -> p c k", p=128)[:, :, k0:k0 + 256]),
                    reads=[stab], writes=[trs])
                for g in range(4):
                    bank = B[4 + g % 2]
                    for c in range(ntc):
                        kb.op("pe", lambda e, g=g, c=c, bank=bank, clh=clh: e.matmul(
                            out=bank.h[:, 0:256], lhsT=Yall[:, c, g * 256:g * 256 + 128], rhs=clh[:, c, :],
                            start=(c == 0), stop=False), reads=[Yall, trc], writes=[bank])
                        kb.op("pe", lambda e, g=g, c=c, bank=bank, ntc=ntc, slh=slh: e.matmul(
                            out=bank.h[:, 0:256], lhsT=Yall[:, c, g * 256 + 128:g * 256 + 256], rhs=slh[:, c, :],
                            start=False, stop=(c == ntc - 1)), reads=[Yall, trs], writes=[bank])
                    if g % 2 == 0:
                        kb.op("act", lambda e, g=g, bank=bank, k0=k0: e.copy(out=mixT[:, 4 + g, k0:k0 + 256], in_=bank.h[:, 0:256]),
                              reads=[bank], writes=[mixT.sub((4 + g, k0))])
                    else:
                        kb.op("dve", lambda e, g=g, bank=bank, k0=k0: e.tensor_copy(out=mixT[:, 4 + g, k0:k0 + 256], in_=bank.h[:, 0:256]),
                              reads=[bank], writes=[mixT.sub((4 + g, k0))])
            kb.dma("pool", lambda e: e.dma_start(out=wout[:], in_=self.ab_w_out.ap.rearrange("(k p) f -> p k f", p=128)),
                   reads=[self.ab_w_out], writes=[wbuf])
            for tc in range(ntc):
                kb.dma("sp", lambda e, src=src, tc=tc: e.dma_start(out=xb[:], in_=src.ap[tc * 128:(tc + 1) * 128, :]),
                       reads=[src], writes=[xb])
                for db in range(2):
                    bank = B[6 + db]
                    for m in range(8):
                        kb.op("pe", lambda e, m=m, db=db, bank=bank, tc=tc: e.matmul(
                            out=bank.h[:, 0:512], lhsT=mixT[:, m, tc * 128:(tc + 1) * 128], rhs=wout[:, m, db * 512:(db + 1) * 512],
                            start=(m == 0), stop=(m == 7)), reads=[mixT, wbuf], writes=[bank])
                    kb.op("dve", lambda e, db=db, bank=bank: e.tensor_tensor(
                        out=t_tmp[:, 0:512], in0=bank.h[:, 0:512], in1=g1b[:, db * 512:(db + 1) * 512], op=ALU.mult),
                        reads=[bank, g1b], writes=[t_tmp])
                    kb.op("pool", lambda e, db=db: e.tensor_tensor(
                        out=xb[:, db * 512:(db + 1) * 512], in0=xb[:, db * 512:(db + 1) * 512], in1=t_tmp[:, 0:512], op=ALU.add),
                        reads=[xb, t_tmp], writes=[xb])
                kb.dma("sp", lambda e, dst=dst, tc=tc: e.dma_start(out=dst.ap[tc * 128:(tc + 1) * 128, :], in_=xb[:]),
                       reads=[xb], writes=[dst.sub(("row", tc))])


    def mla(self):
        kb = self.kb
        l = 1
        B = kb.banks
        NKV = LC + L
        NT = NKV // 128
        SCALE = 96.0 ** -0.5
        cast = lambda dst, src_ap, tr: kb.dma("pool", lambda e: e.dma_start(out=dst[:], in_=src_ap), reads=[tr], writes=[dst])
        wi = kb.alloc("wi", [128, 8, 416], BF16)
        cast(wi, self.mla_w_in.ap.rearrange("(k p) f -> p k f", p=128), self.mla_w_in)
        wuq = kb.alloc("wuq", [128, 2, 1536], BF16)
        cast(wuq, self.mla_w_uq.ap.rearrange("(k p) f -> p k f", p=128), self.mla_w_uq)
        wukv = kb.alloc("wukv", [128, 2048], BF16)
        cast(wukv, self.mla_w_ukv.ap[:, :], self.mla_w_ukv)
        wo = kb.alloc("wo", [128, 8, D], BF16)
        cast(wo, self.mla_w_o.ap.rearrange("(k p) f -> p k f", p=128), self.mla_w_o)
        ropt = kb.alloc("ropt", [128, 16, 32], F32)
        kb.dma("sp", lambda e: e.dma_start(out=ropt[:], in_=self.c_rope.ap.rearrange("(c p) f -> p c f", p=128)),
               reads=[self.c_rope], writes=[ropt])
        qgc = kb.alloc("qgc", [128, 2], F32)
        kb.dma("sp", lambda e: e.dma_start(out=qgc[:], in_=self.mla_qg.ap.rearrange("(j p) -> p j", p=128)), reads=[self.mla_qg], writes=[qgc])
        kvgc = kb.alloc("kvgc", [128, 1], F32)
        kb.dma("sp", lambda e: e.dma_start(out=kvgc[:], in_=self.mla_kvg.ap.rearrange("(j p) -> p j", p=128)), reads=[self.mla_kvg], writes=[kvgc])
        ones1 = kb.alloc("ones1", [128, 128], BF16)
        kb.op("dve", lambda e: e.memset(ones1[:], 1.0), writes=[ones1])
        bufs = self.norm_bufs(nx=2)
        xb = bufs[0][0]
        uT = kb.alloc("uTall", [128, 8, NKV], BF16)
        cqnT = kb.alloc("cqnT", [128, 2, L], BF16)
        ckvnT = kb.alloc("ckvnT", [128, NKV], BF16)
        KTs = [kb.alloc(f"KT{i}", [128, NKV], BF16) for i in range(2)]
        QTs = [kb.alloc(f"QT{i}", [128, L], BF16) for i in range(2)]
        qtoks = [kb.alloc(f"qtok{i}", [128, 8, 96], BF16) for i in range(2)]
        Vaug = [kb.alloc(f"Vaug{i}", [128, NT, 128], BF16) for i in range(2)]
        kb.op("dve", lambda e: e.memset(Vaug[0][:].rearrange("p a b -> p (a b)"), 1.0), writes=[Vaug[0]])
        kb.op("dve", lambda e: e.memset(Vaug[1][:].rearrange("p a b -> p (a b)"), 1.0), writes=[Vaug[1]])
        pT = [kb.alloc(f"pT{i}", [128, 1024], BF16) for i in range(3)]
        oTs = kb.alloc("oTs", [128, 512], F32)
        onesf = kb.alloc("onesf", [128, 512], F32)
        kb.op("dve", lambda e: e.memset(onesf[:], 1.0), writes=[onesf])
        recf = kb.alloc("recf", [128, 512], F32)
        rech = kb.alloc("rech", [128, 512], BF16)
        recl = kb.alloc("recl", [128, 512], BF16)
        attnT = kb.alloc("attnT", [128, 8, L], BF16)
        g1b = kb.alloc("g1bm", [128, D], F32)
        t_tmp = kb.alloc("t_tmpm", [128, 512], F32)
        zts = [kb.alloc(f"zt{i}", [128, 416], F32) for i in range(2)]
        cqns = [kb.alloc(f"cqn{i}", [128, 256], BF16) for i in range(2)]
        ckvns = [kb.alloc(f"ckvn{i}", [128, 128], BF16) for i in range(2)]
        ktoks = [kb.alloc(f"ktok{i}", [128, 96], BF16) for i in range(2)]
        for kt_ in ktoks:
            kb.op("dve", lambda e, kt_=kt_: e.memset(kt_[:], 0.0), writes=[kt_])
        rtk = [[kb.alloc(f"rtk{i}_{j}", [128, 16], F32) for j in range(4)] for i in range(2)]
        rt = [kb.alloc(f"rt{i}", [128, 4, 16], F32) for i in range(4)]
        st2 = kb.alloc("st2", [128, 8 * NT], F32)
        kb.op("dve", lambda e: e.memset(st2[:], 0.0), writes=[st2])
        junk2 = kb.alloc("junk2", [128, 256], BF16)
        addc1 = lambda k, r: self.modc[l][:, k, r:r + 1]
        mulc1 = self.mul1c[l]
        npt = 0
        for s in range(NS):
            kb.dma("sp", lambda e, s=s: e.dma_start(out=g1b[:], in_=self.modd.ap[l, s, 2 * D:3 * D].partition_broadcast(128)),
                   reads=[self.modd.sub(l)], writes=[g1b])
            kb.op("dve", lambda e: e.memset(st2[:], 0.0), writes=[st2])
            for b2 in range(LC // 256):
                self.norm_batch(self.xc[s], b2 * 256, 2, mulc1, addc1, 2, uT, b2 * 256, bufs, [B[0], B[1], B[2], B[3]])
            for b2 in range(L // 256):
                self.norm_batch(self.xr[s], b2 * 256, 2, mulc1, addc1, s, uT, LC + b2 * 256, bufs, [B[0], B[1], B[2], B[3]])
            for c in range(NT):
                lat = c >= 2
                cl_ = c - 2
                zt, cqn, ckvn, ktok = zts[c % 2], cqns[c % 2], ckvns[c % 2], ktoks[c % 2]
                rk = rtk[c % 2]
                zb = B[4 + c % 2]
                for k in range(8):
                    kb.op("pe", lambda e, c=c, k=k, zb=zb: e.matmul(out=zb.h[:, 0:416], lhsT=uT[:, k, c * 128:(c + 1) * 128], rhs=wi[:, k, :],
                                                                  start=(k == 0), stop=(k == 7)), reads=[uT, wi], writes=[zb])
                kb.op("act", lambda e, zb=zb, zt=zt: e.copy(out=zt[:], in_=zb.h[:, 0:416]), reads=[zb], writes=[zt])
                so = c * 8
                parts = [(256, 128, 128.0, ckvn, 0)] + ([(0, 256, 256.0, cqn, 4)] if lat else [])
                for (c0, n, nf, dstt, o) in parts:
                    kb.op("act", lambda e, c0=c0, n=n, so=so, o=o, zt=zt: e.activation(out=junk2[:, 0:n], in_=zt[:, c0:c0 + n], func=AF.Square,
                                                                             accum_out=st2[:, so + o:so + o + 1]), reads=[zt], writes=[junk2, st2.sub(so + o)])
                    kb.op("act", lambda e, so=so, o=o, nf=nf: e.activation(out=st2[:, so + o + 1:so + o + 2], in_=st2[:, so + o:so + o + 1], func=AF.Identity,
                                                                         scale=1.0 / nf, bias=self.epsc[:, 0:1]), reads=[st2.sub(so + o), self.epsc], writes=[st2.sub(so + o + 1)])
                    kb.op("act", lambda e, so=so, o=o: e.activation(out=st2[:, so + o + 2:so + o + 3], in_=st2[:, so + o + 1:so + o + 2], func=AF.Sqrt),
                          reads=[st2.sub(so + o + 1)], writes=[st2.sub(so + o + 2)])
                    kb.op("dve", lambda e, so=so, o=o: e.reciprocal(out=st2[:, so + o + 3:so + o + 4], in_=st2[:, so + o + 2:so + o + 3]),
                          reads=[st2.sub(so + o + 2)], writes=[st2.sub(so + o + 3)])
                    kb.op("act", lambda e, c0=c0, n=n, so=so, o=o, dstt=dstt, zt=zt: e.activation(out=dstt[:, 0:n], in_=zt[:, c0:c0 + n], func=AF.Identity,
                                                                                          scale=st2[:, so + o + 3:so + o + 4]), reads=[zt, st2.sub(so + o + 3)], writes=[dstt])
                if lat:
                    krv = zt[:, 384:416].rearrange("p (i two) -> p i two", two=2)
                    xe, xo = krv[:, :, 0], krv[:, :, 1]
                    cs, sn = ropt[:, cl_, 0:16], ropt[:, cl_, 16:32]
                    ko = ktok[:, 64:96].rearrange("p (i two) -> p i two", two=2)
                    a0, a1, a2, a3 = rk[0][:, :], rk[1][:, :], rk[2][:, :], rk[3][:, :]
                    kb.op("pool", lambda e, xe=xe, cs=cs, a0=a0: e.tensor_tensor(out=a0, in0=xe, in1=cs, op=ALU.mult), reads=[zt, ropt], writes=[rk[0]])
                    kb.op("pool", lambda e, xo=xo, sn=sn, a1=a1: e.tensor_tensor(out=a1, in0=xo, in1=sn, op=ALU.mult), reads=[zt, ropt], writes=[rk[1]])
                    kb.op("pool", lambda e, xe=xe, sn=sn, a2=a2: e.tensor_tensor(out=a2, in0=xe, in1=sn, op=ALU.mult), reads=[zt, ropt], writes=[rk[2]])
                    kb.op("pool", lambda e, xo=xo, cs=cs, a3=a3: e.tensor_tensor(out=a3, in0=xo, in1=cs, op=ALU.mult), reads=[zt, ropt], writes=[rk[3]])
                    kb.op("pool", lambda e, ko=ko, a0=a0, a1=a1: e.tensor_tensor(out=ko[:, :, 0], in0=a0, in1=a1, op=ALU.subtract), reads=[rk[0], rk[1]], writes=[ktok.sub(0)])
                    kb.op("pool", lambda e, ko=ko, a2=a2, a3=a3: e.tensor_tensor(out=ko[:, :, 1], in0=a2, in1=a3, op=ALU.add), reads=[rk[2], rk[3]], writes=[ktok.sub(1)])
                else:
                    kb.op("pool", lambda e, ktok=ktok, zt=zt: e.tensor_copy(out=ktok[:, 64:96], in_=zt[:, 384:416]), reads=[zt], writes=[ktok.sub(0)])
                tbk = B[6 + c % 2]
                pv = tbk.h.bitcast(BF16)
                if lat:
                    for qk in range(2):
                        kb.op("pe", lambda e, pv=pv, qk=qk, cqn=cqn: e.transpose(out=pv[:, qk * 128:(qk + 1) * 128], in_=cqn[:, qk * 128:(qk + 1) * 128], identity=self.identb[:]),
                              reads=[cqn, self.identb], writes=[tbk])
                kb.op("pe", lambda e, pv=pv, ckvn=ckvn: e.transpose(out=pv[:, 256:384], in_=ckvn[:, :], identity=self.identb[:]), reads=[ckvn, self.identb], writes=[tbk])
                kb.op("pe", lambda e, pv=pv, ktok=ktok: e.transpose(out=pv[0:96, 384:512], in_=ktok[:, 0:96], identity=self.identb[:]), reads=[ktok, self.identb], writes=[tbk])
                if lat:
                    for qk in range(2):
                        kb.op("act", lambda e, pv=pv, qk=qk, cl_=cl_: e.activation(out=cqnT[:, qk, cl_ * 128:(cl_ + 1) * 128], in_=pv[:, qk * 128:(qk + 1) * 128],
                                                                                 func=AF.Identity, scale=qgc[:, qk:qk + 1]), reads=[tbk, qgc], writes=[cqnT.sub((qk, cl_))])
                kb.op("act", lambda e, pv=pv, c=c: e.activation(out=ckvnT[:, c * 128:(c + 1) * 128], in_=pv[:, 256:384], func=AF.Identity, scale=kvgc[:, 0:1]),
                      reads=[tbk, kvgc], writes=[ckvnT.sub(c)])
                for KTx in KTs:
                    kb.op("act", lambda e, pv=pv, c=c, KTx=KTx: e.copy(out=KTx[64:96, c * 128:(c + 1) * 128], in_=pv[64:96, 384:512]),
                          reads=[tbk], writes=[KTx.sub(("r", c))])
            def projA(h):
                KTh = KTs[h % 2]
                for blk in range(5):
                    n0 = blk * 512
                    nn = min(512, NKV - n0)
                    bk = B[7]
                    kb.op("pe", lambda e, h=h, n0=n0, nn=nn, bk=bk: e.matmul(out=bk.h[0:64, 0:nn], lhsT=wukv[:, h * 128:h * 128 + 64], rhs=ckvnT[:, n0:n0 + nn],
                                                                        start=True, stop=True), reads=[wukv, ckvnT], writes=[bk])
                    kb.op("dve", lambda e, n0=n0, nn=nn, bk=bk, KTh=KTh: e.tensor_copy(out=KTh[0:64, n0:n0 + nn], in_=bk.h[0:64, 0:nn]),
                          reads=[bk], writes=[KTh.sub(("n", blk))])
                va = Vaug[h % 2]
                vo = 0 if h % 2 == 0 else 64
                for vb in range(3):
                    ntl = min(8, NT - vb * 8)
                    bv = B[7]
                    for ci in range(ntl):
                        c = vb * 8 + ci
                        kb.op("pe", lambda e, h=h, c=c, ci=ci, bv=bv: e.matmul(out=bv.h[:, ci * 64:(ci + 1) * 64], lhsT=ckvnT[:, c * 128:(c + 1) * 128],
                                                                             rhs=wukv[:, h * 128 + 64:h * 128 + 128], start=True, stop=True), reads=[ckvnT, wukv], writes=[bv])
                    kb.op("dve", lambda e, vb=vb, ntl=ntl, bv=bv, va=va, vo=vo: e.tensor_copy(
                        out=va[:, vb * 8:vb * 8 + ntl, vo:vo + 64], in_=bv.h[:, 0:ntl * 64].rearrange("p (c f) -> p c f", f=64)),
                        reads=[bv], writes=[va.sub(vb)])

            def projQ(h, half, stage):
                qtk = qtoks[half]
                QTh = QTs[h % 2]
                for bq in range(2):
                    c0 = half * 8 + bq * 4
                    if stage == 0:
                        bqk = B[6 + bq]
                        for ci in range(4):
                            c = c0 + ci
                            for qk in range(2):
                                kb.op("pe", lambda e, h=h, c=c, ci=ci, qk=qk, bqk=bqk: e.matmul(
                                    out=bqk.h[:, ci * 96:(ci + 1) * 96], lhsT=cqnT[:, qk, c * 128:(c + 1) * 128], rhs=wuq[:, qk, h * 96:(h + 1) * 96],
                                    start=(qk == 0), stop=(qk == 1)), reads=[cqnT, wuq], writes=[bqk])
                        qv = bqk.h[:, 0:384].rearrange("p (c f) -> p c f", f=96)
                        lc0 = bq * 4
                        kb.op("dve", lambda e, qv=qv, lc0=lc0, qtk=qtk: e.tensor_copy(out=qtk[:, lc0:lc0 + 4, 0:64], in_=qv[:, :, 0:64]), reads=[bqk], writes=[qtk.sub((lc0, "n"))])
                        xe, xo = qv[:, :, 64:96:2], qv[:, :, 65:96:2]
                        cs, sn = ropt[:, c0:c0 + 4, 0:16], ropt[:, c0:c0 + 4, 16:32]
                        qo = qtk[:, lc0:lc0 + 4, 64:96].rearrange("p c (i two) -> p c i two", two=2)
                        kb.op("dve", lambda e, xe=xe, cs=cs: e.tensor_tensor(out=rt[0][:], in0=xe, in1=cs, op=ALU.mult), reads=[bqk, ropt], writes=[rt[0]])
                        kb.op("dve", lambda e, xo=xo, sn=sn: e.tensor_tensor(out=rt[1][:], in0=xo, in1=sn, op=ALU.mult), reads=[bqk, ropt], writes=[rt[1]])
                        kb.op("dve", lambda e, xe=xe, sn=sn: e.tensor_tensor(out=rt[2][:], in0=xe, in1=sn, op=ALU.mult), reads=[bqk, ropt], writes=[rt[2]])
                        kb.op("dve", lambda e, xo=xo, cs=cs: e.tensor_tensor(out=rt[3][:], in0=xo, in1=cs, op=ALU.mult), reads=[bqk, ropt], writes=[rt[3]])
                        kb.op("pool", lambda e, qo=qo: e.tensor_tensor(out=qo[:, :, :, 0], in0=rt[0][:], in1=rt[1][:], op=ALU.subtract),
                              reads=[rt[0], rt[1]], writes=[qtk.sub((lc0, "e"))])
                        kb.op("pool", lambda e, qo=qo: e.tensor_tensor(out=qo[:, :, :, 1], in0=rt[2][:], in1=rt[3][:], op=ALU.add),
                              reads=[rt[2], rt[3]], writes=[qtk.sub((lc0, "o"))])
                    else:
                        lc0 = bq * 4
                        tq = B[6 + bq]
                        pvq = tq.h.bitcast(BF16)
                        for ci in range(4):
                            kb.op("pe", lambda e, pvq=pvq, lc=lc0 + ci, ci=ci, qtk=qtk: e.transpose(out=pvq[0:96, ci * 128:(ci + 1) * 128], in_=qtk[:, lc, 0:96], identity=self.identb[:]),
                                  reads=[qtk, self.identb], writes=[tq])
                        kb.op("dve", lambda e, pvq=pvq, c0=c0, QTh=QTh: e.tensor_copy(out=QTh[0:96, c0 * 128:(c0 + 4) * 128], in_=pvq[0:96, 0:512]), reads=[tq], writes=[QTh.sub(c0)])

            def normalize1(h, qb, bo):
                kb.op("dve", lambda e, bo=bo: e.tensor_copy(out=oTs[:], in_=bo.h[:, 0:512]), reads=[bo], writes=[oTs])

            def normalize1b(h, qb):
                dp = 64 if (h % 2 == 0) else 0
                kb.op("dve", lambda e, dp=dp: e.reciprocal(out=recf[dp:dp + 1, :], in_=oTs[dp:dp + 1, :]), reads=[oTs], writes=[recf])
                kb.op("pool", lambda e, dp=dp: e.tensor_copy(out=rech[dp:dp + 1, :], in_=recf[dp:dp + 1, :]), reads=[recf], writes=[rech])
                kb.op("pool", lambda e, dp=dp: e.tensor_tensor(out=recl[dp:dp + 1, :], in0=recf[dp:dp + 1, :], in1=rech[dp:dp + 1, :], op=ALU.subtract),
                      reads=[recf, rech], writes=[recl])

            def normalize2(h, qb):
                even = (h % 2 == 0)
                dp = 64 if even else 0
                op_ = 0 if even else 64
                bb = B[6]
                kb.op("pe", lambda e, dp=dp, bb=bb: e.matmul(out=bb.h[:, 0:512], lhsT=ones1[dp:dp + 1, :], rhs=rech[dp:dp + 1, :], start=True, stop=False),
                      reads=[ones1, rech], writes=[bb])
                kb.op("pe", lambda e, dp=dp, bb=bb: e.matmul(out=bb.h[:, 0:512], lhsT=ones1[dp:dp + 1, :], rhs=recl[dp:dp + 1, :], start=False, stop=True),
                      reads=[ones1, recl], writes=[bb])
                kb.op("dve", lambda e, op_=op_, h=h, qb=qb, bb=bb: e.tensor_tensor(
                    out=attnT[op_:op_ + 64, h // 2, qb * 512:(qb + 1) * 512], in0=oTs[op_:op_ + 64, :], in1=bb.h[op_:op_ + 64, 0:512], op=ALU.mult),
                    reads=[oTs, bb], writes=[attnT.sub((h, qb))])

            projA(0)
            for half in range(2):
                projQ(0, half, 0)
                projQ(0, half, 1)
            pend = []
            pendnorm = []
            nstep = 0

            def issue_pv(item):
                (h, qb, c2, bo, pt, va) = item
                for half in range(2):
                    c = c2 * 2 + half
                    kb.op("pe", lambda e, c=c, bo=bo, pt=pt, va=va, half=half: e.matmul(
                        out=bo.h[:, 0:512], lhsT=va[:, c, :], rhs=pt[:, half * 512:(half + 1) * 512],
                        start=(c == 0), stop=(c == NT - 1)), reads=[va, pt], writes=[bo])
                if c2 == NT // 2 - 1:
                    normalize1(h, qb, bo)
                    pendnorm.append((nstep + 1, 1, h, qb))
                    pendnorm.append((nstep + 6, 2, h, qb))
                    pendnorm.sort()

            for h in range(NE):
                KTh = KTs[h % 2]
                QTh = QTs[h % 2]
                va = Vaug[h % 2]
                for qb in range(4):
                    bo = B[4 + qb % 2]
                    for c2 in range(NT // 2):
                        if h + 1 < NE:
                            if qb == 0 and c2 == 4:
                                projA(h + 1)
                            if qb == 1 and c2 == 4:
                                projQ(h + 1, 0, 0)
                            if qb == 2 and c2 == 4:
                                projQ(h + 1, 0, 1)
                            if qb == 3 and c2 == 3:
                                projQ(h + 1, 1, 0)
                            if qb == 3 and c2 == 8:
                                projQ(h + 1, 1, 1)
                        while pendnorm and nstep >= pendnorm[0][0]:
                            (_, stg, hh, qq) = pendnorm.pop(0)
                            (normalize1b if stg == 1 else normalize2)(hh, qq)
                        pi = nstep % 2
                        pt = pT[nstep % 3]
                        nstep += 1
                        for half in range(2):
                            c = c2 * 2 + half
                            bs = B[2 * pi + half]
                            kb.op("pe", lambda e, c=c, qb=qb, bs=bs, KTh=KTh, QTh=QTh: e.matmul(
                                out=bs.h[:, 0:512], lhsT=KTh[0:96, c * 128:(c + 1) * 128], rhs=QTh[0:96, qb * 512:(qb + 1) * 512],
                                start=True, stop=True), reads=[KTh, QTh], writes=[bs])
                        kb.op("act", lambda e, pi=pi, pt=pt: e.activation(out=pt[:, 0:1024], in_=kb.pairs[pi][:, 0:1024], func=AF.Exp, scale=SCALE),
                              reads=[B[2 * pi], B[2 * pi + 1]], writes=[pt])
                        pend.append((h, qb, c2, bo, pt, va))
                        if len(pend) > 1:
                            issue_pv(pend.pop(0))
            while pend:
                issue_pv(pend.pop(0))
            while pendnorm:
                (_, stg, hh, qq) = pendnorm.pop(0)
                (normalize1b if stg == 1 else normalize2)(hh, qq)
            for tc in range(L // 128):
                kb.dma("sp", lambda e, s=s, tc=tc: e.dma_start(out=xb[:], in_=self.xr[s].ap[tc * 128:(tc + 1) * 128, :]),
                       reads=[self.xr[s]], writes=[xb])
                for db in range(2):
                    bank = B[6 + db]
                    for m in range(8):
                        kb.op("pe", lambda e, m=m, db=db, bank=bank, tc=tc: e.matmul(
                            out=bank.h[:, 0:512], lhsT=attnT[:, m, tc * 128:(tc + 1) * 128], rhs=wo[:, m, db * 512:(db + 1) * 512],
                            start=(m == 0), stop=(m == 7)), reads=[attnT, wo], writes=[bank])
                    kb.op("dve", lambda e, db=db, bank=bank: e.tensor_tensor(
                        out=t_tmp[:, 0:512], in0=bank.h[:, 0:512], in1=g1b[:, db * 512:(db + 1) * 512], op=ALU.mult),
                        reads=[bank, g1b], writes=[t_tmp])
                    kb.op("pool", lambda e, db=db: e.tensor_tensor(
                        out=xb[:, db * 512:(db + 1) * 512], in0=xb[:, db * 512:(db + 1) * 512], in1=t_tmp[:, 0:512], op=ALU.add),
                        reads=[xb, t_tmp], writes=[xb])
                kb.dma("sp", lambda e, s=s, tc=tc: e.dma_start(out=self.xr[s].ap[tc * 128:(tc + 1) * 128, :], in_=xb[:]),
                       reads=[xb], writes=[self.xr[s].sub(("row", tc))])


def _consts():
    bf = ml_dtypes.bfloat16
    c = {}
    c["c_identb"] = np.eye(128, dtype=np.float32).astype(bf)
    c["c_identf"] = np.eye(128, dtype=np.float32)
    i = np.arange(128, dtype=np.int64)
    ang = 2.0 * np.pi * ((i[:, None] * i[None, :]) % 128).astype(np.float64) / 128.0
    c["c_csc"] = np.concatenate([np.cos(ang), np.sin(ang)], axis=1).astype(np.float32) / np.float32(np.sqrt(128.0))
    c["c_csc"] = c["c_csc"].astype(bf)
    for nm, n in (("", L), ("c", LC)):
        t = np.arange(n, dtype=np.int64)
        a = 2.0 * np.pi * ((t[:, None] * t[None, :]) % n).astype(np.float64) / n
        c["c_cl" + nm] = (np.cos(a) / np.sqrt(n)).astype(np.float32).astype(bf)
        c["c_sl" + nm] = (-np.sin(a) / np.sqrt(n)).astype(np.float32).astype(bf)
    t = np.arange(L)
    row = (t // 64).astype(np.float32)
    col = (t % 64).astype(np.float32)
    inv = (np.float32(10000.0) ** (-np.arange(8, dtype=np.float32) / np.float32(8))).astype(np.float32)
    angr = np.concatenate([row[:, None] * inv[None, :], col[:, None] * inv[None, :]], axis=1).astype(np.float32)
    c["c_rope"] = np.concatenate([np.cos(angr), np.sin(angr)], axis=1).astype(np.float32)
    c["c_ctxbase"] = np.concatenate([np.zeros(32), np.full(32, LC), NS * LC + np.arange(64)]).astype(np.float32).reshape(128, 1)
    return c


def _in_map(inp, core, consts):
    f = lambda a: np.ascontiguousarray(np.asarray(a, dtype=np.float32))
    s0 = core * NS
    m = {}
    m["x"] = f(inp["x"][s0:s0 + NS])
    m["ctx"] = f(inp["ctx"][s0:s0 + NS])
    cv3 = np.stack([inp["c"][s0], inp["c"][s0 + 1], inp["c_ctx"]], axis=0).astype(np.float32)
    m["cv"] = np.ascontiguousarray(cv3.reshape(3, 8, 128).transpose(2, 1, 0))
    for k in ("mod_w", "mod_b", "norm1_g", "norm2_g", "final_g", "moe_w_router", "moe_w1", "moe_w3", "moe_w2"):
        m[k] = f(inp[k])
    for k in ("ab_w_in", "ab_conv_w", "ab_conv_b", "ab_ln_g", "ab_ln_b", "ab_w_out", "mla_w_in", "mla_q_norm_g",
              "mla_kv_norm_g", "mla_w_uq", "mla_w_ukv", "mla_w_o"):
        m[k] = f(inp[k][0])
    m.update(consts)
    return m


_CACHE = {}


def run_prog(inputs, phases, copy_in=False, ncores=8, debug_route=False, raw=False):
    key = (tuple(phases), copy_in, debug_route)
    if key not in _CACHE:
        p = Prog(phases=phases, copy_in=copy_in)
        p.debug_route = debug_route
        _CACHE[key] = p.build()
    nc = _CACHE[key]
    consts = _consts()
    in_maps = [_in_map(inputs, c, consts) for c in range(ncores)]
    res = run_bass_kernel_spmd(nc, in_maps, core_ids=list(range(ncores)))
    if raw:
        return res.results
    return np.concatenate([np.asarray(r["y"]) for r in res.results], axis=0)


def kernel(**inputs):
    out = run_prog(inputs, ("mix0", "moe0", "mla1", "moe1", "final"))
    return out.astype(np.float32)
```

```python
import numpy as np
import ml_dtypes
from contextlib import ExitStack
import concourse.bass as bass
import concourse.mybir as mybir
from concourse.bass_utils import run_bass_kernel_spmd

F32 = mybir.dt.float32
BF16 = mybir.dt.bfloat16
I32 = mybir.dt.int32
U32 = mybir.dt.uint32
U8 = mybir.dt.uint8
AF = mybir.ActivationFunctionType
ALU = mybir.AluOpType
AX = mybir.AxisListType

D = 1024
L = 2048
LC = 256
NS = 2
NE = 16
EPS = 1e-6
DSZ = {F32: 4, BF16: 2, I32: 4, U32: 4, U8: 1}


class Trk:
    def __init__(self, name):
        self.name = name
        self.w = None
        self.r = []
        self.kids = {}
        self.parent = None

    def sub(self, key):
        if key not in self.kids:
            k = Trk(f"{self.name}.{key}")
            k.parent = self
            self.kids[key] = k
        return self.kids[key]

    def rdeps(self):
        s = set()
        if self.w:
            s.add(self.w)
        if self.parent is not None and self.parent.w:
            s.add(self.parent.w)
        for k in self.kids.values():
            if k.w:
                s.add(k.w)
        return s

    def wdeps(self):
        s = self.rdeps()
        s.update(self.r)
        if self.parent is not None:
            s.update(self.parent.r)
        for k in self.kids.values():
            s.update(k.r)
        return s

    def did_read(self, ev):
        self.r.append(ev)

    def did_write(self, ev):
        self.w = ev
        self.r = []
        for k in self.kids.values():
            k.w = None
            k.r = []


class T(Trk):
    def __init__(self, kb, name, shape, dtype, off):
        super().__init__(name)
        self.kb = kb
        self.shape = shape
        self.dtype = dtype
        self.off = off
        self.h = kb.nc.alloc_sbuf_tensor_at(name, list(shape), dtype, offset=off)

    def view(self, name, shape, dtype, boff=0):
        return self.kb.nc.alloc_sbuf_tensor_at(
            self.kb.uname(name), list(shape), dtype, offset=self.off + boff)

    def __getitem__(self, k):
        return self.h[k]


class Lane:
    def __init__(self, key, sem):
        self.key = key
        self.sem = sem
        self.count = 0


class KB:
    COMPUTE = ["pe", "act", "dve", "pool"]
    QUEUES = ["sp", "pool", "act"]

    def __init__(self, n_lanes=8):
        self.nc = bass.Bass("TRN2", target_bir_lowering=False)
        nc = self.nc
        self.es = ExitStack()
        self.uid = 0
        self.semobj = {}
        self.cnt = {}
        for e in self.COMPUTE:
            self.semobj[e] = self.es.enter_context(nc.semaphore("s_" + e))
            self.cnt[e] = 0
        self.lanes = {}
        self.lane_rr = {}
        for q in self.QUEUES:
            self.lanes[q] = []
            for i in range(n_lanes):
                key = f"d_{q}{i}"
                self.semobj[key] = self.es.enter_context(nc.semaphore(key))
                self.lanes[q].append(Lane(key, self.semobj[key]))
            self.lane_rr[q] = 0
        self.prog = {e: [] for e in ["pe", "act", "dve", "pool", "sp"]}
        self.waited = {e: {} for e in ["pe", "act", "dve", "pool", "sp"]}
        self.arena_bytes = 206 * 1024
        ah = nc.alloc_sbuf_tensor("arena", [128, self.arena_bytes], U8)
        self.abase = nc.lookup_mloc(ah).addr
        self.atop = 0
        self.bnd = {}
        for n in (L - 1, NS * LC + 128 - 1):
            reg = self.es.enter_context(nc.gpsimd.register(f"bnd{n}"))
            self.bnd[n] = reg
            self.prog["pool"].append(lambda en, reg=reg, n=n: en.reg_mov(reg, n))
        self.banks = []
        self.pairs = []
        for i in range(4):
            ph = self.es.enter_context(nc.psum_tensor(f"pbank{i}", [128, 1024], F32))
            self.pairs.append(ph)
            for j in range(2):
                t = Trk(f"bank{2 * i + j}")
                t.h = ph[:, j * 512:(j + 1) * 512]
                t.psum = True
                self.banks.append(t)

    def uname(self, n):
        self.uid += 1
        return f"{n}_{self.uid}"

    def alloc(self, name, shape, dtype):
        nbytes = int(np.prod(shape[1:])) * DSZ[dtype]
        nbytes = (nbytes + 63) // 64 * 64
        off = self.atop
        assert off + nbytes <= self.arena_bytes, f"SBUF arena overflow at {name}: {off}+{nbytes}"
        self.atop += nbytes
        return T(self, self.uname(name), shape, dtype, self.abase + off)

    def mark(self):
        return self.atop

    def release(self, m):
        self.barrier()
        self.atop = m

    def dram(self, name, shape, dtype, kind="Internal"):
        if kind == "Internal":
            h = self.nc.dram_tensor(name, list(shape), dtype)
        else:
            h = self.nc.dram_tensor(name, list(shape), dtype, kind=kind)
        t = Trk(name)
        t.h = h
        t.ap = h.ap()
        return t

    def _waits(self, eng, evs):
        best = {}
        for (k, v) in evs:
            if v > best.get(k, 0):
                best[k] = v
        for k, v in best.items():
            if k == "pe" and eng == "pe":
                continue
            if self.waited[eng].get(k, 0) >= v:
                continue
            self.waited[eng][k] = v
            sem = self.semobj[k]
            self.prog[eng].append(lambda e, sem=sem, v=v: e.wait_ge(sem, v))

    def _deps(self, reads, writes, eng=None):
        evs = set()
        for t in reads:
            evs |= t.rdeps()
            root = t if t.parent is None else t.parent
            if getattr(root, "psum", False):
                for ev in root.r:
                    if ev[0] != eng:
                        evs.add(ev)
                for k in root.kids.values():
                    for ev in k.r:
                        if ev[0] != eng:
                            evs.add(ev)
        for t in writes:
            evs |= t.wdeps()
        return evs

    def op(self, eng, fn, reads=(), writes=()):
        evs = self._deps(reads, writes, eng)
        self._waits(eng, evs)
        self.cnt[eng] += 1
        sem = self.semobj[eng]
        self.prog[eng].append(lambda e, fn=fn, sem=sem: fn(e).then_inc(sem, 1))
        ev = (eng, self.cnt[eng])
        for t in reads:
            t.did_read(ev)
        for t in writes:
            t.did_write(ev)
        return ev

    def dma(self, q, fn, reads=(), writes=()):
        evs = self._deps(reads, writes)
        lanes = self.lanes[q]
        lane = lanes[self.lane_rr[q] % len(lanes)]
        self.lane_rr[q] += 1
        if lane.count > 0:
            evs.add((lane.key, lane.count))
        self._waits(q, evs)
        lane.count += 16
        sem = lane.sem
        def run(e, fn=fn, sem=sem):
            try:
                ins = fn(e)
            except Exception:
                print("DMA BUILD FAIL line", fn.__code__.co_firstlineno, "defaults", [str(d)[:80] for d in (fn.__defaults__ or ())])
                raise
            ins.then_inc(sem, 16)
        self.prog[q].append(run)
        ev = (lane.key, lane.count)
        for t in reads:
            t.did_read(ev)
        for t in writes:
            t.did_write(ev)
        return ev

    def barrier(self):
        evs = set()
        for e in self.COMPUTE:
            if self.cnt[e] > 0:
                evs.add((e, self.cnt[e]))
        for q in self.QUEUES:
            for ln in self.lanes[q]:
                if ln.count > 0:
                    evs.add((ln.key, ln.count))
        for e in ["pe", "act", "dve", "pool", "sp"]:
            self._waits(e, evs)

    def finish(self):
        self.barrier()
        nc = self.nc
        with nc.allow_non_contiguous_dma(reason="small strided constant loads"):
            with nc.Block() as block:
                @block.sync
                def _(e):
                    for f in self.prog["sp"]:
                        f(e)

                @block.tensor
                def _(e):
                    for f in self.prog["pe"]:
                        f(e)

                @block.scalar
                def _(e):
                    for f in self.prog["act"]:
                        f(e)

                @block.vector
                def _(e):
                    for f in self.prog["dve"]:
                        f(e)

                @block.gpsimd
                def _(e):
                    for f in self.prog["pool"]:
                        f(e)
        self.es.close()
        return nc


class Prog:
    def __init__(self, phases=("mix0", "moe0", "mla1", "moe1", "final"), copy_in=False):
        self.kb = KB()
        self.phases = phases
        self.copy_in = copy_in
        kb = self.kb
        di = lambda n, s, d=F32: kb.dram(n, s, d, kind="ExternalInput")
        self.x = di("x", [NS, L, D])
        self.ctx = di("ctx", [NS, LC, D])
        self.cv = di("cv", [128, 8, 3])
        self.mod_w = di("mod_w", [2, D, 6 * D])
        self.mod_b = di("mod_b", [2, 6 * D])
        self.n1g = di("norm1_g", [2, D])
        self.n2g = di("norm2_g", [2, D])
        self.final_g = di("final_g", [D])
        self.ab_w_in = di("ab_w_in", [D, 1536])
        self.ab_conv_w = di("ab_conv_w", [31, 512])
        self.ab_conv_b = di("ab_conv_b", [512])
        self.ab_ln_g = di("ab_ln_g", [512])
        self.ab_ln_b = di("ab_ln_b", [512])
        self.ab_w_out = di("ab_w_out", [D, D])
        self.mla_w_in = di("mla_w_in", [D, 416])
        self.mla_qg = di("mla_q_norm_g", [256])
        self.mla_kvg = di("mla_kv_norm_g", [128])
        self.mla_w_uq = di("mla_w_uq", [256, 1536])
        self.mla_w_ukv = di("mla_w_ukv", [128, 2048])
        self.mla_w_o = di("mla_w_o", [D, D])
        self.w_router = di("moe_w_router", [2, D, NE])
        self.w1 = di("moe_w1", [2, NE, D, D])
        self.w3 = di("moe_w3", [2, NE, D, D])
        self.w2 = di("moe_w2", [2, NE, D, D])
        self.c_identb = di("c_identb", [128, 128], BF16)
        self.c_identf = di("c_identf", [128, 128], F32)
        self.c_csc = di("c_csc", [128, 256], BF16)
        self.c_cl = di("c_cl", [L, L], BF16)
        self.c_sl = di("c_sl", [L, L], BF16)
        self.c_clc = di("c_clc", [LC, LC], BF16)
        self.c_slc = di("c_slc", [LC, LC], BF16)
        self.c_rope = di("c_rope", [L, 32], F32)
        self.c_ctxbase = di("c_ctxbase", [128, 1], F32)
        self.out = kb.dram("y", [NS, L, D], F32, kind="ExternalOutput")
        self.xr = [kb.dram(f"xr{s}", [L, D], F32) for s in range(NS)]
        self.xc_all = kb.dram("xc_all", [NS * LC + 128, D], F32)
        self.xnc_all = kb.dram("xnc_all", [NS * LC + 128, D], BF16)
        self.xc = []
        self.xnl = [kb.dram(f"xnl{s}", [L, D], BF16) for s in range(NS)]
        self.xnc = []
        for s in range(NS):
            t = self.xc_all.sub(s); t.ap = self.xc_all.ap[s * LC:(s + 1) * LC, :]; self.xc.append(t)
            t = self.xnc_all.sub(s); t.ap = self.xnc_all.ap[s * LC:(s + 1) * LC, :]; self.xnc.append(t)
        self.xin = []
        self.cin = []
        for s in range(NS):
            t = self.x.sub(s); t.ap = self.x.ap[s]; self.xin.append(t)
            t = self.ctx.sub(s); t.ap = self.ctx.ap[s]; self.cin.append(t)
        self.modd = kb.dram("modd", [2, 3, 6 * D], F32)

    def build(self):
        kb = self.kb
        self.prologue()
        if self.copy_in:
            for s in range(NS):
                kb.dma("sp", lambda e, s=s: e.dma_start(out=self.xr[s].ap, in_=self.x.ap[s]),
                       reads=[self.x], writes=[self.xr[s]])
                kb.dma("sp", lambda e, s=s: e.dma_start(out=self.xc[s].ap, in_=self.ctx.ap[s]),
                       reads=[self.ctx], writes=[self.xc[s]])
            kb.barrier()
        for ph in self.phases:
            m = kb.mark()
            if ph == "mix0":
                self.mixer0()
            elif ph == "moe0":
                self.moe(0, with_ctx=True)
            elif ph == "mla1":
                self.mla()
            elif ph == "moe1":
                self.moe(1, with_ctx=False)
            elif ph == "final":
                self.final()
            elif ph == "dump":
                self.dump()
            kb.release(m)
        return kb.finish()

    def prologue(self):
        kb = self.kb
        self.identb = kb.alloc("identb", [128, 128], BF16)
        self.identf = kb.alloc("identf", [128, 128], F32)
        kb.dma("sp", lambda e: e.dma_start(out=self.identb[:], in_=self.c_identb.ap[:, :]),
               reads=[self.c_identb], writes=[self.identb])
        kb.dma("sp", lambda e: e.dma_start(out=self.identf[:], in_=self.c_identf.ap[:, :]),
               reads=[self.c_identf], writes=[self.identf])
        self.epsc = kb.alloc("epsc", [128, 1], F32)
        kb.op("dve", lambda e: e.memset(self.epsc[:], EPS), writes=[self.epsc])
        self.zeroc = kb.alloc("zeroc", [128, 1], F32)
        kb.op("dve", lambda e: e.memset(self.zeroc[:], 0.0), writes=[self.zeroc])
        self.modc = [kb.alloc(f"modc{l}", [128, 48, 3], F32) for l in range(2)]
        self.mul1c = [kb.alloc(f"mul1c{l}", [128, 8, 3], F32) for l in range(2)]
        self.mul2c = [kb.alloc(f"mul2c{l}", [128, 8, 3], F32) for l in range(2)]
        self.n1gc = kb.alloc("n1gc", [128, 2, 8], F32)
        self.n2gc = kb.alloc("n2gc", [128, 2, 8], F32)
        kb.dma("sp", lambda e: e.dma_start(out=self.n1gc[:], in_=self.n1g.ap.rearrange("l (k p) -> p l k", p=128)),
               reads=[self.n1g], writes=[self.n1gc])
        kb.dma("sp", lambda e: e.dma_start(out=self.n2gc[:], in_=self.n2g.ap.rearrange("l (k p) -> p l k", p=128)),
               reads=[self.n2g], writes=[self.n2gc])
        m0 = kb.mark()
        zf = kb.alloc("zf", [128, D], F32)
        zb = kb.alloc("zb", [128, D], BF16)
        kb.op("dve", lambda e: e.memset(zf[:], 0.0), writes=[zf])
        kb.op("dve", lambda e: e.memset(zb[:], 0.0), writes=[zb])
        kb.dma("sp", lambda e: e.dma_start(out=self.xc_all.ap[NS * LC:NS * LC + 128, :], in_=zf[:]),
               reads=[zf], writes=[self.xc_all.sub("pad")])
        kb.dma("sp", lambda e: e.dma_start(out=self.xnc_all.ap[NS * LC:NS * LC + 128, :], in_=zb[:]),
               reads=[zb], writes=[self.xnc_all.sub("pad")])
        cvt = kb.alloc("cvt", [128, 24], F32)
        sct = kb.alloc("sct", [128, 24], BF16)
        kb.dma("sp", lambda e: e.dma_start(out=cvt[:], in_=self.cv.ap.rearrange("p k r -> p (k r)")),
               reads=[self.cv], writes=[cvt])
        kb.op("act", lambda e: e.activation(out=sct[:], in_=cvt[:], func=AF.Silu), reads=[cvt], writes=[sct])
        mwt = [kb.alloc(f"mwt{i}", [128, 8, 1536], BF16) for i in range(3)]
        mrow = kb.alloc("mrow", [3, 6 * D], F32)
        mb3 = kb.alloc("mb3", [3, 6 * D], F32)
        it = 0
        for l in range(2):
            kb.dma("sp", lambda e, l=l: e.dma_start(out=mb3[:], in_=self.mod_b.ap[l].partition_broadcast(3)),
                   reads=[self.mod_b], writes=[mb3])
            for pc in range(4):
                wt = mwt[it % 3]
                it += 1
                src = self.mod_w.ap[l].rearrange("(k p) n -> p k n", p=128)[:, :, pc * 1536:(pc + 1) * 1536]
                kb.dma("pool", lambda e, wt=wt, src=src: e.dma_start(out=wt[:], in_=src),
                       reads=[self.mod_w], writes=[wt])
                for nb in range(3):
                    bank = kb.banks[(pc * 3 + nb) % 2]
                    for k in range(8):
                        kb.op("pe", lambda e, bank=bank, wt=wt, k=k, nb=nb: e.matmul(
                            out=bank.h[0:3, 0:512], lhsT=sct[:, k * 3:(k + 1) * 3],
                            rhs=wt[:, k, nb * 512:(nb + 1) * 512], start=(k == 0), stop=(k == 7)),
                            reads=[sct, wt], writes=[bank])
                    c0 = pc * 1536 + nb * 512
                    kb.op("dve", lambda e, bank=bank, c0=c0: e.tensor_tensor(
                        out=mrow[0:3, c0:c0 + 512], in0=bank.h[0:3, 0:512], in1=mb3[0:3, c0:c0 + 512], op=ALU.add),
                        reads=[bank, mb3], writes=[mrow.sub(c0)])
            kb.dma("sp", lambda e, l=l: e.dma_start(out=self.modd.ap[l], in_=mrow[0:3, :]),
                   reads=[mrow], writes=[self.modd.sub(l)])
            for r in range(3):
                kb.dma("sp", lambda e, l=l, r=r: e.dma_start(
                    out=self.modc[l][:, :, r], in_=self.modd.ap[l, r].rearrange("(c p) -> p c", p=128)),
                    reads=[self.modd.sub(l)], writes=[self.modc[l].sub(r)])
            for (mulc, gc, v) in ((self.mul1c[l], self.n1gc, 1), (self.mul2c[l], self.n2gc, 4)):
                kb.op("dve", lambda e, mulc=mulc, v=v, l=l: e.tensor_scalar(
                    out=mulc[:], in0=self.modc[l][:, v * 8:(v + 1) * 8, :], scalar1=1.0, scalar2=None, op0=ALU.add),
                    reads=[self.modc[l]], writes=[mulc])
                for r in range(3):
                    kb.op("dve", lambda e, mulc=mulc, gc=gc, r=r, l=l: e.tensor_tensor(
                        out=mulc[:, :, r], in0=mulc[:, :, r], in1=gc[:, l, :], op=ALU.mult),
                        reads=[mulc, gc], writes=[mulc])
        kb.release(m0)

    def norm_batch(self, src, row0, nt, mulc, addc, r, uT, ucol0, bufs, banks, xn_dst=None):
        kb = self.kb
        xb, xnb, junk, stat = bufs
        tiles = []
        for j in range(nt):
            xt = xb[self._nb % len(xb)]
            xn = xnb[self._nb % len(xnb)]
            sc = self._nb % 64
            self._nb += 1
            tiles.append((j, xt, xn, sc))
            rr = row0 + j * 128
            kb.dma("sp", lambda e, xt=xt, rr=rr: e.dma_start(out=xt[:], in_=src.ap[rr:rr + 128, :]),
                   reads=[src], writes=[xt])
            kb.op("act", lambda e, xt=xt, sc=sc: e.activation(
                out=junk[:], in_=xt[:], func=AF.Square, accum_out=stat[:, sc:sc + 1]),
                reads=[xt], writes=[junk, stat.sub(sc)])
            kb.op("act", lambda e, sc=sc: e.activation(
                out=stat[:, 64 + sc:65 + sc], in_=stat[:, sc:sc + 1], func=AF.Identity, scale=1.0 / D, bias=self.epsc[:, 0:1]),
                reads=[stat.sub(sc), self.epsc], writes=[stat.sub(64 + sc)])
            kb.op("act", lambda e, sc=sc: e.activation(
                out=stat[:, 128 + sc:129 + sc], in_=stat[:, 64 + sc:65 + sc], func=AF.Sqrt),
                reads=[stat.sub(64 + sc)], writes=[stat.sub(128 + sc)])
            kb.op("dve", lambda e, sc=sc: e.reciprocal(out=stat[:, 192 + sc:193 + sc], in_=stat[:, 128 + sc:129 + sc]),
                  reads=[stat.sub(128 + sc)], writes=[stat.sub(192 + sc)])
            if len(xb) == 1:
                self._norm_tail(tiles.pop(), src, row0, mulc, addc, r, uT, ucol0, banks, xn_dst)
        for t in tiles:
            self._norm_tail(t, src, row0, mulc, addc, r, uT, ucol0, banks, xn_dst)

    def _norm_tail(self, t, src, row0, mulc, addc, r, uT, ucol0, banks, xn_dst):
        kb = self.kb
        (j, xt, xn, sc) = t
        stat = self._stat
        kb.op("dve", lambda e, xt=xt, xn=xn, sc=sc: e.tensor_scalar(
            out=xn[:], in0=xt[:], scalar1=stat[:, 192 + sc:193 + sc], scalar2=None, op0=ALU.mult),
            reads=[xt, stat.sub(192 + sc)], writes=[xn])
        if xn_dst is not None:
            dt, drow = xn_dst
            dr0 = drow + j * 128
            kb.dma("pool", lambda e, xn=xn, dt=dt, dr=dr0: e.dma_start(
                out=dt.ap[dr:dr + 128, :], in_=xn[:]), reads=[xn], writes=[dt.sub(dr0)])
        for k in range(8):
            bank = banks[2 * j + k // 4]
            pv = bank.h.bitcast(BF16)
            kk = k % 4
            kb.op("pe", lambda e, pv=pv, xn=xn, k=k, kk=kk: e.transpose(
                out=pv[:, kk * 128:(kk + 1) * 128], in_=xn[:, k * 128:(k + 1) * 128], identity=self.identb[:]),
                reads=[xn, self.identb], writes=[bank])
        for k in range(8):
            bank = banks[2 * j + k // 4]
            pv = bank.h.bitcast(BF16)
            kk = k % 4
            dst = uT[:, k, ucol0 + j * 128: ucol0 + (j + 1) * 128]
            if k < 4:
                kb.op("act", lambda e, dst=dst, pv=pv, k=k, kk=kk: e.activation(
                    out=dst, in_=pv[:, kk * 128:(kk + 1) * 128], func=AF.Identity,
                    scale=mulc[:, k, r:r + 1], bias=addc(k, r)),
                    reads=[bank, mulc], writes=[uT.sub((k, ucol0 + j * 128))])
            else:
                kb.op("dve", lambda e, dst=dst, pv=pv, k=k, kk=kk: e.tensor_scalar(
                    out=dst, in0=pv[:, kk * 128:(kk + 1) * 128], scalar1=mulc[:, k, r:r + 1],
                    scalar2=addc(k, r), op0=ALU.mult, op1=ALU.add),
                    reads=[bank, mulc], writes=[uT.sub((k, ucol0 + j * 128))])

    def norm_bufs(self, nx=2):
        kb = self.kb
        self._nb = 0
        xb = [kb.alloc(f"xb{i}", [128, D], F32) for i in range(nx)]
        xnb = [kb.alloc(f"xnb{i}", [128, D], BF16) for i in range(nx)]
        junk = kb.alloc("junk", [128, D], BF16)
        stat = kb.alloc("stat", [128, 256], F32)
        kb.op("dve", lambda e: e.memset(stat[:], 0.0), writes=[stat])
        self._stat = stat
        return (xb, xnb, junk, stat)

    def dump(self):
        kb = self.kb
        yc = kb.dram("yc", [NS * LC, D], F32, kind="ExternalOutput")
        kb.dma("sp", lambda e: e.dma_start(out=yc.ap[:, :], in_=self.xc_all.ap[0:NS * LC, :]),
               reads=[self.xc_all], writes=[yc])
        for s in range(NS):
            kb.dma("sp", lambda e, s=s: e.dma_start(out=self.out.ap[s], in_=self.xr[s].ap),
                   reads=[self.xr[s]], writes=[self.out.sub(s)])

    def final(self):
        kb = self.kb
        fgb = kb.alloc("fgb", [128, D], F32)
        kb.dma("sp", lambda e: e.dma_start(out=fgb[:], in_=self.final_g.ap.partition_broadcast(128)),
               reads=[self.final_g], writes=[fgb])
        NB = 8
        xb = [kb.alloc(f"fxb{i}", [128, D], F32) for i in range(NB)]
        yb = [kb.alloc(f"fyb{i}", [128, D], F32) for i in range(NB)]
        junk = kb.alloc("fjunk", [128, D], BF16)
        stat = kb.alloc("fstat", [128, 4 * 64], F32)
        kb.op("dve", lambda e: e.memset(stat[:], 0.0), writes=[stat])
        tiles = [(s, t) for s in range(NS) for t in range(L // 128)]
        for b0 in range(0, len(tiles), 4):
            batch = tiles[b0:b0 + 4]
            info = []
            for bi, (s, t) in enumerate(batch):
                i = b0 + bi
                xt = xb[i % NB]
                yt = yb[i % NB]
                sc = i % 64
                info.append((s, t, xt, yt, sc))
                kb.dma("sp", lambda e, xt=xt, s=s, t=t: e.dma_start(out=xt[:], in_=self.xr[s].ap[t * 128:(t + 1) * 128, :]),
                       reads=[self.xr[s]], writes=[xt])
                kb.op("act", lambda e, xt=xt, sc=sc: e.activation(
                    out=junk[:], in_=xt[:], func=AF.Square, accum_out=stat[:, sc:sc + 1]),
                    reads=[xt], writes=[junk, stat.sub(sc)])
                kb.op("act", lambda e, sc=sc: e.activation(
                    out=stat[:, 64 + sc:65 + sc], in_=stat[:, sc:sc + 1], func=AF.Identity, scale=1.0 / D, bias=self.epsc[:, 0:1]),
                    reads=[stat.sub(sc), self.epsc], writes=[stat.sub(64 + sc)])
                kb.op("act", lambda e, sc=sc: e.activation(
                    out=stat[:, 128 + sc:129 + sc], in_=stat[:, 64 + sc:65 + sc], func=AF.Sqrt),
                    reads=[stat.sub(64 + sc)], writes=[stat.sub(128 + sc)])
                kb.op("dve", lambda e, sc=sc: e.reciprocal(out=stat[:, 192 + sc:193 + sc], in_=stat[:, 128 + sc:129 + sc]),
                      reads=[stat.sub(128 + sc)], writes=[stat.sub(192 + sc)])
            for bi, (s, t, xt, yt, sc) in enumerate(info):
                eng = "dve"
                kb.op(eng, lambda e, xt=xt, yt=yt, sc=sc: e.scalar_tensor_tensor(
                    out=yt[:], in0=xt[:], scalar=stat[:, 192 + sc:193 + sc], in1=fgb[:], op0=ALU.mult, op1=ALU.mult),
                    reads=[xt, stat.sub(192 + sc), fgb], writes=[yt])
                kb.dma("pool", lambda e, yt=yt, s=s, t=t: e.dma_start(out=self.out.ap[s, t * 128:(t + 1) * 128, :], in_=yt[:]),
                       reads=[yt], writes=[self.out.sub((s, t))])

    def moe(self, l, with_ctx):
        kb = self.kb
        IOA = bass.IndirectOffsetOnAxis
        wbufs = [[kb.alloc(f"w{n}_{i}", [128, 8, D], BF16) for n in (1, 3, 2)] for i in range(2)]
        wsrc = (self.w1, self.w3, self.w2)

        def load_w(e):
            for n in range(3):
                src = wsrc[n].ap[l, e].rearrange("(k p) f -> p k f", p=128)
                wt = wbufs[e % 2][n]
                kb.dma("pool", lambda en, wt=wt, src=src: en.dma_start(out=wt[:], in_=src),
                       reads=[wsrc[n]], writes=[wt])

        nr = 3 if with_ctx else 2
        g2b = [kb.alloc(f"g2b{r}", [128, D], F32) for r in range(nr)]
        for r in range(nr):
            kb.dma("sp", lambda e, r=r: e.dma_start(
                out=g2b[r][:], in_=self.modd.ap[l, r, 5 * D:6 * D].partition_broadcast(128)),
                reads=[self.modd.sub(l)], writes=[g2b[r]])
        idxT = kb.alloc("idxT", [128, 2, 48], I32)
        gT = kb.alloc("gT", [128, 2, 48], F32)
        idxC = kb.alloc("idxC", [128, NE], I32)
        gC = kb.alloc("gC", [128, NE], F32)
        load_w(0)
        load_w(1)
        addc2 = lambda k, r: self.modc[l][:, 24 + k, r:r + 1]
        mulc2 = self.mul2c[l]

        dbg = getattr(self, "debug_route", 0)
        if dbg == 10:
            return
        m1 = kb.mark()
        bufs = self.norm_bufs(nx=4)
        uTb = [kb.alloc(f"uTb{i}", [128, 8, 256], BF16) for i in range(2)]
        wr = kb.alloc("wr", [128, 8, NE], BF16)
        wrf = kb.alloc("wrf", [128, 8, NE], F32)
        kb.dma("sp", lambda e: e.dma_start(out=wrf[:], in_=self.w_router.ap[l].rearrange("(k p) e -> p k e", p=128)),
               reads=[self.w_router], writes=[wrf])
        kb.op("dve", lambda e: e.tensor_copy(out=wr[:], in_=wrf[:]), reads=[wrf], writes=[wr])
        aff2 = kb.alloc("aff2", [128, 16, 64], F32)
        affc = kb.alloc("affc", [128, 2, 64], F32)
        kb.op("dve", lambda e: e.memset(aff2[:].rearrange("p a b -> p (a b)"), 0.0), writes=[aff2])
        kb.op("dve", lambda e: e.memset(affc[:].rearrange("p a b -> p (a b)"), 0.0), writes=[affc])
        lg = kb.alloc("lg", [128, 16, 16], F32)
        mx = kb.alloc("mx", [128, 16], F32)
        sm = kb.alloc("sm", [128, 16], F32)
        rs = kb.alloc("rs", [128, 16], F32)
        work = kb.alloc("work", [48, L], F32)
        workc = kb.alloc("workc", [48, LC], F32)
        topv = kb.alloc("topv", [48, 256], F32)
        topi = kb.alloc("topi", [48, 256], U32)
        topif = kb.alloc("topif", [48, 256], F32)
        topvc = kb.alloc("topvc", [48, 32], F32)
        topic = kb.alloc("topic", [48, 32], U32)
        topicf = kb.alloc("topicf", [48, 32], F32)
        nbatch = 0
        seqs = []
        for s in range(NS):
            seqs.append((self.xr[s], self.xnl[s], L // 128, s, aff2, s * 32, kb.banks[4 + s], 0))
        if with_ctx:
            for s in range(NS):
                seqs.append((self.xc[s], self.xnc[s], LC // 128, 2, affc, s * 32, kb.banks[6], s * 32))
        for (src, xnd, ntl, r, afft, acol, lbank, lcol0) in seqs:
            for b in range((ntl + 1) // 2):
                nt = min(2, ntl - b * 2)
                ub = uTb[nbatch % 2]
                nbatch += 1
                self.norm_batch(src, b * 256, nt, mulc2, addc2, r, ub, 0, bufs,
                                [kb.banks[j] for j in range(2 * nt)], xn_dst=(xnd, b * 256))
                for j in range(nt):
                    if dbg == 11:
                        continue
                    c = b * 2 + j
                    for k in range(8):
                        kb.op("pe", lambda e, lbank=lbank, lc=lcol0 + c * 16, ub=ub, k=k, j=j: e.matmul(
                            out=lbank.h[:, lc:lc + 16], lhsT=ub[:, k, j * 128:(j + 1) * 128], rhs=wr[:, k, :],
                            start=(k == 0), stop=(k == 7)),
                            reads=[ub.sub((k, j * 128)), wr], writes=[lbank])
            if dbg == 11:
                continue
            lv = lbank.h[:, lcol0:lcol0 + ntl * 16].rearrange("p (c e) -> p c e", e=16)
            bc = lambda t, ntl=ntl: t[:, 0:ntl].unsqueeze(2).to_broadcast([128, ntl, 16])
            kb.op("dve", lambda e, lv=lv, ntl=ntl: e.tensor_reduce(out=mx[:, 0:ntl], in_=lv, axis=AX.X, op=ALU.max),
                  reads=[lbank], writes=[mx])
            kb.op("dve", lambda e, lv=lv, bc=bc, ntl=ntl: e.tensor_tensor(out=lg[:, 0:ntl, :], in0=lv, in1=bc(mx), op=ALU.subtract),
                  reads=[lbank, mx], writes=[lg])
            kb.op("act", lambda e, ntl=ntl: e.activation(out=lg[:, 0:ntl, :], in_=lg[:, 0:ntl, :], func=AF.Exp),
                  reads=[lg], writes=[lg])
            kb.op("dve", lambda e, ntl=ntl: e.tensor_reduce(out=sm[:, 0:ntl], in_=lg[:, 0:ntl, :], axis=AX.X, op=ALU.add),
                  reads=[lg], writes=[sm])
            kb.op("dve", lambda e, ntl=ntl: e.reciprocal(out=rs[:, 0:ntl], in_=sm[:, 0:ntl]), reads=[sm], writes=[rs])
            kb.op("dve", lambda e, afft=afft, acol=acol, bc=bc, ntl=ntl: e.tensor_tensor(
                out=afft[:, 0:ntl, acol:acol + 16], in0=lg[:, 0:ntl, :], in1=bc(rs), op=ALU.mult),
                reads=[lg, rs], writes=[afft])
        if dbg == 11:
            kb.release(m1)
            return
        if dbg == 1:
            da = kb.dram("dbg_aff", [128, 1024], F32, kind="ExternalOutput")
            kb.dma("sp", lambda e: e.dma_start(out=da.ap[:, :], in_=aff2[:].rearrange("p h c -> p (h c)")), reads=[aff2], writes=[da])
            kb.release(m1)
            return
        for c in range(16):
            bank = kb.banks[c // 4]
            kb.op("pe", lambda e, bank=bank, c=c: e.transpose(
                out=bank.h[0:48, (c % 4) * 128:(c % 4 + 1) * 128], in_=aff2[:, c, 0:48], identity=self.identf[:]),
                reads=[aff2, self.identf], writes=[bank])
        for q in range(4):
            eng = "act" if q % 2 == 0 else "dve"
            if eng == "act":
                kb.op("act", lambda e, q=q: e.copy(out=work[0:48, q * 512:(q + 1) * 512], in_=kb.banks[q].h[0:48, 0:512]),
                      reads=[kb.banks[q]], writes=[work.sub(q)])
            else:
                kb.op("dve", lambda e, q=q: e.tensor_copy(out=work[0:48, q * 512:(q + 1) * 512], in_=kb.banks[q].h[0:48, 0:512]),
                      reads=[kb.banks[q]], writes=[work.sub(q)])
        if with_ctx:
            for c in range(2):
                kb.op("pe", lambda e, c=c: e.transpose(
                    out=kb.banks[7].h[0:48, c * 128:(c + 1) * 128], in_=affc[:, c, 0:48], identity=self.identf[:]),
                    reads=[affc, self.identf], writes=[kb.banks[7]])
            kb.op("act", lambda e: e.copy(out=workc[0:48, :], in_=kb.banks[7].h[0:48, 0:256]),
                  reads=[kb.banks[7]], writes=[workc])

        def topk(wk, tv, ti, niter):
            for it in range(niter):
                sl = slice(it * 8, (it + 1) * 8)
                kb.op("dve", lambda e, sl=sl: e.max(out=tv[:, sl], in_=wk[:]), reads=[wk], writes=[tv.sub(it)])
                kb.op("dve", lambda e, sl=sl: e.max_index(out=ti[:, sl], in_max=tv[:, sl], in_values=wk[:]),
                      reads=[wk, tv.sub(it)], writes=[ti.sub(it)])
                kb.op("dve", lambda e, sl=sl: e.match_replace(out=wk[:], in_to_replace=tv[:, sl], in_values=wk[:], imm_value=-1.0),
                      reads=[tv.sub(it), wk], writes=[wk])

        if dbg == 2:
            da = kb.dram("dbg_work", [48, L], F32, kind="ExternalOutput")
            kb.dma("sp", lambda e: e.dma_start(out=da.ap[:, :], in_=work[:]), reads=[work], writes=[da])
            kb.release(m1)
            return
        topk(work, topv, topi, 32)
        if dbg == 3:
            da = kb.dram("dbg_topv", [48, 256], F32, kind="ExternalOutput")
            kb.dma("sp", lambda e: e.dma_start(out=da.ap[:, :], in_=topv[:]), reads=[topv], writes=[da])
            db_ = kb.dram("dbg_topi", [48, 256], U32, kind="ExternalOutput")
            kb.dma("sp", lambda e: e.dma_start(out=db_.ap[:, :], in_=topi[:]), reads=[topi], writes=[db_])
            kb.release(m1)
            return
        kb.op("dve", lambda e: e.tensor_copy(out=topif[:], in_=topi[:]), reads=[topi], writes=[topif])
        tb = kb.banks[5]
        for h in range(2):
            kb.op("pe", lambda e, h=h: e.transpose(out=tb.h[:, h * 48:(h + 1) * 48], in_=topif[0:48, h * 128:(h + 1) * 128],
                                                   identity=self.identf[0:48, 0:48]),
                  reads=[topif, self.identf], writes=[tb])
            kb.op("pe", lambda e, h=h: e.transpose(out=tb.h[:, 128 + h * 48:128 + (h + 1) * 48], in_=topv[0:48, h * 128:(h + 1) * 128],
                                                   identity=self.identf[0:48, 0:48]),
                  reads=[topv, self.identf], writes=[tb])
        kb.op("dve", lambda e: e.tensor_copy(out=idxT[:], in_=tb.h[:, 0:96].rearrange("p (h c) -> p h c", h=2)),
              reads=[tb], writes=[idxT])
        kb.op("dve", lambda e: e.tensor_copy(out=gT[:], in_=tb.h[:, 128:224].rearrange("p (h c) -> p h c", h=2)),
              reads=[tb], writes=[gT])
        if with_ctx:
            topk(workc, topvc, topic, 4)
            kb.op("dve", lambda e: e.tensor_copy(out=topicf[:], in_=topic[:]), reads=[topic], writes=[topicf])
            cbase = kb.alloc("cbase", [128, 1], F32)
            kb.dma("sp", lambda e: e.dma_start(out=cbase[:], in_=self.c_ctxbase.ap[:, :]), reads=[self.c_ctxbase], writes=[cbase])
            for (srcT, dstT, isidx) in ((topicf, idxC, True), (topvc, gC, False)):
                M = kb.alloc("Mc", [48, 128], F32)
                kb.op("dve", lambda e, M=M: e.memset(M[:], 0.0), writes=[M])
                kb.op("dve", lambda e, M=M, srcT=srcT: e.tensor_copy(out=M[0:16, 0:32], in_=srcT[0:16, 0:32]), reads=[srcT], writes=[M])
                kb.op("dve", lambda e, M=M, srcT=srcT: e.tensor_copy(out=M[32:48, 32:64], in_=srcT[32:48, 0:32]), reads=[srcT], writes=[M])
                kb.op("pe", lambda e, M=M: e.transpose(out=tb.h[:, 256:304], in_=M[0:48, :], identity=self.identf[0:48, 0:48]),
                      reads=[M, self.identf], writes=[tb])
                tcp = kb.alloc("tcp", [128, 48], F32)
                kb.op("dve", lambda e, tcp=tcp: e.tensor_copy(out=tcp[:], in_=tb.h[:, 256:304]), reads=[tb], writes=[tcp])
                tsum = kb.alloc("tsum", [128, NE], F32)
                kb.op("dve", lambda e, tcp=tcp, tsum=tsum: e.tensor_tensor(out=tsum[:], in0=tcp[:, 0:16], in1=tcp[:, 32:48], op=ALU.add),
                      reads=[tcp], writes=[tsum])
                if isidx:
                    kb.op("dve", lambda e, tsum=tsum: e.tensor_scalar(out=tsum[:], in0=tsum[:], scalar1=cbase[:, 0:1], scalar2=None, op0=ALU.add),
                          reads=[tsum, cbase], writes=[tsum])
                kb.op("dve", lambda e, tsum=tsum, dstT=dstT: e.tensor_copy(out=dstT[:], in_=tsum[:]), reads=[tsum], writes=[dstT])
        if dbg == 4:
            di = kb.dram("dbg_idx", [128, 96], I32, kind="ExternalOutput")
            dg = kb.dram("dbg_g", [128, 96], F32, kind="ExternalOutput")
            da = kb.dram("dbg_aff", [128, 1024], F32, kind="ExternalOutput")
            kb.dma("sp", lambda e: e.dma_start(out=di.ap[:, :], in_=idxT[:].rearrange("p h c -> p (h c)")), reads=[idxT], writes=[di])
            kb.dma("sp", lambda e: e.dma_start(out=dg.ap[:, :], in_=gT[:].rearrange("p h c -> p (h c)")), reads=[gT], writes=[dg])
            kb.dma("sp", lambda e: e.dma_start(out=da.ap[:, :], in_=aff2[:].rearrange("p h c -> p (h c)")), reads=[aff2], writes=[da])
            kb.release(m1)
            return
        kb.release(m1)

        G = []
        for s in range(NS):
            for h in range(2):
                G.append(dict(co=s * 256 + h * 128, idx=lambda e, s=s, h=h: idxT[:, h, s * 32 + e:s * 32 + e + 1],
                              gate=lambda e, s=s, h=h: gT[:, h, s * 32 + e:s * 32 + e + 1], r=s,
                              xn=self.xnl[s], dst=self.xr[s], n=L))
        if with_ctx:
            G.append(dict(co=512, idx=lambda e: idxC[:, e:e + 1], gate=lambda e: gC[:, e:e + 1], r=2,
                          xn=self.xnc_all, dst=self.xc_all, n=NS * LC + 128))
        NSL = 128 * len(G)
        HW = NSL // 2
        halves = [(0, HW), (HW, HW)]
        Xg = [[kb.alloc(f"xg{i}_{gi}", [128, D], BF16) for gi in range(len(G))] for i in range(2)]
        XeT = [kb.alloc(f"xeT{i}", [128, 8, NSL], BF16) for i in range(2)]
        hidT = kb.alloc("hidT", [128, 8, NSL], BF16)
        sgt = [kb.alloc(f"sgt{i}", [128, HW], F32) for i in range(2)]
        yo = [kb.alloc(f"yo{i}", [128, D], F32) for i in range(3)]
        nyo = 0
        nyb = 0

        def gathers(e):
            for gi, g in enumerate(G):
                xg = Xg[e % 2][gi]
                kb.dma("pool", lambda en, xg=xg, g=g, e=e: en.indirect_dma_start(
                    out=xg[:], out_offset=None, in_=g["xn"].ap[:, :], in_offset=IOA(ap=g["idx"](e), axis=0)),
                    reads=[g["xn"], idxT, idxC], writes=[xg])

        def transposes(e, gi):
            g = G[gi]
            co, r = g["co"], g["r"]
            xg = Xg[e % 2][gi]
            xe_ = XeT[e % 2]
            for k in range(8):
                bank = kb.banks[6 + k // 4]
                pv = bank.h.bitcast(BF16)
                kk = k % 4
                kb.op("pe", lambda en, pv=pv, xg=xg, k=k, kk=kk: en.transpose(
                    out=pv[:, kk * 128:(kk + 1) * 128], in_=xg[:, k * 128:(k + 1) * 128],
                    identity=self.identb[:]), reads=[xg, self.identb], writes=[bank])
            for k in range(8):
                bank = kb.banks[6 + k // 4]
                pv = bank.h.bitcast(BF16)
                kk = k % 4
                dst = xe_[:, k, co:co + 128]
                if k < 4:
                    kb.op("act", lambda en, dst=dst, pv=pv, kk=kk, r=r, k=k: en.activation(
                        out=dst, in_=pv[:, kk * 128:(kk + 1) * 128], func=AF.Identity,
                        scale=mulc2[:, k, r:r + 1], bias=addc2(k, r)),
                        reads=[bank], writes=[xe_.sub((k, co))])
                else:
                    kb.op("dve", lambda en, dst=dst, pv=pv, kk=kk, r=r, k=k: en.tensor_scalar(
                        out=dst, in0=pv[:, kk * 128:(kk + 1) * 128], scalar1=mulc2[:, k, r:r + 1],
                        scalar2=addc2(k, r), op0=ALU.mult, op1=ALU.add),
                        reads=[bank], writes=[xe_.sub((k, co))])

        gathers(0)
        gathers(1)
        for gi in range(len(G)):
            transposes(0, gi)
        for e in range(NE):
            if e + 2 < NE:
                pass
            w1t, w3t, w2t = wbufs[e % 2]
            xe = XeT[e % 2]
            for fc in range(8):
                for hi, (h0, hw) in enumerate(halves):
                    b1 = kb.banks[hi]
                    b3 = kb.banks[2 + hi]
                    for (wt, bm) in ((w1t, b1), (w3t, b3)):
                        for k in range(8):
                            kb.op("pe", lambda en, wt=wt, bm=bm, k=k, fc=fc, xe=xe, h0=h0, hw=hw: en.matmul(
                                out=bm.h[:, 0:hw], lhsT=wt[:, k, fc * 128:(fc + 1) * 128], rhs=xe[:, k, h0:h0 + hw],
                                start=(k == 0), stop=(k == 7)), reads=[wt, xe], writes=[bm])
                    sg = sgt[hi]
                    kb.op("act", lambda en, sg=sg, b1=b1, hw=hw: en.activation(out=sg[:, 0:hw], in_=b1.h[:, 0:hw], func=AF.Silu),
                          reads=[b1], writes=[sg])
                    kb.op("dve", lambda en, sg=sg, b3=b3, fc=fc, h0=h0, hw=hw: en.tensor_tensor(
                        out=hidT[:, fc, h0:h0 + hw], in0=sg[:, 0:hw], in1=b3.h[:, 0:hw], op=ALU.mult),
                        reads=[sg, b3], writes=[hidT.sub((fc, h0))])
            for gi, g in enumerate(G):
                if e + 1 < NE:
                    transposes(e + 1, gi)
                co, r = g["co"], g["r"]
                yt = yo[nyo % 3]
                nyo += 1
                for db in range(2):
                    by = kb.banks[4 + nyb % 2]
                    nyb += 1
                    for fc in range(8):
                        kb.op("pe", lambda en, by=by, fc=fc, co=co, db=db, w2t=w2t: en.matmul(
                            out=by.h[:, 0:512], lhsT=hidT[:, fc, co:co + 128], rhs=w2t[:, fc, db * 512:(db + 1) * 512],
                            start=(fc == 0), stop=(fc == 7)), reads=[hidT, w2t], writes=[by])
                    kb.op("dve", lambda en, by=by, yt=yt, db=db, g=g, e=e, r=r: en.scalar_tensor_tensor(
                        out=yt[:, db * 512:(db + 1) * 512], in0=by.h[:, 0:512], scalar=g["gate"](e),
                        in1=g2b[r][:, db * 512:(db + 1) * 512], op0=ALU.mult, op1=ALU.mult),
                        reads=[by, gT, gC, g2b[r]], writes=[yt.sub(db)])
                kb.dma("pool", lambda en, yt=yt, g=g, e=e: en.indirect_dma_start(
                    out=g["dst"].ap[:, :], out_offset=IOA(ap=g["idx"](e), axis=0), in_=yt[:, :], in_offset=None,
                    compute_op=ALU.add, bounds_check=kb.bnd[g["n"] - 1], oob_is_err=True),
                    reads=[yt, idxT, idxC], writes=[g["dst"]])
            if e + 2 < NE:
                gathers(e + 2)
                load_w(e + 2)

    def mixer0(self):
        kb = self.kb
        l = 0
        wbuf = kb.alloc("wbuf", [128, 8, 1536], BF16)
        wout = wbuf.view("woutv", [128, 8, D], BF16)
        csc = kb.alloc("csc", [128, 256], BF16)
        kb.dma("sp", lambda e: e.dma_start(out=csc[:], in_=self.c_csc.ap[:, :]), reads=[self.c_csc], writes=[csc])
        cwc = kb.alloc("cwc", [128, 4, 31], F32)
        for j in range(4):
            kb.dma("sp", lambda e, j=j: e.dma_start(out=cwc[:, j, :], in_=self.ab_conv_w.ap[:, j * 128:(j + 1) * 128].rearrange("k p -> p k")),
                   reads=[self.ab_conv_w], writes=[cwc.sub(j)])
        cols = {}
        for nm, dr in (("cb", self.ab_conv_b), ("lg", self.ab_ln_g), ("lb", self.ab_ln_b)):
            t = kb.alloc(nm + "c", [128, 4], F32)
            kb.dma("sp", lambda e, t=t, dr=dr: e.dma_start(out=t[:], in_=dr.ap.rearrange("(j p) -> p j", p=128)), reads=[dr], writes=[t])
            cols[nm] = t
        onesb = kb.alloc("onesb", [128, 128], BF16)
        kb.op("dve", lambda e: e.memset(onesb[:], 1.0 / 512.0), writes=[onesb])
        diag = kb.alloc("diag", [128, 4 * 31, 128], BF16)
        for j in range(4):
            for k in range(31):
                kb.op("dve", lambda e, j=j, k=k: e.tensor_scalar(
                    out=diag[:, j * 31 + k, :], in0=self.identb[:], scalar1=cwc[:, j, k:k + 1], scalar2=None, op0=ALU.mult),
                    reads=[self.identb, cwc], writes=[diag.sub((j, k))])
        bufs = self.norm_bufs(nx=2)
        xb = bufs[0][0]
        uTb = kb.alloc("uTb", [128, 8, 512], BF16)
        aT = kb.alloc("aT", [128, 4, L + 32], BF16)
        ufTb = kb.alloc("ufTb", [128, 4, 512], BF16)
        Yall = kb.alloc("Yall", [128, 16, 1024], BF16)
        mixT = kb.alloc("mixT", [128, 8, L], BF16)
        clb = kb.alloc("clb", [128, 16, 256], BF16)
        slb = kb.alloc("slb", [128, 16, 256], BF16)
        g1b = kb.alloc("g1b", [128, D], F32)
        cl2h = aT.view("cl2", [128, 16, 256], BF16, boff=0)
        sl2h = aT.view("sl2", [128, 16, 256], BF16, boff=8192)
        NBC = 256
        cT = kb.alloc("cT", [128, 4, NBC], F32)
        cbt = kb.alloc("cbt", [128, 4, NBC], BF16)
        c2t = kb.alloc("c2t", [128, 4, NBC], BF16)
        t_mean = kb.alloc("t_mean", [128, NBC], F32)
        t_rstd = kb.alloc("t_rstd", [128, NBC], F32)
        t_tmp = kb.alloc("t_tmp", [128, 512], F32)
        t_tmp2 = kb.alloc("t_tmp2", [128, NBC], F32)
        addc1 = lambda k, r: self.modc[l][:, k, r:r + 1]
        mulc1 = self.mul1c[l]
        B = kb.banks
        seqs = [(self.xin[0], self.xr[0], L, 0, self.c_cl, self.c_sl), (self.xin[1], self.xr[1], L, 1, self.c_cl, self.c_sl),
                (self.cin[0], self.xc[0], LC, 2, self.c_clc, self.c_slc), (self.cin[1], self.xc[1], LC, 2, self.c_clc, self.c_slc)]
        for (src, dst, Ls, r, ctab, stab) in seqs:
            kb.dma("pool", lambda e: e.dma_start(out=wbuf[:], in_=self.ab_w_in.ap.rearrange("(k p) f -> p k f", p=128)),
                   reads=[self.ab_w_in], writes=[wbuf])
            kb.dma("sp", lambda e, r=r: e.dma_start(out=g1b[:], in_=self.modd.ap[l, r, 2 * D:3 * D].partition_broadcast(128)),
                   reads=[self.modd.sub(l)], writes=[g1b])
            for j in range(4):
                kb.op("dve", lambda e, j=j: e.memset(aT[:, j, 0:15], 0.0), writes=[aT.sub((j, "h0"))])
                kb.op("dve", lambda e, j=j, Ls=Ls: e.memset(aT[:, j, 15 + Ls:32 + Ls], 0.0), writes=[aT.sub((j, "h1"))])
            nb = min(512, Ls)
            for blk in range(Ls // nb):
                t0 = blk * nb
                for sb in range(nb // 256):
                    self.norm_batch(src, t0 + sb * 256, 2, mulc1, addc1, r, uTb, sb * 256, bufs, [B[0], B[1], B[2], B[3]])
                def zmm(j, bank):
                    for k in range(8):
                        kb.op("pe", lambda e, j=j, k=k, bank=bank, nb=nb: e.matmul(
                            out=bank.h[:, 0:nb], lhsT=wbuf[:, k, j * 128:(j + 1) * 128], rhs=uTb[:, k, 0:nb],
                            start=(k == 0), stop=(k == 7)), reads=[wbuf, uTb], writes=[bank])
                for jj in range(4):
                    zmm(jj, B[4])
                    zmm(jj + 4, B[5])
                    kb.op("act", lambda e, nb=nb: e.activation(out=t_tmp[:, 0:nb], in_=B[5].h[:, 0:nb], func=AF.Sigmoid),
                          reads=[B[5]], writes=[t_tmp])
                    kb.op("dve", lambda e, jj=jj, nb=nb, t0=t0: e.tensor_tensor(
                        out=aT[:, jj, 15 + t0:15 + t0 + nb], in0=t_tmp[:, 0:nb], in1=B[4].h[:, 0:nb], op=ALU.mult),
                        reads=[t_tmp, B[4]], writes=[aT.sub((jj, t0))])
                for g in range(4):
                    zmm(8 + g, B[6])
                    kb.op("act", lambda e, g=g, nb=nb: e.copy(out=ufTb[:, g, 0:nb], in_=B[6].h[:, 0:nb]),
                          reads=[B[6]], writes=[ufTb.sub(g)])
                for tc in range(nb // 128):
                    c = (t0 // 128) + tc
                    for gp in range(2):
                        for gg in range(2):
                            g = gp * 2 + gg
                            kb.op("pe", lambda e, g=g, gg=gg, tc=tc: e.matmul(
                                out=B[7].h[:, gg * 256:(gg + 1) * 256], lhsT=ufTb[:, g, tc * 128:(tc + 1) * 128], rhs=csc[:, :],
                                start=True, stop=True), reads=[ufTb, csc], writes=[B[7]])
                        kb.op("dve", lambda e, c=c, gp=gp: e.tensor_copy(out=Yall[:, c, gp * 512:(gp + 1) * 512], in_=B[7].h[:, 0:512]),
                              reads=[B[7]], writes=[Yall.sub((c, gp))])
            nbc = min(NBC, Ls)
            for blk in range(Ls // nbc):
                t0 = blk * nbc
                for j in range(4):
                    bank = B[j % 2]
                    for k in range(31):
                        kb.op("pe", lambda e, j=j, k=k, bank=bank, t0=t0, nbc=nbc: e.matmul(
                            out=bank.h[:, 0:nbc], lhsT=diag[:, j * 31 + k, :], rhs=aT[:, j, t0 + k:t0 + k + nbc],
                            start=(k == 0), stop=(k == 30)), reads=[diag, aT], writes=[bank])
                    kb.op("act", lambda e, j=j, bank=bank, nbc=nbc: e.activation(
                        out=cT[:, j, 0:nbc], in_=bank.h[:, 0:nbc], func=AF.Identity, bias=cols["cb"][:, j:j + 1]),
                        reads=[bank, cols["cb"]], writes=[cT.sub(j)])
                    kb.op("pool", lambda e, j=j, nbc=nbc: e.tensor_copy(out=cbt[:, j, 0:nbc], in_=cT[:, j, 0:nbc]),
                          reads=[cT.sub(j)], writes=[cbt.sub(j)])
                    kb.op("pool", lambda e, j=j, nbc=nbc: e.tensor_tensor(out=c2t[:, j, 0:nbc], in0=cT[:, j, 0:nbc], in1=cT[:, j, 0:nbc], op=ALU.mult),
                          reads=[cT.sub(j)], writes=[c2t.sub(j)])
                for j in range(4):
                    kb.op("pe", lambda e, j=j, nbc=nbc: e.matmul(out=B[2].h[:, 0:nbc], lhsT=onesb[:], rhs=cbt[:, j, 0:nbc],
                                                                 start=(j == 0), stop=(j == 3)), reads=[onesb, cbt], writes=[B[2]])
                for j in range(4):
                    kb.op("pe", lambda e, j=j, nbc=nbc: e.matmul(out=B[3].h[:, 0:nbc], lhsT=onesb[:], rhs=c2t[:, j, 0:nbc],
                                                                 start=(j == 0), stop=(j == 3)), reads=[onesb, c2t], writes=[B[3]])
                kb.op("dve", lambda e, nbc=nbc: e.tensor_copy(out=t_mean[:, 0:nbc], in_=B[2].h[:, 0:nbc]), reads=[B[2]], writes=[t_mean])
                kb.op("dve", lambda e, nbc=nbc: e.tensor_tensor(out=t_tmp2[:, 0:nbc], in0=t_mean[:, 0:nbc], in1=t_mean[:, 0:nbc], op=ALU.mult),
                      reads=[t_mean], writes=[t_tmp2])
                kb.op("dve", lambda e, nbc=nbc: e.tensor_tensor(out=t_rstd[:, 0:nbc], in0=B[3].h[:, 0:nbc], in1=t_tmp2[:, 0:nbc], op=ALU.subtract),
                      reads=[B[3], t_tmp2], writes=[t_rstd])
                kb.op("act", lambda e, nbc=nbc: e.activation(out=t_rstd[:, 0:nbc], in_=t_rstd[:, 0:nbc], func=AF.Identity, bias=self.epsc[:, 0:1]),
                      reads=[t_rstd, self.epsc], writes=[t_rstd])
                kb.op("act", lambda e, nbc=nbc: e.activation(out=t_rstd[:, 0:nbc], in_=t_rstd[:, 0:nbc], func=AF.Sqrt),
                      reads=[t_rstd], writes=[t_rstd])
                kb.op("dve", lambda e, nbc=nbc: e.reciprocal(out=t_rstd[:, 0:nbc], in_=t_rstd[:, 0:nbc]), reads=[t_rstd], writes=[t_rstd])
                for j in range(4):
                    kb.op("pool", lambda e, j=j, nbc=nbc: e.tensor_tensor(out=cT[:, j, 0:nbc], in0=cT[:, j, 0:nbc], in1=t_mean[:, 0:nbc], op=ALU.subtract),
                          reads=[cT.sub(j), t_mean], writes=[cT.sub(j)])
                    kb.op("dve", lambda e, j=j, nbc=nbc: e.tensor_tensor(out=cT[:, j, 0:nbc], in0=cT[:, j, 0:nbc], in1=t_rstd[:, 0:nbc], op=ALU.mult),
                          reads=[cT.sub(j), t_rstd], writes=[cT.sub(j)])
                    kb.op("act", lambda e, j=j, nbc=nbc, t0=t0: e.activation(
                        out=mixT[:, j, t0:t0 + nbc], in_=cT[:, j, 0:nbc], func=AF.Silu,
                        scale=cols["lg"][:, j:j + 1], bias=cols["lb"][:, j:j + 1]),
                        reads=[cT.sub(j), cols["lg"], cols["lb"]], writes=[mixT.sub((j, t0))])
            ntc = Ls // 128
            for kbi in range(Ls // 256):
                k0 = kbi * 256
                if kbi % 2 == 0:
                    clh, slh, trc, trs = clb.h, slb.h, clb, slb
                else:
                    clh, slh, trc, trs = cl2h, sl2h, aT, aT
                kb.dma("sp", lambda e, ctab=ctab, k0=k0, ntc=ntc, clh=clh: e.dma_start(
                    out=clh[:, 0:ntc, :], in_=ctab.ap.rearrange("(c p) k -> p c k", p=128)[:, :, k0:k0 + 256]),
                    reads=[ctab], writes=[trc])
                kb.dma("sp", lambda e, stab=stab, k0=k0, ntc=ntc, slh=slh: e.dma_start(
                    out=slh[:, 0:ntc, :], in_=stab.ap.rearrange("(c p) k -> p c k", p=128)[:, :, k0:k0 + 256]),
                    reads=[stab], writes=[trs])
                for g in range(4):
                    bank = B[4 + g % 2]
                    for c in range(ntc):
                        kb.op("pe", lambda e, g=g, c=c, bank=bank, clh=clh: e.matmul(
                            out=bank.h[:, 0:256], lhsT=Yall[:, c, g * 256:g * 256 + 128], rhs=clh[:, c, :],
                            start=(c == 0), stop=False), reads=[Yall, trc], writes=[bank])
                        kb.op("pe", lambda e, g=g, c=c, bank=bank, ntc=ntc, slh=slh: e.matmul(
                            out=bank.h[:, 0:256], lhsT=Yall[:, c, g * 256 + 128:g * 256 + 256], rhs=slh[:, c, :],
                            start=False, stop=(c == ntc - 1)), reads=[Yall, trs], writes=[bank])
                    if g % 2 == 0:
                        kb.op("act", lambda e, g=g, bank=bank, k0=k0: e.copy(out=mixT[:, 4 + g, k0:k0 + 256], in_=bank.h[:, 0:256]),
                              reads=[bank], writes=[mixT.sub((4 + g, k0))])
                    else:
                        kb.op("dve", lambda e, g=g, bank=bank, k0=k0: e.tensor_copy(out=mixT[:, 4 + g, k0:k0 + 256], in_=bank.h[:, 0:256]),
                              reads=[bank], writes=[mixT.sub((4 + g, k0))])
            kb.dma("pool", lambda e: e.dma_start(out=wout[:], in_=self.ab_w_out.ap.rearrange("(k p) f -> p k f", p=128)),
                   reads=[self.ab_w_out], writes=[wbuf])
            for tc in range(ntc):
                xb = bufs[0][tc % 2]
                kb.dma("sp", lambda e, src=src, tc=tc, xb=xb: e.dma_start(out=xb[:], in_=src.ap[tc * 128:(tc + 1) * 128, :]),
                       reads=[src], writes=[xb])
                for db in range(2):
                    bank = B[6 + db]
                    for m in range(8):
                        kb.op("pe", lambda e, m=m, db=db, bank=bank, tc=tc: e.matmul(
                            out=bank.h[:, 0:512], lhsT=mixT[:, m, tc * 128:(tc + 1) * 128], rhs=wout[:, m, db * 512:(db + 1) * 512],
                            start=(m == 0), stop=(m == 7)), reads=[mixT, wbuf], writes=[bank])
                    kb.op("dve", lambda e, db=db, bank=bank: e.tensor_tensor(
                        out=t_tmp[:, 0:512], in0=bank.h[:, 0:512], in1=g1b[:, db * 512:(db + 1) * 512], op=ALU.mult),
                        reads=[bank, g1b], writes=[t_tmp])
                    kb.op("pool", lambda e, db=db, xb=xb: e.tensor_tensor(
                        out=xb[:, db * 512:(db + 1) * 512], in0=xb[:, db * 512:(db + 1) * 512], in1=t_tmp[:, 0:512], op=ALU.add),
                        reads=[xb, t_tmp], writes=[xb])
                kb.dma("pool", lambda e, dst=dst, tc=tc, xb=xb: e.dma_start(out=dst.ap[tc * 128:(tc + 1) * 128, :], in_=xb[:]),
                       reads=[xb], writes=[dst.sub(("row", tc))])


    def mla(self):
        kb = self.kb
        l = 1
        B = kb.banks
        NKV = LC + L
        NT = NKV // 128
        SCALE = 96.0 ** -0.5
        cast = lambda dst, src_ap, tr: kb.dma("pool", lambda e: e.dma_start(out=dst[:], in_=src_ap), reads=[tr], writes=[dst])
        wi = kb.alloc("wi", [128, 8, 416], BF16)
        cast(wi, self.mla_w_in.ap.rearrange("(k p) f -> p k f", p=128), self.mla_w_in)
        wuq = kb.alloc("wuq", [128, 2, 1536], BF16)
        cast(wuq, self.mla_w_uq.ap.rearrange("(k p) f -> p k f", p=128), self.mla_w_uq)
        wukv = kb.alloc("wukv", [128, 2048], BF16)
        cast(wukv, self.mla_w_ukv.ap[:, :], self.mla_w_ukv)
        wo = kb.alloc("wo", [128, 8, D], BF16)
        cast(wo, self.mla_w_o.ap.rearrange("(k p) f -> p k f", p=128), self.mla_w_o)
        ropt = kb.alloc("ropt", [128, 16, 32], F32)
        kb.dma("sp", lambda e: e.dma_start(out=ropt[:], in_=self.c_rope.ap.rearrange("(c p) f -> p c f", p=128)),
               reads=[self.c_rope], writes=[ropt])
        qgc = kb.alloc("qgc", [128, 2], F32)
        kb.dma("sp", lambda e: e.dma_start(out=qgc[:], in_=self.mla_qg.ap.rearrange("(j p) -> p j", p=128)), reads=[self.mla_qg], writes=[qgc])
        kvgc = kb.alloc("kvgc", [128, 1], F32)
        kb.dma("sp", lambda e: e.dma_start(out=kvgc[:], in_=self.mla_kvg.ap.rearrange("(j p) -> p j", p=128)), reads=[self.mla_kvg], writes=[kvgc])
        ones1 = kb.alloc("ones1", [128, 128], BF16)
        kb.op("dve", lambda e: e.memset(ones1[:], 1.0), writes=[ones1])
        bufs = self.norm_bufs(nx=2)
        xb = bufs[0][0]
        uT = kb.alloc("uTall", [128, 8, NKV], BF16)
        cqnT = kb.alloc("cqnT", [128, 2, L], BF16)
        ckvnT = kb.alloc("ckvnT", [128, NKV], BF16)
        KTs = [kb.alloc(f"KT{i}", [128, NKV], BF16) for i in range(2)]
        QTs = [kb.alloc(f"QT{i}", [128, L], BF16) for i in range(2)]
        qtoks = [kb.alloc(f"qtok{i}", [128, 8, 96], BF16) for i in range(2)]
        Vaug = [kb.alloc(f"Vaug{i}", [128, NT, 128], BF16) for i in range(2)]
        kb.op("dve", lambda e: e.memset(Vaug[0][:].rearrange("p a b -> p (a b)"), 1.0), writes=[Vaug[0]])
        kb.op("dve", lambda e: e.memset(Vaug[1][:].rearrange("p a b -> p (a b)"), 1.0), writes=[Vaug[1]])
        pT = [kb.alloc(f"pT{i}", [128, 1024], BF16) for i in range(3)]
        oTs = kb.alloc("oTs", [128, 512], F32)
        onesf = kb.alloc("onesf", [128, 512], F32)
        kb.op("dve", lambda e: e.memset(onesf[:], 1.0), writes=[onesf])
        recf = kb.alloc("recf", [128, 512], F32)
        rech = kb.alloc("rech", [128, 512], BF16)
        recl = kb.alloc("recl", [128, 512], BF16)
        attnT = kb.alloc("attnT", [128, 8, L], BF16)
        g1b = kb.alloc("g1bm", [128, D], F32)
        t_tmp = kb.alloc("t_tmpm", [128, 512], F32)
        zts = [kb.alloc(f"zt{i}", [128, 416], F32) for i in range(2)]
        cqns = [kb.alloc(f"cqn{i}", [128, 256], BF16) for i in range(2)]
        ckvns = [kb.alloc(f"ckvn{i}", [128, 128], BF16) for i in range(2)]
        ktoks = [kb.alloc(f"ktok{i}", [128, 96], BF16) for i in range(2)]
        for kt_ in ktoks:
            kb.op("dve", lambda e, kt_=kt_: e.memset(kt_[:], 0.0), writes=[kt_])
        rtk = [[kb.alloc(f"rtk{i}_{j}", [128, 16], F32) for j in range(4)] for i in range(2)]
        rt = [kb.alloc(f"rt{i}", [128, 4, 16], F32) for i in range(4)]
        st2 = kb.alloc("st2", [128, 8 * NT], F32)
        kb.op("dve", lambda e: e.memset(st2[:], 0.0), writes=[st2])
        junk2 = kb.alloc("junk2", [128, 256], BF16)
        addc1 = lambda k, r: self.modc[l][:, k, r:r + 1]
        mulc1 = self.mul1c[l]
        npt = 0
        for s in range(NS):
            kb.dma("sp", lambda e, s=s: e.dma_start(out=g1b[:], in_=self.modd.ap[l, s, 2 * D:3 * D].partition_broadcast(128)),
                   reads=[self.modd.sub(l)], writes=[g1b])
            kb.op("dve", lambda e: e.memset(st2[:], 0.0), writes=[st2])
            for b2 in range(LC // 256):
                self.norm_batch(self.xc[s], b2 * 256, 2, mulc1, addc1, 2, uT, b2 * 256, bufs, [B[0], B[1], B[2], B[3]])
            for b2 in range(L // 256):
                self.norm_batch(self.xr[s], b2 * 256, 2, mulc1, addc1, s, uT, LC + b2 * 256, bufs, [B[0], B[1], B[2], B[3]])
            for c in range(NT):
                lat = c >= 2
                cl_ = c - 2
                zt, cqn, ckvn, ktok = zts[c % 2], cqns[c % 2], ckvns[c % 2], ktoks[c % 2]
                rk = rtk[c % 2]
                zb = B[4 + c % 2]
                for k in range(8):
                    kb.op("pe", lambda e, c=c, k=k, zb=zb: e.matmul(out=zb.h[:, 0:416], lhsT=uT[:, k, c * 128:(c + 1) * 128], rhs=wi[:, k, :],
                                                                  start=(k == 0), stop=(k == 7)), reads=[uT, wi], writes=[zb])
                kb.op("act", lambda e, zb=zb, zt=zt: e.copy(out=zt[:], in_=zb.h[:, 0:416]), reads=[zb], writes=[zt])
                so = c * 8
                parts = [(256, 128, 128.0, ckvn, 0)] + ([(0, 256, 256.0, cqn, 4)] if lat else [])
                for (c0, n, nf, dstt, o) in parts:
                    kb.op("act", lambda e, c0=c0, n=n, so=so, o=o, zt=zt: e.activation(out=junk2[:, 0:n], in_=zt[:, c0:c0 + n], func=AF.Square,
                                                                             accum_out=st2[:, so + o:so + o + 1]), reads=[zt], writes=[junk2, st2.sub(so + o)])
                    kb.op("act", lambda e, so=so, o=o, nf=nf: e.activation(out=st2[:, so + o + 1:so + o + 2], in_=st2[:, so + o:so + o + 1], func=AF.Identity,
                                                                         scale=1.0 / nf, bias=self.epsc[:, 0:1]), reads=[st2.sub(so + o), self.epsc], writes=[st2.sub(so + o + 1)])
                    kb.op("act", lambda e, so=so, o=o: e.activation(out=st2[:, so + o + 2:so + o + 3], in_=st2[:, so + o + 1:so + o + 2], func=AF.Sqrt),
                          reads=[st2.sub(so + o + 1)], writes=[st2.sub(so + o + 2)])
                    kb.op("dve", lambda e, so=so, o=o: e.reciprocal(out=st2[:, so + o + 3:so + o + 4], in_=st2[:, so + o + 2:so + o + 3]),
                          reads=[st2.sub(so + o + 2)], writes=[st2.sub(so + o + 3)])
                    kb.op("act", lambda e, c0=c0, n=n, so=so, o=o, dstt=dstt, zt=zt: e.activation(out=dstt[:, 0:n], in_=zt[:, c0:c0 + n], func=AF.Identity,
                                                                                          scale=st2[:, so + o + 3:so + o + 4]), reads=[zt, st2.sub(so + o + 3)], writes=[dstt])
                if lat:
                    krv = zt[:, 384:416].rearrange("p (i two) -> p i two", two=2)
                    xe, xo = krv[:, :, 0], krv[:, :, 1]
                    cs, sn = ropt[:, cl_, 0:16], ropt[:, cl_, 16:32]
                    ko = ktok[:, 64:96].rearrange("p (i two) -> p i two", two=2)
                    a0, a1, a2, a3 = rk[0][:, :], rk[1][:, :], rk[2][:, :], rk[3][:, :]
                    kb.op("pool", lambda e, xe=xe, cs=cs, a0=a0: e.tensor_tensor(out=a0, in0=xe, in1=cs, op=ALU.mult), reads=[zt, ropt], writes=[rk[0]])
                    kb.op("pool", lambda e, xo=xo, sn=sn, a1=a1: e.tensor_tensor(out=a1, in0=xo, in1=sn, op=ALU.mult), reads=[zt, ropt], writes=[rk[1]])
                    kb.op("pool", lambda e, xe=xe, sn=sn, a2=a2: e.tensor_tensor(out=a2, in0=xe, in1=sn, op=ALU.mult), reads=[zt, ropt], writes=[rk[2]])
                    kb.op("pool", lambda e, xo=xo, cs=cs, a3=a3: e.tensor_tensor(out=a3, in0=xo, in1=cs, op=ALU.mult), reads=[zt, ropt], writes=[rk[3]])
                    kb.op("pool", lambda e, ko=ko, a0=a0, a1=a1: e.tensor_tensor(out=ko[:, :, 0], in0=a0, in1=a1, op=ALU.subtract), reads=[rk[0], rk[1]], writes=[ktok.sub(0)])
                    kb.op("pool", lambda e, ko=ko, a2=a2, a3=a3: e.tensor_tensor(out=ko[:, :, 1], in0=a2, in1=a3, op=ALU.add), reads=[rk[2], rk[3]], writes=[ktok.sub(1)])
                else:
                    kb.op("pool", lambda e, ktok=ktok, zt=zt: e.tensor_copy(out=ktok[:, 64:96], in_=zt[:, 384:416]), reads=[zt], writes=[ktok.sub(0)])
                tbk = B[6 + c % 2]
                pv = tbk.h.bitcast(BF16)
                if lat:
                    for qk in range(2):
                        kb.op("pe", lambda e, pv=pv, qk=qk, cqn=cqn: e.transpose(out=pv[:, qk * 128:(qk + 1) * 128], in_=cqn[:, qk * 128:(qk + 1) * 128], identity=self.identb[:]),
                              reads=[cqn, self.identb], writes=[tbk])
                kb.op("pe", lambda e, pv=pv, ckvn=ckvn: e.transpose(out=pv[:, 256:384], in_=ckvn[:, :], identity=self.identb[:]), reads=[ckvn, self.identb], writes=[tbk])
                kb.op("pe", lambda e, pv=pv, ktok=ktok: e.transpose(out=pv[0:96, 384:512], in_=ktok[:, 0:96], identity=self.identb[:]), reads=[ktok, self.identb], writes=[tbk])
                if lat:
                    for qk in range(2):
                        kb.op("act", lambda e, pv=pv, qk=qk, cl_=cl_: e.activation(out=cqnT[:, qk, cl_ * 128:(cl_ + 1) * 128], in_=pv[:, qk * 128:(qk + 1) * 128],
                                                                                 func=AF.Identity, scale=qgc[:, qk:qk + 1]), reads=[tbk, qgc], writes=[cqnT.sub((qk, cl_))])
                kb.op("act", lambda e, pv=pv, c=c: e.activation(out=ckvnT[:, c * 128:(c + 1) * 128], in_=pv[:, 256:384], func=AF.Identity, scale=kvgc[:, 0:1]),
                      reads=[tbk, kvgc], writes=[ckvnT.sub(c)])
                for KTx in KTs:
                    kb.op("act", lambda e, pv=pv, c=c, KTx=KTx: e.copy(out=KTx[64:96, c * 128:(c + 1) * 128], in_=pv[64:96, 384:512]),
                          reads=[tbk], writes=[KTx.sub(("r", c))])
            def projA(h):
                KTh = KTs[h % 2]
                for blk in range(5):
                    n0 = blk * 512
                    nn = min(512, NKV - n0)
                    bk = B[7]
                    kb.op("pe", lambda e, h=h, n0=n0, nn=nn, bk=bk: e.matmul(out=bk.h[0:64, 0:nn], lhsT=wukv[:, h * 128:h * 128 + 64], rhs=ckvnT[:, n0:n0 + nn],
                                                                        start=True, stop=True), reads=[wukv, ckvnT], writes=[bk])
                    kb.op("dve", lambda e, n0=n0, nn=nn, bk=bk, KTh=KTh: e.tensor_copy(out=KTh[0:64, n0:n0 + nn], in_=bk.h[0:64, 0:nn]),
                          reads=[bk], writes=[KTh.sub(("n", blk))])
                va = Vaug[h % 2]
                vo = 0 if h % 2 == 0 else 64
                for vb in range(3):
                    ntl = min(8, NT - vb * 8)
                    bv = B[7]
                    for ci in range(ntl):
                        c = vb * 8 + ci
                        kb.op("pe", lambda e, h=h, c=c, ci=ci, bv=bv: e.matmul(out=bv.h[:, ci * 64:(ci + 1) * 64], lhsT=ckvnT[:, c * 128:(c + 1) * 128],
                                                                             rhs=wukv[:, h * 128 + 64:h * 128 + 128], start=True, stop=True), reads=[ckvnT, wukv], writes=[bv])
                    kb.op("dve", lambda e, vb=vb, ntl=ntl, bv=bv, va=va, vo=vo: e.tensor_copy(
                        out=va[:, vb * 8:vb * 8 + ntl, vo:vo + 64], in_=bv.h[:, 0:ntl * 64].rearrange("p (c f) -> p c f", f=64)),
                        reads=[bv], writes=[va.sub(vb)])

            def projQ(h, half, stage):
                qtk = qtoks[half]
                QTh = QTs[h % 2]
                for bq in range(2):
                    c0 = half * 8 + bq * 4
                    if stage == 0:
                        bqk = B[6 + bq]
                        for ci in range(4):
                            c = c0 + ci
                            for qk in range(2):
                                kb.op("pe", lambda e, h=h, c=c, ci=ci, qk=qk, bqk=bqk: e.matmul(
                                    out=bqk.h[:, ci * 96:(ci + 1) * 96], lhsT=cqnT[:, qk, c * 128:(c + 1) * 128], rhs=wuq[:, qk, h * 96:(h + 1) * 96],
                                    start=(qk == 0), stop=(qk == 1)), reads=[cqnT, wuq], writes=[bqk])
                        qv = bqk.h[:, 0:384].rearrange("p (c f) -> p c f", f=96)
                        lc0 = bq * 4
                        kb.op("dve", lambda e, qv=qv, lc0=lc0, qtk=qtk: e.tensor_copy(out=qtk[:, lc0:lc0 + 4, 0:64], in_=qv[:, :, 0:64]), reads=[bqk], writes=[qtk.sub((lc0, "n"))])
                        xe, xo = qv[:, :, 64:96:2], qv[:, :, 65:96:2]
                        cs, sn = ropt[:, c0:c0 + 4, 0:16], ropt[:, c0:c0 + 4, 16:32]
                        qo = qtk[:, lc0:lc0 + 4, 64:96].rearrange("p c (i two) -> p c i two", two=2)
                        kb.op("dve", lambda e, xe=xe, cs=cs: e.tensor_tensor(out=rt[0][:], in0=xe, in1=cs, op=ALU.mult), reads=[bqk, ropt], writes=[rt[0]])
                        kb.op("dve", lambda e, xo=xo, sn=sn: e.tensor_tensor(out=rt[1][:], in0=xo, in1=sn, op=ALU.mult), reads=[bqk, ropt], writes=[rt[1]])
                        kb.op("dve", lambda e, xe=xe, sn=sn: e.tensor_tensor(out=rt[2][:], in0=xe, in1=sn, op=ALU.mult), reads=[bqk, ropt], writes=[rt[2]])
                        kb.op("dve", lambda e, xo=xo, cs=cs: e.tensor_tensor(out=rt[3][:], in0=xo, in1=cs, op=ALU.mult), reads=[bqk, ropt], writes=[rt[3]])
                        kb.op("pool", lambda e, qo=qo: e.tensor_tensor(out=qo[:, :, :, 0], in0=rt[0][:], in1=rt[1][:], op=ALU.subtract),
                              reads=[rt[0], rt[1]], writes=[qtk.sub((lc0, "e"))])
                        kb.op("pool", lambda e, qo=qo: e.tensor_tensor(out=qo[:, :, :, 1], in0=rt[2][:], in1=rt[3][:], op=ALU.add),
                              reads=[rt[2], rt[3]], writes=[qtk.sub((lc0, "o"))])
                    else:
                        lc0 = bq * 4
                        tq = B[6 + bq]
                        pvq = tq.h.bitcast(BF16)
                        for ci in range(4):
                            kb.op("pe", lambda e, pvq=pvq, lc=lc0 + ci, ci=ci, qtk=qtk: e.transpose(out=pvq[0:96, ci * 128:(ci + 1) * 128], in_=qtk[:, lc, 0:96], identity=self.identb[:]),
                                  reads=[qtk, self.identb], writes=[tq])
                        kb.op("dve", lambda e, pvq=pvq, c0=c0, QTh=QTh: e.tensor_copy(out=QTh[0:96, c0 * 128:(c0 + 4) * 128], in_=pvq[0:96, 0:512]), reads=[tq], writes=[QTh.sub(c0)])

            def normalize1(h, qb, bo):
                kb.op("dve", lambda e, bo=bo: e.tensor_copy(out=oTs[:], in_=bo.h[:, 0:512]), reads=[bo], writes=[oTs])

            def normalize1b(h, qb):
                dp = 64 if (h % 2 == 0) else 0
                kb.op("dve", lambda e, dp=dp: e.reciprocal(out=recf[dp:dp + 1, :], in_=oTs[dp:dp + 1, :]), reads=[oTs], writes=[recf])
                kb.op("pool", lambda e, dp=dp: e.tensor_copy(out=rech[dp:dp + 1, :], in_=recf[dp:dp + 1, :]), reads=[recf], writes=[rech])
                kb.op("pool", lambda e, dp=dp: e.tensor_tensor(out=recl[dp:dp + 1, :], in0=recf[dp:dp + 1, :], in1=rech[dp:dp + 1, :], op=ALU.subtract),
                      reads=[recf, rech], writes=[recl])

            def normalize2(h, qb):
                even = (h % 2 == 0)
                dp = 64 if even else 0
                op_ = 0 if even else 64
                bb = B[6]
                kb.op("pe", lambda e, dp=dp, bb=bb: e.matmul(out=bb.h[:, 0:512], lhsT=ones1[dp:dp + 1, :], rhs=rech[dp:dp + 1, :], start=True, stop=False),
                      reads=[ones1, rech], writes=[bb])
                kb.op("pe", lambda e, dp=dp, bb=bb: e.matmul(out=bb.h[:, 0:512], lhsT=ones1[dp:dp + 1, :], rhs=recl[dp:dp + 1, :], start=False, stop=True),
                      reads=[ones1, recl], writes=[bb])
                kb.op("dve", lambda e, op_=op_, h=h, qb=qb, bb=bb: e.tensor_tensor(
                    out=attnT[op_:op_ + 64, h // 2, qb * 512:(qb + 1) * 512], in0=oTs[op_:op_ + 64, :], in1=bb.h[op_:op_ + 64, 0:512], op=ALU.mult),
                    reads=[oTs, bb], writes=[attnT.sub((h, qb))])

            projA(0)
            for half in range(2):
                projQ(0, half, 0)
                projQ(0, half, 1)
            pend = []
            pendnorm = []
            nstep = 0

            def issue_pv(item):
                (h, qb, c2, bo, pt, va) = item
                for half in range(2):
                    c = c2 * 2 + half
                    kb.op("pe", lambda e, c=c, bo=bo, pt=pt, va=va, half=half: e.matmul(
                        out=bo.h[:, 0:512], lhsT=va[:, c, :], rhs=pt[:, half * 512:(half + 1) * 512],
                        start=(c == 0), stop=(c == NT - 1)), reads=[va, pt], writes=[bo])
                if c2 == NT // 2 - 1:
                    normalize1(h, qb, bo)
                    pendnorm.append((nstep + 1, 1, h, qb))
                    pendnorm.append((nstep + 6, 2, h, qb))
                    pendnorm.sort()

            for h in range(NE):
                KTh = KTs[h % 2]
                QTh = QTs[h % 2]
                va = Vaug[h % 2]
                for qb in range(4):
                    bo = B[4 + qb % 2]
                    for c2 in range(NT // 2):
                        if h + 1 < NE:
                            if qb == 0 and c2 == 4:
                                projA(h + 1)
                            if qb == 1 and c2 == 4:
                                projQ(h + 1, 0, 0)
                            if qb == 2 and c2 == 4:
                                projQ(h + 1, 0, 1)
                            if qb == 3 and c2 == 3:
                                projQ(h + 1, 1, 0)
                            if qb == 3 and c2 == 8:
                                projQ(h + 1, 1, 1)
                        while pendnorm and nstep >= pendnorm[0][0]:
                            (_, stg, hh, qq) = pendnorm.pop(0)
                            (normalize1b if stg == 1 else normalize2)(hh, qq)
                        pi = nstep % 2
                        pt = pT[nstep % 3]
                        nstep += 1
                        for half in range(2):
                            c = c2 * 2 + half
                            bs = B[2 * pi + half]
                            kb.op("pe", lambda e, c=c, qb=qb, bs=bs, KTh=KTh, QTh=QTh: e.matmul(
                                out=bs.h[:, 0:512], lhsT=KTh[0:96, c * 128:(c + 1) * 128], rhs=QTh[0:96, qb * 512:(qb + 1) * 512],
                                start=True, stop=True), reads=[KTh, QTh], writes=[bs])
                        kb.op("act", lambda e, pi=pi, pt=pt: e.activation(out=pt[:, 0:1024], in_=kb.pairs[pi][:, 0:1024], func=AF.Exp, scale=SCALE),
                              reads=[B[2 * pi], B[2 * pi + 1]], writes=[pt])
                        pend.append((h, qb, c2, bo, pt, va))
                        if len(pend) > 1:
                            issue_pv(pend.pop(0))
            while pend:
                issue_pv(pend.pop(0))
            while pendnorm:
                (_, stg, hh, qq) = pendnorm.pop(0)
                (normalize1b if stg == 1 else normalize2)(hh, qq)
            for tc in range(L // 128):
                xb = bufs[0][tc % 2]
                kb.dma("sp", lambda e, s=s, tc=tc, xb=xb: e.dma_start(out=xb[:], in_=self.xr[s].ap[tc * 128:(tc + 1) * 128, :]),
                       reads=[self.xr[s]], writes=[xb])
                for db in range(2):
                    bank = B[6 + db]
                    for m in range(8):
                        kb.op("pe", lambda e, m=m, db=db, bank=bank, tc=tc: e.matmul(
                            out=bank.h[:, 0:512], lhsT=attnT[:, m, tc * 128:(tc + 1) * 128], rhs=wo[:, m, db * 512:(db + 1) * 512],
                            start=(m == 0), stop=(m == 7)), reads=[attnT, wo], writes=[bank])
                    kb.op("dve", lambda e, db=db, bank=bank: e.tensor_tensor(
                        out=t_tmp[:, 0:512], in0=bank.h[:, 0:512], in1=g1b[:, db * 512:(db + 1) * 512], op=ALU.mult),
                        reads=[bank, g1b], writes=[t_tmp])
                    kb.op("pool", lambda e, db=db, xb=xb: e.tensor_tensor(
                        out=xb[:, db * 512:(db + 1) * 512], in0=xb[:, db * 512:(db + 1) * 512], in1=t_tmp[:, 0:512], op=ALU.add),
                        reads=[xb, t_tmp], writes=[xb])
                kb.dma("pool", lambda e, s=s, tc=tc, xb=xb: e.dma_start(out=self.xr[s].ap[tc * 128:(tc + 1) * 128, :], in_=xb[:]),
                       reads=[xb], writes=[self.xr[s].sub(("row", tc))])


def _consts():
    bf = ml_dtypes.bfloat16
    c = {}
    c["c_identb"] = np.eye(128, dtype=np.float32).astype(bf)
    c["c_identf"] = np.eye(128, dtype=np.float32)
    i = np.arange(128, dtype=np.int64)
    ang = 2.0 * np.pi * ((i[:, None] * i[None, :]) % 128).astype(np.float64) / 128.0
    c["c_csc"] = np.concatenate([np.cos(ang), np.sin(ang)], axis=1).astype(np.float32) / np.float32(np.sqrt(128.0))
    c["c_csc"] = c["c_csc"].astype(bf)
    for nm, n in (("", L), ("c", LC)):
        t = np.arange(n, dtype=np.int64)
        a = 2.0 * np.pi * ((t[:, None] * t[None, :]) % n).astype(np.float64) / n
        c["c_cl" + nm] = (np.cos(a) / np.sqrt(n)).astype(np.float32).astype(bf)
        c["c_sl" + nm] = (-np.sin(a) / np.sqrt(n)).astype(np.float32).astype(bf)
    t = np.arange(L)
    row = (t // 64).astype(np.float32)
    col = (t % 64).astype(np.float32)
    inv = (np.float32(10000.0) ** (-np.arange(8, dtype=np.float32) / np.float32(8))).astype(np.float32)
    angr = np.concatenate([row[:, None] * inv[None, :], col[:, None] * inv[None, :]], axis=1).astype(np.float32)
    c["c_rope"] = np.concatenate([np.cos(angr), np.sin(angr)], axis=1).astype(np.float32)
    c["c_ctxbase"] = np.concatenate([np.zeros(32), np.full(32, LC), NS * LC + np.arange(64)]).astype(np.float32).reshape(128, 1)
    return c


def _in_map(inp, core, consts):
    f = lambda a: np.ascontiguousarray(np.asarray(a, dtype=np.float32))
    s0 = core * NS
    m = {}
    m["x"] = f(inp["x"][s0:s0 + NS])
    m["ctx"] = f(inp["ctx"][s0:s0 + NS])
    cv3 = np.stack([inp["c"][s0], inp["c"][s0 + 1], inp["c_ctx"]], axis=0).astype(np.float32)
    m["cv"] = np.ascontiguousarray(cv3.reshape(3, 8, 128).transpose(2, 1, 0))
    for k in ("mod_w", "mod_b", "norm1_g", "norm2_g", "final_g", "moe_w_router", "moe_w1", "moe_w3", "moe_w2"):
        m[k] = f(inp[k])
    for k in ("ab_w_in", "ab_conv_w", "ab_conv_b", "ab_ln_g", "ab_ln_b", "ab_w_out", "mla_w_in", "mla_q_norm_g",
              "mla_kv_norm_g", "mla_w_uq", "mla_w_ukv", "mla_w_o"):
        m[k] = f(inp[k][0])
    m.update(consts)
    return m


_CACHE = {}


def run_prog(inputs, phases, copy_in=False, ncores=8, debug_route=False, raw=False):
    key = (tuple(phases), copy_in, debug_route)
    if key not in _CACHE:
        p = Prog(phases=phases, copy_in=copy_in)
        p.debug_route = debug_route
        _CACHE[key] = p.build()
    nc = _CACHE[key]
    consts = _consts()
    in_maps = [_in_map(inputs, c, consts) for c in range(ncores)]
    res = run_bass_kernel_spmd(nc, in_maps, core_ids=list(range(ncores)))
    if raw:
        return res.results
    return np.concatenate([np.asarray(r["y"]) for r in res.results], axis=0)


def kernel(**inputs):
    out = run_prog(inputs, ("mix0", "moe0", "mla1", "moe1", "final"))
    return out.astype(np.float32)
```

```python
import numpy as np
import ml_dtypes
from contextlib import ExitStack
import concourse.bass as bass
import concourse.mybir as mybir
from concourse.bass_utils import run_bass_kernel_spmd

F32 = mybir.dt.float32
BF16 = mybir.dt.bfloat16
I32 = mybir.dt.int32
U32 = mybir.dt.uint32
U8 = mybir.dt.uint8
AF = mybir.ActivationFunctionType
ALU = mybir.AluOpType
AX = mybir.AxisListType

D = 1024
L = 2048
LC = 256
NS = 2
NE = 16
EPS = 1e-6
DSZ = {F32: 4, BF16: 2, I32: 4, U32: 4, U8: 1}


class Trk:
    def __init__(self, name):
        self.name = name
        self.w = None
        self.r = []
        self.kids = {}
        self.parent = None

    def sub(self, key):
        if key not in self.kids:
            k = Trk(f"{self.name}.{key}")
            k.parent = self
            self.kids[key] = k
        return self.kids[key]

    def rdeps(self):
        s = set()
        if self.w:
            s.add(self.w)
        if self.parent is not None and self.parent.w:
            s.add(self.parent.w)
        for k in self.kids.values():
            if k.w:
                s.add(k.w)
        return s

    def wdeps(self):
        s = self.rdeps()
        s.update(self.r)
        if self.parent is not None:
            s.update(self.parent.r)
        for k in self.kids.values():
            s.update(k.r)
        return s

    def did_read(self, ev):
        self.r.append(ev)

    def did_write(self, ev):
        self.w = ev
        self.r = []
        for k in self.kids.values():
            k.w = None
            k.r = []


class T(Trk):
    def __init__(self, kb, name, shape, dtype, off):
        super().__init__(name)
        self.kb = kb
        self.shape = shape
        self.dtype = dtype
        self.off = off
        self.h = kb.nc.alloc_sbuf_tensor_at(name, list(shape), dtype, offset=off)

    def view(self, name, shape, dtype, boff=0):
        return self.kb.nc.alloc_sbuf_tensor_at(
            self.kb.uname(name), list(shape), dtype, offset=self.off + boff)

    def __getitem__(self, k):
        return self.h[k]


class Lane:
    def __init__(self, key, sem):
        self.key = key
        self.sem = sem
        self.count = 0


class KB:
    COMPUTE = ["pe", "act", "dve", "pool"]
    QUEUES = ["sp", "pool", "act"]

    def __init__(self, n_lanes=8):
        self.nc = bass.Bass("TRN2", target_bir_lowering=False)
        nc = self.nc
        self.es = ExitStack()
        self.uid = 0
        self.semobj = {}
        self.cnt = {}
        for e in self.COMPUTE:
            self.semobj[e] = self.es.enter_context(nc.semaphore("s_" + e))
            self.cnt[e] = 0
        self.lanes = {}
        self.lane_rr = {}
        for q in self.QUEUES:
            self.lanes[q] = []
            for i in range(n_lanes):
                key = f"d_{q}{i}"
                self.semobj[key] = self.es.enter_context(nc.semaphore(key))
                self.lanes[q].append(Lane(key, self.semobj[key]))
            self.lane_rr[q] = 0
        self.prog = {e: [] for e in ["pe", "act", "dve", "pool", "sp"]}
        self.waited = {e: {} for e in ["pe", "act", "dve", "pool", "sp"]}
        self.arena_bytes = 206 * 1024
        ah = nc.alloc_sbuf_tensor("arena", [128, self.arena_bytes], U8)
        self.abase = nc.lookup_mloc(ah).addr
        self.atop = 0
        self.bnd = {}
        for n in (L - 1, NS * LC + 128 - 1):
            reg = self.es.enter_context(nc.gpsimd.register(f"bnd{n}"))
            self.bnd[n] = reg
            self.prog["pool"].append(lambda en, reg=reg, n=n: en.reg_mov(reg, n))
        self.banks = []
        self.pairs = []
        for i in range(4):
            ph = self.es.enter_context(nc.psum_tensor(f"pbank{i}", [128, 1024], F32))
            self.pairs.append(ph)
            for j in range(2):
                t = Trk(f"bank{2 * i + j}")
                t.h = ph[:, j * 512:(j + 1) * 512]
                t.psum = True
                self.banks.append(t)

    def uname(self, n):
        self.uid += 1
        return f"{n}_{self.uid}"

    def alloc(self, name, shape, dtype):
        nbytes = int(np.prod(shape[1:])) * DSZ[dtype]
        nbytes = (nbytes + 63) // 64 * 64
        off = self.atop
        assert off + nbytes <= self.arena_bytes, f"SBUF arena overflow at {name}: {off}+{nbytes}"
        self.atop += nbytes
        return T(self, self.uname(name), shape, dtype, self.abase + off)

    def mark(self):
        return self.atop

    def release(self, m):
        self.barrier()
        self.atop = m

    def dram(self, name, shape, dtype, kind="Internal"):
        if kind == "Internal":
            h = self.nc.dram_tensor(name, list(shape), dtype)
        else:
            h = self.nc.dram_tensor(name, list(shape), dtype, kind=kind)
        t = Trk(name)
        t.h = h
        t.ap = h.ap()
        return t

    def _waits(self, eng, evs):
        best = {}
        for (k, v) in evs:
            if v > best.get(k, 0):
                best[k] = v
        for k, v in best.items():
            if k == "pe" and eng == "pe":
                continue
            if self.waited[eng].get(k, 0) >= v:
                continue
            self.waited[eng][k] = v
            sem = self.semobj[k]
            self.prog[eng].append(lambda e, sem=sem, v=v: e.wait_ge(sem, v))

    def _deps(self, reads, writes, eng=None):
        evs = set()
        for t in reads:
            evs |= t.rdeps()
            root = t if t.parent is None else t.parent
            if getattr(root, "psum", False):
                for ev in root.r:
                    if ev[0] != eng:
                        evs.add(ev)
                for k in root.kids.values():
                    for ev in k.r:
                        if ev[0] != eng:
                            evs.add(ev)
        for t in writes:
            evs |= t.wdeps()
        return evs

    def op(self, eng, fn, reads=(), writes=()):
        evs = self._deps(reads, writes, eng)
        self._waits(eng, evs)
        self.cnt[eng] += 1
        sem = self.semobj[eng]
        self.prog[eng].append(lambda e, fn=fn, sem=sem: fn(e).then_inc(sem, 1))
        ev = (eng, self.cnt[eng])
        for t in reads:
            t.did_read(ev)
        for t in writes:
            t.did_write(ev)
        return ev

    def dma(self, q, fn, reads=(), writes=()):
        evs = self._deps(reads, writes)
        lanes = self.lanes[q]
        lane = lanes[self.lane_rr[q] % len(lanes)]
        self.lane_rr[q] += 1
        if lane.count > 0:
            evs.add((lane.key, lane.count))
        self._waits(q, evs)
        lane.count += 16
        sem = lane.sem
        def run(e, fn=fn, sem=sem):
            try:
                ins = fn(e)
            except Exception:
                print("DMA BUILD FAIL line", fn.__code__.co_firstlineno, "defaults", [str(d)[:80] for d in (fn.__defaults__ or ())])
                raise
            ins.then_inc(sem, 16)
        self.prog[q].append(run)
        ev = (lane.key, lane.count)
        for t in reads:
            t.did_read(ev)
        for t in writes:
            t.did_write(ev)
        return ev

    def barrier(self):
        evs = set()
        for e in self.COMPUTE:
            if self.cnt[e] > 0:
                evs.add((e, self.cnt[e]))
        for q in self.QUEUES:
            for ln in self.lanes[q]:
                if ln.count > 0:
                    evs.add((ln.key, ln.count))
        for e in ["pe", "act", "dve", "pool", "sp"]:
            self._waits(e, evs)

    def finish(self):
        self.barrier()
        nc = self.nc
        with nc.allow_non_contiguous_dma(reason="small strided constant loads"):
            with nc.Block() as block:
                @block.sync
                def _(e):
                    for f in self.prog["sp"]:
                        f(e)

                @block.tensor
                def _(e):
                    for f in self.prog["pe"]:
                        f(e)

                @block.scalar
                def _(e):
                    for f in self.prog["act"]:
                        f(e)

                @block.vector
                def _(e):
                    for f in self.prog["dve"]:
                        f(e)

                @block.gpsimd
                def _(e):
                    for f in self.prog["pool"]:
                        f(e)
        self.es.close()
        return nc


class Prog:
    def __init__(self, phases=("mix0", "moe0", "mla1", "moe1", "final"), copy_in=False):
        self.kb = KB()
        self.phases = phases
        self.copy_in = copy_in
        kb = self.kb
        di = lambda n, s, d=F32: kb.dram(n, s, d, kind="ExternalInput")
        self.x = di("x", [NS, L, D])
        self.ctx = di("ctx", [NS, LC, D])
        self.cv = di("cv", [128, 8, 3])
        self.mod_w = di("mod_w", [2, D, 6 * D])
        self.mod_b = di("mod_b", [2, 6 * D])
        self.n1g = di("norm1_g", [2, D])
        self.n2g = di("norm2_g", [2, D])
        self.final_g = di("final_g", [D])
        self.ab_w_in = di("ab_w_in", [D, 1536])
        self.ab_conv_w = di("ab_conv_w", [31, 512])
        self.ab_conv_b = di("ab_conv_b", [512])
        self.ab_ln_g = di("ab_ln_g", [512])
        self.ab_ln_b = di("ab_ln_b", [512])
        self.ab_w_out = di("ab_w_out", [D, D])
        self.mla_w_in = di("mla_w_in", [D, 416])
        self.mla_qg = di("mla_q_norm_g", [256])
        self.mla_kvg = di("mla_kv_norm_g", [128])
        self.mla_w_uq = di("mla_w_uq", [256, 1536])
        self.mla_w_ukv = di("mla_w_ukv", [128, 2048])
        self.mla_w_o = di("mla_w_o", [D, D])
        self.w_router = di("moe_w_router", [2, D, NE])
        self.w1 = di("moe_w1", [2, NE, D, D])
        self.w3 = di("moe_w3", [2, NE, D, D])
        self.w2 = di("moe_w2", [2, NE, D, D])
        self.c_identb = di("c_identb", [128, 128], BF16)
        self.c_identf = di("c_identf", [128, 128], F32)
        self.c_csc = di("c_csc", [128, 256], BF16)
        self.c_cl = di("c_cl", [L, L], BF16)
        self.c_sl = di("c_sl", [L, L], BF16)
        self.c_clc = di("c_clc", [LC, LC], BF16)
        self.c_slc = di("c_slc", [LC, LC], BF16)
        self.c_rope = di("c_rope", [L, 32], F32)
        self.c_ctxbase = di("c_ctxbase", [128, 1], F32)
        self.out = kb.dram("y", [NS, L, D], F32, kind="ExternalOutput")
        self.xr = [kb.dram(f"xr{s}", [L, D], F32) for s in range(NS)]
        self.xc_all = kb.dram("xc_all", [NS * LC + 128, D], F32)
        self.xnc_all = kb.dram("xnc_all", [NS * LC + 128, D], BF16)
        self.xc = []
        self.xnl = [kb.dram(f"xnl{s}", [L, D], BF16) for s in range(NS)]
        self.xnc = []
        for s in range(NS):
            t = self.xc_all.sub(s); t.ap = self.xc_all.ap[s * LC:(s + 1) * LC, :]; self.xc.append(t)
            t = self.xnc_all.sub(s); t.ap = self.xnc_all.ap[s * LC:(s + 1) * LC, :]; self.xnc.append(t)
        self.xin = []
        self.cin = []
        for s in range(NS):
            t = self.x.sub(s); t.ap = self.x.ap[s]; self.xin.append(t)
            t = self.ctx.sub(s); t.ap = self.ctx.ap[s]; self.cin.append(t)
        self.modd = kb.dram("modd", [2, 3, 6 * D], F32)

    def build(self):
        kb = self.kb
        self.prologue()
        if self.copy_in:
            for s in range(NS):
                kb.dma("sp", lambda e, s=s: e.dma_start(out=self.xr[s].ap, in_=self.x.ap[s]),
                       reads=[self.x], writes=[self.xr[s]])
                kb.dma("sp", lambda e, s=s: e.dma_start(out=self.xc[s].ap, in_=self.ctx.ap[s]),
                       reads=[self.ctx], writes=[self.xc[s]])
            kb.barrier()
        for ph in self.phases:
            m = kb.mark()
            if ph == "mix0":
                self.mixer0()
            elif ph == "moe0":
                self.moe(0, with_ctx=True)
            elif ph == "mla1":
                self.mla()
            elif ph == "moe1":
                self.moe(1, with_ctx=False)
            elif ph == "final":
                self.final()
            elif ph == "dump":
                self.dump()
            kb.release(m)
        return kb.finish()

    def prologue(self):
        kb = self.kb
        self.identb = kb.alloc("identb", [128, 128], BF16)
        self.identf = kb.alloc("identf", [128, 128], F32)
        kb.dma("sp", lambda e: e.dma_start(out=self.identb[:], in_=self.c_identb.ap[:, :]),
               reads=[self.c_identb], writes=[self.identb])
        kb.dma("sp", lambda e: e.dma_start(out=self.identf[:], in_=self.c_identf.ap[:, :]),
               reads=[self.c_identf], writes=[self.identf])
        self.epsc = kb.alloc("epsc", [128, 1], F32)
        kb.op("dve", lambda e: e.memset(self.epsc[:], EPS), writes=[self.epsc])
        self.zeroc = kb.alloc("zeroc", [128, 1], F32)
        kb.op("dve", lambda e: e.memset(self.zeroc[:], 0.0), writes=[self.zeroc])
        self.modc = [kb.alloc(f"modc{l}", [128, 48, 3], F32) for l in range(2)]
        self.mul1c = [kb.alloc(f"mul1c{l}", [128, 8, 3], F32) for l in range(2)]
        self.mul2c = [kb.alloc(f"mul2c{l}", [128, 8, 3], F32) for l in range(2)]
        self.n1gc = kb.alloc("n1gc", [128, 2, 8], F32)
        self.n2gc = kb.alloc("n2gc", [128, 2, 8], F32)
        kb.dma("sp", lambda e: e.dma_start(out=self.n1gc[:], in_=self.n1g.ap.rearrange("l (k p) -> p l k", p=128)),
               reads=[self.n1g], writes=[self.n1gc])
        kb.dma("sp", lambda e: e.dma_start(out=self.n2gc[:], in_=self.n2g.ap.rearrange("l (k p) -> p l k", p=128)),
               reads=[self.n2g], writes=[self.n2gc])
        m0 = kb.mark()
        zf = kb.alloc("zf", [128, D], F32)
        zb = kb.alloc("zb", [128, D], BF16)
        kb.op("dve", lambda e: e.memset(zf[:], 0.0), writes=[zf])
        kb.op("dve", lambda e: e.memset(zb[:], 0.0), writes=[zb])
        kb.dma("sp", lambda e: e.dma_start(out=self.xc_all.ap[NS * LC:NS * LC + 128, :], in_=zf[:]),
               reads=[zf], writes=[self.xc_all.sub("pad")])
        kb.dma("sp", lambda e: e.dma_start(out=self.xnc_all.ap[NS * LC:NS * LC + 128, :], in_=zb[:]),
               reads=[zb], writes=[self.xnc_all.sub("pad")])
        cvt = kb.alloc("cvt", [128, 24], F32)
        sct = kb.alloc("sct", [128, 24], BF16)
        kb.dma("sp", lambda e: e.dma_start(out=cvt[:], in_=self.cv.ap.rearrange("p k r -> p (k r)")),
               reads=[self.cv], writes=[cvt])
        kb.op("act", lambda e: e.activation(out=sct[:], in_=cvt[:], func=AF.Silu), reads=[cvt], writes=[sct])
        mwt = [kb.alloc(f"mwt{i}", [128, 8, 1536], BF16) for i in range(3)]
        mrow = kb.alloc("mrow", [3, 6 * D], F32)
        mb3 = kb.alloc("mb3", [3, 6 * D], F32)
        it = 0
        for l in range(2):
            kb.dma("sp", lambda e, l=l: e.dma_start(out=mb3[:], in_=self.mod_b.ap[l].partition_broadcast(3)),
                   reads=[self.mod_b], writes=[mb3])
            for pc in range(4):
                wt = mwt[it % 3]
                it += 1
                src = self.mod_w.ap[l].rearrange("(k p) n -> p k n", p=128)[:, :, pc * 1536:(pc + 1) * 1536]
                kb.dma("pool", lambda e, wt=wt, src=src: e.dma_start(out=wt[:], in_=src),
                       reads=[self.mod_w], writes=[wt])
                for nb in range(3):
                    bank = kb.banks[(pc * 3 + nb) % 2]
                    for k in range(8):
                        kb.op("pe", lambda e, bank=bank, wt=wt, k=k, nb=nb: e.matmul(
                            out=bank.h[0:3, 0:512], lhsT=sct[:, k * 3:(k + 1) * 3],
                            rhs=wt[:, k, nb * 512:(nb + 1) * 512], start=(k == 0), stop=(k == 7)),
                            reads=[sct, wt], writes=[bank])
                    c0 = pc * 1536 + nb * 512
                    kb.op("dve", lambda e, bank=bank, c0=c0: e.tensor_tensor(
                        out=mrow[0:3, c0:c0 + 512], in0=bank.h[0:3, 0:512], in1=mb3[0:3, c0:c0 + 512], op=ALU.add),
                        reads=[bank, mb3], writes=[mrow.sub(c0)])
            kb.dma("sp", lambda e, l=l: e.dma_start(out=self.modd.ap[l], in_=mrow[0:3, :]),
                   reads=[mrow], writes=[self.modd.sub(l)])
            for r in range(3):
                kb.dma("sp", lambda e, l=l, r=r: e.dma_start(
                    out=self.modc[l][:, :, r], in_=self.modd.ap[l, r].rearrange("(c p) -> p c", p=128)),
                    reads=[self.modd.sub(l)], writes=[self.modc[l].sub(r)])
            for (mulc, gc, v) in ((self.mul1c[l], self.n1gc, 1), (self.mul2c[l], self.n2gc, 4)):
                kb.op("dve", lambda e, mulc=mulc, v=v, l=l: e.tensor_scalar(
                    out=mulc[:], in0=self.modc[l][:, v * 8:(v + 1) * 8, :], scalar1=1.0, scalar2=None, op0=ALU.add),
                    reads=[self.modc[l]], writes=[mulc])
                for r in range(3):
                    kb.op("dve", lambda e, mulc=mulc, gc=gc, r=r, l=l: e.tensor_tensor(
                        out=mulc[:, :, r], in0=mulc[:, :, r], in1=gc[:, l, :], op=ALU.mult),
                        reads=[mulc, gc], writes=[mulc])
        kb.release(m0)

    def norm_batch(self, src, row0, nt, mulc, addc, r, uT, ucol0, bufs, banks, xn_dst=None):
        kb = self.kb
        xb, xnb, junk, stat = bufs
        tiles = []
        for j in range(nt):
            xt = xb[self._nb % len(xb)]
            xn = xnb[self._nb % len(xnb)]
            sc = self._nb % 64
            self._nb += 1
            tiles.append((j, xt, xn, sc))
            rr = row0 + j * 128
            kb.dma("sp", lambda e, xt=xt, rr=rr: e.dma_start(out=xt[:], in_=src.ap[rr:rr + 128, :]),
                   reads=[src], writes=[xt])
            kb.op("act", lambda e, xt=xt, sc=sc: e.activation(
                out=junk[:], in_=xt[:], func=AF.Square, accum_out=stat[:, sc:sc + 1]),
                reads=[xt], writes=[junk, stat.sub(sc)])
            kb.op("act", lambda e, sc=sc: e.activation(
                out=stat[:, 64 + sc:65 + sc], in_=stat[:, sc:sc + 1], func=AF.Identity, scale=1.0 / D, bias=self.epsc[:, 0:1]),
                reads=[stat.sub(sc), self.epsc], writes=[stat.sub(64 + sc)])
            kb.op("act", lambda e, sc=sc: e.activation(
                out=stat[:, 128 + sc:129 + sc], in_=stat[:, 64 + sc:65 + sc], func=AF.Sqrt),
                reads=[stat.sub(64 + sc)], writes=[stat.sub(128 + sc)])
            kb.op("dve", lambda e, sc=sc: e.reciprocal(out=stat[:, 192 + sc:193 + sc], in_=stat[:, 128 + sc:129 + sc]),
                  reads=[stat.sub(128 + sc)], writes=[stat.sub(192 + sc)])
            if len(xb) == 1:
                self._norm_tail(tiles.pop(), src, row0, mulc, addc, r, uT, ucol0, banks, xn_dst)
        for t in tiles:
            self._norm_tail(t, src, row0, mulc, addc, r, uT, ucol0, banks, xn_dst)

    def _norm_tail(self, t, src, row0, mulc, addc, r, uT, ucol0, banks, xn_dst):
        kb = self.kb
        (j, xt, xn, sc) = t
        stat = self._stat
        kb.op("dve", lambda e, xt=xt, xn=xn, sc=sc: e.tensor_scalar(
            out=xn[:], in0=xt[:], scalar1=stat[:, 192 + sc:193 + sc], scalar2=None, op0=ALU.mult),
            reads=[xt, stat.sub(192 + sc)], writes=[xn])
        if xn_dst is not None:
            dt, drow = xn_dst
            dr0 = drow + j * 128
            kb.dma("pool", lambda e, xn=xn, dt=dt, dr=dr0: e.dma_start(
                out=dt.ap[dr:dr + 128, :], in_=xn[:]), reads=[xn], writes=[dt.sub(dr0)])
        for k in range(8):
            bank = banks[2 * j + k // 4]
            pv = bank.h.bitcast(BF16)
            kk = k % 4
            kb.op("pe", lambda e, pv=pv, xn=xn, k=k, kk=kk: e.transpose(
                out=pv[:, kk * 128:(kk + 1) * 128], in_=xn[:, k * 128:(k + 1) * 128], identity=self.identb[:]),
                reads=[xn, self.identb], writes=[bank])
        for k in range(8):
            bank = banks[2 * j + k // 4]
            pv = bank.h.bitcast(BF16)
            kk = k % 4
            dst = uT[:, k, ucol0 + j * 128: ucol0 + (j + 1) * 128]
            if k < 4:
                kb.op("act", lambda e, dst=dst, pv=pv, k=k, kk=kk: e.activation(
                    out=dst, in_=pv[:, kk * 128:(kk + 1) * 128], func=AF.Identity,
                    scale=mulc[:, k, r:r + 1], bias=addc(k, r)),
                    reads=[bank, mulc], writes=[uT.sub((k, ucol0 + j * 128))])
            else:
                kb.op("dve", lambda e, dst=dst, pv=pv, k=k, kk=kk: e.tensor_scalar(
                    out=dst, in0=pv[:, kk * 128:(kk + 1) * 128], scalar1=mulc[:, k, r:r + 1],
                    scalar2=addc(k, r), op0=ALU.mult, op1=ALU.add),
                    reads=[bank, mulc], writes=[uT.sub((k, ucol0 + j * 128))])

    def norm_bufs(self, nx=2):
        kb = self.kb
        self._nb = 0
        xb = [kb.alloc(f"xb{i}", [128, D], F32) for i in range(nx)]
        xnb = [kb.alloc(f"xnb{i}", [128, D], BF16) for i in range(nx)]
        junk = kb.alloc("junk", [128, D], BF16)
        stat = kb.alloc("stat", [128, 256], F32)
        kb.op("dve", lambda e: e.memset(stat[:], 0.0), writes=[stat])
        self._stat = stat
        return (xb, xnb, junk, stat)

    def dump(self):
        kb = self.kb
        yc = kb.dram("yc", [NS * LC, D], F32, kind="ExternalOutput")
        kb.dma("sp", lambda e: e.dma_start(out=yc.ap[:, :], in_=self.xc_all.ap[0:NS * LC, :]),
               reads=[self.xc_all], writes=[yc])
        for s in range(NS):
            kb.dma("sp", lambda e, s=s: e.dma_start(out=self.out.ap[s], in_=self.xr[s].ap),
                   reads=[self.xr[s]], writes=[self.out.sub(s)])

    def final(self):
        kb = self.kb
        fgb = kb.alloc("fgb", [128, D], F32)
        kb.dma("sp", lambda e: e.dma_start(out=fgb[:], in_=self.final_g.ap.partition_broadcast(128)),
               reads=[self.final_g], writes=[fgb])
        NB = 8
        xb = [kb.alloc(f"fxb{i}", [128, D], F32) for i in range(NB)]
        yb = [kb.alloc(f"fyb{i}", [128, D], F32) for i in range(NB)]
        junk = kb.alloc("fjunk", [128, D], BF16)
        stat = kb.alloc("fstat", [128, 4 * 64], F32)
        kb.op("dve", lambda e: e.memset(stat[:], 0.0), writes=[stat])
        tiles = [(s, t) for s in range(NS) for t in range(L // 128)]
        for b0 in range(0, len(tiles), 4):
            batch = tiles[b0:b0 + 4]
            info = []
            for bi, (s, t) in enumerate(batch):
                i = b0 + bi
                xt = xb[i % NB]
                yt = yb[i % NB]
                sc = i % 64
                info.append((s, t, xt, yt, sc))
                kb.dma("sp", lambda e, xt=xt, s=s, t=t: e.dma_start(out=xt[:], in_=self.xr[s].ap[t * 128:(t + 1) * 128, :]),
                       reads=[self.xr[s]], writes=[xt])
                kb.op("act", lambda e, xt=xt, sc=sc: e.activation(
                    out=junk[:], in_=xt[:], func=AF.Square, accum_out=stat[:, sc:sc + 1]),
                    reads=[xt], writes=[junk, stat.sub(sc)])
                kb.op("act", lambda e, sc=sc: e.activation(
                    out=stat[:, 64 + sc:65 + sc], in_=stat[:, sc:sc + 1], func=AF.Identity, scale=1.0 / D, bias=self.epsc[:, 0:1]),
                    reads=[stat.sub(sc), self.epsc], writes=[stat.sub(64 + sc)])
                kb.op("act", lambda e, sc=sc: e.activation(
                    out=stat[:, 128 + sc:129 + sc], in_=stat[:, 64 + sc:65 + sc], func=AF.Sqrt),
                    reads=[stat.sub(64 + sc)], writes=[stat.sub(128 + sc)])
                kb.op("dve", lambda e, sc=sc: e.reciprocal(out=stat[:, 192 + sc:193 + sc], in_=stat[:, 128 + sc:129 + sc]),
                      reads=[stat.sub(128 + sc)], writes=[stat.sub(192 + sc)])
            for bi, (s, t, xt, yt, sc) in enumerate(info):
                eng = "dve"
                kb.op(eng, lambda e, xt=xt, yt=yt, sc=sc: e.scalar_tensor_tensor(
                    out=yt[:], in0=xt[:], scalar=stat[:, 192 + sc:193 + sc], in1=fgb[:], op0=ALU.mult, op1=ALU.mult),
                    reads=[xt, stat.sub(192 + sc), fgb], writes=[yt])
                kb.dma("pool", lambda e, yt=yt, s=s, t=t: e.dma_start(out=self.out.ap[s, t * 128:(t + 1) * 128, :], in_=yt[:]),
                       reads=[yt], writes=[self.out.sub((s, t))])

    def moe(self, l, with_ctx):
        kb = self.kb
        IOA = bass.IndirectOffsetOnAxis
        wbufs = [[kb.alloc(f"w{n}_{i}", [128, 8, D], BF16) for n in (1, 3, 2)] for i in range(2)]
        wsrc = (self.w1, self.w3, self.w2)

        def load_w(e):
            for n in range(3):
                src = wsrc[n].ap[l, e].rearrange("(k p) f -> p k f", p=128)
                wt = wbufs[e % 2][n]
                kb.dma("pool", lambda en, wt=wt, src=src: en.dma_start(out=wt[:], in_=src),
                       reads=[wsrc[n]], writes=[wt])

        nr = 3 if with_ctx else 2
        g2b = [kb.alloc(f"g2b{r}", [128, D], F32) for r in range(nr)]
        for r in range(nr):
            kb.dma("sp", lambda e, r=r: e.dma_start(
                out=g2b[r][:], in_=self.modd.ap[l, r, 5 * D:6 * D].partition_broadcast(128)),
                reads=[self.modd.sub(l)], writes=[g2b[r]])
        idxT = kb.alloc("idxT", [128, 2, 48], I32)
        gT = kb.alloc("gT", [128, 2, 48], F32)
        idxC = kb.alloc("idxC", [128, NE], I32)
        gC = kb.alloc("gC", [128, NE], F32)
        load_w(0)
        load_w(1)
        addc2 = lambda k, r: self.modc[l][:, 24 + k, r:r + 1]
        mulc2 = self.mul2c[l]

        dbg = getattr(self, "debug_route", 0)
        if dbg == 10:
            return
        m1 = kb.mark()
        bufs = self.norm_bufs(nx=4)
        uTb = [kb.alloc(f"uTb{i}", [128, 8, 256], BF16) for i in range(2)]
        wr = kb.alloc("wr", [128, 8, NE], BF16)
        wrf = kb.alloc("wrf", [128, 8, NE], F32)
        kb.dma("sp", lambda e: e.dma_start(out=wrf[:], in_=self.w_router.ap[l].rearrange("(k p) e -> p k e", p=128)),
               reads=[self.w_router], writes=[wrf])
        kb.op("dve", lambda e: e.tensor_copy(out=wr[:], in_=wrf[:]), reads=[wrf], writes=[wr])
        aff2 = kb.alloc("aff2", [128, 16, 64], F32)
        affc = kb.alloc("affc", [128, 2, 64], F32)
        kb.op("dve", lambda e: e.memset(aff2[:].rearrange("p a b -> p (a b)"), 0.0), writes=[aff2])
        kb.op("dve", lambda e: e.memset(affc[:].rearrange("p a b -> p (a b)"), 0.0), writes=[affc])
        lg = kb.alloc("lg", [128, 16, 16], F32)
        mx = kb.alloc("mx", [128, 16], F32)
        sm = kb.alloc("sm", [128, 16], F32)
        rs = kb.alloc("rs", [128, 16], F32)
        work = kb.alloc("work", [48, L], F32)
        workc = kb.alloc("workc", [48, LC], F32)
        topv = kb.alloc("topv", [48, 256], F32)
        topi = kb.alloc("topi", [48, 256], U32)
        topif = kb.alloc("topif", [48, 256], F32)
        topvc = kb.alloc("topvc", [48, 32], F32)
        topic = kb.alloc("topic", [48, 32], U32)
        topicf = kb.alloc("topicf", [48, 32], F32)
        nbatch = 0
        seqs = []
        for s in range(NS):
            seqs.append((self.xr[s], self.xnl[s], L // 128, s, aff2, s * 32, kb.banks[4 + s], 0))
        if with_ctx:
            for s in range(NS):
                seqs.append((self.xc[s], self.xnc[s], LC // 128, 2, affc, s * 32, kb.banks[6], s * 32))
        for (src, xnd, ntl, r, afft, acol, lbank, lcol0) in seqs:
            for b in range((ntl + 1) // 2):
                nt = min(2, ntl - b * 2)
                ub = uTb[nbatch % 2]
                nbatch += 1
                self.norm_batch(src, b * 256, nt, mulc2, addc2, r, ub, 0, bufs,
                                [kb.banks[j] for j in range(2 * nt)], xn_dst=(xnd, b * 256))
                for j in range(nt):
                    if dbg == 11:
                        continue
                    c = b * 2 + j
                    for k in range(8):
                        kb.op("pe", lambda e, lbank=lbank, lc=lcol0 + c * 16, ub=ub, k=k, j=j: e.matmul(
                            out=lbank.h[:, lc:lc + 16], lhsT=ub[:, k, j * 128:(j + 1) * 128], rhs=wr[:, k, :],
                            start=(k == 0), stop=(k == 7)),
                            reads=[ub.sub((k, j * 128)), wr], writes=[lbank])
            if dbg == 11:
                continue
            lv = lbank.h[:, lcol0:lcol0 + ntl * 16].rearrange("p (c e) -> p c e", e=16)
            bc = lambda t, ntl=ntl: t[:, 0:ntl].unsqueeze(2).to_broadcast([128, ntl, 16])
            kb.op("dve", lambda e, lv=lv, ntl=ntl: e.tensor_reduce(out=mx[:, 0:ntl], in_=lv, axis=AX.X, op=ALU.max),
                  reads=[lbank], writes=[mx])
            kb.op("dve", lambda e, lv=lv, bc=bc, ntl=ntl: e.tensor_tensor(out=lg[:, 0:ntl, :], in0=lv, in1=bc(mx), op=ALU.subtract),
                  reads=[lbank, mx], writes=[lg])
            kb.op("act", lambda e, ntl=ntl: e.activation(out=lg[:, 0:ntl, :], in_=lg[:, 0:ntl, :], func=AF.Exp),
                  reads=[lg], writes=[lg])
            kb.op("dve", lambda e, ntl=ntl: e.tensor_reduce(out=sm[:, 0:ntl], in_=lg[:, 0:ntl, :], axis=AX.X, op=ALU.add),
                  reads=[lg], writes=[sm])
            kb.op("dve", lambda e, ntl=ntl: e.reciprocal(out=rs[:, 0:ntl], in_=sm[:, 0:ntl]), reads=[sm], writes=[rs])
            kb.op("dve", lambda e, afft=afft, acol=acol, bc=bc, ntl=ntl: e.tensor_tensor(
                out=afft[:, 0:ntl, acol:acol + 16], in0=lg[:, 0:ntl, :], in1=bc(rs), op=ALU.mult),
                reads=[lg, rs], writes=[afft])
        if dbg == 11:
            kb.release(m1)
            return
        if dbg == 1:
            da = kb.dram("dbg_aff", [128, 1024], F32, kind="ExternalOutput")
            kb.dma("sp", lambda e: e.dma_start(out=da.ap[:, :], in_=aff2[:].rearrange("p h c -> p (h c)")), reads=[aff2], writes=[da])
            kb.release(m1)
            return
        for c in range(16):
            bank = kb.banks[c // 4]
            kb.op("pe", lambda e, bank=bank, c=c: e.transpose(
                out=bank.h[0:48, (c % 4) * 128:(c % 4 + 1) * 128], in_=aff2[:, c, 0:48], identity=self.identf[:]),
                reads=[aff2, self.identf], writes=[bank])
        for q in range(4):
            eng = "act" if q % 2 == 0 else "dve"
            if eng == "act":
                kb.op("act", lambda e, q=q: e.copy(out=work[0:48, q * 512:(q + 1) * 512], in_=kb.banks[q].h[0:48, 0:512]),
                      reads=[kb.banks[q]], writes=[work.sub(q)])
            else:
                kb.op("dve", lambda e, q=q: e.tensor_copy(out=work[0:48, q * 512:(q + 1) * 512], in_=kb.banks[q].h[0:48, 0:512]),
                      reads=[kb.banks[q]], writes=[work.sub(q)])
        if with_ctx:
            for c in range(2):
                kb.op("pe", lambda e, c=c: e.transpose(
                    out=kb.banks[7].h[0:48, c * 128:(c + 1) * 128], in_=affc[:, c, 0:48], identity=self.identf[:]),
                    reads=[affc, self.identf], writes=[kb.banks[7]])
            kb.op("act", lambda e: e.copy(out=workc[0:48, :], in_=kb.banks[7].h[0:48, 0:256]),
                  reads=[kb.banks[7]], writes=[workc])

        def topk(wk, tv, ti, niter):
            for it in range(niter):
                sl = slice(it * 8, (it + 1) * 8)
                kb.op("dve", lambda e, sl=sl: e.max(out=tv[:, sl], in_=wk[:]), reads=[wk], writes=[tv.sub(it)])
                kb.op("dve", lambda e, sl=sl: e.max_index(out=ti[:, sl], in_max=tv[:, sl], in_values=wk[:]),
                      reads=[wk, tv.sub(it)], writes=[ti.sub(it)])
                kb.op("dve", lambda e, sl=sl: e.match_replace(out=wk[:], in_to_replace=tv[:, sl], in_values=wk[:], imm_value=-1.0),
                      reads=[tv.sub(it), wk], writes=[wk])

        if dbg == 2:
            da = kb.dram("dbg_work", [48, L], F32, kind="ExternalOutput")
            kb.dma("sp", lambda e: e.dma_start(out=da.ap[:, :], in_=work[:]), reads=[work], writes=[da])
            kb.release(m1)
            return
        topk(work, topv, topi, 32)
        if dbg == 3:
            da = kb.dram("dbg_topv", [48, 256], F32, kind="ExternalOutput")
            kb.dma("sp", lambda e: e.dma_start(out=da.ap[:, :], in_=topv[:]), reads=[topv], writes=[da])
            db_ = kb.dram("dbg_topi", [48, 256], U32, kind="ExternalOutput")
            kb.dma("sp", lambda e: e.dma_start(out=db_.ap[:, :], in_=topi[:]), reads=[topi], writes=[db_])
            kb.release(m1)
            return
        kb.op("dve", lambda e: e.tensor_copy(out=topif[:], in_=topi[:]), reads=[topi], writes=[topif])
        tb = kb.banks[5]
        for h in range(2):
            kb.op("pe", lambda e, h=h: e.transpose(out=tb.h[:, h * 48:(h + 1) * 48], in_=topif[0:48, h * 128:(h + 1) * 128],
                                                   identity=self.identf[0:48, 0:48]),
                  reads=[topif, self.identf], writes=[tb])
            kb.op("pe", lambda e, h=h: e.transpose(out=tb.h[:, 128 + h * 48:128 + (h + 1) * 48], in_=topv[0:48, h * 128:(h + 1) * 128],
                                                   identity=self.identf[0:48, 0:48]),
                  reads=[topv, self.identf], writes=[tb])
        kb.op("dve", lambda e: e.tensor_copy(out=idxT[:], in_=tb.h[:, 0:96].rearrange("p (h c) -> p h c", h=2)),
              reads=[tb], writes=[idxT])
        kb.op("dve", lambda e: e.tensor_copy(out=gT[:], in_=tb.h[:, 128:224].rearrange("p (h c) -> p h c", h=2)),
              reads=[tb], writes=[gT])
        if with_ctx:
            topk(workc, topvc, topic, 4)
            kb.op("dve", lambda e: e.tensor_copy(out=topicf[:], in_=topic[:]), reads=[topic], writes=[topicf])
            cbase = kb.alloc("cbase", [128, 1], F32)
            kb.dma("sp", lambda e: e.dma_start(out=cbase[:], in_=self.c_ctxbase.ap[:, :]), reads=[self.c_ctxbase], writes=[cbase])
            for (srcT, dstT, isidx) in ((topicf, idxC, True), (topvc, gC, False)):
                M = kb.alloc("Mc", [48, 128], F32)
                kb.op("dve", lambda e, M=M: e.memset(M[:], 0.0), writes=[M])
                kb.op("dve", lambda e, M=M, srcT=srcT: e.tensor_copy(out=M[0:16, 0:32], in_=srcT[0:16, 0:32]), reads=[srcT], writes=[M])
                kb.op("dve", lambda e, M=M, srcT=srcT: e.tensor_copy(out=M[32:48, 32:64], in_=srcT[32:48, 0:32]), reads=[srcT], writes=[M])
                kb.op("pe", lambda e, M=M: e.transpose(out=tb.h[:, 256:304], in_=M[0:48, :], identity=self.identf[0:48, 0:48]),
                      reads=[M, self.identf], writes=[tb])
                tcp = kb.alloc("tcp", [128, 48], F32)
                kb.op("dve", lambda e, tcp=tcp: e.tensor_copy(out=tcp[:], in_=tb.h[:, 256:304]), reads=[tb], writes=[tcp])
                tsum = kb.alloc("tsum", [128, NE], F32)
                kb.op("dve", lambda e, tcp=tcp, tsum=tsum: e.tensor_tensor(out=tsum[:], in0=tcp[:, 0:16], in1=tcp[:, 32:48], op=ALU.add),
                      reads=[tcp], writes=[tsum])
                if isidx:
                    kb.op("dve", lambda e, tsum=tsum: e.tensor_scalar(out=tsum[:], in0=tsum[:], scalar1=cbase[:, 0:1], scalar2=None, op0=ALU.add),
                          reads=[tsum, cbase], writes=[tsum])
                kb.op("dve", lambda e, tsum=tsum, dstT=dstT: e.tensor_copy(out=dstT[:], in_=tsum[:]), reads=[tsum], writes=[dstT])
        if dbg == 4:
            di = kb.dram("dbg_idx", [128, 96], I32, kind="ExternalOutput")
            dg = kb.dram("dbg_g", [128, 96], F32, kind="ExternalOutput")
            da = kb.dram("dbg_aff", [128, 1024], F32, kind="ExternalOutput")
            kb.dma("sp", lambda e: e.dma_start(out=di.ap[:, :], in_=idxT[:].rearrange("p h c -> p (h c)")), reads=[idxT], writes=[di])
            kb.dma("sp", lambda e: e.dma_start(out=dg.ap[:, :], in_=gT[:].rearrange("p h c -> p (h c)")), reads=[gT], writes=[dg])
            kb.dma("sp", lambda e: e.dma_start(out=da.ap[:, :], in_=aff2[:].rearrange("p h c -> p (h c)")), reads=[aff2], writes=[da])
            kb.release(m1)
            return
        kb.release(m1)

        G = []
        for s in range(NS):
            for h in range(2):
                G.append(dict(co=s * 256 + h * 128, idx=lambda e, s=s, h=h: idxT[:, h, s * 32 + e:s * 32 + e + 1],
                              gate=lambda e, s=s, h=h: gT[:, h, s * 32 + e:s * 32 + e + 1], r=s,
                              xn=self.xnl[s], dst=self.xr[s], n=L))
        if with_ctx:
            G.append(dict(co=512, idx=lambda e: idxC[:, e:e + 1], gate=lambda e: gC[:, e:e + 1], r=2,
                          xn=self.xnc_all, dst=self.xc_all, n=NS * LC + 128))
        NSL = 128 * len(G)
        HW = NSL // 2
        halves = [(0, HW), (HW, HW)]
        Xg = [[kb.alloc(f"xg{i}_{gi}", [128, D], BF16) for gi in range(len(G))] for i in range(2)]
        XeT = [kb.alloc(f"xeT{i}", [128, 8, NSL], BF16) for i in range(2)]
        hidT = kb.alloc("hidT", [128, 8, NSL], BF16)
        sgt = [kb.alloc(f"sgt{i}", [128, HW], F32) for i in range(2)]
        yo = [kb.alloc(f"yo{i}", [128, D], F32) for i in range(3)]
        nyo = 0
        nyb = 0

        def gathers(e):
            for gi, g in enumerate(G):
                xg = Xg[e % 2][gi]
                kb.dma("pool", lambda en, xg=xg, g=g, e=e: en.indirect_dma_start(
                    out=xg[:], out_offset=None, in_=g["xn"].ap[:, :], in_offset=IOA(ap=g["idx"](e), axis=0)),
                    reads=[g["xn"], idxT, idxC], writes=[xg])

        def transposes(e, gi):
            g = G[gi]
            co, r = g["co"], g["r"]
            xg = Xg[e % 2][gi]
            xe_ = XeT[e % 2]
            for k in range(8):
                bank = kb.banks[6 + k // 4]
                pv = bank.h.bitcast(BF16)
                kk = k % 4
                kb.op("pe", lambda en, pv=pv, xg=xg, k=k, kk=kk: en.transpose(
                    out=pv[:, kk * 128:(kk + 1) * 128], in_=xg[:, k * 128:(k + 1) * 128],
                    identity=self.identb[:]), reads=[xg, self.identb], writes=[bank])
            for k in range(8):
                bank = kb.banks[6 + k // 4]
                pv = bank.h.bitcast(BF16)
                kk = k % 4
                dst = xe_[:, k, co:co + 128]
                if k < 4:
                    kb.op("act", lambda en, dst=dst, pv=pv, kk=kk, r=r, k=k: en.activation(
                        out=dst, in_=pv[:, kk * 128:(kk + 1) * 128], func=AF.Identity,
                        scale=mulc2[:, k, r:r + 1], bias=addc2(k, r)),
                        reads=[bank], writes=[xe_.sub((k, co))])
                else:
                    kb.op("dve", lambda en, dst=dst, pv=pv, kk=kk, r=r, k=k: en.tensor_scalar(
                        out=dst, in0=pv[:, kk * 128:(kk + 1) * 128], scalar1=mulc2[:, k, r:r + 1],
                        scalar2=addc2(k, r), op0=ALU.mult, op1=ALU.add),
                        reads=[bank], writes=[xe_.sub((k, co))])

        gathers(0)
        gathers(1)
        for gi in range(len(G)):
            transposes(0, gi)
        for e in range(NE):
            if e + 2 < NE:
                pass
            w1t, w3t, w2t = wbufs[e % 2]
            xe = XeT[e % 2]
            for fc in range(8):
                for hi, (h0, hw) in enumerate(halves):
                    b1 = kb.banks[hi]
                    b3 = kb.banks[2 + hi]
                    for (wt, bm) in ((w1t, b1), (w3t, b3)):
                        for k in range(8):
                            kb.op("pe", lambda en, wt=wt, bm=bm, k=k, fc=fc, xe=xe, h0=h0, hw=hw: en.matmul(
                                out=bm.h[:, 0:hw], lhsT=wt[:, k, fc * 128:(fc + 1) * 128], rhs=xe[:, k, h0:h0 + hw],
                                start=(k == 0), stop=(k == 7)), reads=[wt, xe], writes=[bm])
                    sg = sgt[hi]
                    kb.op("act", lambda en, sg=sg, b1=b1, hw=hw: en.activation(out=sg[:, 0:hw], in_=b1.h[:, 0:hw], func=AF.Silu),
                          reads=[b1], writes=[sg])
                    kb.op("dve", lambda en, sg=sg, b3=b3, fc=fc, h0=h0, hw=hw: en.tensor_tensor(
                        out=hidT[:, fc, h0:h0 + hw], in0=sg[:, 0:hw], in1=b3.h[:, 0:hw], op=ALU.mult),
                        reads=[sg, b3], writes=[hidT.sub((fc, h0))])
            for gi, g in enumerate(G):
                if e + 1 < NE:
                    transposes(e + 1, gi)
                co, r = g["co"], g["r"]
                yt = yo[nyo % 3]
                nyo += 1
                for db in range(2):
                    by = kb.banks[4 + nyb % 2]
                    nyb += 1
                    for fc in range(8):
                        kb.op("pe", lambda en, by=by, fc=fc, co=co, db=db, w2t=w2t: en.matmul(
                            out=by.h[:, 0:512], lhsT=hidT[:, fc, co:co + 128], rhs=w2t[:, fc, db * 512:(db + 1) * 512],
                            start=(fc == 0), stop=(fc == 7)), reads=[hidT, w2t], writes=[by])
                    kb.op("dve", lambda en, by=by, yt=yt, db=db, g=g, e=e, r=r: en.scalar_tensor_tensor(
                        out=yt[:, db * 512:(db + 1) * 512], in0=by.h[:, 0:512], scalar=g["gate"](e),
                        in1=g2b[r][:, db * 512:(db + 1) * 512], op0=ALU.mult, op1=ALU.mult),
                        reads=[by, gT, gC, g2b[r]], writes=[yt.sub(db)])
                kb.dma("pool", lambda en, yt=yt, g=g, e=e: en.indirect_dma_start(
                    out=g["dst"].ap[:, :], out_offset=IOA(ap=g["idx"](e), axis=0), in_=yt[:, :], in_offset=None,
                    compute_op=ALU.add, bounds_check=kb.bnd[g["n"] - 1], oob_is_err=True),
                    reads=[yt, idxT, idxC], writes=[g["dst"]])
            if e + 2 < NE:
                gathers(e + 2)
                load_w(e + 2)

    def mixer0(self):
        kb = self.kb
        l = 0
        wbuf = kb.alloc("wbuf", [128, 8, 1536], BF16)
        wout = wbuf.view("woutv", [128, 8, D], BF16)
        csc = kb.alloc("csc", [128, 256], BF16)
        kb.dma("sp", lambda e: e.dma_start(out=csc[:], in_=self.c_csc.ap[:, :]), reads=[self.c_csc], writes=[csc])
        cwc = kb.alloc("cwc", [128, 4, 31], F32)
        for j in range(4):
            kb.dma("sp", lambda e, j=j: e.dma_start(out=cwc[:, j, :], in_=self.ab_conv_w.ap[:, j * 128:(j + 1) * 128].rearrange("k p -> p k")),
                   reads=[self.ab_conv_w], writes=[cwc.sub(j)])
        cols = {}
        for nm, dr in (("cb", self.ab_conv_b), ("lg", self.ab_ln_g), ("lb", self.ab_ln_b)):
            t = kb.alloc(nm + "c", [128, 4], F32)
            kb.dma("sp", lambda e, t=t, dr=dr: e.dma_start(out=t[:], in_=dr.ap.rearrange("(j p) -> p j", p=128)), reads=[dr], writes=[t])
            cols[nm] = t
        onesb = kb.alloc("onesb", [128, 128], BF16)
        kb.op("dve", lambda e: e.memset(onesb[:], 1.0 / 512.0), writes=[onesb])
        diag = kb.alloc("diag", [128, 4 * 31, 128], BF16)
        for j in range(4):
            for k in range(31):
                kb.op("dve", lambda e, j=j, k=k: e.tensor_scalar(
                    out=diag[:, j * 31 + k, :], in0=self.identb[:], scalar1=cwc[:, j, k:k + 1], scalar2=None, op0=ALU.mult),
                    reads=[self.identb, cwc], writes=[diag.sub((j, k))])
        bufs = self.norm_bufs(nx=2)
        xb = bufs[0][0]
        uTb = kb.alloc("uTb", [128, 8, 512], BF16)
        aT = kb.alloc("aT", [128, 4, L + 32], BF16)
        ufTb = kb.alloc("ufTb", [128, 4, 512], BF16)
        Yall = kb.alloc("Yall", [128, 16, 1024], BF16)
        mixT = kb.alloc("mixT", [128, 8, L], BF16)
        clb = kb.alloc("clb", [128, 16, 256], BF16)
        slb = kb.alloc("slb", [128, 16, 256], BF16)
        g1b = kb.alloc("g1b", [128, D], F32)
        cl2h = aT.view("cl2", [128, 16, 256], BF16, boff=0)
        sl2h = aT.view("sl2", [128, 16, 256], BF16, boff=8192)
        NBC = 256
        cT = kb.alloc("cT", [128, 4, NBC], F32)
        cbt = kb.alloc("cbt", [128, 4, NBC], BF16)
        c2t = kb.alloc("c2t", [128, 4, NBC], BF16)
        t_mean = kb.alloc("t_mean", [128, NBC], F32)
        t_rstd = kb.alloc("t_rstd", [128, NBC], F32)
        t_tmp = kb.alloc("t_tmp", [128, 512], F32)
        t_tmp2 = kb.alloc("t_tmp2", [128, NBC], F32)
        addc1 = lambda k, r: self.modc[l][:, k, r:r + 1]
        mulc1 = self.mul1c[l]
        B = kb.banks
        seqs = [(self.xin[0], self.xr[0], L, 0, self.c_cl, self.c_sl), (self.xin[1], self.xr[1], L, 1, self.c_cl, self.c_sl),
                (self.cin[0], self.xc[0], LC, 2, self.c_clc, self.c_slc), (self.cin[1], self.xc[1], LC, 2, self.c_clc, self.c_slc)]
        for (src, dst, Ls, r, ctab, stab) in seqs:
            kb.dma("pool", lambda e: e.dma_start(out=wbuf[:], in_=self.ab_w_in.ap.rearrange("(k p) f -> p k f", p=128)),
                   reads=[self.ab_w_in], writes=[wbuf])
            kb.dma("sp", lambda e, r=r: e.dma_start(out=g1b[:], in_=self.modd.ap[l, r, 2 * D:3 * D].partition_broadcast(128)),
                   reads=[self.modd.sub(l)], writes=[g1b])
            for j in range(4):
                kb.op("dve", lambda e, j=j: e.memset(aT[:, j, 0:15], 0.0), writes=[aT.sub((j, "h0"))])
                kb.op("dve", lambda e, j=j, Ls=Ls: e.memset(aT[:, j, 15 + Ls:32 + Ls], 0.0), writes=[aT.sub((j, "h1"))])
            nb = min(512, Ls)
            for blk in range(Ls // nb):
                t0 = blk * nb
                for sb in range(nb // 256):
                    self.norm_batch(src, t0 + sb * 256, 2, mulc1, addc1, r, uTb, sb * 256, bufs, [B[0], B[1], B[2], B[3]])
                def zmm(j, bank):
                    for k in range(8):
                        kb.op("pe", lambda e, j=j, k=k, bank=bank, nb=nb: e.matmul(
                            out=bank.h[:, 0:nb], lhsT=wbuf[:, k, j * 128:(j + 1) * 128], rhs=uTb[:, k, 0:nb],
                            start=(k == 0), stop=(k == 7)), reads=[wbuf, uTb], writes=[bank])
                for jj in range(4):
                    zmm(jj, B[4])
                    zmm(jj + 4, B[5])
                    kb.op("act", lambda e, nb=nb: e.activation(out=t_tmp[:, 0:nb], in_=B[5].h[:, 0:nb], func=AF.Sigmoid),
                          reads=[B[5]], writes=[t_tmp])
                    kb.op("dve", lambda e, jj=jj, nb=nb, t0=t0: e.tensor_tensor(
                        out=aT[:, jj, 15 + t0:15 + t0 + nb], in0=t_tmp[:, 0:nb], in1=B[4].h[:, 0:nb], op=ALU.mult),
                        reads=[t_tmp, B[4]], writes=[aT.sub((jj, t0))])
                for g in range(4):
                    zmm(8 + g, B[6])
                    kb.op("act", lambda e, g=g, nb=nb: e.copy(out=ufTb[:, g, 0:nb], in_=B[6].h[:, 0:nb]),
                          reads=[B[6]], writes=[ufTb.sub(g)])
                for tc in range(nb // 128):
                    c = (t0 // 128) + tc
                    for gp in range(2):
                        for gg in range(2):
                            g = gp * 2 + gg
                            kb.op("pe", lambda e, g=g, gg=gg, tc=tc: e.matmul(
                                out=B[7].h[:, gg * 256:(gg + 1) * 256], lhsT=ufTb[:, g, tc * 128:(tc + 1) * 128], rhs=csc[:, :],
                                start=True, stop=True), reads=[ufTb, csc], writes=[B[7]])
                        kb.op("dve", lambda e, c=c, gp=gp: e.tensor_copy(out=Yall[:, c, gp * 512:(gp + 1) * 512], in_=B[7].h[:, 0:512]),
                              reads=[B[7]], writes=[Yall.sub((c, gp))])
            nbc = min(NBC, Ls)
            for blk in range(Ls // nbc):
                t0 = blk * nbc
                for j in range(4):
                    bank = B[j % 2]
                    for k in range(31):
                        kb.op("pe", lambda e, j=j, k=k, bank=bank, t0=t0, nbc=nbc: e.matmul(
                            out=bank.h[:, 0:nbc], lhsT=diag[:, j * 31 + k, :], rhs=aT[:, j, t0 + k:t0 + k + nbc],
                            start=(k == 0), stop=(k == 30)), reads=[diag, aT], writes=[bank])
                    kb.op("act", lambda e, j=j, bank=bank, nbc=nbc: e.activation(
                        out=cT[:, j, 0:nbc], in_=bank.h[:, 0:nbc], func=AF.Identity, bias=cols["cb"][:, j:j + 1]),
                        reads=[bank, cols["cb"]], writes=[cT.sub(j)])
                    kb.op("pool", lambda e, j=j, nbc=nbc: e.tensor_copy(out=cbt[:, j, 0:nbc], in_=cT[:, j, 0:nbc]),
                          reads=[cT.sub(j)], writes=[cbt.sub(j)])
                    kb.op("pool", lambda e, j=j, nbc=nbc: e.tensor_tensor(out=c2t[:, j, 0:nbc], in0=cT[:, j, 0:nbc], in1=cT[:, j, 0:nbc], op=ALU.mult),
                          reads=[cT.sub(j)], writes=[c2t.sub(j)])
                for j in range(4):
                    kb.op("pe", lambda e, j=j, nbc=nbc: e.matmul(out=B[2].h[:, 0:nbc], lhsT=onesb[:], rhs=cbt[:, j, 0:nbc],
                                                                 start=(j == 0), stop=(j == 3)), reads=[onesb, cbt], writes=[B[2]])
                for j in range(4):
                    kb.op("pe", lambda e, j=j, nbc=nbc: e.matmul(out=B[3].h[:, 0:nbc], lhsT=onesb[:], rhs=c2t[:, j, 0:nbc],
                                                                 start=(j == 0), stop=(j == 3)), reads=[onesb, c2t], writes=[B[3]])
                kb.op("dve", lambda e, nbc=nbc: e.tensor_copy(out=t_mean[:, 0:nbc], in_=B[2].h[:, 0:nbc]), reads=[B[2]], writes=[t_mean])
                kb.op("dve", lambda e, nbc=nbc: e.tensor_tensor(out=t_tmp2[:, 0:nbc], in0=t_mean[:, 0:nbc], in1=t_mean[:, 0:nbc], op=ALU.mult),
                      reads=[t_mean], writes=[t_tmp2])
                kb.op("dve", lambda e, nbc=nbc: e.tensor_tensor(out=t_rstd[:, 0:nbc], in0=B[3].h[:, 0:nbc], in1=t_tmp2[:, 0:nbc], op=ALU.subtract),
                      reads=[B[3], t_tmp2], writes=[t_rstd])
                kb.op("act", lambda e, nbc=nbc: e.activation(out=t_rstd[:, 0:nbc], in_=t_rstd[:, 0:nbc], func=AF.Identity, bias=self.epsc[:, 0:1]),
                      reads=[t_rstd, self.epsc], writes=[t_rstd])
                kb.op("act", lambda e, nbc=nbc: e.activation(out=t_rstd[:, 0:nbc], in_=t_rstd[:, 0:nbc], func=AF.Sqrt),
                      reads=[t_rstd], writes=[t_rstd])
                kb.op("dve", lambda e, nbc=nbc: e.reciprocal(out=t_rstd[:, 0:nbc], in_=t_rstd[:, 0:nbc]), reads=[t_rstd], writes=[t_rstd])
                for j in range(4):
                    kb.op("pool", lambda e, j=j, nbc=nbc: e.tensor_tensor(out=cT[:, j, 0:nbc], in0=cT[:, j, 0:nbc], in1=t_mean[:, 0:nbc], op=ALU.subtract),
                          reads=[cT.sub(j), t_mean], writes=[cT.sub(j)])
                    kb.op("dve", lambda e, j=j, nbc=nbc: e.tensor_tensor(out=cT[:, j, 0:nbc], in0=cT[:, j, 0:nbc], in1=t_rstd[:, 0:nbc], op=ALU.mult),
                          reads=[cT.sub(j), t_rstd], writes=[cT.sub(j)])
                    kb.op("act", lambda e, j=j, nbc=nbc, t0=t0: e.activation(
                        out=mixT[:, j, t0:t0 + nbc], in_=cT[:, j, 0:nbc], func=AF.Silu,
                        scale=cols["lg"][:, j:j + 1], bias=cols["lb"][:, j:j + 1]),
                        reads=[cT.sub(j), cols["lg"], cols["lb"]], writes=[mixT.sub((j, t0))])
            ntc = Ls // 128
            for kbi in range(Ls // 256):
                k0 = kbi * 256
                if kbi % 2 == 0:
                    clh, slh, trc, trs = clb.h, slb.h, clb, slb
                else:
                    clh, slh, trc, trs = cl2h, sl2h, aT, aT
                kb.dma("sp", lambda e, ctab=ctab, k0=k0, ntc=ntc, clh=clh: e.dma_start(
                    out=clh[:, 0:ntc, :], in_=ctab.ap.rearrange("(c p) k -> p c k", p=128)[:, :, k0:k0 + 256]),
                    reads=[ctab], writes=[trc])
                kb.dma("sp", lambda e, stab=stab, k0=k0, ntc=ntc, slh=slh: e.dma_start(
                    out=slh[:, 0:ntc, :], in_=stab.ap.rearrange("(c p) k -> p c k", p=128)[:, :, k0:k0 + 256]),
                    reads=[stab], writes=[trs])
                for g in range(4):
                    bank = B[4 + g % 2]
                    for c in range(ntc):
                        kb.op("pe", lambda e, g=g, c=c, bank=bank, clh=clh: e.matmul(
                            out=bank.h[:, 0:256], lhsT=Yall[:, c, g * 256:g * 256 + 128], rhs=clh[:, c, :],
                            start=(c == 0), stop=False), reads=[Yall, trc], writes=[bank])
                        kb.op("pe", lambda e, g=g, c=c, bank=bank, ntc=ntc, slh=slh: e.matmul(
                            out=bank.h[:, 0:256], lhsT=Yall[:, c, g * 256 + 128:g * 256 + 256], rhs=slh[:, c, :],
                            start=False, stop=(c == ntc - 1)), reads=[Yall, trs], writes=[bank])
                    if g % 2 == 0:
                        kb.op("act", lambda e, g=g, bank=bank, k0=k0: e.copy(out=mixT[:, 4 + g, k0:k0 + 256], in_=bank.h[:, 0:256]),
                              reads=[bank], writes=[mixT.sub((4 + g, k0))])
                    else:
                        kb.op("dve", lambda e, g=g, bank=bank, k0=k0: e.tensor_copy(out=mixT[:, 4 + g, k0:k0 + 256], in_=bank.h[:, 0:256]),
                              reads=[bank], writes=[mixT.sub((4 + g, k0))])
            kb.dma("pool", lambda e: e.dma_start(out=wout[:], in_=self.ab_w_out.ap.rearrange("(k p) f -> p k f", p=128)),
                   reads=[self.ab_w_out], writes=[wbuf])
            for tc in range(ntc):
                xb = bufs[0][tc % 2]
                kb.dma("sp", lambda e, src=src, tc=tc, xb=xb: e.dma_start(out=xb[:], in_=src.ap[tc * 128:(tc + 1) * 128, :]),
                       reads=[src], writes=[xb])
                for db in range(2):
                    bank = B[6 + db]
                    for m in range(8):
                        kb.op("pe", lambda e, m=m, db=db, bank=bank, tc=tc: e.matmul(
                            out=bank.h[:, 0:512], lhsT=mixT[:, m, tc * 128:(tc + 1) * 128], rhs=wout[:, m, db * 512:(db + 1) * 512],
                            start=(m == 0), stop=(m == 7)), reads=[mixT, wbuf], writes=[bank])
                    kb.op("dve", lambda e, db=db, bank=bank: e.tensor_tensor(
                        out=t_tmp[:, 0:512], in0=bank.h[:, 0:512], in1=g1b[:, db * 512:(db + 1) * 512], op=ALU.mult),
                        reads=[bank, g1b], writes=[t_tmp])
                    kb.op("pool", lambda e, db=db, xb=xb: e.tensor_tensor(
                        out=xb[:, db * 512:(db + 1) * 512], in0=xb[:, db * 512:(db + 1) * 512], in1=t_tmp[:, 0:512], op=ALU.add),
                        reads=[xb, t_tmp], writes=[xb])
                kb.dma("pool", lambda e, dst=dst, tc=tc, xb=xb: e.dma_start(out=dst.ap[tc * 128:(tc + 1) * 128, :], in_=xb[:]),
                       reads=[xb], writes=[dst.sub(("row", tc))])


    def mla(self):
        kb = self.kb
        l = 1
        B = kb.banks
        NKV = LC + L
        NT = NKV // 128
        SCALE = 96.0 ** -0.5
        cast = lambda dst, src_ap, tr: kb.dma("pool", lambda e: e.dma_start(out=dst[:], in_=src_ap), reads=[tr], writes=[dst])
        wi = kb.alloc("wi", [128, 8, 416], BF16)
        cast(wi, self.mla_w_in.ap.rearrange("(k p) f -> p k f", p=128), self.mla_w_in)
        wuq = kb.alloc("wuq", [128, 2, 1536], BF16)
        cast(wuq, self.mla_w_uq.ap.rearrange("(k p) f -> p k f", p=128), self.mla_w_uq)
        wukv = kb.alloc("wukv", [128, 2048], BF16)
        cast(wukv, self.mla_w_ukv.ap[:, :], self.mla_w_ukv)
        wo = kb.alloc("wo", [128, 8, D], BF16)
        cast(wo, self.mla_w_o.ap.rearrange("(k p) f -> p k f", p=128), self.mla_w_o)
        ropt = kb.alloc("ropt", [128, 16, 32], F32)
        kb.dma("sp", lambda e: e.dma_start(out=ropt[:], in_=self.c_rope.ap.rearrange("(c p) f -> p c f", p=128)),
               reads=[self.c_rope], writes=[ropt])
        qgc = kb.alloc("qgc", [128, 2], F32)
        kb.dma("sp", lambda e: e.dma_start(out=qgc[:], in_=self.mla_qg.ap.rearrange("(j p) -> p j", p=128)), reads=[self.mla_qg], writes=[qgc])
        kvgc = kb.alloc("kvgc", [128, 1], F32)
        kb.dma("sp", lambda e: e.dma_start(out=kvgc[:], in_=self.mla_kvg.ap.rearrange("(j p) -> p j", p=128)), reads=[self.mla_kvg], writes=[kvgc])
        ones1 = kb.alloc("ones1", [128, 128], BF16)
        kb.op("dve", lambda e: e.memset(ones1[:], 1.0), writes=[ones1])
        bufs = self.norm_bufs(nx=2)
        xb = bufs[0][0]
        uT = kb.alloc("uTall", [128, 8, NKV], BF16)
        cqnT = kb.alloc("cqnT", [128, 2, L], BF16)
        ckvnT = kb.alloc("ckvnT", [128, NKV], BF16)
        KTs = [kb.alloc(f"KT{i}", [128, NKV], BF16) for i in range(2)]
        QTs = [kb.alloc(f"QT{i}", [128, L], BF16) for i in range(2)]
        qtoks = [kb.alloc(f"qtok{i}", [128, 8, 96], BF16) for i in range(2)]
        Vaug = [kb.alloc(f"Vaug{i}", [128, NT, 128], BF16) for i in range(2)]
        kb.op("dve", lambda e: e.memset(Vaug[0][:].rearrange("p a b -> p (a b)"), 1.0), writes=[Vaug[0]])
        kb.op("dve", lambda e: e.memset(Vaug[1][:].rearrange("p a b -> p (a b)"), 1.0), writes=[Vaug[1]])
        pT = [kb.alloc(f"pT{i}", [128, 1024], BF16) for i in range(3)]
        oTs = kb.alloc("oTs", [128, 512], F32)
        onesf = kb.alloc("onesf", [128, 512], F32)
        kb.op("dve", lambda e: e.memset(onesf[:], 1.0), writes=[onesf])
        recf = kb.alloc("recf", [128, 512], F32)
        rech = kb.alloc("rech", [128, 512], BF16)
        recl = kb.alloc("recl", [128, 512], BF16)
        attnT = kb.alloc("attnT", [128, 8, L], BF16)
        g1b = kb.alloc("g1bm", [128, D], F32)
        t_tmp = kb.alloc("t_tmpm", [128, 512], F32)
        zts = [kb.alloc(f"zt{i}", [128, 416], F32) for i in range(2)]
        cqns = [kb.alloc(f"cqn{i}", [128, 256], BF16) for i in range(2)]
        ckvns = [kb.alloc(f"ckvn{i}", [128, 128], BF16) for i in range(2)]
        ktoks = [kb.alloc(f"ktok{i}", [128, 96], BF16) for i in range(2)]
        for kt_ in ktoks:
            kb.op("dve", lambda e, kt_=kt_: e.memset(kt_[:], 0.0), writes=[kt_])
        rtk = [[kb.alloc(f"rtk{i}_{j}", [128, 16], F32) for j in range(4)] for i in range(2)]
        rt = [kb.alloc(f"rt{i}", [128, 4, 16], F32) for i in range(4)]
        st2 = kb.alloc("st2", [128, 8 * NT], F32)
        kb.op("dve", lambda e: e.memset(st2[:], 0.0), writes=[st2])
        junk2 = kb.alloc("junk2", [128, 256], BF16)
        addc1 = lambda k, r: self.modc[l][:, k, r:r + 1]
        mulc1 = self.mul1c[l]
        npt = 0
        def stepA(s):
            kb.op("dve", lambda e: e.memset(st2[:], 0.0), writes=[st2])
            for b2 in range(LC // 256):
                self.norm_batch(self.xc[s], b2 * 256, 2, mulc1, addc1, 2, uT, b2 * 256, bufs, [B[0], B[1], B[2], B[3]])
            for b2 in range(L // 256):
                self.norm_batch(self.xr[s], b2 * 256, 2, mulc1, addc1, s, uT, LC + b2 * 256, bufs, [B[0], B[1], B[2], B[3]])

        for s in range(NS):
            kb.dma("sp", lambda e, s=s: e.dma_start(out=g1b[:], in_=self.modd.ap[l, s, 2 * D:3 * D].partition_broadcast(128)),
                   reads=[self.modd.sub(l)], writes=[g1b])
            def stepB_tile(c):
                lat = c >= 2
                cl_ = c - 2
                zt, cqn, ckvn, ktok = zts[c % 2], cqns[c % 2], ckvns[c % 2], ktoks[c % 2]
                rk = rtk[c % 2]
                zb = B[4 + c % 2]
                for k in range(8):
                    kb.op("pe", lambda e, c=c, k=k, zb=zb: e.matmul(out=zb.h[:, 0:416], lhsT=uT[:, k, c * 128:(c + 1) * 128], rhs=wi[:, k, :],
                                                                  start=(k == 0), stop=(k == 7)), reads=[uT, wi], writes=[zb])
                kb.op("act", lambda e, zb=zb, zt=zt: e.copy(out=zt[:], in_=zb.h[:, 0:416]), reads=[zb], writes=[zt])
                so = c * 8
                parts = [(256, 128, 128.0, ckvn, 0)] + ([(0, 256, 256.0, cqn, 4)] if lat else [])
                for (c0, n, nf, dstt, o) in parts:
                    kb.op("act", lambda e, c0=c0, n=n, so=so, o=o, zt=zt: e.activation(out=junk2[:, 0:n], in_=zt[:, c0:c0 + n], func=AF.Square,
                                                                             accum_out=st2[:, so + o:so + o + 1]), reads=[zt], writes=[junk2, st2.sub(so + o)])
                    kb.op("act", lambda e, so=so, o=o, nf=nf: e.activation(out=st2[:, so + o + 1:so + o + 2], in_=st2[:, so + o:so + o + 1], func=AF.Identity,
                                                                         scale=1.0 / nf, bias=self.epsc[:, 0:1]), reads=[st2.sub(so + o), self.epsc], writes=[st2.sub(so + o + 1)])
                    kb.op("act", lambda e, so=so, o=o: e.activation(out=st2[:, so + o + 2:so + o + 3], in_=st2[:, so + o + 1:so + o + 2], func=AF.Sqrt),
                          reads=[st2.sub(so + o + 1)], writes=[st2.sub(so + o + 2)])
                    kb.op("dve", lambda e, so=so, o=o: e.reciprocal(out=st2[:, so + o + 3:so + o + 4], in_=st2[:, so + o + 2:so + o + 3]),
                          reads=[st2.sub(so + o + 2)], writes=[st2.sub(so + o + 3)])
                    kb.op("act", lambda e, c0=c0, n=n, so=so, o=o, dstt=dstt, zt=zt: e.activation(out=dstt[:, 0:n], in_=zt[:, c0:c0 + n], func=AF.Identity,
                                                                                          scale=st2[:, so + o + 3:so + o + 4]), reads=[zt, st2.sub(so + o + 3)], writes=[dstt])
                if lat:
                    krv = zt[:, 384:416].rearrange("p (i two) -> p i two", two=2)
                    xe, xo = krv[:, :, 0], krv[:, :, 1]
                    cs, sn = ropt[:, cl_, 0:16], ropt[:, cl_, 16:32]
                    ko = ktok[:, 64:96].rearrange("p (i two) -> p i two", two=2)
                    a0, a1, a2, a3 = rk[0][:, :], rk[1][:, :], rk[2][:, :], rk[3][:, :]
                    kb.op("pool", lambda e, xe=xe, cs=cs, a0=a0: e.tensor_tensor(out=a0, in0=xe, in1=cs, op=ALU.mult), reads=[zt, ropt], writes=[rk[0]])
                    kb.op("pool", lambda e, xo=xo, sn=sn, a1=a1: e.tensor_tensor(out=a1, in0=xo, in1=sn, op=ALU.mult), reads=[zt, ropt], writes=[rk[1]])
                    kb.op("pool", lambda e, xe=xe, sn=sn, a2=a2: e.tensor_tensor(out=a2, in0=xe, in1=sn, op=ALU.mult), reads=[zt, ropt], writes=[rk[2]])
                    kb.op("pool", lambda e, xo=xo, cs=cs, a3=a3: e.tensor_tensor(out=a3, in0=xo, in1=cs, op=ALU.mult), reads=[zt, ropt], writes=[rk[3]])
                    kb.op("pool", lambda e, ko=ko, a0=a0, a1=a1: e.tensor_tensor(out=ko[:, :, 0], in0=a0, in1=a1, op=ALU.subtract), reads=[rk[0], rk[1]], writes=[ktok.sub(0)])
                    kb.op("pool", lambda e, ko=ko, a2=a2, a3=a3: e.tensor_tensor(out=ko[:, :, 1], in0=a2, in1=a3, op=ALU.add), reads=[rk[2], rk[3]], writes=[ktok.sub(1)])
                else:
                    kb.op("pool", lambda e, ktok=ktok, zt=zt: e.tensor_copy(out=ktok[:, 64:96], in_=zt[:, 384:416]), reads=[zt], writes=[ktok.sub(0)])
                tbk = B[6 + c % 2]
                pv = tbk.h.bitcast(BF16)
                if lat:
                    for qk in range(2):
                        kb.op("pe", lambda e, pv=pv, qk=qk, cqn=cqn: e.transpose(out=pv[:, qk * 128:(qk + 1) * 128], in_=cqn[:, qk * 128:(qk + 1) * 128], identity=self.identb[:]),
                              reads=[cqn, self.identb], writes=[tbk])
                kb.op("pe", lambda e, pv=pv, ckvn=ckvn: e.transpose(out=pv[:, 256:384], in_=ckvn[:, :], identity=self.identb[:]), reads=[ckvn, self.identb], writes=[tbk])
                kb.op("pe", lambda e, pv=pv, ktok=ktok: e.transpose(out=pv[0:96, 384:512], in_=ktok[:, 0:96], identity=self.identb[:]), reads=[ktok, self.identb], writes=[tbk])
                if lat:
                    for qk in range(2):
                        kb.op("act", lambda e, pv=pv, qk=qk, cl_=cl_: e.activation(out=cqnT[:, qk, cl_ * 128:(cl_ + 1) * 128], in_=pv[:, qk * 128:(qk + 1) * 128],
                                                                                 func=AF.Identity, scale=qgc[:, qk:qk + 1]), reads=[tbk, qgc], writes=[cqnT.sub((qk, cl_))])
                kb.op("act", lambda e, pv=pv, c=c: e.activation(out=ckvnT[:, c * 128:(c + 1) * 128], in_=pv[:, 256:384], func=AF.Identity, scale=kvgc[:, 0:1]),
                      reads=[tbk, kvgc], writes=[ckvnT.sub(c)])
                for KTx in KTs:
                    kb.op("act", lambda e, pv=pv, c=c, KTx=KTx: e.copy(out=KTx[64:96, c * 128:(c + 1) * 128], in_=pv[64:96, 384:512]),
                          reads=[tbk], writes=[KTx.sub(("r", c))])
            if s == 0:
                stepA(0)
                for c in range(NT):
                    stepB_tile(c)
            def projA(h):
                KTh = KTs[h % 2]
                for blk in range(5):
                    n0 = blk * 512
                    nn = min(512, NKV - n0)
                    bk = B[7]
                    kb.op("pe", lambda e, h=h, n0=n0, nn=nn, bk=bk: e.matmul(out=bk.h[0:64, 0:nn], lhsT=wukv[:, h * 128:h * 128 + 64], rhs=ckvnT[:, n0:n0 + nn],
                                                                        start=True, stop=True), reads=[wukv, ckvnT], writes=[bk])
                    kb.op("dve", lambda e, n0=n0, nn=nn, bk=bk, KTh=KTh: e.tensor_copy(out=KTh[0:64, n0:n0 + nn], in_=bk.h[0:64, 0:nn]),
                          reads=[bk], writes=[KTh.sub(("n", blk))])
                va = Vaug[h % 2]
                vo = 0 if h % 2 == 0 else 64
                for vb in range(3):
                    ntl = min(8, NT - vb * 8)
                    bv = B[7]
                    for ci in range(ntl):
                        c = vb * 8 + ci
                        kb.op("pe", lambda e, h=h, c=c, ci=ci, bv=bv: e.matmul(out=bv.h[:, ci * 64:(ci + 1) * 64], lhsT=ckvnT[:, c * 128:(c + 1) * 128],
                                                                             rhs=wukv[:, h * 128 + 64:h * 128 + 128], start=True, stop=True), reads=[ckvnT, wukv], writes=[bv])
                    kb.op("dve", lambda e, vb=vb, ntl=ntl, bv=bv, va=va, vo=vo: e.tensor_copy(
                        out=va[:, vb * 8:vb * 8 + ntl, vo:vo + 64], in_=bv.h[:, 0:ntl * 64].rearrange("p (c f) -> p c f", f=64)),
                        reads=[bv], writes=[va.sub(vb)])

            def projQ(h, half, stage):
                qtk = qtoks[half]
                QTh = QTs[h % 2]
                for bq in range(2):
                    c0 = half * 8 + bq * 4
                    if stage == 0:
                        bqk = B[6 + bq]
                        for ci in range(4):
                            c = c0 + ci
                            for qk in range(2):
                                kb.op("pe", lambda e, h=h, c=c, ci=ci, qk=qk, bqk=bqk: e.matmul(
                                    out=bqk.h[:, ci * 96:(ci + 1) * 96], lhsT=cqnT[:, qk, c * 128:(c + 1) * 128], rhs=wuq[:, qk, h * 96:(h + 1) * 96],
                                    start=(qk == 0), stop=(qk == 1)), reads=[cqnT, wuq], writes=[bqk])
                        qv = bqk.h[:, 0:384].rearrange("p (c f) -> p c f", f=96)
                        lc0 = bq * 4
                        kb.op("dve", lambda e, qv=qv, lc0=lc0, qtk=qtk: e.tensor_copy(out=qtk[:, lc0:lc0 + 4, 0:64], in_=qv[:, :, 0:64]), reads=[bqk], writes=[qtk.sub((lc0, "n"))])
                        xe, xo = qv[:, :, 64:96:2], qv[:, :, 65:96:2]
                        cs, sn = ropt[:, c0:c0 + 4, 0:16], ropt[:, c0:c0 + 4, 16:32]
                        qo = qtk[:, lc0:lc0 + 4, 64:96].rearrange("p c (i two) -> p c i two", two=2)
                        kb.op("dve", lambda e, xe=xe, cs=cs: e.tensor_tensor(out=rt[0][:], in0=xe, in1=cs, op=ALU.mult), reads=[bqk, ropt], writes=[rt[0]])
                        kb.op("dve", lambda e, xo=xo, sn=sn: e.tensor_tensor(out=rt[1][:], in0=xo, in1=sn, op=ALU.mult), reads=[bqk, ropt], writes=[rt[1]])
                        kb.op("dve", lambda e, xe=xe, sn=sn: e.tensor_tensor(out=rt[2][:], in0=xe, in1=sn, op=ALU.mult), reads=[bqk, ropt], writes=[rt[2]])
                        kb.op("dve", lambda e, xo=xo, cs=cs: e.tensor_tensor(out=rt[3][:], in0=xo, in1=cs, op=ALU.mult), reads=[bqk, ropt], writes=[rt[3]])
                        kb.op("pool", lambda e, qo=qo: e.tensor_tensor(out=qo[:, :, :, 0], in0=rt[0][:], in1=rt[1][:], op=ALU.subtract),
                              reads=[rt[0], rt[1]], writes=[qtk.sub((lc0, "e"))])
                        kb.op("pool", lambda e, qo=qo: e.tensor_tensor(out=qo[:, :, :, 1], in0=rt[2][:], in1=rt[3][:], op=ALU.add),
                              reads=[rt[2], rt[3]], writes=[qtk.sub((lc0, "o"))])
                    else:
                        lc0 = bq * 4
                        tq = B[6 + bq]
                        pvq = tq.h.bitcast(BF16)
                        for ci in range(4):
                            kb.op("pe", lambda e, pvq=pvq, lc=lc0 + ci, ci=ci, qtk=qtk: e.transpose(out=pvq[0:96, ci * 128:(ci + 1) * 128], in_=qtk[:, lc, 0:96], identity=self.identb[:]),
                                  reads=[qtk, self.identb], writes=[tq])
                        kb.op("dve", lambda e, pvq=pvq, c0=c0, QTh=QTh: e.tensor_copy(out=QTh[0:96, c0 * 128:(c0 + 4) * 128], in_=pvq[0:96, 0:512]), reads=[tq], writes=[QTh.sub(c0)])

            def normalize1(h, qb, bo):
                kb.op("dve", lambda e, bo=bo: e.tensor_copy(out=oTs[:], in_=bo.h[:, 0:512]), reads=[bo], writes=[oTs])

            def normalize1b(h, qb):
                dp = 64 if (h % 2 == 0) else 0
                kb.op("dve", lambda e, dp=dp: e.reciprocal(out=recf[dp:dp + 1, :], in_=oTs[dp:dp + 1, :]), reads=[oTs], writes=[recf])
                kb.op("pool", lambda e, dp=dp: e.tensor_copy(out=rech[dp:dp + 1, :], in_=recf[dp:dp + 1, :]), reads=[recf], writes=[rech])
                kb.op("pool", lambda e, dp=dp: e.tensor_tensor(out=recl[dp:dp + 1, :], in0=recf[dp:dp + 1, :], in1=rech[dp:dp + 1, :], op=ALU.subtract),
                      reads=[recf, rech], writes=[recl])

            def normalize2(h, qb):
                even = (h % 2 == 0)
                dp = 64 if even else 0
                op_ = 0 if even else 64
                bb = B[6]
                kb.op("pe", lambda e, dp=dp, bb=bb: e.matmul(out=bb.h[:, 0:512], lhsT=ones1[dp:dp + 1, :], rhs=rech[dp:dp + 1, :], start=True, stop=False),
                      reads=[ones1, rech], writes=[bb])
                kb.op("pe", lambda e, dp=dp, bb=bb: e.matmul(out=bb.h[:, 0:512], lhsT=ones1[dp:dp + 1, :], rhs=recl[dp:dp + 1, :], start=False, stop=True),
                      reads=[ones1, recl], writes=[bb])
                kb.op("dve", lambda e, op_=op_, h=h, qb=qb, bb=bb: e.tensor_tensor(
                    out=attnT[op_:op_ + 64, h // 2, qb * 512:(qb + 1) * 512], in0=oTs[op_:op_ + 64, :], in1=bb.h[op_:op_ + 64, 0:512], op=ALU.mult),
                    reads=[oTs, bb], writes=[attnT.sub((h, qb))])

            projA(0)
            for half in range(2):
                projQ(0, half, 0)
                projQ(0, half, 1)
            pend = []
            pendnorm = []
            nstep = 0

            def issue_pv(item):
                (h, qb, c2, bo, pt, va) = item
                for half in range(2):
                    c = c2 * 2 + half
                    kb.op("pe", lambda e, c=c, bo=bo, pt=pt, va=va, half=half: e.matmul(
                        out=bo.h[:, 0:512], lhsT=va[:, c, :], rhs=pt[:, half * 512:(half + 1) * 512],
                        start=(c == 0), stop=(c == NT - 1)), reads=[va, pt], writes=[bo])
                if c2 == NT // 2 - 1:
                    normalize1(h, qb, bo)
                    pendnorm.append((nstep + 1, 1, h, qb))
                    pendnorm.append((nstep + 6, 2, h, qb))
                    pendnorm.sort()

            for h in range(NE):
                KTh = KTs[h % 2]
                QTh = QTs[h % 2]
                va = Vaug[h % 2]
                for qb in range(4):
                    bo = B[4 + qb % 2]
                    for c2 in range(NT // 2):
                        if h + 1 < NE:
                            if qb == 0 and c2 == 4:
                                projA(h + 1)
                            if qb == 1 and c2 == 4:
                                projQ(h + 1, 0, 0)
                            if qb == 2 and c2 == 4:
                                projQ(h + 1, 0, 1)
                            if qb == 3 and c2 == 3:
                                projQ(h + 1, 1, 0)
                            if qb == 3 and c2 == 8:
                                projQ(h + 1, 1, 1)
                        while pendnorm and nstep >= pendnorm[0][0]:
                            (_, stg, hh, qq) = pendnorm.pop(0)
                            (normalize1b if stg == 1 else normalize2)(hh, qq)
                        pi = nstep % 2
                        pt = pT[nstep % 3]
                        nstep += 1
                        for half in range(2):
                            c = c2 * 2 + half
                            bs = B[2 * pi + half]
                            kb.op("pe", lambda e, c=c, qb=qb, bs=bs, KTh=KTh, QTh=QTh: e.matmul(
                                out=bs.h[:, 0:512], lhsT=KTh[0:96, c * 128:(c + 1) * 128], rhs=QTh[0:96, qb * 512:(qb + 1) * 512],
                                start=True, stop=True), reads=[KTh, QTh], writes=[bs])
                        kb.op("act", lambda e, pi=pi, pt=pt: e.activation(out=pt[:, 0:1024], in_=kb.pairs[pi][:, 0:1024], func=AF.Exp, scale=SCALE),
                              reads=[B[2 * pi], B[2 * pi + 1]], writes=[pt])
                        pend.append((h, qb, c2, bo, pt, va))
                        if len(pend) > 1:
                            issue_pv(pend.pop(0))
            while pend:
                issue_pv(pend.pop(0))
            while pendnorm:
                (_, stg, hh, qq) = pendnorm.pop(0)
                (normalize1b if stg == 1 else normalize2)(hh, qq)
            def stepD_tile(tc, s=s):
                xb = bufs[0][tc % 2]
                kb.dma("sp", lambda e, s=s, tc=tc, xb=xb: e.dma_start(out=xb[:], in_=self.xr[s].ap[tc * 128:(tc + 1) * 128, :]),
                       reads=[self.xr[s]], writes=[xb])
                for db in range(2):
                    bank = B[db]
                    for m in range(8):
                        kb.op("pe", lambda e, m=m, db=db, bank=bank, tc=tc: e.matmul(
                            out=bank.h[:, 0:512], lhsT=attnT[:, m, tc * 128:(tc + 1) * 128], rhs=wo[:, m, db * 512:(db + 1) * 512],
                            start=(m == 0), stop=(m == 7)), reads=[attnT, wo], writes=[bank])
                    kb.op("dve", lambda e, db=db, bank=bank: e.tensor_tensor(
                        out=t_tmp[:, 0:512], in0=bank.h[:, 0:512], in1=g1b[:, db * 512:(db + 1) * 512], op=ALU.mult),
                        reads=[bank, g1b], writes=[t_tmp])
                    kb.op("pool", lambda e, db=db, xb=xb: e.tensor_tensor(
                        out=xb[:, db * 512:(db + 1) * 512], in0=xb[:, db * 512:(db + 1) * 512], in1=t_tmp[:, 0:512], op=ALU.add),
                        reads=[xb, t_tmp], writes=[xb])
                kb.dma("pool", lambda e, s=s, tc=tc, xb=xb: e.dma_start(out=self.xr[s].ap[tc * 128:(tc + 1) * 128, :], in_=xb[:]),
                       reads=[xb], writes=[self.xr[s].sub(("row", tc))])

            if s + 1 < NS:
                stepA(s + 1)
                for i in range(NT):
                    stepB_tile(i)
                    if i < L // 128:
                        stepD_tile(i)
            else:
                for tc in range(L // 128):
                    stepD_tile(tc)


def _consts():
    bf = ml_dtypes.bfloat16
    c = {}
    c["c_identb"] = np.eye(128, dtype=np.float32).astype(bf)
    c["c_identf"] = np.eye(128, dtype=np.float32)
    i = np.arange(128, dtype=np.int64)
    ang = 2.0 * np.pi * ((i[:, None] * i[None, :]) % 128).astype(np.float64) / 128.0
    c["c_csc"] = np.concatenate([np.cos(ang), np.sin(ang)], axis=1).astype(np.float32) / np.float32(np.sqrt(128.0))
    c["c_csc"] = c["c_csc"].astype(bf)
    for nm, n in (("", L), ("c", LC)):
        t = np.arange(n, dtype=np.int64)
        a = 2.0 * np.pi * ((t[:, None] * t[None, :]) % n).astype(np.float64) / n
        c["c_cl" + nm] = (np.cos(a) / np.sqrt(n)).astype(np.float32).astype(bf)
        c["c_sl" + nm] = (-np.sin(a) / np.sqrt(n)).astype(np.float32).astype(bf)
    t = np.arange(L)
    row = (t // 64).astype(np.float32)
    col = (t % 64).astype(np.float32)
    inv = (np.float32(10000.0) ** (-np.arange(8, dtype=np.float32) / np.float32(8))).astype(np.float32)
    angr = np.concatenate([row[:, None] * inv[None, :], col[:, None] * inv[None, :]], axis=1).astype(np.float32)
    c["c_rope"] = np.concatenate([np.cos(angr), np.sin(angr)], axis=1).astype(np.float32)
    c["c_ctxbase"] = np.concatenate([np.zeros(32), np.full(32, LC), NS * LC + np.arange(64)]).astype(np.float32).reshape(128, 1)
    return c


def _in_map(inp, core, consts):
    f = lambda a: np.ascontiguousarray(np.asarray(a, dtype=np.float32))
    s0 = core * NS
    m = {}
    m["x"] = f(inp["x"][s0:s0 + NS])
    m["ctx"] = f(inp["ctx"][s0:s0 + NS])
    cv3 = np.stack([inp["c"][s0], inp["c"][s0 + 1], inp["c_ctx"]], axis=0).astype(np.float32)
    m["cv"] = np.ascontiguousarray(cv3.reshape(3, 8, 128).transpose(2, 1, 0))
    for k in ("mod_w", "mod_b", "norm1_g", "norm2_g", "final_g", "moe_w_router", "moe_w1", "moe_w3", "moe_w2"):
        m[k] = f(inp[k])
    for k in ("ab_w_in", "ab_conv_w", "ab_conv_b", "ab_ln_g", "ab_ln_b", "ab_w_out", "mla_w_in", "mla_q_norm_g",
              "mla_kv_norm_g", "mla_w_uq", "mla_w_ukv", "mla_w_o"):
        m[k] = f(inp[k][0])
    m.update(consts)
    return m


_CACHE = {}


def run_prog(inputs, phases, copy_in=False, ncores=8, debug_route=False, raw=False):
    key = (tuple(phases), copy_in, debug_route)
    if key not in _CACHE:
        p = Prog(phases=phases, copy_in=copy_in)
        p.debug_route = debug_route
        _CACHE[key] = p.build()
    nc = _CACHE[key]
    consts = _consts()
    in_maps = [_in_map(inputs, c, consts) for c in range(ncores)]
    res = run_bass_kernel_spmd(nc, in_maps, core_ids=list(range(ncores)))
    if raw:
        return res.results
    return np.concatenate([np.asarray(r["y"]) for r in res.results], axis=0)


def kernel(**inputs):
    out = run_prog(inputs, ("mix0", "moe0", "mla1", "moe1", "final"))
    return out.astype(np.float32)
```
